# Optimizing a Trainium2 kernel written in Bass

```python
import math
import jax, jax.numpy as jnp
from jax import lax
import numpy as np

D_MODEL = 1024
BATCH = 2
SEQ = 8192
DEPTH = 2

LRU_WIDTH = D_MODEL // 2
LRU_HEADS = 8
LRU_HEAD_DIM = LRU_WIDTH // LRU_HEADS
CONV_WIDTH = 4
LRU_C = 8.0
RWKV_WIDTH = D_MODEL // 2
RWKV_HEAD_DIM = 64
RWKV_HEADS = RWKV_WIDTH // RWKV_HEAD_DIM
DECAY_LORA = 64
AAA_LORA = 64
GATE_LORA = 128
RWKV_IN = 3 * RWKV_WIDTH + DECAY_LORA + AAA_LORA + GATE_LORA
EVEN_IN = 2 * LRU_WIDTH + RWKV_IN
GN_EPS = 64e-5
DIFF_HEAD_DIM = 64
DIFF_HEADS = D_MODEL // (2 * DIFF_HEAD_DIM)
DIFF_V_DIM = 2 * DIFF_HEAD_DIM
QKV_DIM = 2 * DIFF_HEADS * 2 * DIFF_HEAD_DIM + DIFF_HEADS * DIFF_V_DIM
ROPE_DIM = DIFF_HEAD_DIM // 4
ROPE_THETA = 500000.0
Q_BLOCK = 128
FFN_DIM = 2816
N_EXPERTS = 8
TOP_K = 2
EXPERT_DIM = 3584
RMS_EPS = 1e-6

kernel_name = 'hybrid_rglru_rwkv7_diffattn_moe'

F32 = jnp.float32


def rms_norm(x, g, eps=RMS_EPS):
    xf = x.astype(F32)
    y = xf * lax.rsqrt(jnp.mean(jnp.square(xf), axis=-1, keepdims=True) + eps)
    return (y * g.astype(F32)).astype(x.dtype)


def token_shift(x):
    return jnp.pad(x, ((0, 0), (1, 0), (0, 0)))[:, :-1]


def swiglu(x, wg, wu, wd):
    return (jax.nn.silu(x @ wg) * (x @ wu)) @ wd


def causal_dwconv(x, w, b):
    y = lax.conv_general_dilated(x, w[:, None, :].astype(x.dtype), window_strides=(1,),
                                 padding=[(CONV_WIDTH - 1, 0)],
                                 dimension_numbers=('NWC', 'WIO', 'NWC'),
                                 feature_group_count=x.shape[-1])
    return y + b


def rg_lru(x, wa, ba, wx, bx, lam):
    B, T, _ = x.shape
    xf = x.astype(F32)
    xh = xf.reshape(B, T, LRU_HEADS, LRU_HEAD_DIM)
    gate_a = jnp.einsum('bthi,hij->bthj', xh, wa.astype(F32)).reshape(B, T, LRU_WIDTH) + ba
    gate_x = jnp.einsum('bthi,hij->bthj', xh, wx.astype(F32)).reshape(B, T, LRU_WIDTH) + bx
    log_a = -LRU_C * jax.nn.sigmoid(gate_a) * jax.nn.softplus(-lam.astype(F32))
    a = jnp.exp(log_a)
    u = jnp.sqrt(-jnp.expm1(2.0 * log_a)) * jax.nn.sigmoid(gate_x) * xf

    def combine(l, r):
        return (l[0] * r[0], r[0] * l[1] + r[1])

    _, h = lax.associative_scan(combine, (a, u), axis=1)
    return h.astype(x.dtype)


def rwkv7_scan(r, w, k, v, kk, a):
    B, T, H, N = r.shape

    def step(S, inp):
        r_t, w_t, k_t, v_t, kk_t, a_t = inp
        sa = jnp.einsum('bhvk,bhk->bhv', S, -kk_t)
        S = S * w_t[:, :, None, :] + sa[..., None] * (kk_t * a_t)[:, :, None, :] \
            + v_t[..., None] * k_t[:, :, None, :]
        o = jnp.einsum('bhvk,bhk->bhv', S, r_t)
        return S, o

    xs = tuple(jnp.moveaxis(t, 1, 0) for t in (r, w, k, v, kk, a))
    S0 = jnp.zeros((B, H, N, N), F32)
    _, o = lax.scan(step, S0, xs)
    return jnp.moveaxis(o, 0, 1)


def rwkv7_mixer(p, mu, w0, w2, a0, a2, g2, k_k, k_a, r_k, gn_w, gn_b):
    B, T, _ = p.shape
    pf = p.astype(F32)
    ps = pf + (token_shift(pf) - pf) * mu
    idx = np.cumsum([RWKV_WIDTH, RWKV_WIDTH, RWKV_WIDTH, DECAY_LORA, AAA_LORA]).tolist()
    r, k, v, xw, xa, xg = jnp.split(ps, idx, axis=-1)
    wlog = -jax.nn.softplus(-(w0 + jnp.tanh(xw) @ w2)) - 0.5
    decay = jnp.exp(-jnp.exp(wlog))
    a = jax.nn.sigmoid(a0 + xa @ a2)
    g = jax.nn.sigmoid(xg) @ g2

    def hs(t):
        return t.reshape(B, T, RWKV_HEADS, RWKV_HEAD_DIM)

    kk = hs(k * k_k)
    kk = kk / jnp.maximum(jnp.linalg.norm(kk, axis=-1, keepdims=True), 1e-12)
    k = k * (1.0 + (a - 1.0) * k_a)
    rh, kh, vh = hs(r), hs(k), hs(v)
    o = rwkv7_scan(rh, hs(decay), kh, vh, kk, hs(a))
    mean = jnp.mean(o, axis=-1, keepdims=True)
    var = jnp.mean(jnp.square(o - mean), axis=-1, keepdims=True)
    on = ((o - mean) * lax.rsqrt(var + GN_EPS)).reshape(B, T, RWKV_WIDTH) * gn_w + gn_b
    bonus = (jnp.sum(rh * kh * r_k, axis=-1, keepdims=True) * vh).reshape(B, T, RWKV_WIDTH)
    return (on + bonus) * g


def even_mixer(xn, w_in, conv_w, conv_b, gate_a_w, gate_a_b, gate_x_w, gate_x_b, lru_lambda,
               shift_mu, w0, w2, a0, a2, g2, k_k, k_a, r_k, gn_w, gn_b, w_out):
    proj = xn @ w_in
    lru_x, lru_g, rw = jnp.split(proj, [LRU_WIDTH, 2 * LRU_WIDTH], axis=-1)
    h = rg_lru(causal_dwconv(lru_x, conv_w, conv_b), gate_a_w, gate_a_b, gate_x_w, gate_x_b, lru_lambda)
    y_lru = jax.nn.gelu(lru_g) * h
    y_rwkv = rwkv7_mixer(rw, shift_mu, w0, w2, a0, a2, g2, k_k, k_a, r_k, gn_w, gn_b)
    y = jnp.concatenate([y_lru, y_rwkv.astype(y_lru.dtype)], axis=-1)
    return y @ w_out


def partial_rope(x, positions):
    half = ROPE_DIM // 2
    inv_freq = ROPE_THETA ** (-jnp.arange(0, ROPE_DIM, 2, dtype=F32) / ROPE_DIM)
    ang = positions.astype(F32)[:, :, None] * inv_freq
    cos = jnp.cos(ang)[:, :, None, None, :]
    sin = jnp.sin(ang)[:, :, None, None, :]
    xf = x.astype(F32)
    x1 = xf[..., :half]
    x2 = xf[..., half:ROPE_DIM]
    out = jnp.concatenate([x1 * cos - x2 * sin, x2 * cos + x1 * sin, xf[..., ROPE_DIM:]], axis=-1)
    return out.astype(x.dtype)


def diff_attention(xn, positions, w_qkv, q_norm, k_norm, lq1, lk1, lq2, lk2, subln, w_o, lambda_init):
    B, T, _ = xn.shape
    qkv = xn @ w_qkv
    qd = DIFF_HEADS * 2 * DIFF_HEAD_DIM
    q, k, v = jnp.split(qkv, [qd, 2 * qd], axis=-1)
    q = q.reshape(B, T, DIFF_HEADS, 2, DIFF_HEAD_DIM)
    k = k.reshape(B, T, DIFF_HEADS, 2, DIFF_HEAD_DIM)
    v = v.reshape(B, T, DIFF_HEADS, DIFF_V_DIM).astype(F32)
    q = partial_rope(rms_norm(q, q_norm), positions)
    k = partial_rope(rms_norm(k, k_norm), positions)
    lam = jnp.exp(jnp.sum(lq1.astype(F32) * lk1.astype(F32))) \
        - jnp.exp(jnp.sum(lq2.astype(F32) * lk2.astype(F32))) + lambda_init
    n_blk = T // Q_BLOCK
    qb = jnp.moveaxis(q.reshape(B, n_blk, Q_BLOCK, DIFF_HEADS, 2, DIFF_HEAD_DIM), 1, 0)
    kpos = jnp.arange(T)
    scale = DIFF_HEAD_DIM ** -0.5

    def block(args):
        q_blk, bi = args
        s = jnp.einsum('bqhcd,bkhcd->bhcqk', q_blk, k, preferred_element_type=F32) * scale
        qpos = bi * Q_BLOCK + jnp.arange(Q_BLOCK)
        s = jnp.where(kpos[None, :] <= qpos[:, None], s, -jnp.inf)
        p = jax.nn.softmax(s, axis=-1)
        attn = p[:, :, 0] - lam * p[:, :, 1]
        return jnp.einsum('bhqk,bkhe->bqhe', attn, v)

    o = lax.map(block, (qb, jnp.arange(n_blk)))
    o = jnp.moveaxis(o, 0, 1).reshape(B, T, DIFF_HEADS, DIFF_V_DIM)
    o = rms_norm(o, subln) * (1.0 - lambda_init)
    return o.reshape(B, T, DIFF_HEADS * DIFF_V_DIM).astype(xn.dtype) @ w_o


def moe_swiglu(xn, router, wg, wu, wd):
    B, T, D = xn.shape
    xt = xn.reshape(B * T, D)
    logits = (xt @ router).astype(F32)
    topv, topi = lax.top_k(logits, TOP_K)
    gates = jax.nn.softmax(topv, axis=-1)
    combine = jnp.sum(jax.nn.one_hot(topi, N_EXPERTS, dtype=F32) * gates[..., None], axis=1)
    y = jnp.zeros((B * T, D), F32)
    for e in range(N_EXPERTS):
        y = y + combine[:, e:e + 1] * swiglu(xt, wg[e], wu[e], wd[e]).astype(F32)
    return y.reshape(B, T, D).astype(xn.dtype)


def setup_inputs(seed: int = 0) -> dict:
    key = jax.random.key(seed)
    keys = jax.random.split(key, 64)
    counter = [0]

    def nk():
        kk = keys[counter[0]]
        counter[0] += 1
        return kk

    def nrm(shape, scale):
        return jax.random.normal(nk(), shape, F32) * scale

    def gain(shape):
        return 1.0 + nrm(shape, 0.02)

    ne = (DEPTH + 1) // 2
    no = DEPTH // 2
    D = D_MODEL
    x = jax.random.normal(nk(), (BATCH, SEQ, D), F32)
    positions = jnp.broadcast_to(jnp.arange(SEQ, dtype=jnp.int32), (BATCH, SEQ))
    u = jax.random.uniform(nk(), (ne, LRU_WIDTH), F32, 0.9, 0.999)
    s = u ** (1.0 / LRU_C)
    return {
        'x': x,
        'positions': positions,
        'e_ln_mix': gain((ne, D)),
        'e_w_in': nrm((ne, D, EVEN_IN), D ** -0.5),
        'e_conv_w': nrm((ne, CONV_WIDTH, LRU_WIDTH), CONV_WIDTH ** -0.5),
        'e_conv_b': nrm((ne, LRU_WIDTH), 0.01),
        'e_gate_a_w': nrm((ne, LRU_HEADS, LRU_HEAD_DIM, LRU_HEAD_DIM), LRU_HEAD_DIM ** -0.5),
        'e_gate_a_b': nrm((ne, LRU_WIDTH), 0.01),
        'e_gate_x_w': nrm((ne, LRU_HEADS, LRU_HEAD_DIM, LRU_HEAD_DIM), LRU_HEAD_DIM ** -0.5),
        'e_gate_x_b': nrm((ne, LRU_WIDTH), 0.01),
        'e_lru_lambda': jnp.log(s) - jnp.log1p(-s),
        'e_shift_mu': jax.random.uniform(nk(), (ne, RWKV_IN), F32),
        'e_w0': jax.random.uniform(nk(), (ne, RWKV_WIDTH), F32, -6.0, 1.0),
        'e_w2': nrm((ne, DECAY_LORA, RWKV_WIDTH), 0.1),
        'e_a0': nrm((ne, RWKV_WIDTH), 0.1),
        'e_a2': nrm((ne, AAA_LORA, RWKV_WIDTH), 0.1),
        'e_g2': nrm((ne, GATE_LORA, RWKV_WIDTH), GATE_LORA ** -0.5),
        'e_k_k': 0.85 + nrm((ne, RWKV_WIDTH), 0.02),
        'e_k_a': gain((ne, RWKV_WIDTH)),
        'e_r_k': nrm((ne, RWKV_HEADS, RWKV_HEAD_DIM), 0.1),
        'e_gn_w': gain((ne, RWKV_WIDTH)),
        'e_gn_b': nrm((ne, RWKV_WIDTH), 0.01),
        'e_w_out': nrm((ne, D, D), D ** -0.5),
        'e_ln_ffn': gain((ne, D)),
        'e_ffn_gate': nrm((ne, D, FFN_DIM), D ** -0.5),
        'e_ffn_up': nrm((ne, D, FFN_DIM), D ** -0.5),
        'e_ffn_down': nrm((ne, FFN_DIM, D), FFN_DIM ** -0.5),
        'o_ln_mix': gain((no, D)),
        'o_w_qkv': nrm((no, D, QKV_DIM), D ** -0.5),
        'o_q_norm': gain((no, DIFF_HEAD_DIM)),
        'o_k_norm': gain((no, DIFF_HEAD_DIM)),
        'o_lambda_q1': nrm((no, DIFF_HEAD_DIM), 0.1),
        'o_lambda_k1': nrm((no, DIFF_HEAD_DIM), 0.1),
        'o_lambda_q2': nrm((no, DIFF_HEAD_DIM), 0.1),
        'o_lambda_k2': nrm((no, DIFF_HEAD_DIM), 0.1),
        'o_subln': gain((no, DIFF_V_DIM)),
        'o_w_o': nrm((no, DIFF_HEADS * DIFF_V_DIM, D), D ** -0.5),
        'o_ln_ffn': gain((no, D)),
        'o_router': nrm((no, D, N_EXPERTS), D ** -0.5),
        'o_moe_gate': nrm((no, N_EXPERTS, D, EXPERT_DIM), D ** -0.5),
        'o_moe_up': nrm((no, N_EXPERTS, D, EXPERT_DIM), D ** -0.5),
        'o_moe_down': nrm((no, N_EXPERTS, EXPERT_DIM, D), EXPERT_DIM ** -0.5),
    }


def reference(x, positions, e_ln_mix, e_w_in, e_conv_w, e_conv_b, e_gate_a_w, e_gate_a_b,
              e_gate_x_w, e_gate_x_b, e_lru_lambda, e_shift_mu, e_w0, e_w2, e_a0, e_a2, e_g2,
              e_k_k, e_k_a, e_r_k, e_gn_w, e_gn_b, e_w_out, e_ln_ffn, e_ffn_gate, e_ffn_up,
              e_ffn_down, o_ln_mix, o_w_qkv, o_q_norm, o_k_norm, o_lambda_q1, o_lambda_k1,
              o_lambda_q2, o_lambda_k2, o_subln, o_w_o, o_ln_ffn, o_router, o_moe_gate,
              o_moe_up, o_moe_down):
    for i in range(DEPTH):
        j = i // 2
        if i % 2 == 0:
            h = rms_norm(x, e_ln_mix[j])
            x = x + even_mixer(h, e_w_in[j], e_conv_w[j], e_conv_b[j], e_gate_a_w[j], e_gate_a_b[j],
                               e_gate_x_w[j], e_gate_x_b[j], e_lru_lambda[j], e_shift_mu[j], e_w0[j],
                               e_w2[j], e_a0[j], e_a2[j], e_g2[j], e_k_k[j], e_k_a[j], e_r_k[j],
                               e_gn_w[j], e_gn_b[j], e_w_out[j])
            h = rms_norm(x, e_ln_ffn[j])
            x = x + swiglu(h, e_ffn_gate[j], e_ffn_up[j], e_ffn_down[j])
        else:
            lambda_init = 0.8 - 0.6 * math.exp(-0.3 * i)
            h = rms_norm(x, o_ln_mix[j])
            x = x + diff_attention(h, positions, o_w_qkv[j], o_q_norm[j], o_k_norm[j],
                                   o_lambda_q1[j], o_lambda_k1[j], o_lambda_q2[j], o_lambda_k2[j],
                                   o_subln[j], o_w_o[j], lambda_init)
            h = rms_norm(x, o_ln_ffn[j])
            x = x + moe_swiglu(h, o_router[j], o_moe_gate[j], o_moe_up[j], o_moe_down[j])
    return x
```

```python
import contextlib
import numpy as np
import ml_dtypes
import concourse.bass as bass
import concourse.mybir as mybir
from concourse.alu_op_type import AluOpType as ALU
from concourse.bass_utils import run_bass_kernel_spmd

AF = mybir.ActivationFunctionType
F32 = mybir.dt.float32
BF16 = mybir.dt.bfloat16
I32 = mybir.dt.int32
AX = mybir.AxisListType

COMPUTE = ("pe", "act", "dve", "pool")


class _Op:
    __slots__ = ("eng", "fn", "reads", "writes", "kind", "waits", "signal", "val", "dkey", "seq")

    def __init__(self, eng, fn, reads, writes, kind):
        self.eng = eng
        self.fn = fn
        self.reads = tuple(reads)
        self.writes = tuple(writes)
        self.kind = kind
        self.waits = {}
        self.signal = False
        self.val = None
        self.dkey = None


class Prog:
    def __init__(self, nc):
        self.nc = nc
        self.ops = []
        self.last_w = {}
        self.readers = {}
        self.deps = []

    def _add(self, op):
        deps = set()
        for k in op.reads:
            w = self.last_w.get(k)
            if w is not None:
                deps.add((w, "raw"))
        for k in op.writes:
            w = self.last_w.get(k)
            if w is not None:
                deps.add((w, "waw"))
            for r in self.readers.get(k, ()):
                if r is not op:
                    deps.add((r, "war"))
        for k in op.reads:
            self.readers.setdefault(k, []).append(op)
        for k in op.writes:
            self.last_w[k] = op
            self.readers[k] = []
        op.seq = len(self.ops)
        self.ops.append(op)
        self.deps.append(deps)
        return op

    def add(self, eng, fn, reads=(), writes=()):
        return self._add(_Op(eng, fn, reads, writes, "c"))

    def dma(self, q, fn, reads=(), writes=(), key=None):
        op = _Op(q, fn, reads, writes, "d")
        op.dkey = key
        return self._add(op)

    def emit(self, final_keys=()):
        nc = self.nc
        ops = self.ops
        for op, deps in zip(ops, self.deps):
            for d, kind in deps:
                if d.kind == "c":
                    if d.eng == op.eng and op.kind == "c":
                        if op.eng == "pe" or kind == "war":
                            continue
                    d.signal = True
        finals = [self.last_w[k] for k in final_keys if k in self.last_w]
        for d in finals:
            if d.kind == "c":
                d.signal = True
        cnt = {e: 0 for e in COMPUTE}
        dcnt = {}
        dkeys = []
        for op in ops:
            if op.kind == "c":
                if op.signal:
                    cnt[op.eng] += 1
                    op.val = cnt[op.eng]
            else:
                if op.dkey not in dcnt:
                    dcnt[op.dkey] = 0
                    dkeys.append(op.dkey)
                dcnt[op.dkey] += 16
                op.val = dcnt[op.dkey]
        with contextlib.ExitStack() as st:
            csem = {e: st.enter_context(nc.semaphore("cs_" + e)) for e in COMPUTE}
            dsem = {k: st.enter_context(nc.semaphore("ds%d" % i)) for i, k in enumerate(dkeys)}

            def semof(d):
                return csem[d.eng] if d.kind == "c" else dsem[d.dkey]

            streams = {}
            waited = {}
            for op, deps in zip(ops, self.deps):
                need = {}
                for d, kind in deps:
                    if d.kind == "c" and d.eng == op.eng and op.kind == "c":
                        if op.eng == "pe" or kind == "war":
                            continue
                    s = semof(d)
                    sid = id(s)
                    if need.get(sid, (None, 0))[1] < d.val:
                        need[sid] = (s, d.val)
                w = waited.setdefault(op.eng, {})
                op.waits = []
                for sid, (s, v) in need.items():
                    if w.get(sid, 0) < v:
                        w[sid] = v
                        op.waits.append((s, v))
                streams.setdefault(op.eng, []).append(op)
            fin_waits = []
            for d in finals:
                fin_waits.append((semof(d), d.val))

            def run_stream(name, engine, extra=None):
                for op in streams.get(name, []):
                    for s, v in op.waits:
                        engine.wait_ge(s, v)
                    ins = op.fn(engine)
                    if op.kind == "c":
                        if op.signal:
                            ins.then_inc(csem[op.eng], 1)
                    else:
                        ins.then_inc(dsem[op.dkey], 16)
                if extra:
                    for s, v in extra:
                        engine.wait_ge(s, v)

            with nc.Block() as block:
                @block.tensor
                def _(e):
                    run_stream("pe", e)

                @block.scalar
                def _(e):
                    run_stream("act", e)

                @block.vector
                def _(e):
                    run_stream("dve", e)

                @block.gpsimd
                def _(e):
                    run_stream("pool", e)

                @block.sync
                def _(e):
                    run_stream("sp", e, extra=fin_waits)


class KL(list):
    pass


KEYMAP = {"PS0": KL([("PS0", 0), ("PS0", 1)]), "PS1": KL([("PS1", 0), ("PS1", 1)])}


def _ak(x):
    if isinstance(x, tuple):
        ap, k = x
        return (ap, k if isinstance(k, KL) else KL([k]))
    n = x.tensor.name
    return (x, KEYMAP.get(n) or KL([n]))


class B:
    def __init__(self, nc):
        self.nc = nc
        self.P = Prog(nc)
        self.st = contextlib.ExitStack()
        self.nout = 0

    def sb(self, name, shape, dt=F32):
        return self.st.enter_context(self.nc.sbuf_tensor(name, shape, dt))[:]

    def ps(self, name, shape, dt=F32):
        return self.st.enter_context(self.nc.psum_tensor(name, shape, dt))[:]

    def mm(self, out, lhsT, rhs, start=True, stop=True):
        (o, ok), (l, lk), (r, rk) = _ak(out), _ak(lhsT), _ak(rhs)
        self.P.add("pe", lambda e: e.matmul(o, l, r, start=start, stop=stop), reads=lk + rk, writes=ok)

    def tr(self, out, in_, ident):
        (o, ok), (i, ik), (d, dk) = _ak(out), _ak(in_), _ak(ident)
        self.P.add("pe", lambda e: e.transpose(o, i, d), reads=ik + dk, writes=ok)

    def act(self, out, in_, func, scale=None, bias=None, accum=None, eng="act"):
        (o, ok), (i, ik) = _ak(out), _ak(in_)
        reads = list(ik)
        writes = list(ok)
        kw = {}
        if scale is not None:
            if isinstance(scale, (int, float)):
                kw["scale"] = float(scale)
            else:
                s, sk = _ak(scale)
                kw["scale"] = s
                reads.extend(sk)
        if bias is not None:
            if isinstance(bias, (int, float)):
                kw["bias"] = float(bias)
            else:
                b_, bk = _ak(bias)
                kw["bias"] = b_
                reads.extend(bk)
        if accum is not None:
            a_, ak_ = _ak(accum)
            kw["accum_out"] = a_
            writes.extend(ak_)
        self.P.add("act", lambda e: e.activation(out=o, in_=i, func=func, **kw), reads=reads, writes=writes)

    def tt(self, out, in0, in1, op, eng="dve"):
        (o, ok), (a, ak_), (b_, bk) = _ak(out), _ak(in0), _ak(in1)
        self.P.add(eng, lambda e: e.tensor_tensor(out=o, in0=a, in1=b_, op=op), reads=ak_ + bk, writes=ok)

    def ts(self, out, in0, s1, s2=None, op0=ALU.mult, op1=None, eng="dve", accum=None):
        (o, ok), (a, ak_) = _ak(out), _ak(in0)
        reads = list(ak_)
        writes = list(ok)

        def sc(s):
            if s is None or isinstance(s, (int, float)):
                return s
            ap, k = _ak(s)
            reads.extend(k)
            return ap
        v1, v2 = sc(s1), sc(s2)
        kw = {}
        if op1 is not None:
            kw["op1"] = op1
        if accum is not None:
            a2, a2k = _ak(accum)
            kw["accum_out"] = a2
            writes.extend(a2k)
        self.P.add(eng, lambda e: e.tensor_scalar(out=o, in0=a, scalar1=v1, scalar2=v2, op0=op0, **kw), reads=reads, writes=writes)

    def stt(self, out, in0, scalar, in1, op0, op1):
        (o, ok), (a, ak_), (b_, bk) = _ak(out), _ak(in0), _ak(in1)
        reads = list(ak_ + bk)
        if isinstance(scalar, (int, float)):
            s = float(scalar)
        else:
            s, sk = _ak(scalar)
            reads.extend(sk)
        self.P.add("dve", lambda e: e.scalar_tensor_tensor(out=o, in0=a, scalar=s, in1=b_, op0=op0, op1=op1), reads=reads, writes=ok)

    def copy(self, out, in_, eng="dve"):
        (o, ok), (i, ik) = _ak(out), _ak(in_)
        if eng == "act":
            self.P.add("act", lambda e: e.activation(out=o, in_=i, func=AF.Copy), reads=ik, writes=ok)
        else:
            self.P.add(eng, lambda e: e.tensor_copy(out=o, in_=i), reads=ik, writes=ok)

    def scan(self, out, d0, d1, init):
        (o, ok), (a, ak_), (b_, bk) = _ak(out), _ak(d0), _ak(d1)
        reads = list(ak_ + bk)
        if isinstance(init, (int, float)):
            iv = float(init)
        else:
            iv, ik = _ak(init)
            reads.extend(ik)
        self.P.add("dve", lambda e: e.tensor_tensor_scan(out=o, data0=a, data1=b_, initial=iv, op0=ALU.mult, op1=ALU.add), reads=reads, writes=ok)

    def recip(self, out, in_):
        (o, ok), (i, ik) = _ak(out), _ak(in_)
        self.P.add("dve", lambda e: e.reciprocal(out=o, in_=i), reads=ik, writes=ok)

    def memset(self, out, val, eng="dve"):
        (o, ok) = _ak(out)
        self.P.add(eng, lambda e: e.memset(o, val), writes=ok)

    def load(self, out, in_, q="sp"):
        (o, ok) = _ak(out)
        self.P.dma(q, lambda e: e.dma_start(out=o, in_=in_), writes=ok, key=ok[0])

    def store(self, out_dram, in_, q="sp"):
        (i, ik) = _ak(in_)
        self.nout += 1
        k = ("__out", self.nout)
        self.P.dma(q, lambda e: e.dma_start(out=out_dram, in_=i), reads=ik, writes=[k], key=ik[0])

    def finish(self):
        self.P.emit(final_keys=[("__out", i + 1) for i in range(self.nout)])
        self.st.close()


def rms_tile_T(b, xt, xs, ss, rstd, PT, xnT_dst, identb, junk, eps=1e-6, D=1024):
    b.act(junk, xt, AF.Square, accum=ss)
    b.act(rstd, ss, AF.Sqrt, scale=1.0 / D, bias=eps)
    b.recip(rstd, rstd)
    b.ts(xs, xt, rstd, None, op0=ALU.mult)
    n = D // 128
    for kc in range(n):
        b.tr((PT[:, kc, :], (PT.tensor.name, kc)), xs[:, kc * 128:(kc + 1) * 128], identb)
    b.copy(xnT_dst, (PT[:, :, :], KL([(PT.tensor.name, kc) for kc in range(n)])), eng="act")


T_SEQ = 8192
SEG = 512
CH = 64
C0 = 0.6065306597126334
NV1 = 20


def build_l1(nseg=T_SEQ // SEG, phase=99):
    nc = bass.Bass("TRN2", target_bir_lowering=False)
    dr = lambda n, s, dt=F32, kind="ExternalInput": nc.dram_tensor(n, s, dt, kind=kind).ap()
    x = dr("x", [T_SEQ, 1024])
    w_in = dr("w_in", [1024, 896])
    gmix = dr("gmix", [128, 8])
    cvec = dr("cvec", [128, NV1])
    wa_d = dr("wa", [128, 128])
    wx_d = dr("wx", [128, 128])
    w2a2_d = dr("w2a2", [128, 128])
    g2_d = dr("g2", [128, 128])
    ident_d = dr("ident", [128, 128])
    bones_d = dr("bones", [128, 128])
    maska_d = dr("maska", [128, 512])
    maskb_d = dr("maskb", [128, 256])
    rmask_d = dr("rmask", [128, 512])
    id2_d = dr("id2", [128, 128])
    yT = dr("yT", [256, T_SEQ], kind="ExternalOutput")

    b = B(nc)
    sb, ps = b.sb, b.ps
    wb = sb("wb", [128, 8, 896], BF16)
    wst = [sb("wst%d" % i, [128, 896]) for i in range(2)]
    gm = sb("gm", [128, 8])
    cv = sb("cv", [128, NV1 + 4])
    wa = sb("wa_s", [128, 128]); wx = sb("wx_s", [128, 128])
    w2a2 = sb("w2a2_s", [128, 128]); g2 = sb("g2_s", [128, 128])
    ident = sb("ident_s", [128, 128]); identb = sb("identb", [128, 128], BF16)
    bones = sb("bones_s", [128, 128]); rkbd = sb("rkbd", [128, 128])
    id2 = sb("id2_s", [128, 2, 64])
    maska = sb("maska_s", [128, 512]); maskb = sb("maskb_s", [128, 256]); rmask = sb("rmask_s", [128, 512])
    for t_, d_ in ((gm, gmix), (cv[:, 0:NV1], cvec), (wa, wa_d), (wx, wx_d), (w2a2, w2a2_d), (g2, g2_d), (ident, ident_d),
                   (bones, bones_d), ((id2[:, :, :].rearrange("p h s -> p (h s)"), "id2_s"), id2_d), (maska, maska_d), (maskb, maskb_d), (rmask, rmask_d)):
        b.load(t_, d_)
    b.load(identb, ident_d, q="pool")
    for kc in range(8):
        b.load(wst[kc % 2], w_in[kc * 128:(kc + 1) * 128, :])
        b.ts((wb[:, kc, :], ("wb", kc)), wst[kc % 2], gm[:, kc:kc + 1], None, op0=ALU.mult)
    WBK = lambda kc: ("wb", kc)
    col = lambda i: cv[:, i:i + 1]
    CCH, OMKA, TWOC = NV1, NV1 + 1, NV1 + 2
    b.act(col(CCH), col(7), AF.Exp, scale=-1.0)
    b.act(col(CCH), col(CCH), AF.Ln, bias=1.0)
    b.ts(col(TWOC), col(CCH), -16.0, None, op0=ALU.mult)
    b.ts(col(CCH), col(CCH), -8.0, None, op0=ALU.mult)
    b.ts(col(OMKA), col(16), -1.0, 1.0, op0=ALU.mult, op1=ALU.add)
    b.ts(rkbd, bones, col(17), None, op0=ALU.mult)

    xt = [sb("xt%d" % i, [128, 1024]) for i in range(2)]
    xs = [sb("xs%d" % i, [128, 1024], BF16) for i in range(2)]
    junk = sb("junk", [128, 1024], BF16)
    ss = sb("ss", [128, 1]); rstd = sb("rstd", [128, 1])
    xnT = sb("xnT", [128, 8, SEG], BF16)
    pj = [[sb("pj%d_%d" % (i, c), [128, 4 + SEG]) for c in range(7)] for i in range(2)]
    NT = 27
    tmp = [sb("t%d" % i, [128, SEG]) for i in range(NT)]
    hh = [sb("hh%d" % i, [128, SEG]) for i in range(2)]
    TM = sb("TM", [64, 2, 4, 128])
    SA = sb("SA", [64, 2, 4, 2, 64]); SBm = sb("SBm", [64, 256])
    PQ = [sb("PQ%d" % i, [64, 2, 2, 2, 64]) for i in range(2)]
    Tb = [sb("Tb%d" % i, [64, 2, 2, 64]) for i in range(2)]
    ZS = sb("ZS", [64, 2, 2, 64]); MS = sb("MS", [64, 2, 2, 2, 64])
    GP = sb("GP", [64, 2, 2, 2, 64]); GS = GP[:, 0, :, :, :]; PSm = GP[:, 1, :, :, :]
    STT = [sb("STT%d" % i, [64, 2, 64]) for i in range(2)]
    AFx = sb("AFx", [128, SEG // CH, 2, CH]); RFx = sb("RFx", [128, SEG // CH, 2, CH]); BTx = sb("BTx", [128, SEG // CH, 2, CH])
    RFh = sb("RFh", [64, 2, SEG]); WCh = sb("WCh", [64, 2, SEG // CH])
    OS = sb("OS", [128, SEG])
    PP = ps("PP", [128, 512]); PT = ps("PT", [128, 8, 128], BF16)
    PS0 = ps("PS0", [128, 512]); PS1 = ps("PS1", [128, 512])
    PA = ps("PA", [128, 512]); PC = ps("PC", [128, 512]); PX = ps("PX", [128, 512]); PO = ps("PO", [128, 512])

    for i in range(2):
        for c in range(7):
            b.memset((pj[i][c][:, 0:4], ("pjh", i, c)), 0.0)
    b.memset((STT[0][:, :, :], "STT0"), 0.0)
    for tx in (AFx, RFx, BTx):
        b.memset((tx[:, :, :, :], tx.tensor.name), 0.0, eng="pool")

    st_i = 0
    PS0a_ = ("PS0", 0)
    for s in range(nseg):
        cur = s % 2
        nxt = 1 - cur
        pjc = pj[cur]
        PJ = lambda c: ("pj", cur, c)
        PJH = lambda c: ("pjh", cur, c)
        for j in range(4):
            tix = (s * 4 + j) % 2
            b.load(xt[tix], x[s * SEG + j * 128: s * SEG + (j + 1) * 128, :])
            rms_tile_T(b, xt[tix], xs[tix], ss, rstd, PT, (xnT[:, :, j * 128:(j + 1) * 128], ("xnT", j)), identb, junk)
        XN = KL([("xnT", j) for j in range(4)])
        for cc in range(7):
            for kc in range(8):
                b.mm(PP, (wb[:, kc, cc * 128:(cc + 1) * 128], WBK(kc)), (xnT[:, kc, :], XN), start=(kc == 0), stop=(kc == 7))
            b.copy((pjc[cc][:, 4:4 + SEG], PJ(cc)), PP, eng=("act" if cc % 2 == 0 else "dve"))
            b.copy((pj[nxt][cc][:, 0:4], ("pjh", nxt, cc)), (pjc[cc][:, SEG:SEG + 4], PJ(cc)), eng="pool")
        if phase < 2:
            continue
        cur_v = lambda c: (pjc[c][:, 4:4 + SEG], PJ(c))
        sh_v = lambda c, k: (pjc[c][:, 4 - k:4 - k + SEG], KL([PJ(c), PJH(c)]))
        t = tmp
        xc = t[0]
        b.ts(xc, sh_v(0, 3), col(0), col(4), op0=ALU.mult, op1=ALU.add)
        b.stt(xc, sh_v(0, 2), col(1), xc, ALU.mult, ALU.add)
        b.stt(xc, sh_v(0, 1), col(2), xc, ALU.mult, ALU.add)
        b.stt(xc, cur_v(0), col(3), xc, ALU.mult, ALU.add)
        b.mm(PS0, wa, xc)
        b.mm(PS1, wx, xc)
        ra = t[1]
        b.act(ra, PS0, AF.Sigmoid, bias=col(5))
        av_ = t[2]
        b.act(av_, ra, AF.Exp, scale=col(CCH))
        a2_ = t[3]
        b.act(a2_, ra, AF.Exp, scale=col(TWOC))
        b.act(a2_, a2_, AF.Sqrt, scale=-1.0, bias=1.0)
        ix = t[1]
        b.act(ix, PS1, AF.Sigmoid, bias=col(6))
        b.tt(a2_, a2_, ix, ALU.mult)
        b.tt(a2_, a2_, xc, ALU.mult)
        hcur = hh[cur]
        b.scan(hcur, av_, a2_, 0.0 if s == 0 else hh[nxt][:, SEG - 1:SEG])
        gq = t[0]
        b.act(gq, cur_v(1), AF.Square)
        b.ts(gq, gq, 0.044715, 1.0, op0=ALU.mult, op1=ALU.add)
        b.tt(gq, gq, cur_v(1), ALU.mult)
        b.act(gq, gq, AF.Sigmoid, scale=1.5957691216057308)
        b.tt(gq, gq, cur_v(1), ALU.mult)
        ylru = t[1]
        b.tt(ylru, gq, hcur, ALU.mult)
        b.store(yT[0:128, s * SEG:(s + 1) * SEG], ylru)
        if phase < 3:
            continue
        shf = []
        for i, c in enumerate((2, 3, 4, 5, 6)):
            d = t[4 + i]
            b.tt(d, sh_v(c, 1), cur_v(c), ALU.subtract)
            b.stt(d, d, col(8 + i), cur_v(c), ALU.mult, ALU.add)
            shf.append(d)
        rs, ks, vs, xwa, xgs = shf
        tw = t[9]
        b.act(tw[0:64, :], xwa[0:64, :], AF.Tanh)
        b.mm(PS0, w2a2[0:64, :], tw[0:64, :])
        b.mm(PS1, w2a2[64:128, :], xwa[64:128, :])
        sgz = t[9]
        b.act(sgz, PS0, AF.Sigmoid, bias=col(13))
        avv = t[10]
        b.act(avv, PS1, AF.Sigmoid, bias=col(14))
        sg = t[11]
        b.act(sg, xgs, AF.Sigmoid)
        cs = t[12]
        b.scan(cs, rmask, sgz, 0.0)
        csm1 = t[13]
        b.tt(csm1, cs, sgz, ALU.subtract)
        Wt = t[14]; iW = t[15]; Wm1 = t[13]
        b.act(Wt, cs, AF.Exp, scale=-C0)
        b.act(iW, cs, AF.Exp, scale=C0)
        b.act(Wm1, csm1, AF.Exp, scale=-C0)
        b.mm(PS0, g2, sg)
        gv = t[11]
        b.copy(gv, PS0, eng="act")
        kq = t[9]
        b.ts(kq, ks, col(15), None, op0=ALU.mult)
        kq2 = t[12]
        b.act(kq2, kq, AF.Square)
        b.mm(PS1, bones, kq2)
        rn = t[12]
        b.act(rn, PS1, AF.Sqrt)
        b.ts(rn, rn, 1e-12, None, op0=ALU.max)
        b.recip(rn, rn)
        kkn = t[9]
        b.tt(kkn, kq, rn, ALU.mult)
        kmod = t[12]
        b.ts(kmod, avv, col(16), col(OMKA), op0=ALU.mult, op1=ALU.add)
        b.tt(kmod, kmod, ks, ALU.mult)
        bb = t[10]
        b.tt(bb, kkn, avv, ALU.mult)
        AFm = t[16]; RF = t[17]; BT = t[18]; KT = t[19]; Bh = t[20]; Kh = t[21]
        b.stt(AFm, kkn, -1.0, Wm1, ALU.mult, ALU.mult)
        b.tt(RF, rs, Wt, ALU.mult)
        b.tt(BT, bb, iW, ALU.mult)
        b.tt(KT, kmod, iW, ALU.mult)
        v3 = lambda tl: tl[:, :].rearrange("p (c s) -> p c s", s=CH)
        wcb = (v3(Wt)[:, :, CH - 1:CH].broadcast_to([128, SEG // CH, CH]), Wt.tensor.name)
        b.tt((v3(Bh), Bh.tensor.name), (v3(BT), BT.tensor.name), wcb, ALU.mult)
        b.tt((v3(Kh), Kh.tensor.name), (v3(KT), KT.tensor.name), wcb, ALU.mult)
        rk_ = t[9]
        b.tt(rk_, rs, kmod, ALU.mult)
        b.mm(PS1, rkbd, rk_)
        bonus = t[22]
        b.tt(bonus, PS1, vs, ALU.mult)
        if phase < 4:
            continue
        for h in range(2):
            b.mm((PS1[0:64, :], KEYMAP["PS1"]), ident[:, 64 * h:64 * h + 64], RF)
            b.copy((RFh[:, h, :], "RFh"), (PS1[0:64, :], KEYMAP["PS1"]), eng="act")
            b.mm((PS0[0:64, h * 8:h * 8 + 8], PS0a_), ident[:, 64 * h:64 * h + 64], (v3(Wt)[:, :, CH - 1], Wt.tensor.name))
        b.copy((WCh[:, :, :].rearrange("p h c -> p (h c)"), "WCh"), (PS0[0:64, 0:16], PS0a_), eng="act")
        for tl, tx in ((AFm, AFx), (RF, RFx), (BT, BTx)):
            b.copy((tx[0:64, :, 0, :], tx.tensor.name), (v3(tl)[0:64], tl.tensor.name), eng="pool")
            b.copy((tx[64:128, :, 1, :], tx.tensor.name), (v3(tl)[64:128], tl.tensor.name), eng="pool")
        PS0a, PS0b, PS1a, PS1b = ("PS0", 0), ("PS0", 1), ("PS1", 0), ("PS1", 1)
        sel = [ident[:, 0:64], ident[:, 64:128]]
        for cp in range(SEG // 128):
            tok = slice(cp * 128, (cp + 1) * 128)
            for q in range(2):
                c_ = cp * 2 + q
                ck = slice(c_ * CH, (c_ + 1) * CH)
                for qi, src in enumerate((AFm, Bh, Kh, vs)):
                    b.tr((PX[0:64, qi * 128:(qi + 1) * 128], "PX"), src[:, ck], ident)
                b.copy((TM[:, q, :, :].rearrange("p a b -> p (a b)"), ("TM", q)), (PX[0:64, :], "PX"), eng="act")
                for j, (l_, rx) in enumerate(((BT, AFx), (BT, RFx), (KT, AFx), (KT, RFx))):
                    b.mm((PA[0:64, j * 128:(j + 1) * 128], "PA"), l_[:, ck], rx[:, c_, :, :].rearrange("p h s -> p (h s)"))
                b.mm((PS1[0:64, q * 128:(q + 1) * 128], PS1a), AFm[:, ck], BTx[:, c_, :, :].rearrange("p h s -> p (h s)"))
                b.tt((SA[:, q, :, :, :].rearrange("p j h s -> p (j h s)"), ("SA", q)), (PA[0:64, :], "PA"), maska[0:64, :], ALU.mult)
            b.tt(SBm, (PS1[0:64, 0:256], PS1a), maskb[0:64, :], ALU.mult)
            TMv = lambda qi, q, h: (TM[:, q, qi, 64 * h:64 * h + 64], ("TM", q))
            SAv = lambda j, q, h: (SA[:, q, j, h, :], ("SA", q))
            SAK = KL([("SA", 0), ("SA", 1)])
            QH = [(q, h) for q in range(2) for h in range(2)]
            for q, h in QH:
                b.mm((PS1[0:64, 256 + (q * 2 + h) * 64:256 + (q * 2 + h) * 64 + 64], PS1b), SAv(2, q, h), TMv(3, q, h))
            b.copy((ZS[:, :, :, :].rearrange("p q h s -> p (q h s)"), "ZS"), (PS1[0:64, 256:512], PS1b), eng="act")
            if phase < 5:
                continue
            b.copy((PQ[0][:, 0, :, :, :], "PQ0"), (SA[:, :, 0, :, :], SAK), eng="pool")
            b.copy((PQ[0][:, 1, :, :, :].rearrange("p q h s -> p (q h s)"), "PQ0"), SBm, eng="pool")
            idb = (ident[0:64, 0:64].rearrange("p (a c s) -> p a c s", a=1, c=1).broadcast_to([64, 2, 2, 64]), "ident_s")
            b.tt((Tb[0][:, :, :, :], "Tb0"), (SA[:, :, 0, :, :], SAK), idb, ALU.add)
            PC5 = PC[0:64, :].rearrange("p (j q h s) -> p j q h s", j=2, q=2, h=2)
            POt = PO[0:64, 256:512].rearrange("p (q h s) -> p q h s", q=2, h=2)
            for k in range(0, 6):
                i = k % 2
                pqk = "PQ%d" % i
                if k >= 1:
                    for q, h in QH:
                        b.mm((POt[:, q, h, :], ("PO", 2)), (PQ[i][:, 1, q, h, :], pqk), (Tb[1 - i][:, q, h, :], "Tb%d" % (1 - i)))
                if k <= 3:
                    for q, h in QH:
                        b.mm((PC5[:, 0, q, h, :], ("PC", 0)), (PQ[i][:, 1, q, h, :], pqk), (PQ[i][:, 0, q, h, :], pqk))
                if k <= 4:
                    for q, h in QH:
                        b.mm((PC5[:, 1, q, h, :], ("PC", 1)), (PQ[i][:, 0, q, h, :], pqk), (PQ[i][:, 1, q, h, :], pqk))
                if k <= 3:
                    b.copy((PQ[1 - i][:, :, :, :, :].rearrange("p j q h s -> p (j q h s)"), "PQ%d" % (1 - i)),
                           (PC[0:64, :], KL([("PC", 0), ("PC", 1)])), eng="act")
                elif k == 4:
                    b.copy((PQ[1 - i][:, 1, :, :, :].rearrange("p q h s -> p (q h s)"), "PQ%d" % (1 - i)),
                           (PC[0:64, 256:512], ("PC", 1)), eng="act")
                if k >= 1:
                    b.tt((Tb[i][:, :, :, :].rearrange("p q h s -> p (q h s)"), "Tb%d" % i),
                         (Tb[1 - i][:, :, :, :].rearrange("p q h s -> p (q h s)"), "Tb%d" % (1 - i)),
                         (PO[0:64, 256:512], ("PO", 2)), ALU.add)
            if phase < 5.2:
                continue
            for q, h in QH:
                o0 = ((q * 2 + h) * 2) * 64
                b.mm((PX[0:64, o0:o0 + 64], "PX"), (Tb[1][:, q, h, :], "Tb1"), TMv(0, q, h))
                b.mm((PX[0:64, o0 + 64:o0 + 128], "PX"), (Tb[1][:, q, h, :], "Tb1"), (ZS[:, q, h, :], "ZS"))
            b.copy((MS[:, :, :, :, :].rearrange("p q h m s -> p (q h m s)"), "MS"), (PX[0:64, :], "PX"), eng="act")
            M1T = lambda q, h: (MS[:, q, h, 0, :], "MS")
            M2T = lambda q, h: (MS[:, q, h, 1, :], "MS")
            if phase < 5.4:
                continue
            for q, h in QH:
                o0 = (q * 2 + h) * 64
                b.mm((PS0[0:64, o0:o0 + 64], PS0a), M1T(q, h), TMv(1, q, h))
                b.mm((PS0[0:64, 256 + o0:256 + o0 + 64], PS0b), M1T(q, h), SAv(1, q, h))
            for q, h in QH:
                c_ = cp * 2 + q
                o0 = (q * 2 + h) * 64
                b.stt((GS[:, q, h, :], "GS"), ident[0:64, 0:64], (WCh[:, h, c_:c_ + 1], "WCh"), (PS0[0:64, o0:o0 + 64], PS0a), ALU.mult, ALU.add)
            b.tt((PSm[:, :, :, :], "PSm"), (PS0[0:64, 256:512].rearrange("p (q h s) -> p q h s", q=2, h=2), PS0b),
                 (RFh[:, :, tok].rearrange("p h (q s) -> p q h s", q=2), "RFh"), ALU.add)
            if phase < 5.6:
                continue
            for q in range(2):
                ocol = slice(q * 64, q * 64 + 64)
                stn = "STT%d" % st_i
                for h in range(2):
                    if 5.8 <= phase < 5.9:
                        continue
                    pr = slice(64 * h, 64 * h + 64)
                    b.mm((PO[pr, ocol], ("PO", 0)), M2T(q, h), SAv(1, q, h), start=True, stop=False)
                    b.mm((PO[pr, ocol], ("PO", 0)), TMv(3, q, h), SAv(3, q, h), start=False, stop=False)
                    b.mm((PO[pr, ocol], ("PO", 0)), (STT[st_i][:, h, :], stn), (PSm[:, q, h, :], "PSm"), start=False, stop=True)
                for h in range(2):
                    if phase == 5.7:
                        continue
                    so = slice(h * 64, h * 64 + 64)
                    b.mm((PS1[0:64, so], PS1a), TMv(1, q, h), M2T(q, h), start=True, stop=False)
                    b.mm((PS1[0:64, so], PS1a), TMv(2, q, h), TMv(3, q, h), start=False, stop=False)
                    b.mm((PS1[0:64, so], PS1a), (GS[:, q, h, :], "GS"), (STT[st_i][:, h, :], stn), start=False, stop=True)
                b.copy((STT[1 - st_i][:, :, :].rearrange("p h s -> p (h s)"), "STT%d" % (1 - st_i)), (PS1[0:64, 0:128], PS1a), eng="dve")
                st_i = 1 - st_i
            b.copy((OS[:, tok], ("OS", cp)), (PO[:, 0:128], ("PO", 0)), eng="dve")
        if phase < 6:
            continue
        OSK = KL([("OS", i) for i in range(4)])
        b.mm(PS0, bones, (OS[:, :], OSK))
        cen = t[23]
        b.stt(cen, PS0, -1.0 / 64, (OS[:, :], OSK), ALU.mult, ALU.add)
        sq = t[24]
        b.act(sq, cen, AF.Square)
        b.mm(PS1, bones, sq)
        b.act(sq, PS1, AF.Sqrt, scale=1.0 / 64, bias=64e-5)
        b.recip(sq, sq)
        b.tt(cen, cen, sq, ALU.mult)
        b.ts(cen, cen, col(18), col(19), op0=ALU.mult, op1=ALU.add)
        b.tt(cen, cen, bonus, ALU.add)
        yrw = t[25 + (s % 2)]
        b.tt(yrw, cen, gv, ALU.mult)
        b.store(yT[128:256, s * SEG:(s + 1) * SEG], yrw)
    b.finish()
    return nc


def _consts_l1():
    p = np.arange(128)
    ident = np.eye(128, dtype=np.float32)
    bones = (p[:, None] // 64 == p[None, :] // 64).astype(np.float32)
    s_ = (p % 64)[:, None]
    t_ = np.arange(64)[None, :]
    lt = (s_ < t_).astype(np.float32)
    le = (s_ <= t_).astype(np.float32)
    gt = (s_ > t_).astype(np.float32)
    eq = (s_ == t_).astype(np.float32)
    maska = np.concatenate([lt, lt, le, le, lt, lt, le, le], axis=1)
    maskb = np.concatenate([gt, gt, gt, gt], axis=1)
    id2 = np.concatenate([eq, eq], axis=1)
    rmask = np.ones((128, 512), np.float32)
    rmask[:, ::64] = 0.0
    return dict(ident=ident, bones=bones, maska=np.ascontiguousarray(maska), maskb=np.ascontiguousarray(maskb),
                id2=np.ascontiguousarray(id2), rmask=rmask)


def _blockdiag2(w2):
    o = np.zeros((128, 128), np.float32)
    o[0:64, 0:64] = w2[0]
    o[64:128, 64:128] = w2[1]
    return o


def l1_inputs(inp, bi, g):
    f = lambda k: np.asarray(inp[k][0], np.float32)
    ls = slice(g * 128, (g + 1) * 128)
    W = f("e_w_in")
    rw0 = 1024
    cols = np.concatenate([np.arange(512)[ls], 512 + np.arange(512)[ls], rw0 + np.arange(512)[ls], rw0 + 512 + np.arange(512)[ls],
                           rw0 + 1024 + np.arange(512)[ls], rw0 + 1536 + np.arange(128), rw0 + 1664 + np.arange(128)])
    mu = f("e_shift_mu")
    cvec = np.zeros((128, NV1), np.float32)
    cvec[:, 0:4] = f("e_conv_w")[:, ls].T
    cvec[:, 4] = f("e_conv_b")[ls]
    cvec[:, 5] = f("e_gate_a_b")[ls]
    cvec[:, 6] = f("e_gate_x_b")[ls]
    cvec[:, 7] = f("e_lru_lambda")[ls]
    cvec[:, 8] = mu[0:512][ls]
    cvec[:, 9] = mu[512:1024][ls]
    cvec[:, 10] = mu[1024:1536][ls]
    cvec[:, 11] = mu[1536:1664]
    cvec[:, 12] = mu[1664:1792]
    cvec[:, 13] = f("e_w0")[ls]
    cvec[:, 14] = f("e_a0")[ls]
    cvec[:, 15] = f("e_k_k")[ls]
    cvec[:, 16] = f("e_k_a")[ls]
    cvec[:, 17] = f("e_r_k").reshape(-1)[ls]
    cvec[:, 18] = f("e_gn_w")[ls]
    cvec[:, 19] = f("e_gn_b")[ls]
    d = dict(
        x=np.ascontiguousarray(inp["x"][bi]),
        w_in=np.ascontiguousarray(W[:, cols]),
        gmix=np.ascontiguousarray(f("e_ln_mix").reshape(8, 128).T),
        cvec=cvec,
        wa=_blockdiag2(f("e_gate_a_w")[2 * g:2 * g + 2]),
        wx=_blockdiag2(f("e_gate_x_w")[2 * g:2 * g + 2]),
        w2a2=np.ascontiguousarray(np.concatenate([f("e_w2")[:, ls], f("e_a2")[:, ls]], axis=0)),
        g2=np.ascontiguousarray(f("e_g2")[:, ls]),
    )
    d.update(_consts_l1())
    return d


def build_ffn(NT=2048, F=2816, E=1, G=512, moe=False, ngroups=None, phase=99):
    nc = bass.Bass("TRN2", target_bir_lowering=False)
    dr = lambda n, s, dt=F32, kind="ExternalInput": nc.dram_tensor(n, s, dt, kind=kind).ap()
    nF = F // 128
    TG = G // 128
    xres = dr("xres", [NT, 1024])
    aT_d = dr("aT", [1024, NT])
    wproj = dr("wproj", [1024, 1024])
    gain_d = dr("gain", [1, 1024])
    wg_d = dr("wg", [E, 1024, F])
    wu_d = dr("wu", [E, 1024, F])
    wd_d = dr("wd", [E, F, 1024])
    ident_d = dr("ident", [128, 128])
    if moe:
        router_d = dr("router", [1024, 8])
    out = dr("out", [NT, 1024], kind="ExternalOutput")

    b = B(nc)
    sb, ps = b.sb, b.ps
    wo = sb("wo", [128, 8, 1024], BF16)
    gbc = sb("gbc", [128, 1024])
    ident = sb("ident_s", [128, 128])
    identb = sb("identb", [128, 128], BF16)
    b.load(gbc, gain_d.partition_broadcast(128))
    b.load(ident, ident_d)
    b.load(identb, ident_d, q="pool")
    wpv = wproj.rearrange("(kc p) n -> p kc n", p=128)
    for kc in range(8):
        b.load((wo[:, kc, :], ("wo", kc)), wpv[:, kc, :], q="pool")
    WOK = lambda kc: ("wo", kc)
    if moe:
        rt = sb("rt", [128, 8, 8])
        b.load(rt, router_d.rearrange("(kc p) e -> p kc e", p=128))
        hT32 = sb("hT32", [128, 8, 128])
        lg = sb("lg", [128, 8]); lg2 = sb("lg2", [128, 8]); mk1 = sb("mk1", [128, 8]); mk2 = sb("mk2", [128, 8])
        sm = sb("sm", [128, 8])
        comb = sb("comb", [128, TG, 8])
        xs32 = sb("xs32", [128, 1024])
        PTa = ps("PTa", [128, 4, 128]); PTb = ps("PTb", [128, 4, 128])
    else:
        xs = sb("xs", [128, 1024], BF16)
        PT = ps("PT", [128, 8, 128], BF16)
    xt = [sb("xt%d" % i, [128, 1024]) for i in range(2)]
    at = [sb("at%d" % i, [128, 8, 128], BF16) for i in range(2)]
    acc = [sb("acc%d" % i, [128, 1024]) for i in range(TG)]
    junk = sb("junk", [128, 1024], BF16)
    ss = sb("ss", [128, 1]); rstd = sb("rstd", [128, 1])
    hT = sb("hT", [128, 8, G], BF16)
    hid = sb("hid", [128, nF, G], BF16)
    NWB = 3
    wgb = [sb("wgb%d" % i, [128, 8, 128], BF16) for i in range(NWB)]
    wub = [sb("wub%d" % i, [128, 8, 128], BF16) for i in range(NWB)]
    wdh = [sb("wdh%d" % i, [128, nF, 512], BF16) for i in range(2)]
    sg = [sb("sg%d" % i, [128, G]) for i in range(2)]
    PP = [ps("PP%d" % i, [128, 512]) for i in range(2)]
    PG = [ps("PG%d" % i, [128, 512]) for i in range(2)]
    PU = [ps("PU%d" % i, [128, 512]) for i in range(2)]
    aTv = aT_d.rearrange("(kc p) t -> p kc t", p=128)
    wi = 0
    di = 0
    for g in range(ngroups if ngroups is not None else NT // G):
        for j in range(TG):
            tok0 = g * G + j * 128
            x_ = xt[j % 2]
            a_ = at[j % 2]
            b.load(x_, xres[tok0:tok0 + 128, :])
            b.load(a_, aTv[:, :, tok0:tok0 + 128], q="pool")
            for n in range(2):
                for kc in range(8):
                    b.mm(PP[n], a_[:, kc, :], (wo[:, kc, n * 512:(n + 1) * 512], WOK(kc)), start=(kc == 0), stop=(kc == 7))
                b.tt((acc[j][:, n * 512:(n + 1) * 512], ("acc", j, n)), PP[n], x_[:, n * 512:(n + 1) * 512], ALU.add)
            ACCK = KL([("acc", j, 0), ("acc", j, 1)])
            b.act(junk, (acc[j], ACCK), AF.Square, accum=ss)
            b.act(rstd, ss, AF.Sqrt, scale=1.0 / 1024, bias=1e-6)
            b.recip(rstd, rstd)
            if not moe:
                b.stt(xs, (acc[j], ACCK), rstd, gbc, ALU.mult, ALU.mult)
                for kc in range(8):
                    b.tr((PT[:, kc, :], ("PT", kc)), xs[:, kc * 128:(kc + 1) * 128], identb)
                b.copy((hT[:, :, j * 128:(j + 1) * 128], ("hT", j)), (PT, KL([("PT", kc) for kc in range(8)])), eng="act")
            else:
                b.stt(xs32, (acc[j], ACCK), rstd, gbc, ALU.mult, ALU.mult)
                for kc in range(8):
                    pt_ = PTa if kc < 4 else PTb
                    b.tr((pt_[:, kc % 4, :], (pt_.tensor.name, kc % 4)), xs32[:, kc * 128:(kc + 1) * 128], ident)
                for kc in range(8):
                    pt_ = PTa if kc < 4 else PTb
                    pass
                b.copy((hT32[:, 0:4, :], ("hT32", 0)), (PTa, KL([("PTa", i) for i in range(4)])), eng="act")
                b.copy((hT32[:, 4:8, :], ("hT32", 1)), (PTb, KL([("PTb", i) for i in range(4)])), eng="dve")
                H32 = KL([("hT32", 0), ("hT32", 1)])
                b.copy((hT[:, :, j * 128:(j + 1) * 128], ("hT", j)), (hT32, H32), eng="act")
                for kc in range(8):
                    b.mm((PP[0][:, 0:8], "PP0"), (hT32[:, kc, :], ("hT32", kc // 4)), rt[:, kc, :], start=(kc == 0), stop=(kc == 7))
                b.copy(lg, (PP[0][:, 0:8], "PP0"))
                b.P.add("dve", (lambda o, i: (lambda e: e.reduce_max(out=o, in_=i, axis=AX.X)))(sm[:, 0:1], lg), reads=["lg"], writes=["sm"])
                b.ts(mk1, lg, sm[:, 0:1], None, op0=ALU.is_equal)
                b.stt(lg2, mk1, -1e30, lg, ALU.mult, ALU.add)
                b.P.add("dve", (lambda o, i: (lambda e: e.reduce_max(out=o, in_=i, axis=AX.X)))(sm[:, 1:2], lg2), reads=["lg2"], writes=["sm"])
                b.ts(mk2, lg2, sm[:, 1:2], None, op0=ALU.is_equal)
                b.ts(sm[:, 2:3], sm[:, 0:1], -1.0, None, op0=ALU.mult)
                b.act(sm[:, 3:4], sm[:, 1:2], AF.Exp, bias=sm[:, 2:3])
                b.ts(sm[:, 4:5], sm[:, 3:4], 1.0, None, op0=ALU.add)
                b.recip(sm[:, 4:5], sm[:, 4:5])
                b.tt(sm[:, 5:6], sm[:, 3:4], sm[:, 4:5], ALU.mult)
                b.ts(mk1, mk1, sm[:, 4:5], None, op0=ALU.mult)
                b.stt((comb[:, j, :], ("comb", j)), mk2, sm[:, 5:6], mk1, ALU.mult, ALU.add)
        HT = KL([("hT", j) for j in range(TG)])
        for e in range(E if phase >= 2 else 0):
            wgv = wg_d[e].rearrange("(kc p) f -> p kc f", p=128)
            wuv = wu_d[e].rearrange("(kc p) f -> p kc f", p=128)
            wdv = wd_d[e].rearrange("(fc p) n -> p fc n", p=128)
            for fc in range(nF):
                w1, w2 = wgb[wi % NWB], wub[wi % NWB]
                pg, pu, sg_ = PG[wi % 2], PU[wi % 2], sg[wi % 2]
                wi += 1
                b.load(w1, wgv[:, :, fc * 128:(fc + 1) * 128], q="pool")
                b.load(w2, wuv[:, :, fc * 128:(fc + 1) * 128], q="pool")
                for kc in range(8):
                    b.mm(pg[:, 0:G], w1[:, kc, :], (hT[:, kc, :], HT), start=(kc == 0), stop=(kc == 7))
                for kc in range(8):
                    b.mm(pu[:, 0:G], w2[:, kc, :], (hT[:, kc, :], HT), start=(kc == 0), stop=(kc == 7))
                b.act(sg_, pg[:, 0:G], AF.Silu)
                b.tt((hid[:, fc, :], ("hid", fc)), sg_, pu[:, 0:G], ALU.mult)
            HID = KL([("hid", fc) for fc in range(nF)])
            for n in range(2 if phase >= 3 else 0):
                wd_ = wdh[di % 2]
                di += 1
                for q4 in range(4):
                    f0, f1 = (nF * q4) // 4, (nF * (q4 + 1)) // 4
                    b.load((wd_[:, f0:f1, :], (wd_.tensor.name, q4)), wdv[:, f0:f1, n * 512:(n + 1) * 512], q="pool")
                WDK = KL([(wd_.tensor.name, q4) for q4 in range(4)])
                for j in range(TG):
                    pp = PP[j % 2]
                    for fc in range(nF):
                        b.mm(pp, (hid[:, fc, j * 128:(j + 1) * 128], HID), (wd_[:, fc, :], WDK), start=(fc == 0), stop=(fc == nF - 1))
                    av = (acc[j][:, n * 512:(n + 1) * 512], ("acc", j, n))
                    if moe:
                        b.stt(av, pp, (comb[:, j, e:e + 1], ("comb", j)), av, ALU.mult, ALU.add)
                    else:
                        b.tt(av, pp, av, ALU.add)
        for j in range(TG):
            tok0 = g * G + j * 128
            b.store(out[tok0:tok0 + 128, :], (acc[j], KL([("acc", j, 0), ("acc", j, 1)])))
    b.finish()
    return nc


LAMBDA_INIT1 = 0.8 - 0.6 * float(np.exp(-0.3 * 1))
TWO_PI = 6.283185307179586
CW1 = 6.28125
CW2 = TWO_PI - 6.28125
MAGIC = 12582912.0


def build_l3(nblk=T_SEQ // 512, phase=99):
    nc = bass.Bass("TRN2", target_bir_lowering=False)
    dr = lambda n, s, dt=F32, kind="ExternalInput": nc.dram_tensor(n, s, dt, kind=kind).ap()
    x = dr("x", [T_SEQ, 1024])
    pos_d = dr("pos", [1, T_SEQ], I32)
    w_d = dr("w", [1024, 768])
    gmix = dr("gmix", [128, 8])
    cvec = dr("cvec", [128, 4])
    lams = dr("lams", [1, 256])
    ident_d = dr("ident", [128, 128])
    bones_d = dr("bones", [128, 128])
    rot_d = dr("rot", [128, 128])
    cmask_d = dr("cmask", [128, 4 * 512])
    oT = dr("oT", [256, T_SEQ], kind="ExternalOutput")

    b = B(nc)
    sb, ps = b.sb, b.ps
    wb = sb("wb", [128, 8, 768], BF16)
    wst = [sb("wst%d" % i, [128, 768]) for i in range(2)]
    gm = sb("gm", [128, 8]); cv = sb("cv", [128, 8])
    ident = sb("ident_s", [128, 128]); identb = sb("identb", [128, 128], BF16)
    bones = sb("bones_s", [128, 128]); rot = sb("rot_s", [128, 128])
    ones_f = sb("ones_f", [128, 128]); ones_b = sb("ones_b", [128, 128], BF16)
    cmask = sb("cmask_s", [128, 4, 512], BF16)
    lm = sb("lm", [128, 256]); lmp = sb("lmp", [128, 128]); lsc = sb("lsc", [128, 8])
    for t_, d_ in ((gm, gmix), (cv[:, 0:4], cvec), (ident, ident_d), (bones, bones_d), (rot, rot_d), (lm, lams.partition_broadcast(128))):
        b.load(t_, d_)
    b.load(identb, ident_d, q="pool")
    b.load((cmask[:, :, :].rearrange("p a b -> p (a b)"), "cmask_s"), cmask_d, q="pool")
    b.memset(ones_f, 1.0)
    b.memset(ones_b, 1.0)
    for kc in range(8):
        b.load(wst[kc % 2], w_d[kc * 128:(kc + 1) * 128, :])
        b.ts((wb[:, kc, :], ("wb", kc)), wst[kc % 2], gm[:, kc:kc + 1], None, op0=ALU.mult)
    WBK = lambda kc: ("wb", kc)
    col = lambda i: cv[:, i:i + 1]
    b.tt((lmp[:, 0:64], "lmp"), lm[:, 0:64], lm[:, 64:128], ALU.mult)
    b.tt((lmp[:, 64:128], "lmp"), lm[:, 128:192], lm[:, 192:256], ALU.mult)
    b.P.add("dve", lambda e: e.reduce_sum(out=lsc[:, 0:1], in_=lmp[:, 0:64], axis=AX.X), reads=["lmp"], writes=["lsc"])
    b.P.add("dve", lambda e: e.reduce_sum(out=lsc[:, 1:2], in_=lmp[:, 64:128], axis=AX.X), reads=["lmp"], writes=["lsc"])
    b.act(lsc[:, 0:2], lsc[:, 0:2], AF.Exp)
    b.tt(lsc[:, 2:3], lsc[:, 0:1], lsc[:, 1:2], ALU.subtract)
    b.ts(lsc[:, 3:4], lsc[:, 2:3], LAMBDA_INIT1, -1.0, op0=ALU.add, op1=ALU.mult)
    b.ts(col(4), col(2), 1.0 - LAMBDA_INIT1, None, op0=ALU.mult)
    NEGLAM = lsc[:, 3:4]

    TA = max(nblk, 1) * 512
    QT = [sb("QT%d" % h, [128, TA], BF16) for h in range(2)]
    KT = [sb("KT%d" % h, [128, TA], BF16) for h in range(2)]
    VS = [sb("VS%d" % h, [128, TA // 128, 128], BF16) for h in range(2)]
    xt = [sb("xt%d" % i, [128, 1024]) for i in range(2)]
    xs = [sb("xs%d" % i, [128, 1024], BF16) for i in range(2)]
    junk = sb("junk", [128, 1024], BF16)
    ss = sb("ss", [128, 1]); rstd = sb("rstd", [128, 1])
    xnT = sb("xnT", [128, 8, 512], BF16)
    posi = sb("posi", [128, 512], I32)
    tmp = [sb("t%d" % i, [128, 512]) for i in range(8)]
    Eb = [sb("Eb%d" % i, [128, 512], BF16) for i in range(4)]
    PSB = [ps("PSB%d" % i, [128, 512]) for i in range(2)]
    PSX = ps("PSX", [128, 512])
    PVA = [ps("PVA%d" % i, [128, 512]) for i in range(2)]
    PSM = [ps("PSM%d" % i, [128, 512]) for i in range(2)]
    PTB = ps("PTB", [128, 8, 128], BF16)
    for s in range(nblk):
        for j in range(4):
            tix = (s * 4 + j) % 2
            b.load(xt[tix], x[s * 512 + j * 128: s * 512 + (j + 1) * 128, :])
            rms_tile_T(b, xt[tix], xs[tix], ss, rstd, PTB, (xnT[:, :, j * 128:(j + 1) * 128], ("xnT", j)), identb, junk)
        XN = KL([("xnT", j) for j in range(4)])
        if phase < 0.4:
            continue
        b.load(posi, pos_d[:, s * 512:(s + 1) * 512].partition_broadcast(128))
        ang = tmp[2]
        b.copy(ang, posi)
        b.ts(ang, ang, col(3), None, op0=ALU.mult)
        for which, shift in ((0, 0.0), (1, 1.5707963267948966)):
            a_ = tmp[3]
            kf = tmp[4]
            if shift:
                b.ts(a_, ang, shift, None, op0=ALU.add)
            else:
                a_ = ang
            b.ts(kf, a_, 1.0 / TWO_PI, MAGIC, op0=ALU.mult, op1=ALU.add)
            b.ts(kf, kf, -MAGIC, None, op0=ALU.add)
            r_ = tmp[5]
            b.stt(r_, kf, -CW1, a_, ALU.mult, ALU.add)
            b.stt(r_, kf, -CW2, r_, ALU.mult, ALU.add)
            b.ts(r_, r_, 3.1415925, -3.1415925, op0=ALU.min, op1=ALU.max)
            b.act(tmp[which], r_, AF.Sin)
        SIN, COS = tmp[0], tmp[1]
        if phase < 0.6:
            continue
        for cc in range(4):
            pp = PSB[cc % 2]
            for kc in range(8):
                b.mm(pp, (wb[:, kc, cc * 128:(cc + 1) * 128], WBK(kc)), (xnT[:, kc, :], XN), start=(kc == 0), stop=(kc == 7))
            sq = tmp[2]
            b.act(sq, pp, AF.Square)
            b.mm(PSX, bones, sq)
            b.act(sq, PSX, AF.Sqrt, scale=1.0 / 64, bias=1e-6)
            b.recip(sq, sq)
            qn = tmp[3]
            b.stt(qn, pp, col(0 if cc < 2 else 1), sq, ALU.mult, ALU.mult)
            if phase < 0.7:
                continue
            b.mm(PVA[0], rot, qn)
            t1 = tmp[4]
            b.tt(t1, qn, COS, ALU.mult)
            t2 = tmp[5]
            b.tt(t2, PVA[0], SIN, ALU.mult)
            if phase < 0.75:
                continue
            dst = (QT if cc < 2 else KT)[cc % 2]
            b.stt((dst[:, s * 512:(s + 1) * 512], (dst.tensor.name, s)), t1, 1.0, t2, ALU.mult, ALU.add)
        if phase < 0.8:
            continue
        for j in range(4):
            pp = PSB[j % 2]
            for kc in range(8):
                b.mm(pp[:, 0:256], (xnT[:, kc, j * 128:(j + 1) * 128], XN), (wb[:, kc, 512:768], WBK(kc)), start=(kc == 0), stop=(kc == 7))
            for h in range(2):
                b.copy((VS[h][:, s * 4 + j, :], (VS[h].tensor.name, s)), pp[:, h * 128:(h + 1) * 128], eng="act")
    if phase < 2:
        nb2 = 0
    else:
        nb2 = nblk
    scale = 64 ** -0.5
    ei = 0
    for h in range(2):
        for i in range(nb2):
            qs = slice(i * 512, (i + 1) * 512)
            QK = (QT[h].tensor.name, i)
            nkb = 4 * i + 4
            for kb in range(nkb):
                KK = (KT[h].tensor.name, kb // 4)
                VK = (VS[h].tensor.name, kb // 4)
                for c in range(2):
                    pr = slice(64 * c, 64 * c + 64)
                    psb = PSB[c]
                    b.mm(psb, (KT[h][pr, kb * 128:(kb + 1) * 128], KK), (QT[h][pr, qs], QK))
                    e_ = Eb[ei % 4]
                    ei += 1
                    b.act(e_, psb, AF.Exp, scale=scale)
                    if kb >= 4 * i:
                        b.tt(e_, e_, (cmask[:, kb - 4 * i, :], "cmask_s"), ALU.mult)
                    b.mm(PVA[c], (VS[h][:, kb, :], VK), e_, start=(kb == 0), stop=(kb == nkb - 1))
                    b.mm(PSM[c], ones_b, e_, start=(kb == 0), stop=(kb == nkb - 1))
            r0, r1, o_ = tmp[0], tmp[1], tmp[2]
            b.recip(r0, PSM[0])
            b.tt(r0, PVA[0], r0, ALU.mult)
            b.recip(r1, PSM[1])
            b.tt(r1, PVA[1], r1, ALU.mult)
            b.stt(o_, r1, NEGLAM, r0, ALU.mult, ALU.add)
            sq = tmp[3]
            b.act(sq, o_, AF.Square)
            b.mm(PSX, ones_f, sq)
            b.act(sq, PSX, AF.Sqrt, scale=1.0 / 128, bias=1e-6)
            b.recip(sq, sq)
            on = tmp[4 + (i % 2)]
            b.stt(on, o_, col(4), sq, ALU.mult, ALU.mult)
            b.store(oT[h * 128:(h + 1) * 128, qs], on)
    b.finish()
    return nc


def _consts_l3():
    p = np.arange(128)
    bones = (p[:, None] // 64 == p[None, :] // 64).astype(np.float32)
    rot = np.zeros((128, 128), np.float32)
    for d in range(128):
        dm = d % 64
        if dm < 8:
            rot[d + 8, d] = -1.0
        elif dm < 16:
            rot[d - 8, d] = 1.0
    invf = np.zeros((128,), np.float32)
    for q in range(128):
        if q % 64 < 16:
            invf[q] = np.float32(500000.0) ** np.float32(-(2 * (q % 8)) / 16.0)
    kp = np.arange(128)[:, None]
    qc = np.arange(512)[None, :]
    cm = np.concatenate([((128 * j + kp) <= qc).astype(np.float32) for j in range(4)], axis=1)
    return dict(ident=np.eye(128, dtype=np.float32), bones=bones, rot=rot, cmask=np.ascontiguousarray(cm)), invf


def l3_inputs(inp, x1, bi, g):
    f = lambda k: np.asarray(inp[k][0], np.float32)
    W = f("o_w_qkv")
    hs = [2 * g, 2 * g + 1]
    cols = np.concatenate([np.arange(h * 128, (h + 1) * 128) for h in hs] + [1024 + np.arange(h * 128, (h + 1) * 128) for h in hs]
                          + [2048 + np.arange(h * 128, (h + 1) * 128) for h in hs])
    consts, invf = _consts_l3()
    cvec = np.zeros((128, 4), np.float32)
    cvec[:, 0] = np.tile(f("o_q_norm"), 2)
    cvec[:, 1] = np.tile(f("o_k_norm"), 2)
    cvec[:, 2] = f("o_subln")
    cvec[:, 3] = invf
    d = dict(x=np.ascontiguousarray(x1[bi]), pos=np.ascontiguousarray(inp["positions"][bi].reshape(1, -1).astype(np.int32)),
             w=np.ascontiguousarray(W[:, cols]), gmix=np.ascontiguousarray(f("o_ln_mix").reshape(8, 128).T), cvec=cvec,
             lams=np.concatenate([f("o_lambda_q1"), f("o_lambda_k1"), f("o_lambda_q2"), f("o_lambda_k2")]).reshape(1, 256))
    d.update(consts)
    return d


N_CORES = 8


def _run(nc, in_maps):
    res = run_bass_kernel_spmd(nc, in_maps, core_ids=list(range(N_CORES)))
    return res.results


def kernel(**inp):
    inp = {k: np.asarray(v) for k, v in inp.items()}
    x = np.ascontiguousarray(inp["x"], dtype=np.float32)
    Bn, T, D = x.shape
    f = lambda k: np.asarray(inp[k][0], np.float32)
    ident = np.eye(128, dtype=np.float32)
    nc1 = build_l1()
    r1 = _run(nc1, [l1_inputs(inp, c // 4, c % 4) for c in range(N_CORES)])
    yT = np.zeros((Bn, 1024, T), np.float32)
    for c in range(N_CORES):
        bi, g = c // 4, c % 4
        yT[bi, g * 128:(g + 1) * 128] = r1[c]["yT"][0:128]
        yT[bi, 512 + g * 128:512 + (g + 1) * 128] = r1[c]["yT"][128:256]
    del r1
    nc2 = build_ffn(NT=2048, F=2816, E=1, G=512, moe=False)
    maps = []
    for c in range(N_CORES):
        bi, tq = c // 4, c % 4
        ts_ = slice(tq * 2048, (tq + 1) * 2048)
        maps.append(dict(xres=np.ascontiguousarray(x[bi, ts_]), aT=np.ascontiguousarray(yT[bi][:, ts_]), wproj=f("e_w_out"),
                         gain=np.ascontiguousarray(f("e_ln_ffn").reshape(1, 1024)), wg=np.asarray(inp["e_ffn_gate"], np.float32),
                         wu=np.asarray(inp["e_ffn_up"], np.float32), wd=np.asarray(inp["e_ffn_down"], np.float32), ident=ident))
    r2 = _run(nc2, maps)
    x1 = np.zeros((Bn, T, D), np.float32)
    for c in range(N_CORES):
        bi, tq = c // 4, c % 4
        x1[bi, tq * 2048:(tq + 1) * 2048] = r2[c]["out"]
    del r2, maps
    nc3 = build_l3()
    r3 = _run(nc3, [l3_inputs(inp, x1, c // 4, c % 4) for c in range(N_CORES)])
    oT = np.zeros((Bn, 1024, T), np.float32)
    for c in range(N_CORES):
        bi, g = c // 4, c % 4
        oT[bi, g * 256:(g + 1) * 256] = r3[c]["oT"]
    del r3
    nc4 = build_ffn(NT=2048, F=3584, E=8, G=512, moe=True)
    wg = np.asarray(inp["o_moe_gate"][0], np.float32)
    wu = np.asarray(inp["o_moe_up"][0], np.float32)
    wd = np.asarray(inp["o_moe_down"][0], np.float32)
    maps = []
    for c in range(N_CORES):
        bi, tq = c // 4, c % 4
        ts_ = slice(tq * 2048, (tq + 1) * 2048)
        maps.append(dict(xres=np.ascontiguousarray(x1[bi, ts_]), aT=np.ascontiguousarray(oT[bi][:, ts_]), wproj=f("o_w_o"),
                         gain=np.ascontiguousarray(f("o_ln_ffn").reshape(1, 1024)), wg=wg, wu=wu, wd=wd, router=f("o_router"), ident=ident))
    r4 = _run(nc4, maps)
    out = np.zeros((Bn, T, D), np.float32)
    for c in range(N_CORES):
        bi, tq = c // 4, c % 4
        out[bi, tq * 2048:(tq + 1) * 2048] = r4[c]["out"]
    return out
```

```python
import contextlib
import numpy as np
import ml_dtypes
import concourse.bass as bass
import concourse.mybir as mybir
from concourse.alu_op_type import AluOpType as ALU
from concourse.bass_utils import run_bass_kernel_spmd

AF = mybir.ActivationFunctionType
F32 = mybir.dt.float32
BF16 = mybir.dt.bfloat16
I32 = mybir.dt.int32
AX = mybir.AxisListType

COMPUTE = ("pe", "act", "dve", "pool")


class _Op:
    __slots__ = ("eng", "fn", "reads", "writes", "kind", "waits", "signal", "val", "dkey", "seq")

    def __init__(self, eng, fn, reads, writes, kind):
        self.eng = eng
        self.fn = fn
        self.reads = tuple(reads)
        self.writes = tuple(writes)
        self.kind = kind
        self.waits = {}
        self.signal = False
        self.val = None
        self.dkey = None


class Prog:
    def __init__(self, nc):
        self.nc = nc
        self.ops = []
        self.last_w = {}
        self.readers = {}
        self.deps = []

    def _add(self, op):
        deps = set()
        for k in op.reads:
            w = self.last_w.get(k)
            if w is not None:
                deps.add((w, "raw"))
        for k in op.writes:
            w = self.last_w.get(k)
            if w is not None:
                deps.add((w, "waw"))
            for r in self.readers.get(k, ()):
                if r is not op:
                    deps.add((r, "war"))
        for k in op.reads:
            self.readers.setdefault(k, []).append(op)
        for k in op.writes:
            self.last_w[k] = op
            self.readers[k] = []
        op.seq = len(self.ops)
        self.ops.append(op)
        self.deps.append(deps)
        return op

    def add(self, eng, fn, reads=(), writes=()):
        return self._add(_Op(eng, fn, reads, writes, "c"))

    def dma(self, q, fn, reads=(), writes=(), key=None):
        op = _Op(q, fn, reads, writes, "d")
        op.dkey = key
        return self._add(op)

    def emit(self, final_keys=()):
        nc = self.nc
        ops = self.ops
        for op, deps in zip(ops, self.deps):
            for d, kind in deps:
                if d.kind == "c":
                    if d.eng == op.eng and op.kind == "c":
                        if op.eng == "pe" or kind == "war":
                            continue
                    d.signal = True
        finals = [self.last_w[k] for k in final_keys if k in self.last_w]
        for d in finals:
            if d.kind == "c":
                d.signal = True
        cnt = {e: 0 for e in COMPUTE}
        dcnt = {}
        dkeys = []
        for op in ops:
            if op.kind == "c":
                if op.signal:
                    cnt[op.eng] += 1
                    op.val = cnt[op.eng]
            else:
                if op.dkey not in dcnt:
                    dcnt[op.dkey] = 0
                    dkeys.append(op.dkey)
                dcnt[op.dkey] += 16
                op.val = dcnt[op.dkey]
        with contextlib.ExitStack() as st:
            csem = {e: st.enter_context(nc.semaphore("cs_" + e)) for e in COMPUTE}
            dsem = {k: st.enter_context(nc.semaphore("ds%d" % i)) for i, k in enumerate(dkeys)}

            def semof(d):
                return csem[d.eng] if d.kind == "c" else dsem[d.dkey]

            streams = {}
            waited = {}
            for op, deps in zip(ops, self.deps):
                need = {}
                for d, kind in deps:
                    if d.kind == "c" and d.eng == op.eng and op.kind == "c":
                        if op.eng == "pe" or kind == "war":
                            continue
                    s = semof(d)
                    sid = id(s)
                    if need.get(sid, (None, 0))[1] < d.val:
                        need[sid] = (s, d.val)
                w = waited.setdefault(op.eng, {})
                op.waits = []
                for sid, (s, v) in need.items():
                    if w.get(sid, 0) < v:
                        w[sid] = v
                        op.waits.append((s, v))
                streams.setdefault(op.eng, []).append(op)
            fin_waits = []
            for d in finals:
                fin_waits.append((semof(d), d.val))

            def run_stream(name, engine, extra=None):
                for op in streams.get(name, []):
                    for s, v in op.waits:
                        engine.wait_ge(s, v)
                    ins = op.fn(engine)
                    if op.kind == "c":
                        if op.signal:
                            ins.then_inc(csem[op.eng], 1)
                    else:
                        ins.then_inc(dsem[op.dkey], 16)
                if extra:
                    for s, v in extra:
                        engine.wait_ge(s, v)

            with nc.Block() as block:
                @block.tensor
                def _(e):
                    run_stream("pe", e)

                @block.scalar
                def _(e):
                    run_stream("act", e)

                @block.vector
                def _(e):
                    run_stream("dve", e)

                @block.gpsimd
                def _(e):
                    run_stream("pool", e)

                @block.sync
                def _(e):
                    run_stream("sp", e, extra=fin_waits)


class KL(list):
    pass


KEYMAP = {"PS0": KL([("PS0", 0), ("PS0", 1)]), "PS1": KL([("PS1", 0), ("PS1", 1)])}


def _ak(x):
    if isinstance(x, tuple):
        ap, k = x
        return (ap, k if isinstance(k, KL) else KL([k]))
    n = x.tensor.name
    return (x, KEYMAP.get(n) or KL([n]))


class B:
    def __init__(self, nc):
        self.nc = nc
        self.P = Prog(nc)
        self.st = contextlib.ExitStack()
        self.nout = 0

    def sb(self, name, shape, dt=F32):
        return self.st.enter_context(self.nc.sbuf_tensor(name, shape, dt))[:]

    def ps(self, name, shape, dt=F32):
        return self.st.enter_context(self.nc.psum_tensor(name, shape, dt))[:]

    def mm(self, out, lhsT, rhs, start=True, stop=True):
        (o, ok), (l, lk), (r, rk) = _ak(out), _ak(lhsT), _ak(rhs)
        self.P.add("pe", lambda e: e.matmul(o, l, r, start=start, stop=stop), reads=lk + rk, writes=ok)

    def tr(self, out, in_, ident):
        (o, ok), (i, ik), (d, dk) = _ak(out), _ak(in_), _ak(ident)
        self.P.add("pe", lambda e: e.transpose(o, i, d), reads=ik + dk, writes=ok)

    def act(self, out, in_, func, scale=None, bias=None, accum=None, eng="act"):
        (o, ok), (i, ik) = _ak(out), _ak(in_)
        reads = list(ik)
        writes = list(ok)
        kw = {}
        if scale is not None:
            if isinstance(scale, (int, float)):
                kw["scale"] = float(scale)
            else:
                s, sk = _ak(scale)
                kw["scale"] = s
                reads.extend(sk)
        if bias is not None:
            if isinstance(bias, (int, float)):
                kw["bias"] = float(bias)
            else:
                b_, bk = _ak(bias)
                kw["bias"] = b_
                reads.extend(bk)
        if accum is not None:
            a_, ak_ = _ak(accum)
            kw["accum_out"] = a_
            writes.extend(ak_)
        self.P.add("act", lambda e: e.activation(out=o, in_=i, func=func, **kw), reads=reads, writes=writes)

    def tt(self, out, in0, in1, op, eng="dve"):
        (o, ok), (a, ak_), (b_, bk) = _ak(out), _ak(in0), _ak(in1)
        self.P.add(eng, lambda e: e.tensor_tensor(out=o, in0=a, in1=b_, op=op), reads=ak_ + bk, writes=ok)

    def ts(self, out, in0, s1, s2=None, op0=ALU.mult, op1=None, eng="dve", accum=None):
        (o, ok), (a, ak_) = _ak(out), _ak(in0)
        reads = list(ak_)
        writes = list(ok)

        def sc(s):
            if s is None or isinstance(s, (int, float)):
                return s
            ap, k = _ak(s)
            reads.extend(k)
            return ap
        v1, v2 = sc(s1), sc(s2)
        kw = {}
        if op1 is not None:
            kw["op1"] = op1
        if accum is not None:
            a2, a2k = _ak(accum)
            kw["accum_out"] = a2
            writes.extend(a2k)
        self.P.add(eng, lambda e: e.tensor_scalar(out=o, in0=a, scalar1=v1, scalar2=v2, op0=op0, **kw), reads=reads, writes=writes)

    def stt(self, out, in0, scalar, in1, op0, op1):
        (o, ok), (a, ak_), (b_, bk) = _ak(out), _ak(in0), _ak(in1)
        reads = list(ak_ + bk)
        if isinstance(scalar, (int, float)):
            s = float(scalar)
        else:
            s, sk = _ak(scalar)
            reads.extend(sk)
        self.P.add("dve", lambda e: e.scalar_tensor_tensor(out=o, in0=a, scalar=s, in1=b_, op0=op0, op1=op1), reads=reads, writes=ok)

    def copy(self, out, in_, eng="dve"):
        (o, ok), (i, ik) = _ak(out), _ak(in_)
        if eng == "act":
            self.P.add("act", lambda e: e.activation(out=o, in_=i, func=AF.Copy), reads=ik, writes=ok)
        else:
            self.P.add(eng, lambda e: e.tensor_copy(out=o, in_=i), reads=ik, writes=ok)

    def scan(self, out, d0, d1, init):
        (o, ok), (a, ak_), (b_, bk) = _ak(out), _ak(d0), _ak(d1)
        reads = list(ak_ + bk)
        if isinstance(init, (int, float)):
            iv = float(init)
        else:
            iv, ik = _ak(init)
            reads.extend(ik)
        self.P.add("dve", lambda e: e.tensor_tensor_scan(out=o, data0=a, data1=b_, initial=iv, op0=ALU.mult, op1=ALU.add), reads=reads, writes=ok)

    def recip(self, out, in_):
        (o, ok), (i, ik) = _ak(out), _ak(in_)
        self.P.add("dve", lambda e: e.reciprocal(out=o, in_=i), reads=ik, writes=ok)

    def memset(self, out, val, eng="dve"):
        (o, ok) = _ak(out)
        self.P.add(eng, lambda e: e.memset(o, val), writes=ok)

    def load(self, out, in_, q="sp"):
        (o, ok) = _ak(out)
        self.P.dma(q, lambda e: e.dma_start(out=o, in_=in_), writes=ok, key=ok[0])

    def store(self, out_dram, in_, q="sp"):
        (i, ik) = _ak(in_)
        self.nout += 1
        k = ("__out", self.nout)
        self.P.dma(q, lambda e: e.dma_start(out=out_dram, in_=i), reads=ik, writes=[k], key=ik[0])

    def finish(self):
        self.P.emit(final_keys=[("__out", i + 1) for i in range(self.nout)])
        self.st.close()


def rms_tile_T(b, xt, xs, ss, rstd, PT, xnT_dst, identb, junk, eps=1e-6, D=1024):
    b.act(junk, xt, AF.Square, accum=ss)
    b.act(rstd, ss, AF.Sqrt, scale=1.0 / D, bias=eps)
    b.recip(rstd, rstd)
    b.ts(xs, xt, rstd, None, op0=ALU.mult)
    n = D // 128
    for kc in range(n):
        b.tr((PT[:, kc, :], (PT.tensor.name, kc)), xs[:, kc * 128:(kc + 1) * 128], identb)
    b.copy(xnT_dst, (PT[:, :, :], KL([(PT.tensor.name, kc) for kc in range(n)])), eng="act")


T_SEQ = 8192
SEG = 512
CH = 64
C0 = 0.6065306597126334
NV1 = 20


def build_l1(nseg=T_SEQ // SEG, phase=99):
    nc = bass.Bass("TRN2", target_bir_lowering=False)
    dr = lambda n, s, dt=F32, kind="ExternalInput": nc.dram_tensor(n, s, dt, kind=kind).ap()
    x = dr("x", [T_SEQ, 1024])
    w_in = dr("w_in", [1024, 896])
    gmix = dr("gmix", [128, 8])
    cvec = dr("cvec", [128, NV1])
    wa_d = dr("wa", [128, 128])
    wx_d = dr("wx", [128, 128])
    w2a2_d = dr("w2a2", [128, 128])
    g2_d = dr("g2", [128, 128])
    ident_d = dr("ident", [128, 128])
    bones_d = dr("bones", [128, 128])
    maska_d = dr("maska", [128, 512])
    maskb_d = dr("maskb", [128, 256])
    rmask_d = dr("rmask", [128, 512])
    id2_d = dr("id2", [128, 128])
    yT = dr("yT", [256, T_SEQ], kind="ExternalOutput")

    b = B(nc)
    sb, ps = b.sb, b.ps
    wb = sb("wb", [128, 8, 896], BF16)
    wst = [sb("wst%d" % i, [128, 896]) for i in range(2)]
    gm = sb("gm", [128, 8])
    cv = sb("cv", [128, NV1 + 4])
    wa = sb("wa_s", [128, 128]); wx = sb("wx_s", [128, 128])
    w2a2 = sb("w2a2_s", [128, 128]); g2 = sb("g2_s", [128, 128])
    ident = sb("ident_s", [128, 128]); identb = sb("identb", [128, 128], BF16)
    bones = sb("bones_s", [128, 128]); rkbd = sb("rkbd", [128, 128])
    id2 = sb("id2_s", [128, 2, 64])
    maska = sb("maska_s", [128, 512]); maskb = sb("maskb_s", [128, 256]); rmask = sb("rmask_s", [128, 512])
    for t_, d_ in ((gm, gmix), (cv[:, 0:NV1], cvec), (wa, wa_d), (wx, wx_d), (w2a2, w2a2_d), (g2, g2_d), (ident, ident_d),
                   (bones, bones_d), ((id2[:, :, :].rearrange("p h s -> p (h s)"), "id2_s"), id2_d), (maska, maska_d), (maskb, maskb_d), (rmask, rmask_d)):
        b.load(t_, d_)
    b.load(identb, ident_d, q="pool")
    for kc in range(8):
        b.load(wst[kc % 2], w_in[kc * 128:(kc + 1) * 128, :])
        b.ts((wb[:, kc, :], ("wb", kc)), wst[kc % 2], gm[:, kc:kc + 1], None, op0=ALU.mult)
    WBK = lambda kc: ("wb", kc)
    col = lambda i: cv[:, i:i + 1]
    CCH, OMKA, TWOC = NV1, NV1 + 1, NV1 + 2
    b.act(col(CCH), col(7), AF.Exp, scale=-1.0)
    b.act(col(CCH), col(CCH), AF.Ln, bias=1.0)
    b.ts(col(TWOC), col(CCH), -16.0, None, op0=ALU.mult)
    b.ts(col(CCH), col(CCH), -8.0, None, op0=ALU.mult)
    b.ts(col(OMKA), col(16), -1.0, 1.0, op0=ALU.mult, op1=ALU.add)
    b.ts(rkbd, bones, col(17), None, op0=ALU.mult)

    xt = [sb("xt%d" % i, [128, 1024]) for i in range(2)]
    xs = [sb("xs%d" % i, [128, 1024], BF16) for i in range(2)]
    junk = sb("junk", [128, 1024], BF16)
    ss = sb("ss", [128, 1]); rstd = sb("rstd", [128, 1])
    xnT = sb("xnT", [128, 8, SEG], BF16)
    pj = [[sb("pj%d_%d" % (i, c), [128, 4 + SEG]) for c in range(7)] for i in range(2)]
    NT = 27
    tmp = [sb("t%d" % i, [128, SEG]) for i in range(NT)]
    hh = [sb("hh%d" % i, [128, SEG]) for i in range(2)]
    TM = sb("TM", [64, 2, 4, 128])
    SA = sb("SA", [64, 2, 4, 2, 64]); SBm = sb("SBm", [64, 256])
    PQ = [sb("PQ%d" % i, [64, 2, 2, 2, 64]) for i in range(2)]
    Tb = [sb("Tb%d" % i, [64, 2, 2, 64]) for i in range(2)]
    ZS = sb("ZS", [64, 2, 2, 64]); MS = sb("MS", [64, 2, 2, 2, 64])
    GP = sb("GP", [64, 2, 2, 2, 64]); GS = GP[:, 0, :, :, :]; PSm = GP[:, 1, :, :, :]
    STT = [sb("STT%d" % i, [64, 2, 64]) for i in range(2)]
    AFx = sb("AFx", [128, SEG // CH, 2, CH]); RFx = sb("RFx", [128, SEG // CH, 2, CH]); BTx = sb("BTx", [128, SEG // CH, 2, CH])
    RFh = sb("RFh", [64, 2, SEG]); WCh = sb("WCh", [64, 2, SEG // CH])
    OS = sb("OS", [128, SEG])
    PP = ps("PP", [128, 512]); PT = ps("PT", [128, 8, 128], BF16)
    PS0 = ps("PS0", [128, 512]); PS1 = ps("PS1", [128, 512])
    PA = ps("PA", [128, 512]); PC = ps("PC", [128, 512]); PX = ps("PX", [128, 512]); PO = ps("PO", [128, 512])

    for i in range(2):
        for c in range(7):
            b.memset((pj[i][c][:, 0:4], ("pjh", i, c)), 0.0)
    b.memset((STT[0][:, :, :], "STT0"), 0.0)
    for tx in (AFx, RFx, BTx):
        b.memset((tx[:, :, :, :], tx.tensor.name), 0.0, eng="pool")

    st_i = 0
    PS0a_ = ("PS0", 0)
    for s in range(nseg):
        cur = s % 2
        nxt = 1 - cur
        pjc = pj[cur]
        PJ = lambda c: ("pj", cur, c)
        PJH = lambda c: ("pjh", cur, c)
        for j in range(4):
            tix = (s * 4 + j) % 2
            b.load(xt[tix], x[s * SEG + j * 128: s * SEG + (j + 1) * 128, :])
            rms_tile_T(b, xt[tix], xs[tix], ss, rstd, PT, (xnT[:, :, j * 128:(j + 1) * 128], ("xnT", j)), identb, junk)
        XN = KL([("xnT", j) for j in range(4)])
        for cc in range(7):
            for kc in range(8):
                b.mm(PP, (wb[:, kc, cc * 128:(cc + 1) * 128], WBK(kc)), (xnT[:, kc, :], XN), start=(kc == 0), stop=(kc == 7))
            b.copy((pjc[cc][:, 4:4 + SEG], PJ(cc)), PP, eng=("act" if cc % 2 == 0 else "dve"))
            b.copy((pj[nxt][cc][:, 0:4], ("pjh", nxt, cc)), (pjc[cc][:, SEG:SEG + 4], PJ(cc)), eng="pool")
        if phase < 2:
            continue
        cur_v = lambda c: (pjc[c][:, 4:4 + SEG], PJ(c))
        sh_v = lambda c, k: (pjc[c][:, 4 - k:4 - k + SEG], KL([PJ(c), PJH(c)]))
        t = tmp
        xc = t[0]
        b.ts(xc, sh_v(0, 3), col(0), col(4), op0=ALU.mult, op1=ALU.add)
        b.stt(xc, sh_v(0, 2), col(1), xc, ALU.mult, ALU.add)
        b.stt(xc, sh_v(0, 1), col(2), xc, ALU.mult, ALU.add)
        b.stt(xc, cur_v(0), col(3), xc, ALU.mult, ALU.add)
        b.mm(PS0, wa, xc)
        b.mm(PS1, wx, xc)
        ra = t[1]
        b.act(ra, PS0, AF.Sigmoid, bias=col(5))
        av_ = t[2]
        b.act(av_, ra, AF.Exp, scale=col(CCH))
        a2_ = t[3]
        b.act(a2_, ra, AF.Exp, scale=col(TWOC))
        b.act(a2_, a2_, AF.Sqrt, scale=-1.0, bias=1.0)
        ix = t[1]
        b.act(ix, PS1, AF.Sigmoid, bias=col(6))
        b.tt(a2_, a2_, ix, ALU.mult)
        b.tt(a2_, a2_, xc, ALU.mult)
        hcur = hh[cur]
        b.scan(hcur, av_, a2_, 0.0 if s == 0 else hh[nxt][:, SEG - 1:SEG])
        gq = t[0]
        b.act(gq, cur_v(1), AF.Square)
        b.ts(gq, gq, 0.044715, 1.0, op0=ALU.mult, op1=ALU.add)
        b.tt(gq, gq, cur_v(1), ALU.mult)
        b.act(gq, gq, AF.Sigmoid, scale=1.5957691216057308)
        b.tt(gq, gq, cur_v(1), ALU.mult)
        ylru = t[1]
        b.tt(ylru, gq, hcur, ALU.mult)
        b.store(yT[0:128, s * SEG:(s + 1) * SEG], ylru)
        if phase < 3:
            continue
        shf = []
        for i, c in enumerate((2, 3, 4, 5, 6)):
            d = t[4 + i]
            b.tt(d, sh_v(c, 1), cur_v(c), ALU.subtract)
            b.stt(d, d, col(8 + i), cur_v(c), ALU.mult, ALU.add)
            shf.append(d)
        rs, ks, vs, xwa, xgs = shf
        tw = t[9]
        b.act(tw[0:64, :], xwa[0:64, :], AF.Tanh)
        b.mm(PS0, w2a2[0:64, :], tw[0:64, :])
        b.mm(PS1, w2a2[64:128, :], xwa[64:128, :])
        sgz = t[9]
        b.act(sgz, PS0, AF.Sigmoid, bias=col(13))
        avv = t[10]
        b.act(avv, PS1, AF.Sigmoid, bias=col(14))
        sg = t[11]
        b.act(sg, xgs, AF.Sigmoid)
        cs = t[12]
        b.scan(cs, rmask, sgz, 0.0)
        csm1 = t[13]
        b.tt(csm1, cs, sgz, ALU.subtract)
        Wt = t[14]; iW = t[15]; Wm1 = t[13]
        b.act(Wt, cs, AF.Exp, scale=-C0)
        b.act(iW, cs, AF.Exp, scale=C0)
        b.act(Wm1, csm1, AF.Exp, scale=-C0)
        b.mm(PS0, g2, sg)
        gv = t[11]
        b.copy(gv, PS0, eng="act")
        kq = t[9]
        b.ts(kq, ks, col(15), None, op0=ALU.mult)
        kq2 = t[12]
        b.act(kq2, kq, AF.Square)
        b.mm(PS1, bones, kq2)
        rn = t[12]
        b.act(rn, PS1, AF.Sqrt)
        b.ts(rn, rn, 1e-12, None, op0=ALU.max)
        b.recip(rn, rn)
        kkn = t[9]
        b.tt(kkn, kq, rn, ALU.mult)
        kmod = t[12]
        b.ts(kmod, avv, col(16), col(OMKA), op0=ALU.mult, op1=ALU.add)
        b.tt(kmod, kmod, ks, ALU.mult)
        bb = t[10]
        b.tt(bb, kkn, avv, ALU.mult)
        AFm = t[16]; RF = t[17]; BT = t[18]; KT = t[19]; Bh = t[20]; Kh = t[21]
        b.stt(AFm, kkn, -1.0, Wm1, ALU.mult, ALU.mult)
        b.tt(RF, rs, Wt, ALU.mult)
        b.tt(BT, bb, iW, ALU.mult)
        b.tt(KT, kmod, iW, ALU.mult)
        v3 = lambda tl: tl[:, :].rearrange("p (c s) -> p c s", s=CH)
        wcb = (v3(Wt)[:, :, CH - 1:CH].broadcast_to([128, SEG // CH, CH]), Wt.tensor.name)
        b.tt((v3(Bh), Bh.tensor.name), (v3(BT), BT.tensor.name), wcb, ALU.mult)
        b.tt((v3(Kh), Kh.tensor.name), (v3(KT), KT.tensor.name), wcb, ALU.mult)
        rk_ = t[9]
        b.tt(rk_, rs, kmod, ALU.mult)
        b.mm(PS1, rkbd, rk_)
        bonus = t[22]
        b.tt(bonus, PS1, vs, ALU.mult)
        if phase < 4:
            continue
        for h in range(2):
            b.mm((PS1[0:64, :], KEYMAP["PS1"]), ident[:, 64 * h:64 * h + 64], RF)
            b.copy((RFh[:, h, :], "RFh"), (PS1[0:64, :], KEYMAP["PS1"]), eng="act")
            b.mm((PS0[0:64, h * 8:h * 8 + 8], PS0a_), ident[:, 64 * h:64 * h + 64], (v3(Wt)[:, :, CH - 1], Wt.tensor.name))
        b.copy((WCh[:, :, :].rearrange("p h c -> p (h c)"), "WCh"), (PS0[0:64, 0:16], PS0a_), eng="act")
        for tl, tx in ((AFm, AFx), (RF, RFx), (BT, BTx)):
            b.copy((tx[0:64, :, 0, :], tx.tensor.name), (v3(tl)[0:64], tl.tensor.name), eng="pool")
            b.copy((tx[64:128, :, 1, :], tx.tensor.name), (v3(tl)[64:128], tl.tensor.name), eng="pool")
        PS0a, PS0b, PS1a, PS1b = ("PS0", 0), ("PS0", 1), ("PS1", 0), ("PS1", 1)
        sel = [ident[:, 0:64], ident[:, 64:128]]
        for cp in range(SEG // 128):
            tok = slice(cp * 128, (cp + 1) * 128)
            for q in range(2):
                c_ = cp * 2 + q
                ck = slice(c_ * CH, (c_ + 1) * CH)
                for qi, src in enumerate((AFm, Bh, Kh, vs)):
                    b.tr((PX[0:64, qi * 128:(qi + 1) * 128], "PX"), src[:, ck], ident)
                b.copy((TM[:, q, :, :].rearrange("p a b -> p (a b)"), ("TM", q)), (PX[0:64, :], "PX"), eng="act")
                for j, (l_, rx) in enumerate(((BT, AFx), (BT, RFx), (KT, AFx), (KT, RFx))):
                    b.mm((PA[0:64, j * 128:(j + 1) * 128], "PA"), l_[:, ck], rx[:, c_, :, :].rearrange("p h s -> p (h s)"))
                b.mm((PS1[0:64, q * 128:(q + 1) * 128], PS1a), AFm[:, ck], BTx[:, c_, :, :].rearrange("p h s -> p (h s)"))
                b.tt((SA[:, q, :, :, :].rearrange("p j h s -> p (j h s)"), ("SA", q)), (PA[0:64, :], "PA"), maska[0:64, :], ALU.mult)
            b.tt(SBm, (PS1[0:64, 0:256], PS1a), maskb[0:64, :], ALU.mult)
            TMv = lambda qi, q, h: (TM[:, q, qi, 64 * h:64 * h + 64], ("TM", q))
            SAv = lambda j, q, h: (SA[:, q, j, h, :], ("SA", q))
            SAK = KL([("SA", 0), ("SA", 1)])
            QH = [(q, h) for q in range(2) for h in range(2)]
            for q, h in QH:
                b.mm((PS1[0:64, 256 + (q * 2 + h) * 64:256 + (q * 2 + h) * 64 + 64], PS1b), SAv(2, q, h), TMv(3, q, h))
            b.copy((ZS[:, :, :, :].rearrange("p q h s -> p (q h s)"), "ZS"), (PS1[0:64, 256:512], PS1b), eng="act")
            if phase < 5:
                continue
            b.copy((PQ[0][:, 0, :, :, :], "PQ0"), (SA[:, :, 0, :, :], SAK), eng="pool")
            b.copy((PQ[0][:, 1, :, :, :].rearrange("p q h s -> p (q h s)"), "PQ0"), SBm, eng="pool")
            idb = (ident[0:64, 0:64].rearrange("p (a c s) -> p a c s", a=1, c=1).broadcast_to([64, 2, 2, 64]), "ident_s")
            b.tt((Tb[0][:, :, :, :], "Tb0"), (SA[:, :, 0, :, :], SAK), idb, ALU.add)
            PC5 = PC[0:64, :].rearrange("p (j q h s) -> p j q h s", j=2, q=2, h=2)
            POt = PO[0:64, 256:512].rearrange("p (q h s) -> p q h s", q=2, h=2)
            for k in range(0, 6):
                i = k % 2
                pqk = "PQ%d" % i
                if k >= 1:
                    for q, h in QH:
                        b.mm((POt[:, q, h, :], ("PO", 2)), (PQ[i][:, 1, q, h, :], pqk), (Tb[1 - i][:, q, h, :], "Tb%d" % (1 - i)))
                if k <= 3:
                    for q, h in QH:
                        b.mm((PC5[:, 0, q, h, :], ("PC", 0)), (PQ[i][:, 1, q, h, :], pqk), (PQ[i][:, 0, q, h, :], pqk))
                if k <= 4:
                    for q, h in QH:
                        b.mm((PC5[:, 1, q, h, :], ("PC", 1)), (PQ[i][:, 0, q, h, :], pqk), (PQ[i][:, 1, q, h, :], pqk))
                if k <= 3:
                    b.copy((PQ[1 - i][:, :, :, :, :].rearrange("p j q h s -> p (j q h s)"), "PQ%d" % (1 - i)),
                           (PC[0:64, :], KL([("PC", 0), ("PC", 1)])), eng="act")
                elif k == 4:
                    b.copy((PQ[1 - i][:, 1, :, :, :].rearrange("p q h s -> p (q h s)"), "PQ%d" % (1 - i)),
                           (PC[0:64, 256:512], ("PC", 1)), eng="act")
                if k >= 1:
                    b.tt((Tb[i][:, :, :, :].rearrange("p q h s -> p (q h s)"), "Tb%d" % i),
                         (Tb[1 - i][:, :, :, :].rearrange("p q h s -> p (q h s)"), "Tb%d" % (1 - i)),
                         (PO[0:64, 256:512], ("PO", 2)), ALU.add)
            if phase < 5.2:
                continue
            for q, h in QH:
                o0 = ((q * 2 + h) * 2) * 64
                b.mm((PX[0:64, o0:o0 + 64], "PX"), (Tb[1][:, q, h, :], "Tb1"), TMv(0, q, h))
                b.mm((PX[0:64, o0 + 64:o0 + 128], "PX"), (Tb[1][:, q, h, :], "Tb1"), (ZS[:, q, h, :], "ZS"))
            b.copy((MS[:, :, :, :, :].rearrange("p q h m s -> p (q h m s)"), "MS"), (PX[0:64, :], "PX"), eng="act")
            M1T = lambda q, h: (MS[:, q, h, 0, :], "MS")
            M2T = lambda q, h: (MS[:, q, h, 1, :], "MS")
            if phase < 5.4:
                continue
            for q, h in QH:
                o0 = (q * 2 + h) * 64
                b.mm((PS0[0:64, o0:o0 + 64], PS0a), M1T(q, h), TMv(1, q, h))
                b.mm((PS0[0:64, 256 + o0:256 + o0 + 64], PS0b), M1T(q, h), SAv(1, q, h))
            for q, h in QH:
                c_ = cp * 2 + q
                o0 = (q * 2 + h) * 64
                b.stt((GS[:, q, h, :], "GS"), ident[0:64, 0:64], (WCh[:, h, c_:c_ + 1], "WCh"), (PS0[0:64, o0:o0 + 64], PS0a), ALU.mult, ALU.add)
            b.tt((PSm[:, :, :, :], "PSm"), (PS0[0:64, 256:512].rearrange("p (q h s) -> p q h s", q=2, h=2), PS0b),
                 (RFh[:, :, tok].rearrange("p h (q s) -> p q h s", q=2), "RFh"), ALU.add)
            if phase < 5.6:
                continue
            for q in range(2):
                ocol = slice(q * 64, q * 64 + 64)
                stn = "STT%d" % st_i
                for h in range(2):
                    if 5.8 <= phase < 5.9:
                        continue
                    pr = slice(64 * h, 64 * h + 64)
                    ob = (PO[pr, ocol], ("PO", 0)) if h == 0 else (PX[pr, ocol], "PX")
                    b.mm(ob, M2T(q, h), SAv(1, q, h), start=True, stop=False)
                    b.mm(ob, TMv(3, q, h), SAv(3, q, h), start=False, stop=False)
                    b.mm(ob, (STT[st_i][:, h, :], stn), (PSm[:, q, h, :], "PSm"), start=False, stop=True)
                for h in range(2):
                    if phase == 5.7:
                        continue
                    so = slice(h * 64, h * 64 + 64)
                    b.mm((PS1[0:64, so], PS1a), TMv(1, q, h), M2T(q, h), start=True, stop=False)
                    b.mm((PS1[0:64, so], PS1a), TMv(2, q, h), TMv(3, q, h), start=False, stop=False)
                    b.mm((PS1[0:64, so], PS1a), (GS[:, q, h, :], "GS"), (STT[st_i][:, h, :], stn), start=False, stop=True)
                b.copy((STT[1 - st_i][:, :, :].rearrange("p h s -> p (h s)"), "STT%d" % (1 - st_i)), (PS1[0:64, 0:128], PS1a), eng="dve")
                st_i = 1 - st_i
            b.copy((OS[0:64, tok], ("OS", cp)), (PO[0:64, 0:128], ("PO", 0)), eng="dve")
            b.copy((OS[64:128, tok], ("OS", cp)), (PX[64:128, 0:128], "PX"), eng="dve")
        if phase < 6:
            continue
        OSK = KL([("OS", i) for i in range(4)])
        b.mm(PS0, bones, (OS[:, :], OSK))
        cen = t[23]
        b.stt(cen, PS0, -1.0 / 64, (OS[:, :], OSK), ALU.mult, ALU.add)
        sq = t[24]
        b.act(sq, cen, AF.Square)
        b.mm(PS1, bones, sq)
        b.act(sq, PS1, AF.Sqrt, scale=1.0 / 64, bias=64e-5)
        b.recip(sq, sq)
        b.tt(cen, cen, sq, ALU.mult)
        b.ts(cen, cen, col(18), col(19), op0=ALU.mult, op1=ALU.add)
        b.tt(cen, cen, bonus, ALU.add)
        yrw = t[25 + (s % 2)]
        b.tt(yrw, cen, gv, ALU.mult)
        b.store(yT[128:256, s * SEG:(s + 1) * SEG], yrw)
    b.finish()
    return nc


def _consts_l1():
    p = np.arange(128)
    ident = np.eye(128, dtype=np.float32)
    bones = (p[:, None] // 64 == p[None, :] // 64).astype(np.float32)
    s_ = (p % 64)[:, None]
    t_ = np.arange(64)[None, :]
    lt = (s_ < t_).astype(np.float32)
    le = (s_ <= t_).astype(np.float32)
    gt = (s_ > t_).astype(np.float32)
    eq = (s_ == t_).astype(np.float32)
    maska = np.concatenate([lt, lt, le, le, lt, lt, le, le], axis=1)
    maskb = np.concatenate([gt, gt, gt, gt], axis=1)
    id2 = np.concatenate([eq, eq], axis=1)
    rmask = np.ones((128, 512), np.float32)
    rmask[:, ::64] = 0.0
    return dict(ident=ident, bones=bones, maska=np.ascontiguousarray(maska), maskb=np.ascontiguousarray(maskb),
                id2=np.ascontiguousarray(id2), rmask=rmask)


def _blockdiag2(w2):
    o = np.zeros((128, 128), np.float32)
    o[0:64, 0:64] = w2[0]
    o[64:128, 64:128] = w2[1]
    return o


def l1_inputs(inp, bi, g):
    f = lambda k: np.asarray(inp[k][0], np.float32)
    ls = slice(g * 128, (g + 1) * 128)
    W = f("e_w_in")
    rw0 = 1024
    cols = np.concatenate([np.arange(512)[ls], 512 + np.arange(512)[ls], rw0 + np.arange(512)[ls], rw0 + 512 + np.arange(512)[ls],
                           rw0 + 1024 + np.arange(512)[ls], rw0 + 1536 + np.arange(128), rw0 + 1664 + np.arange(128)])
    mu = f("e_shift_mu")
    cvec = np.zeros((128, NV1), np.float32)
    cvec[:, 0:4] = f("e_conv_w")[:, ls].T
    cvec[:, 4] = f("e_conv_b")[ls]
    cvec[:, 5] = f("e_gate_a_b")[ls]
    cvec[:, 6] = f("e_gate_x_b")[ls]
    cvec[:, 7] = f("e_lru_lambda")[ls]
    cvec[:, 8] = mu[0:512][ls]
    cvec[:, 9] = mu[512:1024][ls]
    cvec[:, 10] = mu[1024:1536][ls]
    cvec[:, 11] = mu[1536:1664]
    cvec[:, 12] = mu[1664:1792]
    cvec[:, 13] = f("e_w0")[ls]
    cvec[:, 14] = f("e_a0")[ls]
    cvec[:, 15] = f("e_k_k")[ls]
    cvec[:, 16] = f("e_k_a")[ls]
    cvec[:, 17] = f("e_r_k").reshape(-1)[ls]
    cvec[:, 18] = f("e_gn_w")[ls]
    cvec[:, 19] = f("e_gn_b")[ls]
    d = dict(
        x=np.ascontiguousarray(inp["x"][bi]),
        w_in=np.ascontiguousarray(W[:, cols]),
        gmix=np.ascontiguousarray(f("e_ln_mix").reshape(8, 128).T),
        cvec=cvec,
        wa=_blockdiag2(f("e_gate_a_w")[2 * g:2 * g + 2]),
        wx=_blockdiag2(f("e_gate_x_w")[2 * g:2 * g + 2]),
        w2a2=np.ascontiguousarray(np.concatenate([f("e_w2")[:, ls], f("e_a2")[:, ls]], axis=0)),
        g2=np.ascontiguousarray(f("e_g2")[:, ls]),
    )
    d.update(_consts_l1())
    return d


def build_ffn(NT=2048, F=2816, E=1, G=512, moe=False, ngroups=None, phase=99):
    nc = bass.Bass("TRN2", target_bir_lowering=False)
    dr = lambda n, s, dt=F32, kind="ExternalInput": nc.dram_tensor(n, s, dt, kind=kind).ap()
    nF = F // 128
    TG = G // 128
    xres = dr("xres", [NT, 1024])
    aT_d = dr("aT", [1024, NT])
    wproj = dr("wproj", [1024, 1024])
    gain_d = dr("gain", [1, 1024])
    wg_d = dr("wg", [E, 1024, F])
    wu_d = dr("wu", [E, 1024, F])
    wd_d = dr("wd", [E, F, 1024])
    ident_d = dr("ident", [128, 128])
    if moe:
        router_d = dr("router", [1024, 8])
    out = dr("out", [NT, 1024], kind="ExternalOutput")

    b = B(nc)
    sb, ps = b.sb, b.ps
    wo = sb("wo", [128, 8, 1024], BF16)
    gbc = sb("gbc", [128, 1024])
    ident = sb("ident_s", [128, 128])
    identb = sb("identb", [128, 128], BF16)
    b.load(gbc, gain_d.partition_broadcast(128))
    b.load(ident, ident_d)
    b.load(identb, ident_d, q="pool")
    wpv = wproj.rearrange("(kc p) n -> p kc n", p=128)
    for kc in range(8):
        b.load((wo[:, kc, :], ("wo", kc)), wpv[:, kc, :], q="pool")
    WOK = lambda kc: ("wo", kc)
    if moe:
        rt = sb("rt", [128, 8, 8])
        b.load(rt, router_d.rearrange("(kc p) e -> p kc e", p=128))
        hT32 = sb("hT32", [128, 8, 128])
        lg = sb("lg", [128, 8]); lg2 = sb("lg2", [128, 8]); mk1 = sb("mk1", [128, 8]); mk2 = sb("mk2", [128, 8])
        sm = sb("sm", [128, 8])
        comb = sb("comb", [128, TG, 8])
        xs32 = sb("xs32", [128, 1024])
        PTa = ps("PTa", [128, 4, 128]); PTb = ps("PTb", [128, 4, 128])
    else:
        xs = sb("xs", [128, 1024], BF16)
        PT = ps("PT", [128, 8, 128], BF16)
    xt = [sb("xt%d" % i, [128, 1024]) for i in range(2)]
    at = [sb("at%d" % i, [128, 8, 128], BF16) for i in range(2)]
    acc = [sb("acc%d" % i, [128, 1024]) for i in range(TG)]
    junk = sb("junk", [128, 1024], BF16)
    ss = sb("ss", [128, 1]); rstd = sb("rstd", [128, 1])
    hT = sb("hT", [128, 8, G], BF16)
    hid = sb("hid", [128, nF, G], BF16)
    NWB = 3
    wgb = [sb("wgb%d" % i, [128, 8, 128], BF16) for i in range(NWB)]
    wub = [sb("wub%d" % i, [128, 8, 128], BF16) for i in range(NWB)]
    wdh = [sb("wdh%d" % i, [128, nF, 512], BF16) for i in range(2)]
    sg = [sb("sg%d" % i, [128, G]) for i in range(2)]
    PP = [ps("PP%d" % i, [128, 512]) for i in range(2)]
    PG = [ps("PG%d" % i, [128, 512]) for i in range(2)]
    PU = [ps("PU%d" % i, [128, 512]) for i in range(2)]
    aTv = aT_d.rearrange("(kc p) t -> p kc t", p=128)
    wi = 0
    di = 0
    for g in range(ngroups if ngroups is not None else NT // G):
        for j in range(TG):
            tok0 = g * G + j * 128
            x_ = xt[j % 2]
            a_ = at[j % 2]
            b.load(x_, xres[tok0:tok0 + 128, :])
            b.load(a_, aTv[:, :, tok0:tok0 + 128], q="pool")
            for n in range(2):
                for kc in range(8):
                    b.mm(PP[n], a_[:, kc, :], (wo[:, kc, n * 512:(n + 1) * 512], WOK(kc)), start=(kc == 0), stop=(kc == 7))
                b.tt((acc[j][:, n * 512:(n + 1) * 512], ("acc", j, n)), PP[n], x_[:, n * 512:(n + 1) * 512], ALU.add)
            ACCK = KL([("acc", j, 0), ("acc", j, 1)])
            b.act(junk, (acc[j], ACCK), AF.Square, accum=ss)
            b.act(rstd, ss, AF.Sqrt, scale=1.0 / 1024, bias=1e-6)
            b.recip(rstd, rstd)
            if not moe:
                b.stt(xs, (acc[j], ACCK), rstd, gbc, ALU.mult, ALU.mult)
                for kc in range(8):
                    b.tr((PT[:, kc, :], ("PT", kc)), xs[:, kc * 128:(kc + 1) * 128], identb)
                b.copy((hT[:, :, j * 128:(j + 1) * 128], ("hT", j)), (PT, KL([("PT", kc) for kc in range(8)])), eng="act")
            else:
                b.stt(xs32, (acc[j], ACCK), rstd, gbc, ALU.mult, ALU.mult)
                for kc in range(8):
                    pt_ = PTa if kc < 4 else PTb
                    b.tr((pt_[:, kc % 4, :], (pt_.tensor.name, kc % 4)), xs32[:, kc * 128:(kc + 1) * 128], ident)
                for kc in range(8):
                    pt_ = PTa if kc < 4 else PTb
                    pass
                b.copy((hT32[:, 0:4, :], ("hT32", 0)), (PTa, KL([("PTa", i) for i in range(4)])), eng="act")
                b.copy((hT32[:, 4:8, :], ("hT32", 1)), (PTb, KL([("PTb", i) for i in range(4)])), eng="dve")
                H32 = KL([("hT32", 0), ("hT32", 1)])
                b.copy((hT[:, :, j * 128:(j + 1) * 128], ("hT", j)), (hT32, H32), eng="act")
                for kc in range(8):
                    b.mm((PP[0][:, 0:8], "PP0"), (hT32[:, kc, :], ("hT32", kc // 4)), rt[:, kc, :], start=(kc == 0), stop=(kc == 7))
                b.copy(lg, (PP[0][:, 0:8], "PP0"))
                b.P.add("dve", (lambda o, i: (lambda e: e.reduce_max(out=o, in_=i, axis=AX.X)))(sm[:, 0:1], lg), reads=["lg"], writes=["sm"])
                b.ts(mk1, lg, sm[:, 0:1], None, op0=ALU.is_equal)
                b.stt(lg2, mk1, -1e30, lg, ALU.mult, ALU.add)
                b.P.add("dve", (lambda o, i: (lambda e: e.reduce_max(out=o, in_=i, axis=AX.X)))(sm[:, 1:2], lg2), reads=["lg2"], writes=["sm"])
                b.ts(mk2, lg2, sm[:, 1:2], None, op0=ALU.is_equal)
                b.ts(sm[:, 2:3], sm[:, 0:1], -1.0, None, op0=ALU.mult)
                b.act(sm[:, 3:4], sm[:, 1:2], AF.Exp, bias=sm[:, 2:3])
                b.ts(sm[:, 4:5], sm[:, 3:4], 1.0, None, op0=ALU.add)
                b.recip(sm[:, 4:5], sm[:, 4:5])
                b.tt(sm[:, 5:6], sm[:, 3:4], sm[:, 4:5], ALU.mult)
                b.ts(mk1, mk1, sm[:, 4:5], None, op0=ALU.mult)
                b.stt((comb[:, j, :], ("comb", j)), mk2, sm[:, 5:6], mk1, ALU.mult, ALU.add)
        HT = KL([("hT", j) for j in range(TG)])
        for e in range(E if phase >= 2 else 0):
            wgv = wg_d[e].rearrange("(kc p) f -> p kc f", p=128)
            wuv = wu_d[e].rearrange("(kc p) f -> p kc f", p=128)
            wdv = wd_d[e].rearrange("(fc p) n -> p fc n", p=128)
            for fc in range(nF):
                w1, w2 = wgb[wi % NWB], wub[wi % NWB]
                pg, pu, sg_ = PG[wi % 2], PU[wi % 2], sg[wi % 2]
                wi += 1
                b.load(w1, wgv[:, :, fc * 128:(fc + 1) * 128], q="pool")
                b.load(w2, wuv[:, :, fc * 128:(fc + 1) * 128], q="pool")
                for kc in range(8):
                    b.mm(pg[:, 0:G], w1[:, kc, :], (hT[:, kc, :], HT), start=(kc == 0), stop=(kc == 7))
                for kc in range(8):
                    b.mm(pu[:, 0:G], w2[:, kc, :], (hT[:, kc, :], HT), start=(kc == 0), stop=(kc == 7))
                b.act(sg_, pg[:, 0:G], AF.Silu)
                b.tt((hid[:, fc, :], ("hid", fc)), sg_, pu[:, 0:G], ALU.mult)
            HID = KL([("hid", fc) for fc in range(nF)])
            for n in range(2 if phase >= 3 else 0):
                wd_ = wdh[di % 2]
                di += 1
                for q4 in range(4):
                    f0, f1 = (nF * q4) // 4, (nF * (q4 + 1)) // 4
                    b.load((wd_[:, f0:f1, :], (wd_.tensor.name, q4)), wdv[:, f0:f1, n * 512:(n + 1) * 512], q="pool")
                WDK = KL([(wd_.tensor.name, q4) for q4 in range(4)])
                for j in range(TG):
                    pp = PP[j % 2]
                    for fc in range(nF):
                        b.mm(pp, (hid[:, fc, j * 128:(j + 1) * 128], HID), (wd_[:, fc, :], WDK), start=(fc == 0), stop=(fc == nF - 1))
                    av = (acc[j][:, n * 512:(n + 1) * 512], ("acc", j, n))
                    if moe:
                        b.stt(av, pp, (comb[:, j, e:e + 1], ("comb", j)), av, ALU.mult, ALU.add)
                    else:
                        b.tt(av, pp, av, ALU.add)
        for j in range(TG):
            tok0 = g * G + j * 128
            b.store(out[tok0:tok0 + 128, :], (acc[j], KL([("acc", j, 0), ("acc", j, 1)])))
    b.finish()
    return nc


LAMBDA_INIT1 = 0.8 - 0.6 * float(np.exp(-0.3 * 1))
TWO_PI = 6.283185307179586
CW1 = 6.28125
CW2 = TWO_PI - 6.28125
MAGIC = 12582912.0


def build_l3(nblk=T_SEQ // 512, phase=99):
    nc = bass.Bass("TRN2", target_bir_lowering=False)
    dr = lambda n, s, dt=F32, kind="ExternalInput": nc.dram_tensor(n, s, dt, kind=kind).ap()
    x = dr("x", [T_SEQ, 1024])
    pos_d = dr("pos", [1, T_SEQ], I32)
    w_d = dr("w", [1024, 768])
    gmix = dr("gmix", [128, 8])
    cvec = dr("cvec", [128, 4])
    lams = dr("lams", [1, 256])
    ident_d = dr("ident", [128, 128])
    bones_d = dr("bones", [128, 128])
    rot_d = dr("rot", [128, 128])
    cmask_d = dr("cmask", [128, 4 * 512])
    oT = dr("oT", [256, T_SEQ], kind="ExternalOutput")

    b = B(nc)
    sb, ps = b.sb, b.ps
    wb = sb("wb", [128, 8, 768], BF16)
    wst = [sb("wst%d" % i, [128, 768]) for i in range(2)]
    gm = sb("gm", [128, 8]); cv = sb("cv", [128, 8])
    ident = sb("ident_s", [128, 128]); identb = sb("identb", [128, 128], BF16)
    bones = sb("bones_s", [128, 128]); rot = sb("rot_s", [128, 128])
    ones_f = sb("ones_f", [128, 128]); ones_b = sb("ones_b", [128, 128], BF16)
    cmask = sb("cmask_s", [128, 4, 512], BF16)
    lm = sb("lm", [128, 256]); lmp = sb("lmp", [128, 128]); lsc = sb("lsc", [128, 8])
    for t_, d_ in ((gm, gmix), (cv[:, 0:4], cvec), (ident, ident_d), (bones, bones_d), (rot, rot_d), (lm, lams.partition_broadcast(128))):
        b.load(t_, d_)
    b.load(identb, ident_d, q="pool")
    b.load((cmask[:, :, :].rearrange("p a b -> p (a b)"), "cmask_s"), cmask_d, q="pool")
    b.memset(ones_f, 1.0)
    b.memset(ones_b, 1.0)
    for kc in range(8):
        b.load(wst[kc % 2], w_d[kc * 128:(kc + 1) * 128, :])
        b.ts((wb[:, kc, :], ("wb", kc)), wst[kc % 2], gm[:, kc:kc + 1], None, op0=ALU.mult)
    WBK = lambda kc: ("wb", kc)
    col = lambda i: cv[:, i:i + 1]
    b.tt((lmp[:, 0:64], "lmp"), lm[:, 0:64], lm[:, 64:128], ALU.mult)
    b.tt((lmp[:, 64:128], "lmp"), lm[:, 128:192], lm[:, 192:256], ALU.mult)
    b.P.add("dve", lambda e: e.reduce_sum(out=lsc[:, 0:1], in_=lmp[:, 0:64], axis=AX.X), reads=["lmp"], writes=["lsc"])
    b.P.add("dve", lambda e: e.reduce_sum(out=lsc[:, 1:2], in_=lmp[:, 64:128], axis=AX.X), reads=["lmp"], writes=["lsc"])
    b.act(lsc[:, 0:2], lsc[:, 0:2], AF.Exp)
    b.tt(lsc[:, 2:3], lsc[:, 0:1], lsc[:, 1:2], ALU.subtract)
    b.ts(lsc[:, 3:4], lsc[:, 2:3], LAMBDA_INIT1, -1.0, op0=ALU.add, op1=ALU.mult)
    b.ts(col(4), col(2), 1.0 - LAMBDA_INIT1, None, op0=ALU.mult)
    NEGLAM = lsc[:, 3:4]

    TA = max(nblk, 1) * 512
    QT = [sb("QT%d" % h, [128, TA], BF16) for h in range(2)]
    KT = [sb("KT%d" % h, [128, TA], BF16) for h in range(2)]
    VS = [sb("VS%d" % h, [128, TA // 128, 128], BF16) for h in range(2)]
    xt = [sb("xt%d" % i, [128, 1024]) for i in range(2)]
    xs = [sb("xs%d" % i, [128, 1024], BF16) for i in range(2)]
    junk = sb("junk", [128, 1024], BF16)
    ss = sb("ss", [128, 1]); rstd = sb("rstd", [128, 1])
    xnT = sb("xnT", [128, 8, 512], BF16)
    posi = sb("posi", [128, 512], I32)
    tmp = [sb("t%d" % i, [128, 512]) for i in range(8)]
    Eb = [sb("Eb%d" % i, [128, 512], BF16) for i in range(4)]
    PSB = [ps("PSB%d" % i, [128, 512]) for i in range(2)]
    PSX = ps("PSX", [128, 512])
    PVA = [ps("PVA%d" % i, [128, 512]) for i in range(2)]
    PSM = [ps("PSM%d" % i, [128, 512]) for i in range(2)]
    PTB = ps("PTB", [128, 8, 128], BF16)
    for s in range(nblk):
        for j in range(4):
            tix = (s * 4 + j) % 2
            b.load(xt[tix], x[s * 512 + j * 128: s * 512 + (j + 1) * 128, :])
            rms_tile_T(b, xt[tix], xs[tix], ss, rstd, PTB, (xnT[:, :, j * 128:(j + 1) * 128], ("xnT", j)), identb, junk)
        XN = KL([("xnT", j) for j in range(4)])
        if phase < 0.4:
            continue
        b.load(posi, pos_d[:, s * 512:(s + 1) * 512].partition_broadcast(128))
        ang = tmp[2]
        b.copy(ang, posi)
        b.ts(ang, ang, col(3), None, op0=ALU.mult)
        for which, shift in ((0, 0.0), (1, 1.5707963267948966)):
            a_ = tmp[3]
            kf = tmp[4]
            if shift:
                b.ts(a_, ang, shift, None, op0=ALU.add)
            else:
                a_ = ang
            b.ts(kf, a_, 1.0 / TWO_PI, MAGIC, op0=ALU.mult, op1=ALU.add)
            b.ts(kf, kf, -MAGIC, None, op0=ALU.add)
            r_ = tmp[5]
            b.stt(r_, kf, -CW1, a_, ALU.mult, ALU.add)
            b.stt(r_, kf, -CW2, r_, ALU.mult, ALU.add)
            b.ts(r_, r_, 3.1415925, -3.1415925, op0=ALU.min, op1=ALU.max)
            b.act(tmp[which], r_, AF.Sin)
        SIN, COS = tmp[0], tmp[1]
        if phase < 0.6:
            continue
        for cc in range(4):
            pp = PSB[cc % 2]
            for kc in range(8):
                b.mm(pp, (wb[:, kc, cc * 128:(cc + 1) * 128], WBK(kc)), (xnT[:, kc, :], XN), start=(kc == 0), stop=(kc == 7))
            sq = tmp[2]
            b.act(sq, pp, AF.Square)
            b.mm(PSX, bones, sq)
            b.act(sq, PSX, AF.Sqrt, scale=1.0 / 64, bias=1e-6)
            b.recip(sq, sq)
            qn = tmp[3]
            b.stt(qn, pp, col(0 if cc < 2 else 1), sq, ALU.mult, ALU.mult)
            if phase < 0.7:
                continue
            b.mm(PVA[0], rot, qn)
            t1 = tmp[4]
            b.tt(t1, qn, COS, ALU.mult)
            t2 = tmp[5]
            b.tt(t2, PVA[0], SIN, ALU.mult)
            if phase < 0.75:
                continue
            dst = (QT if cc < 2 else KT)[cc % 2]
            b.stt((dst[:, s * 512:(s + 1) * 512], (dst.tensor.name, s)), t1, 1.0, t2, ALU.mult, ALU.add)
        if phase < 0.8:
            continue
        for j in range(4):
            pp = PSB[j % 2]
            for kc in range(8):
                b.mm(pp[:, 0:256], (xnT[:, kc, j * 128:(j + 1) * 128], XN), (wb[:, kc, 512:768], WBK(kc)), start=(kc == 0), stop=(kc == 7))
            for h in range(2):
                b.copy((VS[h][:, s * 4 + j, :], (VS[h].tensor.name, s)), pp[:, h * 128:(h + 1) * 128], eng="act")
    if phase < 2:
        nb2 = 0
    else:
        nb2 = nblk
    scale = 64 ** -0.5
    ei = 0
    for h in range(2):
        for i in range(nb2):
            qs = slice(i * 512, (i + 1) * 512)
            QK = (QT[h].tensor.name, i)
            nkb = 4 * i + 4
            for kb in range(nkb):
                KK = (KT[h].tensor.name, kb // 4)
                VK = (VS[h].tensor.name, kb // 4)
                for c in range(2):
                    pr = slice(64 * c, 64 * c + 64)
                    psb = PSB[c]
                    b.mm(psb, (KT[h][pr, kb * 128:(kb + 1) * 128], KK), (QT[h][pr, qs], QK))
                    e_ = Eb[ei % 4]
                    ei += 1
                    b.act(e_, psb, AF.Exp, scale=scale)
                    if kb >= 4 * i:
                        b.tt(e_, e_, (cmask[:, kb - 4 * i, :], "cmask_s"), ALU.mult)
                    b.mm(PVA[c], (VS[h][:, kb, :], VK), e_, start=(kb == 0), stop=(kb == nkb - 1))
                    b.mm(PSM[c], ones_b, e_, start=(kb == 0), stop=(kb == nkb - 1))
            r0, r1, o_ = tmp[0], tmp[1], tmp[2]
            b.recip(r0, PSM[0])
            b.tt(r0, PVA[0], r0, ALU.mult)
            b.recip(r1, PSM[1])
            b.tt(r1, PVA[1], r1, ALU.mult)
            b.stt(o_, r1, NEGLAM, r0, ALU.mult, ALU.add)
            sq = tmp[3]
            b.act(sq, o_, AF.Square)
            b.mm(PSX, ones_f, sq)
            b.act(sq, PSX, AF.Sqrt, scale=1.0 / 128, bias=1e-6)
            b.recip(sq, sq)
            on = tmp[4 + (i % 2)]
            b.stt(on, o_, col(4), sq, ALU.mult, ALU.mult)
            b.store(oT[h * 128:(h + 1) * 128, qs], on)
    b.finish()
    return nc


def _consts_l3():
    p = np.arange(128)
    bones = (p[:, None] // 64 == p[None, :] // 64).astype(np.float32)
    rot = np.zeros((128, 128), np.float32)
    for d in range(128):
        dm = d % 64
        if dm < 8:
            rot[d + 8, d] = -1.0
        elif dm < 16:
            rot[d - 8, d] = 1.0
    invf = np.zeros((128,), np.float32)
    for q in range(128):
        if q % 64 < 16:
            invf[q] = np.float32(500000.0) ** np.float32(-(2 * (q % 8)) / 16.0)
    kp = np.arange(128)[:, None]
    qc = np.arange(512)[None, :]
    cm = np.concatenate([((128 * j + kp) <= qc).astype(np.float32) for j in range(4)], axis=1)
    return dict(ident=np.eye(128, dtype=np.float32), bones=bones, rot=rot, cmask=np.ascontiguousarray(cm)), invf


def l3_inputs(inp, x1, bi, g):
    f = lambda k: np.asarray(inp[k][0], np.float32)
    W = f("o_w_qkv")
    hs = [2 * g, 2 * g + 1]
    cols = np.concatenate([np.arange(h * 128, (h + 1) * 128) for h in hs] + [1024 + np.arange(h * 128, (h + 1) * 128) for h in hs]
                          + [2048 + np.arange(h * 128, (h + 1) * 128) for h in hs])
    consts, invf = _consts_l3()
    cvec = np.zeros((128, 4), np.float32)
    cvec[:, 0] = np.tile(f("o_q_norm"), 2)
    cvec[:, 1] = np.tile(f("o_k_norm"), 2)
    cvec[:, 2] = f("o_subln")
    cvec[:, 3] = invf
    d = dict(x=np.ascontiguousarray(x1[bi]), pos=np.ascontiguousarray(inp["positions"][bi].reshape(1, -1).astype(np.int32)),
             w=np.ascontiguousarray(W[:, cols]), gmix=np.ascontiguousarray(f("o_ln_mix").reshape(8, 128).T), cvec=cvec,
             lams=np.concatenate([f("o_lambda_q1"), f("o_lambda_k1"), f("o_lambda_q2"), f("o_lambda_k2")]).reshape(1, 256))
    d.update(consts)
    return d


N_CORES = 8


def _run(nc, in_maps):
    res = run_bass_kernel_spmd(nc, in_maps, core_ids=list(range(N_CORES)))
    return res.results


def kernel(**inp):
    inp = {k: np.asarray(v) for k, v in inp.items()}
    x = np.ascontiguousarray(inp["x"], dtype=np.float32)
    Bn, T, D = x.shape
    f = lambda k: np.asarray(inp[k][0], np.float32)
    ident = np.eye(128, dtype=np.float32)
    nc1 = build_l1()
    r1 = _run(nc1, [l1_inputs(inp, c // 4, c % 4) for c in range(N_CORES)])
    yT = np.zeros((Bn, 1024, T), np.float32)
    for c in range(N_CORES):
        bi, g = c // 4, c % 4
        yT[bi, g * 128:(g + 1) * 128] = r1[c]["yT"][0:128]
        yT[bi, 512 + g * 128:512 + (g + 1) * 128] = r1[c]["yT"][128:256]
    del r1
    nc2 = build_ffn(NT=2048, F=2816, E=1, G=512, moe=False)
    maps = []
    for c in range(N_CORES):
        bi, tq = c // 4, c % 4
        ts_ = slice(tq * 2048, (tq + 1) * 2048)
        maps.append(dict(xres=np.ascontiguousarray(x[bi, ts_]), aT=np.ascontiguousarray(yT[bi][:, ts_]), wproj=f("e_w_out"),
                         gain=np.ascontiguousarray(f("e_ln_ffn").reshape(1, 1024)), wg=np.asarray(inp["e_ffn_gate"], np.float32),
                         wu=np.asarray(inp["e_ffn_up"], np.float32), wd=np.asarray(inp["e_ffn_down"], np.float32), ident=ident))
    r2 = _run(nc2, maps)
    x1 = np.zeros((Bn, T, D), np.float32)
    for c in range(N_CORES):
        bi, tq = c // 4, c % 4
        x1[bi, tq * 2048:(tq + 1) * 2048] = r2[c]["out"]
    del r2, maps
    nc3 = build_l3()
    r3 = _run(nc3, [l3_inputs(inp, x1, c // 4, c % 4) for c in range(N_CORES)])
    oT = np.zeros((Bn, 1024, T), np.float32)
    for c in range(N_CORES):
        bi, g = c // 4, c % 4
        oT[bi, g * 256:(g + 1) * 256] = r3[c]["oT"]
    del r3
    nc4 = build_ffn(NT=2048, F=3584, E=8, G=512, moe=True)
    wg = np.asarray(inp["o_moe_gate"][0], np.float32)
    wu = np.asarray(inp["o_moe_up"][0], np.float32)
    wd = np.asarray(inp["o_moe_down"][0], np.float32)
    maps = []
    for c in range(N_CORES):
        bi, tq = c // 4, c % 4
        ts_ = slice(tq * 2048, (tq + 1) * 2048)
        maps.append(dict(xres=np.ascontiguousarray(x1[bi, ts_]), aT=np.ascontiguousarray(oT[bi][:, ts_]), wproj=f("o_w_o"),
                         gain=np.ascontiguousarray(f("o_ln_ffn").reshape(1, 1024)), wg=wg, wu=wu, wd=wd, router=f("o_router"), ident=ident))
    r4 = _run(nc4, maps)
    out = np.zeros((Bn, T, D), np.float32)
    for c in range(N_CORES):
        bi, tq = c // 4, c % 4
        out[bi, tq * 2048:(tq + 1) * 2048] = r4[c]["out"]
    return out
```

```python
import contextlib
import numpy as np
import ml_dtypes
import concourse.bass as bass
import concourse.mybir as mybir
from concourse.alu_op_type import AluOpType as ALU
from concourse.bass_utils import run_bass_kernel_spmd

AF = mybir.ActivationFunctionType
F32 = mybir.dt.float32
BF16 = mybir.dt.bfloat16
I32 = mybir.dt.int32
AX = mybir.AxisListType

COMPUTE = ("pe", "act", "dve", "pool")


class _Op:
    __slots__ = ("eng", "fn", "reads", "writes", "kind", "waits", "signal", "val", "dkey", "seq")

    def __init__(self, eng, fn, reads, writes, kind):
        self.eng = eng
        self.fn = fn
        self.reads = tuple(reads)
        self.writes = tuple(writes)
        self.kind = kind
        self.waits = {}
        self.signal = False
        self.val = None
        self.dkey = None


class Prog:
    def __init__(self, nc):
        self.nc = nc
        self.ops = []
        self.last_w = {}
        self.readers = {}
        self.deps = []
        self.group_keys = set()

    def _add(self, op):
        deps = set()
        for k in op.reads:
            w = self.last_w.get(k)
            if w is not None:
                deps.add((w, "raw"))
        for k in op.writes:
            w = self.last_w.get(k)
            if w is not None:
                deps.add((w, "waw"))
            for r in self.readers.get(k, ()):
                if r is not op:
                    deps.add((r, "war"))
        for k in op.reads:
            self.readers.setdefault(k, []).append(op)
        for k in op.writes:
            self.last_w[k] = op
            self.readers[k] = []
        op.seq = len(self.ops)
        self.ops.append(op)
        self.deps.append(deps)
        return op

    def add(self, eng, fn, reads=(), writes=()):
        return self._add(_Op(eng, fn, reads, writes, "c"))

    def dma(self, q, fn, reads=(), writes=(), key=None):
        op = _Op(q, fn, reads, writes, "d")
        op.dkey = key
        return self._add(op)

    def emit(self, final_keys=(), sem_stack=None):
        nc = self.nc
        ops = self.ops
        for op, deps in zip(ops, self.deps):
            for d, kind in deps:
                if d.kind == "c":
                    if d.eng == op.eng and op.kind == "c":
                        if op.eng == "pe" or kind == "war":
                            continue
                    d.signal = True
        finals = [self.last_w[k] for k in final_keys if k in self.last_w]
        for d in finals:
            if d.kind == "c":
                d.signal = True
        cnt = {e: 0 for e in COMPUTE}
        dcnt = {}
        dkeys = []
        for op in ops:
            if op.kind == "c":
                if op.signal:
                    cnt[op.eng] += 1
                    op.val = cnt[op.eng]
            else:
                if op.dkey not in dcnt:
                    dcnt[op.dkey] = 0
                    dkeys.append(op.dkey)
                dcnt[op.dkey] += 16
                op.val = dcnt[op.dkey]
        for op in ops:
            if op.kind == "d" and op.dkey in self.group_keys:
                op.val = dcnt[op.dkey]
        with contextlib.ExitStack() as st_local:
            st = sem_stack if sem_stack is not None else st_local
            tag = "_%d" % len(getattr(st, "_exit_callbacks", ())) if sem_stack is not None else ""
            csem = {e: st.enter_context(nc.semaphore("cs_" + e + tag)) for e in COMPUTE}
            dsem = {k: st.enter_context(nc.semaphore("ds%d%s" % (i, tag))) for i, k in enumerate(dkeys)}

            def semof(d):
                return csem[d.eng] if d.kind == "c" else dsem[d.dkey]

            streams = {}
            waited = {}
            for op, deps in zip(ops, self.deps):
                need = {}
                for d, kind in deps:
                    if d.kind == "c" and d.eng == op.eng and op.kind == "c":
                        if op.eng == "pe" or kind == "war":
                            continue
                    s = semof(d)
                    sid = id(s)
                    if need.get(sid, (None, 0))[1] < d.val:
                        need[sid] = (s, d.val)
                w = waited.setdefault(op.eng, {})
                op.waits = []
                for sid, (s, v) in need.items():
                    if w.get(sid, 0) < v:
                        w[sid] = v
                        op.waits.append((s, v))
                streams.setdefault(op.eng, []).append(op)
            fin_waits = []
            for d in finals:
                fin_waits.append((semof(d), d.val))

            def run_stream(name, engine, extra=None):
                for op in streams.get(name, []):
                    for s, v in op.waits:
                        engine.wait_ge(s, v)
                    ins = op.fn(engine)
                    if op.kind == "c":
                        if op.signal:
                            ins.then_inc(csem[op.eng], 1)
                    else:
                        ins.then_inc(dsem[op.dkey], 16)
                if extra:
                    for s, v in extra:
                        engine.wait_ge(s, v)

            with nc.Block() as block:
                @block.tensor
                def _(e):
                    run_stream("pe", e)

                @block.scalar
                def _(e):
                    run_stream("act", e)

                @block.vector
                def _(e):
                    run_stream("dve", e)

                @block.gpsimd
                def _(e):
                    run_stream("pool", e)

                @block.sync
                def _(e):
                    run_stream("sp", e, extra=fin_waits)


class KL(list):
    pass


KEYMAP = {"PS0": KL([("PS0", 0), ("PS0", 1)]), "PS1": KL([("PS1", 0), ("PS1", 1)])}


def _ak(x):
    if isinstance(x, tuple):
        ap, k = x
        return (ap, k if isinstance(k, KL) else KL([k]))
    n = x.tensor.name
    return (x, KEYMAP.get(n.split("__")[-1]) or KL([n]))


class B:
    def __init__(self, nc, pre=""):
        self.nc = nc
        self.pre = pre
        self.P = Prog(nc)
        self.st = contextlib.ExitStack()
        self.nout = 0

    def sb(self, name, shape, dt=F32):
        return self.st.enter_context(self.nc.sbuf_tensor(self.pre + name, shape, dt))[:]

    def ps(self, name, shape, dt=F32):
        return self.st.enter_context(self.nc.psum_tensor(self.pre + name, shape, dt))[:]

    def mm(self, out, lhsT, rhs, start=True, stop=True):
        (o, ok), (l, lk), (r, rk) = _ak(out), _ak(lhsT), _ak(rhs)
        self.P.add("pe", lambda e: e.matmul(o, l, r, start=start, stop=stop), reads=lk + rk, writes=ok)

    def tr(self, out, in_, ident):
        (o, ok), (i, ik), (d, dk) = _ak(out), _ak(in_), _ak(ident)
        self.P.add("pe", lambda e: e.transpose(o, i, d), reads=ik + dk, writes=ok)

    def act(self, out, in_, func, scale=None, bias=None, accum=None, eng="act"):
        (o, ok), (i, ik) = _ak(out), _ak(in_)
        reads = list(ik)
        writes = list(ok)
        kw = {}
        if scale is not None:
            if isinstance(scale, (int, float)):
                kw["scale"] = float(scale)
            else:
                s, sk = _ak(scale)
                kw["scale"] = s
                reads.extend(sk)
        if bias is not None:
            if isinstance(bias, (int, float)):
                kw["bias"] = float(bias)
            else:
                b_, bk = _ak(bias)
                kw["bias"] = b_
                reads.extend(bk)
        if accum is not None:
            a_, ak_ = _ak(accum)
            kw["accum_out"] = a_
            writes.extend(ak_)
        self.P.add("act", lambda e: e.activation(out=o, in_=i, func=func, **kw), reads=reads, writes=writes)

    def tt(self, out, in0, in1, op, eng="dve"):
        (o, ok), (a, ak_), (b_, bk) = _ak(out), _ak(in0), _ak(in1)
        self.P.add(eng, lambda e: e.tensor_tensor(out=o, in0=a, in1=b_, op=op), reads=ak_ + bk, writes=ok)

    def ts(self, out, in0, s1, s2=None, op0=ALU.mult, op1=None, eng="dve", accum=None):
        (o, ok), (a, ak_) = _ak(out), _ak(in0)
        reads = list(ak_)
        writes = list(ok)

        def sc(s):
            if s is None or isinstance(s, (int, float)):
                return s
            ap, k = _ak(s)
            reads.extend(k)
            return ap
        v1, v2 = sc(s1), sc(s2)
        kw = {}
        if op1 is not None:
            kw["op1"] = op1
        if accum is not None:
            a2, a2k = _ak(accum)
            kw["accum_out"] = a2
            writes.extend(a2k)
        self.P.add(eng, lambda e: e.tensor_scalar(out=o, in0=a, scalar1=v1, scalar2=v2, op0=op0, **kw), reads=reads, writes=writes)

    def stt(self, out, in0, scalar, in1, op0, op1):
        (o, ok), (a, ak_), (b_, bk) = _ak(out), _ak(in0), _ak(in1)
        reads = list(ak_ + bk)
        if isinstance(scalar, (int, float)):
            s = float(scalar)
        else:
            s, sk = _ak(scalar)
            reads.extend(sk)
        self.P.add("dve", lambda e: e.scalar_tensor_tensor(out=o, in0=a, scalar=s, in1=b_, op0=op0, op1=op1), reads=reads, writes=ok)

    def copy(self, out, in_, eng="dve"):
        (o, ok), (i, ik) = _ak(out), _ak(in_)
        if eng == "act":
            self.P.add("act", lambda e: e.activation(out=o, in_=i, func=AF.Copy), reads=ik, writes=ok)
        else:
            self.P.add(eng, lambda e: e.tensor_copy(out=o, in_=i), reads=ik, writes=ok)

    def scan(self, out, d0, d1, init):
        (o, ok), (a, ak_), (b_, bk) = _ak(out), _ak(d0), _ak(d1)
        reads = list(ak_ + bk)
        if isinstance(init, (int, float)):
            iv = float(init)
        else:
            iv, ik = _ak(init)
            reads.extend(ik)
        self.P.add("dve", lambda e: e.tensor_tensor_scan(out=o, data0=a, data1=b_, initial=iv, op0=ALU.mult, op1=ALU.add), reads=reads, writes=ok)

    def recip(self, out, in_):
        (o, ok), (i, ik) = _ak(out), _ak(in_)
        self.P.add("dve", lambda e: e.reciprocal(out=o, in_=i), reads=ik, writes=ok)

    def memset(self, out, val, eng="dve"):
        (o, ok) = _ak(out)
        self.P.add(eng, lambda e: e.memset(o, val), writes=ok)

    def load(self, out, in_, q="sp", dkey=None, grp=False):
        (o, ok) = _ak(out)
        if grp:
            self.P.group_keys.add(dkey)
        self.P.dma(q, lambda e: e.dma_start(out=o, in_=in_), writes=ok, key=(dkey if dkey is not None else ok[0]))

    def store(self, out_dram, in_, q="sp"):
        (i, ik) = _ak(in_)
        self.nout += 1
        k = ("__out", self.nout)
        self.P.dma(q, lambda e: e.dma_start(out=out_dram, in_=i), reads=ik, writes=[k], key=ik[0])

    def finish(self, sem_stack=None):
        self.P.emit(final_keys=[("__out", i + 1) for i in range(self.nout)], sem_stack=sem_stack)
        self.st.close()


def rms_tile_T(b, xt, xs, ss, rstd, PT, xnT_dst, identb, junk, eps=1e-6, D=1024):
    b.act(junk, xt, AF.Square, accum=ss)
    b.act(rstd, ss, AF.Sqrt, scale=1.0 / D, bias=eps)
    b.recip(rstd, rstd)
    b.ts(xs, xt, rstd, None, op0=ALU.mult)
    n = D // 128
    for kc in range(n):
        b.tr((PT[:, kc, :], (PT.tensor.name, kc)), xs[:, kc * 128:(kc + 1) * 128], identb)
    b.copy(xnT_dst, (PT[:, :, :], KL([(PT.tensor.name, kc) for kc in range(n)])), eng="act")


T_SEQ = 8192
SEG = 512
CH = 64
C0 = 0.6065306597126334
NV1 = 20


def build_l1(nseg=T_SEQ // SEG, phase=99, nc=None, pre="", fused=None):
    if nc is None:
        nc = bass.Bass("TRN2", target_bir_lowering=False)
    dr = lambda n, s, dt=F32, kind="ExternalInput": nc.dram_tensor(pre + n, s, dt, kind=kind).ap()
    x = dr("x", [T_SEQ, 1024])
    w_in = dr("w_in", [1024, 896])
    gmix = dr("gmix", [128, 8])
    cvec = dr("cvec", [128, NV1])
    wa_d = dr("wa", [128, 128])
    wx_d = dr("wx", [128, 128])
    w2a2_d = dr("w2a2", [128, 128])
    g2_d = dr("g2", [128, 128])
    ident_d = dr("ident", [128, 128])
    bones_d = dr("bones", [128, 128])
    maska_d = dr("maska", [128, 512])
    maskb_d = dr("maskb", [128, 256])
    rmask_d = dr("rmask", [128, 512])
    id2_d = dr("id2", [128, 128])
    if fused is None:
        yT = dr("yT", [256, T_SEQ], kind="ExternalOutput")
    else:
        wop_d = dr("wo_part", [256, 1024])

    b = B(nc, pre)
    sb, ps = b.sb, b.ps
    wb = sb("wb", [128, 8, 896], BF16)
    if fused is not None:
        wop = sb("wop", [128, 2, 1024], BF16)
        ybf = sb("ybf", [128, 2, SEG], BF16)
        p1t = [sb("p1t%d" % i, [128, 1024]) for i in range(2)]
        for kc in range(2):
            b.load((wop[:, kc, :], ("wop", kc)), wop_d[kc * 128:(kc + 1) * 128, :], q="pool", dkey="constp", grp=True)
    wst = [sb("wst%d" % i, [128, 896]) for i in range(2)]
    gm = sb("gm", [128, 8])
    cv = sb("cv", [128, NV1 + 4])
    wa = sb("wa_s", [128, 128]); wx = sb("wx_s", [128, 128])
    w2a2 = sb("w2a2_s", [128, 128]); g2 = sb("g2_s", [128, 128])
    ident = sb("ident_s", [128, 128]); identb = sb("identb", [128, 128], BF16)
    bones = sb("bones_s", [128, 128]); rkbd = sb("rkbd", [128, 128])
    id2 = sb("id2_s", [128, 2, 64])
    maska = sb("maska_s", [128, 512]); maskb = sb("maskb_s", [128, 256]); rmask = sb("rmask_s", [128, 512])
    for t_, d_ in ((gm, gmix), (cv[:, 0:NV1], cvec), (wa, wa_d), (wx, wx_d), (w2a2, w2a2_d), (g2, g2_d), (ident, ident_d),
                   (bones, bones_d), ((id2[:, :, :].rearrange("p h s -> p (h s)"), "id2_s"), id2_d), (maska, maska_d), (maskb, maskb_d), (rmask, rmask_d)):
        b.load(t_, d_, dkey="const", grp=True)
    b.load(identb, ident_d, q="pool", dkey="constp", grp=True)
    for kc in range(8):
        b.load(wst[kc % 2], w_in[kc * 128:(kc + 1) * 128, :])
        b.ts((wb[:, kc, :], ("wb", kc)), wst[kc % 2], gm[:, kc:kc + 1], None, op0=ALU.mult)
    WBK = lambda kc: ("wb", kc)
    col = lambda i: cv[:, i:i + 1]
    CCH, OMKA, TWOC = NV1, NV1 + 1, NV1 + 2
    b.act(col(CCH), col(7), AF.Exp, scale=-1.0)
    b.act(col(CCH), col(CCH), AF.Ln, bias=1.0)
    b.ts(col(TWOC), col(CCH), -16.0, None, op0=ALU.mult)
    b.ts(col(CCH), col(CCH), -8.0, None, op0=ALU.mult)
    b.ts(col(OMKA), col(16), -1.0, 1.0, op0=ALU.mult, op1=ALU.add)
    b.ts(rkbd, bones, col(17), None, op0=ALU.mult)

    xt = [sb("xt%d" % i, [128, 1024]) for i in range(2)]
    xs = [sb("xs%d" % i, [128, 1024], BF16) for i in range(2)]
    junk = sb("junk", [128, 1024], BF16)
    ss = sb("ss", [128, 1]); rstd = sb("rstd", [128, 1])
    xnT = sb("xnT", [128, 8, SEG], BF16)
    pj = [[sb("pj%d_%d" % (i, c), [128, 4 + SEG]) for c in range(7)] for i in range(2)]
    NT = 27
    tmp = [sb("t%d" % i, [128, SEG]) for i in range(NT)]
    hh = [sb("hh%d" % i, [128, SEG]) for i in range(2)]
    TM = sb("TM", [64, 2, 4, 128])
    SA = sb("SA", [64, 2, 4, 2, 64]); SBm = sb("SBm", [64, 256])
    PQ = [sb("PQ%d" % i, [64, 2, 2, 2, 64]) for i in range(2)]
    Tb = [sb("Tb%d" % i, [64, 2, 2, 64]) for i in range(2)]
    ZS = sb("ZS", [64, 2, 2, 64]); MS = sb("MS", [64, 2, 2, 2, 64])
    GP = sb("GP", [64, 2, 2, 2, 64]); GS = GP[:, 0, :, :, :]; PSm = GP[:, 1, :, :, :]
    STT = [sb("STT%d" % i, [64, 2, 64]) for i in range(2)]
    AFx = sb("AFx", [128, SEG // CH, 2, CH]); RFx = sb("RFx", [128, SEG // CH, 2, CH]); BTx = sb("BTx", [128, SEG // CH, 2, CH])
    RFh = sb("RFh", [64, 2, SEG]); WCh = sb("WCh", [64, 2, SEG // CH])
    OS = sb("OS", [128, SEG])
    PP = ps("PP", [128, 512]); PT = ps("PT", [128, 8, 128], BF16)
    PS0 = ps("PS0", [128, 512]); PS1 = ps("PS1", [128, 512])
    PA = ps("PA", [128, 512]); PC = ps("PC", [128, 512]); PX = ps("PX", [128, 512]); PO = ps("PO", [128, 512])

    for i in range(2):
        for c in range(7):
            b.memset((pj[i][c][:, 0:4], ("pjh", i, c)), 0.0)
    b.memset((STT[0][:, :, :], "STT0"), 0.0)
    for tx in (AFx, RFx, BTx):
        b.memset((tx[:, :, :, :], tx.tensor.name), 0.0, eng="pool")

    st_i = 0
    PS0a_ = ("PS0", 0)
    for s in range(nseg):
        cur = s % 2
        nxt = 1 - cur
        pjc = pj[cur]
        PJ = lambda c: ("pj", cur, c)
        PJH = lambda c: ("pjh", cur, c)
        for j in range(4):
            tix = (s * 4 + j) % 2
            b.load(xt[tix], x[s * SEG + j * 128: s * SEG + (j + 1) * 128, :])
            rms_tile_T(b, xt[tix], xs[tix], ss, rstd, PT, (xnT[:, :, j * 128:(j + 1) * 128], ("xnT", j)), identb, junk)
        XN = KL([("xnT", j) for j in range(4)])
        for cc in range(7):
            for kc in range(8):
                b.mm(PP, (wb[:, kc, cc * 128:(cc + 1) * 128], WBK(kc)), (xnT[:, kc, :], XN), start=(kc == 0), stop=(kc == 7))
            b.copy((pjc[cc][:, 4:4 + SEG], PJ(cc)), PP, eng=("act" if cc % 2 == 0 else "dve"))
            b.copy((pj[nxt][cc][:, 0:4], ("pjh", nxt, cc)), (pjc[cc][:, SEG:SEG + 4], PJ(cc)), eng="pool")
        if phase < 2:
            continue
        cur_v = lambda c: (pjc[c][:, 4:4 + SEG], PJ(c))
        sh_v = lambda c, k: (pjc[c][:, 4 - k:4 - k + SEG], KL([PJ(c), PJH(c)]))
        t = tmp
        xc = t[0]
        b.ts(xc, sh_v(0, 3), col(0), col(4), op0=ALU.mult, op1=ALU.add)
        b.stt(xc, sh_v(0, 2), col(1), xc, ALU.mult, ALU.add)
        b.stt(xc, sh_v(0, 1), col(2), xc, ALU.mult, ALU.add)
        b.stt(xc, cur_v(0), col(3), xc, ALU.mult, ALU.add)
        b.mm(PS0, wa, xc)
        b.mm(PS1, wx, xc)
        ra = t[1]
        b.act(ra, PS0, AF.Sigmoid, bias=col(5))
        av_ = t[2]
        b.act(av_, ra, AF.Exp, scale=col(CCH))
        a2_ = t[3]
        b.act(a2_, ra, AF.Exp, scale=col(TWOC))
        b.act(a2_, a2_, AF.Sqrt, scale=-1.0, bias=1.0)
        ix = t[1]
        b.act(ix, PS1, AF.Sigmoid, bias=col(6))
        b.tt(a2_, a2_, ix, ALU.mult)
        b.tt(a2_, a2_, xc, ALU.mult)
        hcur = hh[cur]
        b.scan(hcur, av_, a2_, 0.0 if s == 0 else hh[nxt][:, SEG - 1:SEG])
        gq = t[0]
        b.act(gq, cur_v(1), AF.Square)
        b.ts(gq, gq, 0.044715, 1.0, op0=ALU.mult, op1=ALU.add)
        b.tt(gq, gq, cur_v(1), ALU.mult)
        b.act(gq, gq, AF.Sigmoid, scale=1.5957691216057308)
        b.tt(gq, gq, cur_v(1), ALU.mult)
        ylru = t[1]
        if fused is None:
            b.tt(ylru, gq, hcur, ALU.mult)
            b.store(yT[0:128, s * SEG:(s + 1) * SEG], ylru)
        else:
            b.tt((ybf[:, 0, :], ("ybf", 0)), gq, hcur, ALU.mult)
        if phase < 3:
            continue
        shf = []
        for i, c in enumerate((2, 3, 4, 5, 6)):
            d = t[4 + i]
            b.tt(d, sh_v(c, 1), cur_v(c), ALU.subtract)
            b.stt(d, d, col(8 + i), cur_v(c), ALU.mult, ALU.add)
            shf.append(d)
        rs, ks, vs, xwa, xgs = shf
        tw = t[9]
        b.act(tw[0:64, :], xwa[0:64, :], AF.Tanh)
        b.mm(PS0, w2a2[0:64, :], tw[0:64, :])
        b.mm(PS1, w2a2[64:128, :], xwa[64:128, :])
        sgz = t[9]
        b.act(sgz, PS0, AF.Sigmoid, bias=col(13))
        avv = t[10]
        b.act(avv, PS1, AF.Sigmoid, bias=col(14))
        sg = t[11]
        b.act(sg, xgs, AF.Sigmoid)
        cs = t[12]
        b.scan(cs, rmask, sgz, 0.0)
        csm1 = t[13]
        b.tt(csm1, cs, sgz, ALU.subtract)
        Wt = t[14]; iW = t[15]; Wm1 = t[13]
        b.act(Wt, cs, AF.Exp, scale=-C0)
        b.act(iW, cs, AF.Exp, scale=C0)
        b.act(Wm1, csm1, AF.Exp, scale=-C0)
        b.mm(PS0, g2, sg)
        gv = t[11]
        b.copy(gv, PS0, eng="act")
        kq = t[9]
        b.ts(kq, ks, col(15), None, op0=ALU.mult)
        kq2 = t[12]
        b.act(kq2, kq, AF.Square)
        b.mm(PS1, bones, kq2)
        rn = t[12]
        b.act(rn, PS1, AF.Sqrt)
        b.ts(rn, rn, 1e-12, None, op0=ALU.max)
        b.recip(rn, rn)
        kkn = t[9]
        b.tt(kkn, kq, rn, ALU.mult)
        kmod = t[12]
        b.ts(kmod, avv, col(16), col(OMKA), op0=ALU.mult, op1=ALU.add)
        b.tt(kmod, kmod, ks, ALU.mult)
        bb = t[10]
        b.tt(bb, kkn, avv, ALU.mult)
        AFm = t[16]; RF = t[17]; BT = t[18]; KT = t[19]; Bh = t[20]; Kh = t[21]
        b.stt(AFm, kkn, -1.0, Wm1, ALU.mult, ALU.mult)
        b.tt(RF, rs, Wt, ALU.mult)
        b.tt(BT, bb, iW, ALU.mult)
        b.tt(KT, kmod, iW, ALU.mult)
        v3 = lambda tl: tl[:, :].rearrange("p (c s) -> p c s", s=CH)
        wcb = (v3(Wt)[:, :, CH - 1:CH].broadcast_to([128, SEG // CH, CH]), Wt.tensor.name)
        b.tt((v3(Bh), Bh.tensor.name), (v3(BT), BT.tensor.name), wcb, ALU.mult)
        b.tt((v3(Kh), Kh.tensor.name), (v3(KT), KT.tensor.name), wcb, ALU.mult)
        rk_ = t[9]
        b.tt(rk_, rs, kmod, ALU.mult)
        b.mm(PS1, rkbd, rk_)
        bonus = t[22]
        b.tt(bonus, PS1, vs, ALU.mult)
        if phase < 4:
            continue
        for h in range(2):
            b.mm((PS1[0:64, :], KEYMAP["PS1"]), ident[:, 64 * h:64 * h + 64], RF)
            b.copy((RFh[:, h, :], "RFh"), (PS1[0:64, :], KEYMAP["PS1"]), eng="act")
            b.mm((PS0[0:64, h * 8:h * 8 + 8], PS0a_), ident[:, 64 * h:64 * h + 64], (v3(Wt)[:, :, CH - 1], Wt.tensor.name))
        b.copy((WCh[:, :, :].rearrange("p h c -> p (h c)"), "WCh"), (PS0[0:64, 0:16], PS0a_), eng="act")
        for tl, tx in ((AFm, AFx), (RF, RFx), (BT, BTx)):
            b.copy((tx[0:64, :, 0, :], tx.tensor.name), (v3(tl)[0:64], tl.tensor.name), eng="pool")
            b.copy((tx[64:128, :, 1, :], tx.tensor.name), (v3(tl)[64:128], tl.tensor.name), eng="pool")
        PS0a, PS0b, PS1a, PS1b = ("PS0", 0), ("PS0", 1), ("PS1", 0), ("PS1", 1)
        sel = [ident[:, 0:64], ident[:, 64:128]]
        for cp in range(SEG // 128):
            tok = slice(cp * 128, (cp + 1) * 128)
            for q in range(2):
                c_ = cp * 2 + q
                ck = slice(c_ * CH, (c_ + 1) * CH)
                for qi, src in enumerate((AFm, Bh, Kh, vs)):
                    b.tr((PX[0:64, qi * 128:(qi + 1) * 128], "PX"), src[:, ck], ident)
                b.copy((TM[:, q, :, :].rearrange("p a b -> p (a b)"), ("TM", q)), (PX[0:64, :], "PX"), eng="act")
                for j, (l_, rx) in enumerate(((BT, AFx), (BT, RFx), (KT, AFx), (KT, RFx))):
                    b.mm((PA[0:64, j * 128:(j + 1) * 128], "PA"), l_[:, ck], rx[:, c_, :, :].rearrange("p h s -> p (h s)"))
                b.mm((PS1[0:64, q * 128:(q + 1) * 128], PS1a), AFm[:, ck], BTx[:, c_, :, :].rearrange("p h s -> p (h s)"))
                b.tt((SA[:, q, :, :, :].rearrange("p j h s -> p (j h s)"), ("SA", q)), (PA[0:64, :], "PA"), maska[0:64, :], ALU.mult)
            b.tt(SBm, (PS1[0:64, 0:256], PS1a), maskb[0:64, :], ALU.mult)
            TMv = lambda qi, q, h: (TM[:, q, qi, 64 * h:64 * h + 64], ("TM", q))
            SAv = lambda j, q, h: (SA[:, q, j, h, :], ("SA", q))
            SAK = KL([("SA", 0), ("SA", 1)])
            QH = [(q, h) for q in range(2) for h in range(2)]
            for q, h in QH:
                b.mm((PS1[0:64, 256 + (q * 2 + h) * 64:256 + (q * 2 + h) * 64 + 64], PS1b), SAv(2, q, h), TMv(3, q, h))
            b.copy((ZS[:, :, :, :].rearrange("p q h s -> p (q h s)"), "ZS"), (PS1[0:64, 256:512], PS1b), eng="act")
            if phase < 5:
                continue
            b.copy((PQ[0][:, 0, :, :, :], "PQ0"), (SA[:, :, 0, :, :], SAK), eng="pool")
            b.copy((PQ[0][:, 1, :, :, :].rearrange("p q h s -> p (q h s)"), "PQ0"), SBm, eng="pool")
            idb = (ident[0:64, 0:64].rearrange("p (a c s) -> p a c s", a=1, c=1).broadcast_to([64, 2, 2, 64]), ident.tensor.name)
            b.tt((Tb[0][:, :, :, :], "Tb0"), (SA[:, :, 0, :, :], SAK), idb, ALU.add)
            PC5 = PC[0:64, :].rearrange("p (j q h s) -> p j q h s", j=2, q=2, h=2)
            POt = PO[0:64, 256:512].rearrange("p (q h s) -> p q h s", q=2, h=2)
            for k in range(0, 6):
                i = k % 2
                pqk = "PQ%d" % i
                if k >= 1:
                    for q, h in QH:
                        b.mm((POt[:, q, h, :], ("PO", 2)), (PQ[i][:, 1, q, h, :], pqk), (Tb[1 - i][:, q, h, :], "Tb%d" % (1 - i)))
                if k <= 3:
                    for q, h in QH:
                        b.mm((PC5[:, 0, q, h, :], ("PC", 0)), (PQ[i][:, 1, q, h, :], pqk), (PQ[i][:, 0, q, h, :], pqk))
                if k <= 4:
                    for q, h in QH:
                        b.mm((PC5[:, 1, q, h, :], ("PC", 1)), (PQ[i][:, 0, q, h, :], pqk), (PQ[i][:, 1, q, h, :], pqk))
                if k <= 3:
                    b.copy((PQ[1 - i][:, :, :, :, :].rearrange("p j q h s -> p (j q h s)"), "PQ%d" % (1 - i)),
                           (PC[0:64, :], KL([("PC", 0), ("PC", 1)])), eng="act")
                elif k == 4:
                    b.copy((PQ[1 - i][:, 1, :, :, :].rearrange("p q h s -> p (q h s)"), "PQ%d" % (1 - i)),
                           (PC[0:64, 256:512], ("PC", 1)), eng="act")
                if k >= 1:
                    b.tt((Tb[i][:, :, :, :].rearrange("p q h s -> p (q h s)"), "Tb%d" % i),
                         (Tb[1 - i][:, :, :, :].rearrange("p q h s -> p (q h s)"), "Tb%d" % (1 - i)),
                         (PO[0:64, 256:512], ("PO", 2)), ALU.add)
            if phase < 5.2:
                continue
            for q, h in QH:
                o0 = ((q * 2 + h) * 2) * 64
                b.mm((PX[0:64, o0:o0 + 64], "PX"), (Tb[1][:, q, h, :], "Tb1"), TMv(0, q, h))
                b.mm((PX[0:64, o0 + 64:o0 + 128], "PX"), (Tb[1][:, q, h, :], "Tb1"), (ZS[:, q, h, :], "ZS"))
            b.copy((MS[:, :, :, :, :].rearrange("p q h m s -> p (q h m s)"), "MS"), (PX[0:64, :], "PX"), eng="act")
            M1T = lambda q, h: (MS[:, q, h, 0, :], "MS")
            M2T = lambda q, h: (MS[:, q, h, 1, :], "MS")
            if phase < 5.4:
                continue
            for q, h in QH:
                o0 = (q * 2 + h) * 64
                b.mm((PS0[0:64, o0:o0 + 64], PS0a), M1T(q, h), TMv(1, q, h))
                b.mm((PS0[0:64, 256 + o0:256 + o0 + 64], PS0b), M1T(q, h), SAv(1, q, h))
            for q, h in QH:
                c_ = cp * 2 + q
                o0 = (q * 2 + h) * 64
                b.stt((GS[:, q, h, :], "GS"), ident[0:64, 0:64], (WCh[:, h, c_:c_ + 1], "WCh"), (PS0[0:64, o0:o0 + 64], PS0a), ALU.mult, ALU.add)
            b.tt((PSm[:, :, :, :], "PSm"), (PS0[0:64, 256:512].rearrange("p (q h s) -> p q h s", q=2, h=2), PS0b),
                 (RFh[:, :, tok].rearrange("p h (q s) -> p q h s", q=2), "RFh"), ALU.add)
            if phase < 5.6:
                continue
            for q in range(2):
                ocol = slice(q * 64, q * 64 + 64)
                stn = "STT%d" % st_i
                for h in range(2):
                    if 5.8 <= phase < 5.9:
                        continue
                    pr = slice(64 * h, 64 * h + 64)
                    ob = (PO[pr, ocol], ("PO", 0)) if h == 0 else (PX[pr, ocol], "PX")
                    b.mm(ob, M2T(q, h), SAv(1, q, h), start=True, stop=False)
                    b.mm(ob, TMv(3, q, h), SAv(3, q, h), start=False, stop=False)
                    b.mm(ob, (STT[st_i][:, h, :], stn), (PSm[:, q, h, :], "PSm"), start=False, stop=True)
                for h in range(2):
                    if phase == 5.7:
                        continue
                    so = slice(h * 64, h * 64 + 64)
                    b.mm((PS1[0:64, so], PS1a), TMv(1, q, h), M2T(q, h), start=True, stop=False)
                    b.mm((PS1[0:64, so], PS1a), TMv(2, q, h), TMv(3, q, h), start=False, stop=False)
                    b.mm((PS1[0:64, so], PS1a), (GS[:, q, h, :], "GS"), (STT[st_i][:, h, :], stn), start=False, stop=True)
                b.copy((STT[1 - st_i][:, :, :].rearrange("p h s -> p (h s)"), "STT%d" % (1 - st_i)), (PS1[0:64, 0:128], PS1a), eng="dve")
                st_i = 1 - st_i
            b.copy((OS[0:64, tok], ("OS", cp)), (PO[0:64, 0:128], ("PO", 0)), eng="dve")
            b.copy((OS[64:128, tok], ("OS", cp)), (PX[64:128, 0:128], "PX"), eng="dve")
        if phase < 6:
            continue
        OSK = KL([("OS", i) for i in range(4)])
        b.mm(PS0, bones, (OS[:, :], OSK))
        cen = t[23]
        b.stt(cen, PS0, -1.0 / 64, (OS[:, :], OSK), ALU.mult, ALU.add)
        sq = t[24]
        b.act(sq, cen, AF.Square)
        b.mm(PS1, bones, sq)
        b.act(sq, PS1, AF.Sqrt, scale=1.0 / 64, bias=64e-5)
        b.recip(sq, sq)
        b.tt(cen, cen, sq, ALU.mult)
        b.ts(cen, cen, col(18), col(19), op0=ALU.mult, op1=ALU.add)
        b.tt(cen, cen, bonus, ALU.add)
        if fused is None:
            yrw = t[25 + (s % 2)]
            b.tt(yrw, cen, gv, ALU.mult)
            b.store(yT[128:256, s * SEG:(s + 1) * SEG], yrw)
        else:
            b.tt((ybf[:, 1, :], ("ybf", 1)), cen, gv, ALU.mult)
            for j in range(4):
                pt_ = p1t[j % 2]
                for n in range(2):
                    for kc in range(2):
                        b.mm(PP, (ybf[:, kc, j * 128:(j + 1) * 128], ("ybf", kc)), (wop[:, kc, n * 512:(n + 1) * 512], ("wop", kc)),
                             start=(kc == 0), stop=(kc == 1))
                    b.copy((pt_[:, n * 512:(n + 1) * 512], (pt_.tensor.name, n)), PP, eng=("act" if n == 0 else "dve"))
                b.store(fused["rs_in"][s * SEG + j * 128:s * SEG + (j + 1) * 128, :], (pt_, KL([(pt_.tensor.name, 0), (pt_.tensor.name, 1)])))
    b.finish(sem_stack=(fused or {}).get("sem_stack"))
    return nc


def _consts_l1():
    p = np.arange(128)
    ident = np.eye(128, dtype=np.float32)
    bones = (p[:, None] // 64 == p[None, :] // 64).astype(np.float32)
    s_ = (p % 64)[:, None]
    t_ = np.arange(64)[None, :]
    lt = (s_ < t_).astype(np.float32)
    le = (s_ <= t_).astype(np.float32)
    gt = (s_ > t_).astype(np.float32)
    eq = (s_ == t_).astype(np.float32)
    maska = np.concatenate([lt, lt, le, le, lt, lt, le, le], axis=1)
    maskb = np.concatenate([gt, gt, gt, gt], axis=1)
    id2 = np.concatenate([eq, eq], axis=1)
    rmask = np.ones((128, 512), np.float32)
    rmask[:, ::64] = 0.0
    return dict(ident=ident, bones=bones, maska=np.ascontiguousarray(maska), maskb=np.ascontiguousarray(maskb),
                id2=np.ascontiguousarray(id2), rmask=rmask)


def _blockdiag2(w2):
    o = np.zeros((128, 128), np.float32)
    o[0:64, 0:64] = w2[0]
    o[64:128, 64:128] = w2[1]
    return o


def l1_inputs(inp, bi, g):
    f = lambda k: np.asarray(inp[k][0], np.float32)
    ls = slice(g * 128, (g + 1) * 128)
    W = f("e_w_in")
    rw0 = 1024
    cols = np.concatenate([np.arange(512)[ls], 512 + np.arange(512)[ls], rw0 + np.arange(512)[ls], rw0 + 512 + np.arange(512)[ls],
                           rw0 + 1024 + np.arange(512)[ls], rw0 + 1536 + np.arange(128), rw0 + 1664 + np.arange(128)])
    mu = f("e_shift_mu")
    cvec = np.zeros((128, NV1), np.float32)
    cvec[:, 0:4] = f("e_conv_w")[:, ls].T
    cvec[:, 4] = f("e_conv_b")[ls]
    cvec[:, 5] = f("e_gate_a_b")[ls]
    cvec[:, 6] = f("e_gate_x_b")[ls]
    cvec[:, 7] = f("e_lru_lambda")[ls]
    cvec[:, 8] = mu[0:512][ls]
    cvec[:, 9] = mu[512:1024][ls]
    cvec[:, 10] = mu[1024:1536][ls]
    cvec[:, 11] = mu[1536:1664]
    cvec[:, 12] = mu[1664:1792]
    cvec[:, 13] = f("e_w0")[ls]
    cvec[:, 14] = f("e_a0")[ls]
    cvec[:, 15] = f("e_k_k")[ls]
    cvec[:, 16] = f("e_k_a")[ls]
    cvec[:, 17] = f("e_r_k").reshape(-1)[ls]
    cvec[:, 18] = f("e_gn_w")[ls]
    cvec[:, 19] = f("e_gn_b")[ls]
    d = dict(
        x=np.ascontiguousarray(inp["x"][bi]),
        w_in=np.ascontiguousarray(W[:, cols]),
        gmix=np.ascontiguousarray(f("e_ln_mix").reshape(8, 128).T),
        cvec=cvec,
        wa=_blockdiag2(f("e_gate_a_w")[2 * g:2 * g + 2]),
        wx=_blockdiag2(f("e_gate_x_w")[2 * g:2 * g + 2]),
        w2a2=np.ascontiguousarray(np.concatenate([f("e_w2")[:, ls], f("e_a2")[:, ls]], axis=0)),
        g2=np.ascontiguousarray(f("e_g2")[:, ls]),
    )
    d.update(_consts_l1())
    return d


def build_ffn(NT=2048, F=2816, E=1, G=512, moe=False, ngroups=None, phase=99, nc=None, pre="", fused=None):
    if nc is None:
        nc = bass.Bass("TRN2", target_bir_lowering=False)
    dr = lambda n, s, dt=F32, kind="ExternalInput": nc.dram_tensor(pre + n, s, dt, kind=kind).ap()
    nF = F // 128
    TG = G // 128
    if fused is None or fused.get("xres") is None:
        xres = dr("xres", [NT, 1024])
    else:
        xres = fused["xres"]
    if fused is None:
        aT_d = dr("aT", [1024, NT])
        wproj = dr("wproj", [1024, 1024])
    gain_d = dr("gain", [1, 1024])
    wg_d = dr("wg", [E, 1024, F])
    wu_d = dr("wu", [E, 1024, F])
    wd_d = dr("wd", [E, F, 1024])
    ident_d = dr("ident", [128, 128])
    if moe:
        router_d = dr("router", [1024, 8])
    if fused is None or fused.get("out") is None:
        out = dr("out", [NT, 1024], kind="ExternalOutput")
    else:
        out = fused["out"]

    b = B(nc, pre)
    sb, ps = b.sb, b.ps
    if fused is None:
        wo = sb("wo", [128, 8, 1024], BF16)
    gbc = sb("gbc", [128, 1024])
    ident = sb("ident_s", [128, 128])
    identb = sb("identb", [128, 128], BF16)
    b.load(gbc, gain_d.partition_broadcast(128), dkey="const", grp=True)
    b.load(ident, ident_d, dkey="const", grp=True)
    b.load(identb, ident_d, q="pool", dkey="constp", grp=True)
    if fused is None:
        wpv = wproj.rearrange("(kc p) n -> p kc n", p=128)
        for kc in range(8):
            b.load((wo[:, kc, :], ("wo", kc)), wpv[:, kc, :], q="pool")
    WOK = lambda kc: ("wo", kc)
    if moe:
        rt = sb("rt", [128, 8, 8])
        b.load(rt, router_d.rearrange("(kc p) e -> p kc e", p=128), dkey="const", grp=True)
        hT32 = sb("hT32", [128, 8, 128])
        lg = sb("lg", [128, 8]); lg2 = sb("lg2", [128, 8]); mk1 = sb("mk1", [128, 8]); mk2 = sb("mk2", [128, 8])
        sm = sb("sm", [128, 8])
        comb = sb("comb", [128, TG, 8])
        xs32 = sb("xs32", [128, 1024])
        PTa = ps("PTa", [128, 4, 128]); PTb = ps("PTb", [128, 4, 128])
    else:
        xs = sb("xs", [128, 1024], BF16)
        PT = ps("PT", [128, 8, 128], BF16)
    xt = [sb("xt%d" % i, [128, 1024]) for i in range(2)]
    if fused is None:
        at = [sb("at%d" % i, [128, 8, 128], BF16) for i in range(2)]
    else:
        at = [sb("at%d" % i, [128, 1024]) for i in range(2)]
    acc = [sb("acc%d" % i, [128, 1024]) for i in range(TG)]
    junk = sb("junk", [128, 1024], BF16)
    ss = sb("ss", [128, 1]); rstd = sb("rstd", [128, 1])
    hT = sb("hT", [128, 8, G], BF16)
    hid = sb("hid", [128, nF, G], BF16)
    NWB = 3
    wgb = [sb("wgb%d" % i, [128, 8, 128], BF16) for i in range(NWB)]
    wub = [sb("wub%d" % i, [128, 8, 128], BF16) for i in range(NWB)]
    wdh = [sb("wdh%d" % i, [128, nF, 512], BF16) for i in range(2)]
    sg = [sb("sg%d" % i, [128, G]) for i in range(2)]
    PP = [ps("PP%d" % i, [128, 512]) for i in range(2)]
    PG = [ps("PG%d" % i, [128, 512]) for i in range(2)]
    PU = [ps("PU%d" % i, [128, 512]) for i in range(2)]
    if fused is None:
        aTv = aT_d.rearrange("(kc p) t -> p kc t", p=128)
    wi = 0
    di = 0
    for g in range(ngroups if ngroups is not None else NT // G):
        for j in range(TG):
            tok0 = g * G + j * 128
            x_ = xt[j % 2]
            a_ = at[j % 2]
            b.load(x_, xres(tok0) if callable(xres) else xres[tok0:tok0 + 128, :])
            if fused is None:
                b.load(a_, aTv[:, :, tok0:tok0 + 128], q="pool")
                for n in range(2):
                    for kc in range(8):
                        b.mm(PP[n], a_[:, kc, :], (wo[:, kc, n * 512:(n + 1) * 512], WOK(kc)), start=(kc == 0), stop=(kc == 7))
                    b.tt((acc[j][:, n * 512:(n + 1) * 512], ("acc", j, n)), PP[n], x_[:, n * 512:(n + 1) * 512], ALU.add)
            else:
                b.load(a_, fused["add"][tok0:tok0 + 128, :])
                for n in range(2):
                    b.tt((acc[j][:, n * 512:(n + 1) * 512], ("acc", j, n)), a_[:, n * 512:(n + 1) * 512], x_[:, n * 512:(n + 1) * 512], ALU.add)
            ACCK = KL([("acc", j, 0), ("acc", j, 1)])
            b.act(junk, (acc[j], ACCK), AF.Square, accum=ss)
            b.act(rstd, ss, AF.Sqrt, scale=1.0 / 1024, bias=1e-6)
            b.recip(rstd, rstd)
            if not moe:
                b.stt(xs, (acc[j], ACCK), rstd, gbc, ALU.mult, ALU.mult)
                for kc in range(8):
                    b.tr((PT[:, kc, :], ("PT", kc)), xs[:, kc * 128:(kc + 1) * 128], identb)
                b.copy((hT[:, :, j * 128:(j + 1) * 128], ("hT", j)), (PT, KL([("PT", kc) for kc in range(8)])), eng="act")
            else:
                b.stt(xs32, (acc[j], ACCK), rstd, gbc, ALU.mult, ALU.mult)
                for kc in range(8):
                    pt_ = PTa if kc < 4 else PTb
                    b.tr((pt_[:, kc % 4, :], (pt_.tensor.name, kc % 4)), xs32[:, kc * 128:(kc + 1) * 128], ident)
                for kc in range(8):
                    pt_ = PTa if kc < 4 else PTb
                    pass
                b.copy((hT32[:, 0:4, :], ("hT32", 0)), (PTa, KL([(PTa.tensor.name, i) for i in range(4)])), eng="act")
                b.copy((hT32[:, 4:8, :], ("hT32", 1)), (PTb, KL([(PTb.tensor.name, i) for i in range(4)])), eng="dve")
                H32 = KL([("hT32", 0), ("hT32", 1)])
                b.copy((hT[:, :, j * 128:(j + 1) * 128], ("hT", j)), (hT32, H32), eng="act")
                for kc in range(8):
                    b.mm(PP[0][:, 0:8], (hT32[:, kc, :], ("hT32", kc // 4)), rt[:, kc, :], start=(kc == 0), stop=(kc == 7))
                b.copy(lg, PP[0][:, 0:8])
                b.P.add("dve", (lambda o, i: (lambda e: e.reduce_max(out=o, in_=i, axis=AX.X)))(sm[:, 0:1], lg), reads=[lg.tensor.name], writes=[sm.tensor.name])
                b.ts(mk1, lg, sm[:, 0:1], None, op0=ALU.is_equal)
                b.stt(lg2, mk1, -1e30, lg, ALU.mult, ALU.add)
                b.P.add("dve", (lambda o, i: (lambda e: e.reduce_max(out=o, in_=i, axis=AX.X)))(sm[:, 1:2], lg2), reads=[lg2.tensor.name], writes=[sm.tensor.name])
                b.ts(mk2, lg2, sm[:, 1:2], None, op0=ALU.is_equal)
                b.ts(sm[:, 2:3], sm[:, 0:1], -1.0, None, op0=ALU.mult)
                b.act(sm[:, 3:4], sm[:, 1:2], AF.Exp, bias=sm[:, 2:3])
                b.ts(sm[:, 4:5], sm[:, 3:4], 1.0, None, op0=ALU.add)
                b.recip(sm[:, 4:5], sm[:, 4:5])
                b.tt(sm[:, 5:6], sm[:, 3:4], sm[:, 4:5], ALU.mult)
                b.ts(mk1, mk1, sm[:, 4:5], None, op0=ALU.mult)
                b.stt((comb[:, j, :], ("comb", j)), mk2, sm[:, 5:6], mk1, ALU.mult, ALU.add)
        HT = KL([("hT", j) for j in range(TG)])
        for e in range(E if phase >= 2 else 0):
            wgv = wg_d[e].rearrange("(kc p) f -> p kc f", p=128)
            wuv = wu_d[e].rearrange("(kc p) f -> p kc f", p=128)
            wdv = wd_d[e].rearrange("(fc p) n -> p fc n", p=128)
            for fc in range(nF):
                w1, w2 = wgb[wi % NWB], wub[wi % NWB]
                pg, pu, sg_ = PG[wi % 2], PU[wi % 2], sg[wi % 2]
                wi += 1
                b.load(w1, wgv[:, :, fc * 128:(fc + 1) * 128], q="pool")
                b.load(w2, wuv[:, :, fc * 128:(fc + 1) * 128], q="pool")
                for kc in range(8):
                    b.mm(pg[:, 0:G], w1[:, kc, :], (hT[:, kc, :], HT), start=(kc == 0), stop=(kc == 7))
                for kc in range(8):
                    b.mm(pu[:, 0:G], w2[:, kc, :], (hT[:, kc, :], HT), start=(kc == 0), stop=(kc == 7))
                b.act(sg_, pg[:, 0:G], AF.Silu)
                b.tt((hid[:, fc, :], ("hid", fc)), sg_, pu[:, 0:G], ALU.mult)
            HID = KL([("hid", fc) for fc in range(nF)])
            for n in range(2 if phase >= 3 else 0):
                wd_ = wdh[di % 2]
                di += 1
                for q4 in range(4):
                    f0, f1 = (nF * q4) // 4, (nF * (q4 + 1)) // 4
                    b.load((wd_[:, f0:f1, :], (wd_.tensor.name, q4)), wdv[:, f0:f1, n * 512:(n + 1) * 512], q="pool", dkey=wd_.tensor.name)
                WDK = KL([(wd_.tensor.name, q4) for q4 in range(4)])
                for j in range(TG):
                    pp = PP[j % 2]
                    for fc in range(nF):
                        b.mm(pp, (hid[:, fc, j * 128:(j + 1) * 128], HID), (wd_[:, fc, :], WDK), start=(fc == 0), stop=(fc == nF - 1))
                    av = (acc[j][:, n * 512:(n + 1) * 512], ("acc", j, n))
                    if moe:
                        b.stt(av, pp, (comb[:, j, e:e + 1], ("comb", j)), av, ALU.mult, ALU.add)
                    else:
                        b.tt(av, pp, av, ALU.add)
        for j in range(TG):
            tok0 = g * G + j * 128
            b.store(out(tok0) if callable(out) else out[tok0:tok0 + 128, :], (acc[j], KL([("acc", j, 0), ("acc", j, 1)])))
    b.finish(sem_stack=(fused or {}).get("sem_stack"))
    return nc


LAMBDA_INIT1 = 0.8 - 0.6 * float(np.exp(-0.3 * 1))
TWO_PI = 6.283185307179586
CW1 = 6.28125
CW2 = TWO_PI - 6.28125
MAGIC = 12582912.0


def build_l3(nblk=T_SEQ // 512, phase=99, nc=None, pre="", fused=None):
    if nc is None:
        nc = bass.Bass("TRN2", target_bir_lowering=False)
    dr = lambda n, s, dt=F32, kind="ExternalInput": nc.dram_tensor(pre + n, s, dt, kind=kind).ap()
    if fused is None:
        x = dr("x", [T_SEQ, 1024])
    else:
        x = fused["x"]
        wop_d = dr("wo_part", [256, 1024])
    pos_d = dr("pos", [1, T_SEQ], I32)
    w_d = dr("w", [1024, 768])
    gmix = dr("gmix", [128, 8])
    cvec = dr("cvec", [128, 4])
    lams = dr("lams", [1, 256])
    ident_d = dr("ident", [128, 128])
    bones_d = dr("bones", [128, 128])
    rot_d = dr("rot", [128, 128])
    cmask_d = dr("cmask", [128, 4 * 512])
    if fused is None:
        oT = dr("oT", [256, T_SEQ], kind="ExternalOutput")

    b = B(nc, pre)
    sb, ps = b.sb, b.ps
    wb = sb("wb", [128, 8, 768], BF16)
    if fused is not None:
        wop = sb("wop", [128, 2, 1024], BF16)
        onb = [sb("onb%d" % i, [128, 512], BF16) for i in range(2)]
        p1t = [sb("p1t%d" % i, [128, 1024]) for i in range(2)]
        for kc in range(2):
            b.load((wop[:, kc, :], ("wop", kc)), wop_d[kc * 128:(kc + 1) * 128, :], q="pool", dkey="constp", grp=True)
    wst = [sb("wst%d" % i, [128, 768]) for i in range(2)]
    gm = sb("gm", [128, 8]); cv = sb("cv", [128, 8])
    ident = sb("ident_s", [128, 128]); identb = sb("identb", [128, 128], BF16)
    bones = sb("bones_s", [128, 128]); rot = sb("rot_s", [128, 128])
    ones_f = sb("ones_f", [128, 128]); ones_b = sb("ones_b", [128, 128], BF16)
    cmask = sb("cmask_s", [128, 4, 512], BF16)
    lm = sb("lm", [128, 256]); lmp = sb("lmp", [128, 128]); lsc = sb("lsc", [128, 8])
    for t_, d_ in ((gm, gmix), (cv[:, 0:4], cvec), (ident, ident_d), (bones, bones_d), (rot, rot_d), (lm, lams.partition_broadcast(128))):
        b.load(t_, d_, dkey="const", grp=True)
    b.load(identb, ident_d, q="pool", dkey="constp", grp=True)
    b.load((cmask[:, :, :].rearrange("p a b -> p (a b)"), "cmask_s"), cmask_d, q="pool", dkey="constp", grp=True)
    b.memset(ones_f, 1.0)
    b.memset(ones_b, 1.0)
    for kc in range(8):
        b.load(wst[kc % 2], w_d[kc * 128:(kc + 1) * 128, :])
        b.ts((wb[:, kc, :], ("wb", kc)), wst[kc % 2], gm[:, kc:kc + 1], None, op0=ALU.mult)
    WBK = lambda kc: ("wb", kc)
    col = lambda i: cv[:, i:i + 1]
    b.tt((lmp[:, 0:64], "lmp"), lm[:, 0:64], lm[:, 64:128], ALU.mult)
    b.tt((lmp[:, 64:128], "lmp"), lm[:, 128:192], lm[:, 192:256], ALU.mult)
    b.P.add("dve", lambda e: e.reduce_sum(out=lsc[:, 0:1], in_=lmp[:, 0:64], axis=AX.X), reads=["lmp"], writes=[lsc.tensor.name])
    b.P.add("dve", lambda e: e.reduce_sum(out=lsc[:, 1:2], in_=lmp[:, 64:128], axis=AX.X), reads=["lmp"], writes=[lsc.tensor.name])
    b.act(lsc[:, 0:2], lsc[:, 0:2], AF.Exp)
    b.tt(lsc[:, 2:3], lsc[:, 0:1], lsc[:, 1:2], ALU.subtract)
    b.ts(lsc[:, 3:4], lsc[:, 2:3], LAMBDA_INIT1, -1.0, op0=ALU.add, op1=ALU.mult)
    b.ts(col(4), col(2), 1.0 - LAMBDA_INIT1, None, op0=ALU.mult)
    NEGLAM = lsc[:, 3:4]

    TA = max(nblk, 1) * 512
    QT = [sb("QT%d" % h, [128, TA], BF16) for h in range(2)]
    KT = [sb("KT%d" % h, [128, TA], BF16) for h in range(2)]
    VS = [sb("VS%d" % h, [128, TA // 128, 128], BF16) for h in range(2)]
    xt = [sb("xt%d" % i, [128, 1024]) for i in range(2)]
    xs = [sb("xs%d" % i, [128, 1024], BF16) for i in range(2)]
    junk = sb("junk", [128, 1024], BF16)
    ss = sb("ss", [128, 1]); rstd = sb("rstd", [128, 1])
    xnT = sb("xnT", [128, 8, 512], BF16)
    posi = sb("posi", [128, 512], I32)
    tmp = [sb("t%d" % i, [128, 512]) for i in range(8)]
    Eb = [sb("Eb%d" % i, [128, 512], BF16) for i in range(4)]
    KX = [sb("KX%d" % i, [128, 128], BF16) for i in range(6)]
    PSB = [ps("PSB%d" % i, [128, 512]) for i in range(4)]
    PVA = [ps("PVA%d" % i, [128, 512]) for i in range(2)]
    PSM = [ps("PSM%d" % i, [128, 512]) for i in range(2)]
    PSX = PSM[0]
    PTB = PSB[3].bitcast(BF16).rearrange("p (a b) -> p a b", a=8)
    for s in range(nblk):
        for j in range(4):
            tix = (s * 4 + j) % 2
            t0_ = s * 512 + j * 128
            b.load(xt[tix], x(t0_) if callable(x) else x[t0_:t0_ + 128, :])
            rms_tile_T(b, xt[tix], xs[tix], ss, rstd, PTB, (xnT[:, :, j * 128:(j + 1) * 128], ("xnT", j)), identb, junk)
        XN = KL([("xnT", j) for j in range(4)])
        if phase < 0.4:
            continue
        b.load(posi, pos_d[:, s * 512:(s + 1) * 512].partition_broadcast(128))
        ang = tmp[2]
        b.copy(ang, posi)
        b.ts(ang, ang, col(3), None, op0=ALU.mult)
        for which, shift in ((0, 0.0), (1, 1.5707963267948966)):
            a_ = tmp[3]
            kf = tmp[4]
            if shift:
                b.ts(a_, ang, shift, None, op0=ALU.add)
            else:
                a_ = ang
            b.ts(kf, a_, 1.0 / TWO_PI, MAGIC, op0=ALU.mult, op1=ALU.add)
            b.ts(kf, kf, -MAGIC, None, op0=ALU.add)
            r_ = tmp[5]
            b.stt(r_, kf, -CW1, a_, ALU.mult, ALU.add)
            b.stt(r_, kf, -CW2, r_, ALU.mult, ALU.add)
            b.ts(r_, r_, 3.1415925, -3.1415925, op0=ALU.min, op1=ALU.max)
            b.act(tmp[which], r_, AF.Sin)
        SIN, COS = tmp[0], tmp[1]
        if phase < 0.6:
            continue
        for cc in range(4):
            pp = PSB[cc % 2]
            for kc in range(8):
                b.mm(pp, (wb[:, kc, cc * 128:(cc + 1) * 128], WBK(kc)), (xnT[:, kc, :], XN), start=(kc == 0), stop=(kc == 7))
            sq = tmp[2]
            b.act(sq, pp, AF.Square)
            b.mm(PSX, bones, sq)
            b.act(sq, PSX, AF.Sqrt, scale=1.0 / 64, bias=1e-6)
            b.recip(sq, sq)
            qn = tmp[3]
            b.stt(qn, pp, col(0 if cc < 2 else 1), sq, ALU.mult, ALU.mult)
            if phase < 0.7:
                continue
            b.mm(PVA[0], rot, qn)
            t1 = tmp[4]
            b.tt(t1, qn, COS, ALU.mult)
            t2 = tmp[5]
            b.tt(t2, PVA[0], SIN, ALU.mult)
            if phase < 0.75:
                continue
            dst = (QT if cc < 2 else KT)[cc % 2]
            b.stt((dst[:, s * 512:(s + 1) * 512], (dst.tensor.name, s)), t1, 1.0, t2, ALU.mult, ALU.add)
        if phase < 0.8:
            continue
        for j in range(4):
            pp = PSB[j % 2]
            for kc in range(8):
                b.mm(pp[:, 0:256], (xnT[:, kc, j * 128:(j + 1) * 128], XN), (wb[:, kc, 512:768], WBK(kc)), start=(kc == 0), stop=(kc == 7))
            for h in range(2):
                b.copy((VS[h][:, s * 4 + j, :], (VS[h].tensor.name, s)), pp[:, h * 128:(h + 1) * 128], eng="act")
    if phase < 2:
        nb2 = 0
    else:
        nb2 = nblk
    scale = 64 ** -0.5
    for i in range(nb2):
        for h in range(2):
            qs = slice(i * 512, (i + 1) * 512)
            QK = (QT[h].tensor.name, i)
            nkb = 4 * i + 4
            steps = [(kb, c) for kb in range(nkb) for c in range(2)]
            nst = len(steps)

            def emit_s(t):
                kb, c = steps[t]
                kx = KX[t % 6]
                b.ts(kx, (KT[h][:, kb * 128:(kb + 1) * 128], (KT[h].tensor.name, kb // 4)), bones[:, 64 * c:64 * c + 1], None, op0=ALU.mult)
                b.mm(PSB[t % 4], kx, (QT[h][:, qs], QK))

            def emit_e(t):
                kb, c = steps[t]
                b.act(Eb[t % 4], PSB[t % 4], AF.Exp, scale=scale)
                if kb >= 4 * i:
                    b.tt(Eb[t % 4], Eb[t % 4], (cmask[:, kb - 4 * i, :], "cmask_s"), ALU.mult)

            def emit_pv(t):
                kb, c = steps[t]
                b.mm(PVA[c], (VS[h][:, kb, :], (VS[h].tensor.name, kb // 4)), Eb[t % 4], start=(kb == 0), stop=(kb == nkb - 1))
                b.mm(PSM[c], ones_b, Eb[t % 4], start=(kb == 0), stop=(kb == nkb - 1))
            LA = 3
            for t in range(min(LA, nst)):
                emit_s(t)
            for t in range(nst):
                emit_e(t)
                if t + LA < nst:
                    emit_s(t + LA)
                emit_pv(t)
            r0, r1, o_ = tmp[0], tmp[1], tmp[2]
            b.recip(r0, PSM[0])
            b.tt(r0, PVA[0], r0, ALU.mult)
            b.recip(r1, PSM[1])
            b.tt(r1, PVA[1], r1, ALU.mult)
            b.stt(o_, r1, NEGLAM, r0, ALU.mult, ALU.add)
            sq = tmp[3]
            b.act(sq, o_, AF.Square)
            b.mm(PVA[0], ones_f, sq)
            b.act(sq, PVA[0], AF.Sqrt, scale=1.0 / 128, bias=1e-6)
            b.recip(sq, sq)
            if fused is None:
                on = tmp[4 + ((i * 2 + h) % 2)]
                b.stt(on, o_, col(4), sq, ALU.mult, ALU.mult)
                b.store(oT[h * 128:(h + 1) * 128, qs], on)
            else:
                b.stt(onb[h], o_, col(4), sq, ALU.mult, ALU.mult)
        if fused is not None:
            for j in range(4):
                pt_ = p1t[j % 2]
                for n in range(2):
                    for h in range(2):
                        b.mm(PVA[1], onb[h][:, j * 128:(j + 1) * 128], (wop[:, h, n * 512:(n + 1) * 512], ("wop", h)), start=(h == 0), stop=(h == 1))
                    b.copy((pt_[:, n * 512:(n + 1) * 512], (pt_.tensor.name, n)), PVA[1], eng=("act" if n == 0 else "dve"))
                b.store(fused["rs_in"][i * 512 + j * 128:i * 512 + (j + 1) * 128, :], (pt_, KL([(pt_.tensor.name, 0), (pt_.tensor.name, 1)])))
    b.finish(sem_stack=(fused or {}).get("sem_stack"))
    return nc


def _consts_l3():
    p = np.arange(128)
    bones = (p[:, None] // 64 == p[None, :] // 64).astype(np.float32)
    rot = np.zeros((128, 128), np.float32)
    for d in range(128):
        dm = d % 64
        if dm < 8:
            rot[d + 8, d] = -1.0
        elif dm < 16:
            rot[d - 8, d] = 1.0
    invf = np.zeros((128,), np.float32)
    for q in range(128):
        if q % 64 < 16:
            invf[q] = np.float32(500000.0) ** np.float32(-(2 * (q % 8)) / 16.0)
    kp = np.arange(128)[:, None]
    qc = np.arange(512)[None, :]
    cm = np.concatenate([((128 * j + kp) <= qc).astype(np.float32) for j in range(4)], axis=1)
    return dict(ident=np.eye(128, dtype=np.float32), bones=bones, rot=rot, cmask=np.ascontiguousarray(cm)), invf


def l3_inputs(inp, x1, bi, g):
    f = lambda k: np.asarray(inp[k][0], np.float32)
    W = f("o_w_qkv")
    hs = [2 * g, 2 * g + 1]
    cols = np.concatenate([np.arange(h * 128, (h + 1) * 128) for h in hs] + [1024 + np.arange(h * 128, (h + 1) * 128) for h in hs]
                          + [2048 + np.arange(h * 128, (h + 1) * 128) for h in hs])
    consts, invf = _consts_l3()
    cvec = np.zeros((128, 4), np.float32)
    cvec[:, 0] = np.tile(f("o_q_norm"), 2)
    cvec[:, 1] = np.tile(f("o_k_norm"), 2)
    cvec[:, 2] = f("o_subln")
    cvec[:, 3] = invf
    d = dict(x=(None if x1 is None else np.ascontiguousarray(x1[bi])), pos=np.ascontiguousarray(inp["positions"][bi].reshape(1, -1).astype(np.int32)),
             w=np.ascontiguousarray(W[:, cols]), gmix=np.ascontiguousarray(f("o_ln_mix").reshape(8, 128).T), cvec=cvec,
             lams=np.concatenate([f("o_lambda_q1"), f("o_lambda_k1"), f("o_lambda_q2"), f("o_lambda_k2")]).reshape(1, 256))
    d.update(consts)
    return d


N_CORES = 8
CC_GROUPS = [[0, 1, 2, 3], [4, 5, 6, 7]]


_CC_STATE = {}


def _cc_block(nc, kind, src, dst, name, sem_stack):
    st = _CC_STATE.setdefault(id(nc), {})
    if "sem" not in st:
        st["sem"] = sem_stack.enter_context(nc.semaphore("cc_sem"))
        st["n"] = 0
    sem = st["sem"]
    st["n"] += 1
    cnt = st["n"]
    with nc.Block() as block:
        @block.gpsimd
        def _(g):
            g.collective_compute(kind, ALU.bypass if kind == "AllGather" else ALU.add, replica_groups=CC_GROUPS,
                                 ins=[src.ap().opt()], outs=[dst.ap().opt()]).then_inc(sem)
            g.wait_ge(sem, cnt)


AG_CH = 256


def build_fused():
    nc = bass.Bass("TRN2", target_bir_lowering=False)
    NQ = T_SEQ // 4
    nch = NQ // AG_CH
    rs1_in = nc.dram_tensor("rs1_in", [T_SEQ, 1024], F32)
    rs1_out = nc.dram_tensor("rs1_out", [NQ, 1024], F32)
    ag_in = [nc.dram_tensor("ag_in%d" % k, [AG_CH, 1024], F32) for k in range(nch)]
    ag_out = [nc.dram_tensor("ag_out%d" % k, [4 * AG_CH, 1024], F32) for k in range(nch)]
    rs2_in = nc.dram_tensor("rs2_in", [T_SEQ, 1024], F32)
    rs2_out = nc.dram_tensor("rs2_out", [NQ, 1024], F32)

    def q_tile(tok0):
        return ag_in[tok0 // AG_CH].ap()[tok0 % AG_CH:tok0 % AG_CH + 128, :]

    def full_tile(t0):
        r, w = t0 // NQ, t0 % NQ
        k, i = w // AG_CH, w % AG_CH
        return ag_out[k].ap()[r * AG_CH + i:r * AG_CH + i + 128, :]

    with contextlib.ExitStack() as ss:
        build_l1(nc=nc, pre="a__", fused=dict(rs_in=rs1_in.ap(), sem_stack=ss))
        _cc_block(nc, "ReduceScatter", rs1_in, rs1_out, "cc1", ss)
        build_ffn(NT=2048, F=2816, E=1, G=512, moe=False, nc=nc, pre="b__", fused=dict(add=rs1_out.ap(), xres=None, out=q_tile, sem_stack=ss))
        for k in range(nch):
            _cc_block(nc, "AllGather", ag_in[k], ag_out[k], "cc2_%d" % k, ss)
        build_l3(nc=nc, pre="c__", fused=dict(x=full_tile, rs_in=rs2_in.ap(), sem_stack=ss))
        _cc_block(nc, "ReduceScatter", rs2_in, rs2_out, "cc3", ss)
        build_ffn(NT=2048, F=3584, E=8, G=512, moe=True, nc=nc, pre="d__", fused=dict(add=rs2_out.ap(), xres=q_tile, out=None, sem_stack=ss))
    return nc


def fused_inputs(inp, c):
    bi, g = c // 4, c % 4
    f = lambda k: np.asarray(inp[k][0], np.float32)
    d = {}
    l1 = l1_inputs(inp, bi, g)
    wout = f("e_w_out")
    l1["wo_part"] = np.ascontiguousarray(np.concatenate([wout[g * 128:(g + 1) * 128], wout[512 + g * 128:512 + (g + 1) * 128]], axis=0))
    for k, v in l1.items():
        d["a__" + k] = v
    ident = np.eye(128, dtype=np.float32)
    d["b__xres"] = np.ascontiguousarray(np.asarray(inp["x"], np.float32)[bi, g * 2048:(g + 1) * 2048])
    d["b__gain"] = np.ascontiguousarray(f("e_ln_ffn").reshape(1, 1024))
    d["b__wg"] = np.asarray(inp["e_ffn_gate"], np.float32)
    d["b__wu"] = np.asarray(inp["e_ffn_up"], np.float32)
    d["b__wd"] = np.asarray(inp["e_ffn_down"], np.float32)
    d["b__ident"] = ident
    l3 = l3_inputs(inp, None, bi, g)
    del l3["x"]
    l3["wo_part"] = np.ascontiguousarray(f("o_w_o")[g * 256:(g + 1) * 256])
    for k, v in l3.items():
        d["c__" + k] = v
    d["d__gain"] = np.ascontiguousarray(f("o_ln_ffn").reshape(1, 1024))
    d["d__wg"] = np.asarray(inp["o_moe_gate"][0], np.float32)
    d["d__wu"] = np.asarray(inp["o_moe_up"][0], np.float32)
    d["d__wd"] = np.asarray(inp["o_moe_down"][0], np.float32)
    d["d__router"] = f("o_router")
    d["d__ident"] = ident
    return d


def kernel(**inp):
    inp = {k: np.asarray(v) for k, v in inp.items()}
    Bn, T, D = inp["x"].shape
    nc = build_fused()
    res = run_bass_kernel_spmd(nc, [fused_inputs(inp, c) for c in range(N_CORES)], core_ids=list(range(N_CORES)))
    out = np.zeros((Bn, T, D), np.float32)
    for c in range(N_CORES):
        bi, tq = c // 4, c % 4
        out[bi, tq * 2048:(tq + 1) * 2048] = res.results[c]["d__out"]
    return out
```

```python
import contextlib
import numpy as np
import ml_dtypes
import concourse.bass as bass
import concourse.mybir as mybir
from concourse.alu_op_type import AluOpType as ALU
from concourse.bass_utils import run_bass_kernel_spmd

AF = mybir.ActivationFunctionType
F32 = mybir.dt.float32
BF16 = mybir.dt.bfloat16
I32 = mybir.dt.int32
AX = mybir.AxisListType

COMPUTE = ("pe", "act", "dve", "pool")


class _Op:
    __slots__ = ("eng", "fn", "reads", "writes", "kind", "waits", "signal", "val", "dkey", "seq")

    def __init__(self, eng, fn, reads, writes, kind):
        self.eng = eng
        self.fn = fn
        self.reads = tuple(reads)
        self.writes = tuple(writes)
        self.kind = kind
        self.waits = {}
        self.signal = False
        self.val = None
        self.dkey = None


class Prog:
    def __init__(self, nc):
        self.nc = nc
        self.ops = []
        self.last_w = {}
        self.readers = {}
        self.deps = []
        self.group_keys = set()

    def _add(self, op):
        deps = set()
        for k in op.reads:
            w = self.last_w.get(k)
            if w is not None:
                deps.add((w, "raw"))
        for k in op.writes:
            w = self.last_w.get(k)
            if w is not None:
                deps.add((w, "waw"))
            for r in self.readers.get(k, ()):
                if r is not op:
                    deps.add((r, "war"))
        for k in op.reads:
            self.readers.setdefault(k, []).append(op)
        for k in op.writes:
            self.last_w[k] = op
            self.readers[k] = []
        op.seq = len(self.ops)
        self.ops.append(op)
        self.deps.append(deps)
        return op

    def add(self, eng, fn, reads=(), writes=()):
        return self._add(_Op(eng, fn, reads, writes, "c"))

    def dma(self, q, fn, reads=(), writes=(), key=None):
        op = _Op(q, fn, reads, writes, "d")
        op.dkey = key
        return self._add(op)

    def emit(self, final_keys=(), sem_stack=None):
        nc = self.nc
        ops = self.ops
        for op, deps in zip(ops, self.deps):
            for d, kind in deps:
                if d.kind == "c":
                    if d.eng == op.eng and op.kind == "c":
                        if op.eng == "pe" or kind == "war":
                            continue
                    d.signal = True
        finals = [self.last_w[k] for k in final_keys if k in self.last_w]
        for d in finals:
            if d.kind == "c":
                d.signal = True
        cnt = {e: 0 for e in COMPUTE}
        dcnt = {}
        dkeys = []
        for op in ops:
            if op.kind == "c":
                if op.signal:
                    cnt[op.eng] += 1
                    op.val = cnt[op.eng]
            else:
                if op.dkey not in dcnt:
                    dcnt[op.dkey] = 0
                    dkeys.append(op.dkey)
                dcnt[op.dkey] += 16
                op.val = dcnt[op.dkey]
        for op in ops:
            if op.kind == "d" and op.dkey in self.group_keys:
                op.val = dcnt[op.dkey]
        with contextlib.ExitStack() as st_local:
            st = sem_stack if sem_stack is not None else st_local
            tag = "_%d" % len(getattr(st, "_exit_callbacks", ())) if sem_stack is not None else ""
            csem = {e: st.enter_context(nc.semaphore("cs_" + e + tag)) for e in COMPUTE}
            dsem = {k: st.enter_context(nc.semaphore("ds%d%s" % (i, tag))) for i, k in enumerate(dkeys)}

            def semof(d):
                return csem[d.eng] if d.kind == "c" else dsem[d.dkey]

            streams = {}
            waited = {}
            for op, deps in zip(ops, self.deps):
                need = {}
                for d, kind in deps:
                    if d.kind == "c" and d.eng == op.eng and op.kind == "c":
                        if op.eng == "pe" or kind == "war":
                            continue
                    s = semof(d)
                    sid = id(s)
                    if need.get(sid, (None, 0))[1] < d.val:
                        need[sid] = (s, d.val)
                w = waited.setdefault(op.eng, {})
                op.waits = []
                for sid, (s, v) in need.items():
                    if w.get(sid, 0) < v:
                        w[sid] = v
                        op.waits.append((s, v))
                streams.setdefault(op.eng, []).append(op)
            fin_waits = []
            for d in finals:
                fin_waits.append((semof(d), d.val))

            def run_stream(name, engine, extra=None):
                for op in streams.get(name, []):
                    for s, v in op.waits:
                        engine.wait_ge(s, v)
                    ins = op.fn(engine)
                    if op.kind == "c":
                        if op.signal:
                            ins.then_inc(csem[op.eng], 1)
                    else:
                        ins.then_inc(dsem[op.dkey], 16)
                if extra:
                    for s, v in extra:
                        engine.wait_ge(s, v)

            with nc.Block() as block:
                @block.tensor
                def _(e):
                    run_stream("pe", e)

                @block.scalar
                def _(e):
                    run_stream("act", e)

                @block.vector
                def _(e):
                    run_stream("dve", e)

                @block.gpsimd
                def _(e):
                    run_stream("pool", e)

                @block.sync
                def _(e):
                    run_stream("sp", e, extra=fin_waits)


class KL(list):
    pass


KEYMAP = {"PS0": KL([("PS0", 0), ("PS0", 1)]), "PS1": KL([("PS1", 0), ("PS1", 1)])}


def _ak(x):
    if isinstance(x, tuple):
        ap, k = x
        return (ap, k if isinstance(k, KL) else KL([k]))
    n = x.tensor.name
    return (x, KEYMAP.get(n.split("__")[-1]) or KL([n]))


class B:
    def __init__(self, nc, pre=""):
        self.nc = nc
        self.pre = pre
        self.P = Prog(nc)
        self.st = contextlib.ExitStack()
        self.nout = 0

    def sb(self, name, shape, dt=F32):
        return self.st.enter_context(self.nc.sbuf_tensor(self.pre + name, shape, dt))[:]

    def ps(self, name, shape, dt=F32):
        return self.st.enter_context(self.nc.psum_tensor(self.pre + name, shape, dt))[:]

    def mm(self, out, lhsT, rhs, start=True, stop=True):
        (o, ok), (l, lk), (r, rk) = _ak(out), _ak(lhsT), _ak(rhs)
        self.P.add("pe", lambda e: e.matmul(o, l, r, start=start, stop=stop), reads=lk + rk, writes=ok)

    def tr(self, out, in_, ident):
        (o, ok), (i, ik), (d, dk) = _ak(out), _ak(in_), _ak(ident)
        self.P.add("pe", lambda e: e.transpose(o, i, d), reads=ik + dk, writes=ok)

    def act(self, out, in_, func, scale=None, bias=None, accum=None, eng="act"):
        (o, ok), (i, ik) = _ak(out), _ak(in_)
        reads = list(ik)
        writes = list(ok)
        kw = {}
        if scale is not None:
            if isinstance(scale, (int, float)):
                kw["scale"] = float(scale)
            else:
                s, sk = _ak(scale)
                kw["scale"] = s
                reads.extend(sk)
        if bias is not None:
            if isinstance(bias, (int, float)):
                kw["bias"] = float(bias)
            else:
                b_, bk = _ak(bias)
                kw["bias"] = b_
                reads.extend(bk)
        if accum is not None:
            a_, ak_ = _ak(accum)
            kw["accum_out"] = a_
            writes.extend(ak_)
        self.P.add("act", lambda e: e.activation(out=o, in_=i, func=func, **kw), reads=reads, writes=writes)

    def tt(self, out, in0, in1, op, eng="dve"):
        (o, ok), (a, ak_), (b_, bk) = _ak(out), _ak(in0), _ak(in1)
        self.P.add(eng, lambda e: e.tensor_tensor(out=o, in0=a, in1=b_, op=op), reads=ak_ + bk, writes=ok)

    def ts(self, out, in0, s1, s2=None, op0=ALU.mult, op1=None, eng="dve", accum=None):
        (o, ok), (a, ak_) = _ak(out), _ak(in0)
        reads = list(ak_)
        writes = list(ok)

        def sc(s):
            if s is None or isinstance(s, (int, float)):
                return s
            ap, k = _ak(s)
            reads.extend(k)
            return ap
        v1, v2 = sc(s1), sc(s2)
        kw = {}
        if op1 is not None:
            kw["op1"] = op1
        if accum is not None:
            a2, a2k = _ak(accum)
            kw["accum_out"] = a2
            writes.extend(a2k)
        self.P.add(eng, lambda e: e.tensor_scalar(out=o, in0=a, scalar1=v1, scalar2=v2, op0=op0, **kw), reads=reads, writes=writes)

    def stt(self, out, in0, scalar, in1, op0, op1):
        (o, ok), (a, ak_), (b_, bk) = _ak(out), _ak(in0), _ak(in1)
        reads = list(ak_ + bk)
        if isinstance(scalar, (int, float)):
            s = float(scalar)
        else:
            s, sk = _ak(scalar)
            reads.extend(sk)
        self.P.add("dve", lambda e: e.scalar_tensor_tensor(out=o, in0=a, scalar=s, in1=b_, op0=op0, op1=op1), reads=reads, writes=ok)

    def copy(self, out, in_, eng="dve"):
        (o, ok), (i, ik) = _ak(out), _ak(in_)
        if eng == "act":
            self.P.add("act", lambda e: e.activation(out=o, in_=i, func=AF.Copy), reads=ik, writes=ok)
        else:
            self.P.add(eng, lambda e: e.tensor_copy(out=o, in_=i), reads=ik, writes=ok)

    def scan(self, out, d0, d1, init):
        (o, ok), (a, ak_), (b_, bk) = _ak(out), _ak(d0), _ak(d1)
        reads = list(ak_ + bk)
        if isinstance(init, (int, float)):
            iv = float(init)
        else:
            iv, ik = _ak(init)
            reads.extend(ik)
        self.P.add("dve", lambda e: e.tensor_tensor_scan(out=o, data0=a, data1=b_, initial=iv, op0=ALU.mult, op1=ALU.add), reads=reads, writes=ok)

    def recip(self, out, in_):
        (o, ok), (i, ik) = _ak(out), _ak(in_)
        self.P.add("dve", lambda e: e.reciprocal(out=o, in_=i), reads=ik, writes=ok)

    def memset(self, out, val, eng="dve"):
        (o, ok) = _ak(out)
        self.P.add(eng, lambda e: e.memset(o, val), writes=ok)

    def load(self, out, in_, q="sp", dkey=None, grp=False):
        (o, ok) = _ak(out)
        if grp:
            self.P.group_keys.add(dkey)
        self.P.dma(q, lambda e: e.dma_start(out=o, in_=in_), writes=ok, key=(dkey if dkey is not None else ok[0]))

    def store(self, out_dram, in_, q="sp"):
        (i, ik) = _ak(in_)
        self.nout += 1
        k = ("__out", self.nout)
        self.P.dma(q, lambda e: e.dma_start(out=out_dram, in_=i), reads=ik, writes=[k], key=ik[0])

    def finish(self, sem_stack=None):
        self.P.emit(final_keys=[("__out", i + 1) for i in range(self.nout)], sem_stack=sem_stack)
        self.st.close()


def rms_tile_T(b, xt, xs, ss, rstd, PT, xnT_dst, identb, junk, eps=1e-6, D=1024):
    b.act(junk, xt, AF.Square, accum=ss)
    b.act(rstd, ss, AF.Sqrt, scale=1.0 / D, bias=eps)
    b.recip(rstd, rstd)
    b.ts(xs, xt, rstd, None, op0=ALU.mult)
    n = D // 128
    for kc in range(n):
        b.tr((PT[:, kc, :], (PT.tensor.name, kc)), xs[:, kc * 128:(kc + 1) * 128], identb)
    b.copy(xnT_dst, (PT[:, :, :], KL([(PT.tensor.name, kc) for kc in range(n)])), eng="act")


T_SEQ = 8192
SEG = 512
CH = 64
C0 = 0.6065306597126334
NV1 = 20


def build_l1(nseg=T_SEQ // SEG, phase=99, nc=None, pre="", fused=None):
    if nc is None:
        nc = bass.Bass("TRN2", target_bir_lowering=False)
    dr = lambda n, s, dt=F32, kind="ExternalInput": nc.dram_tensor(pre + n, s, dt, kind=kind).ap()
    x = dr("x", [T_SEQ, 1024])
    w_in = dr("w_in", [1024, 896])
    gmix = dr("gmix", [128, 8])
    cvec = dr("cvec", [128, NV1])
    wa_d = dr("wa", [128, 128])
    wx_d = dr("wx", [128, 128])
    w2a2_d = dr("w2a2", [128, 128])
    g2_d = dr("g2", [128, 128])
    ident_d = dr("ident", [128, 128])
    bones_d = dr("bones", [128, 128])
    maska_d = dr("maska", [128, 512])
    maskb_d = dr("maskb", [128, 256])
    rmask_d = dr("rmask", [128, 512])
    id2_d = dr("id2", [128, 128])
    if fused is None:
        yT = dr("yT", [256, T_SEQ], kind="ExternalOutput")
    else:
        wop_d = dr("wo_part", [256, 1024])

    b = B(nc, pre)
    sb, ps = b.sb, b.ps
    wb = sb("wb", [128, 8, 896], BF16)
    if fused is not None:
        wop = sb("wop", [128, 2, 1024], BF16)
        ybf = sb("ybf", [128, 2, SEG], BF16)
        p1t = [sb("p1t%d" % i, [128, 1024]) for i in range(2)]
        for kc in range(2):
            b.load((wop[:, kc, :], ("wop", kc)), wop_d[kc * 128:(kc + 1) * 128, :], q="pool", dkey="constp", grp=True)
    wst = [sb("wst%d" % i, [128, 896]) for i in range(2)]
    gm = sb("gm", [128, 8])
    cv = sb("cv", [128, NV1 + 4])
    wa = sb("wa_s", [128, 128]); wx = sb("wx_s", [128, 128])
    w2a2 = sb("w2a2_s", [128, 128]); g2 = sb("g2_s", [128, 128])
    ident = sb("ident_s", [128, 128]); identb = sb("identb", [128, 128], BF16)
    bones = sb("bones_s", [128, 128]); rkbd = sb("rkbd", [128, 128])
    id2 = sb("id2_s", [128, 2, 64])
    maska = sb("maska_s", [128, 512]); maskb = sb("maskb_s", [128, 256]); rmask = sb("rmask_s", [128, 512])
    for t_, d_ in ((gm, gmix), (cv[:, 0:NV1], cvec), (wa, wa_d), (wx, wx_d), (w2a2, w2a2_d), (g2, g2_d), (ident, ident_d),
                   (bones, bones_d), ((id2[:, :, :].rearrange("p h s -> p (h s)"), "id2_s"), id2_d), (maska, maska_d), (maskb, maskb_d), (rmask, rmask_d)):
        b.load(t_, d_, dkey="const", grp=True)
    b.load(identb, ident_d, q="pool", dkey="constp", grp=True)
    for kc in range(8):
        b.load(wst[kc % 2], w_in[kc * 128:(kc + 1) * 128, :])
        b.ts((wb[:, kc, :], ("wb", kc)), wst[kc % 2], gm[:, kc:kc + 1], None, op0=ALU.mult)
    WBK = lambda kc: ("wb", kc)
    col = lambda i: cv[:, i:i + 1]
    CCH, OMKA, TWOC = NV1, NV1 + 1, NV1 + 2
    b.act(col(CCH), col(7), AF.Exp, scale=-1.0)
    b.act(col(CCH), col(CCH), AF.Ln, bias=1.0)
    b.ts(col(TWOC), col(CCH), -16.0, None, op0=ALU.mult)
    b.ts(col(CCH), col(CCH), -8.0, None, op0=ALU.mult)
    b.ts(col(OMKA), col(16), -1.0, 1.0, op0=ALU.mult, op1=ALU.add)
    b.ts(rkbd, bones, col(17), None, op0=ALU.mult)

    xt = [sb("xt%d" % i, [128, 1024]) for i in range(2)]
    xs = [sb("xs%d" % i, [128, 1024], BF16) for i in range(2)]
    junk = sb("junk", [128, 1024], BF16)
    ss = sb("ss", [128, 1]); rstd = sb("rstd", [128, 1])
    xnT = sb("xnT", [128, 8, SEG], BF16)
    pj = [[sb("pj%d_%d" % (i, c), [128, 4 + SEG]) for c in range(7)] for i in range(2)]
    NT = 27
    tmp = [sb("t%d" % i, [128, SEG]) for i in range(NT)]
    hh = [sb("hh%d" % i, [128, SEG]) for i in range(2)]
    TM = sb("TM", [64, 2, 4, 128])
    SA = sb("SA", [64, 2, 4, 2, 64]); SBm = sb("SBm", [64, 256])
    PQ = [sb("PQ%d" % i, [64, 2, 2, 2, 64]) for i in range(2)]
    Tb = [sb("Tb%d" % i, [64, 2, 2, 64]) for i in range(2)]
    ZS = sb("ZS", [64, 2, 2, 64]); MS = sb("MS", [64, 2, 2, 2, 64])
    GP = sb("GP", [64, 2, 2, 2, 64]); GS = GP[:, 0, :, :, :]; PSm = GP[:, 1, :, :, :]
    STT = [sb("STT%d" % i, [64, 2, 64]) for i in range(2)]
    AFx = sb("AFx", [128, SEG // CH, 2, CH]); RFx = sb("RFx", [128, SEG // CH, 2, CH]); BTx = sb("BTx", [128, SEG // CH, 2, CH])
    RFh = sb("RFh", [64, 2, SEG]); WCh = sb("WCh", [64, 2, SEG // CH])
    OS = sb("OS", [128, SEG])
    PP = ps("PP", [128, 512]); PT = ps("PT", [128, 8, 128], BF16)
    PS0 = ps("PS0", [128, 512]); PS1 = ps("PS1", [128, 512])
    PA = ps("PA", [128, 512]); PC = ps("PC", [128, 512]); PX = ps("PX", [128, 512]); PO = ps("PO", [128, 512])

    for i in range(2):
        for c in range(7):
            b.memset((pj[i][c][:, 0:4], ("pjh", i, c)), 0.0)
    b.memset((STT[0][:, :, :], "STT0"), 0.0)
    for tx in (AFx, RFx, BTx):
        b.memset((tx[:, :, :, :], tx.tensor.name), 0.0, eng="pool")

    st_i = 0
    PS0a_ = ("PS0", 0)
    for s in range(nseg):
        cur = s % 2
        nxt = 1 - cur
        pjc = pj[cur]
        PJ = lambda c: ("pj", cur, c)
        PJH = lambda c: ("pjh", cur, c)
        for j in range(4):
            tix = (s * 4 + j) % 2
            b.load(xt[tix], x[s * SEG + j * 128: s * SEG + (j + 1) * 128, :])
            rms_tile_T(b, xt[tix], xs[tix], ss, rstd, PT, (xnT[:, :, j * 128:(j + 1) * 128], ("xnT", j)), identb, junk)
        XN = KL([("xnT", j) for j in range(4)])
        for cc in range(7):
            for kc in range(8):
                b.mm(PP, (wb[:, kc, cc * 128:(cc + 1) * 128], WBK(kc)), (xnT[:, kc, :], XN), start=(kc == 0), stop=(kc == 7))
            b.copy((pjc[cc][:, 4:4 + SEG], PJ(cc)), PP, eng=("act" if cc % 2 == 0 else "dve"))
            b.copy((pj[nxt][cc][:, 0:4], ("pjh", nxt, cc)), (pjc[cc][:, SEG:SEG + 4], PJ(cc)), eng="pool")
        if phase < 2:
            continue
        cur_v = lambda c: (pjc[c][:, 4:4 + SEG], PJ(c))
        sh_v = lambda c, k: (pjc[c][:, 4 - k:4 - k + SEG], KL([PJ(c), PJH(c)]))
        t = tmp
        xc = t[0]
        b.ts(xc, sh_v(0, 3), col(0), col(4), op0=ALU.mult, op1=ALU.add)
        b.stt(xc, sh_v(0, 2), col(1), xc, ALU.mult, ALU.add)
        b.stt(xc, sh_v(0, 1), col(2), xc, ALU.mult, ALU.add)
        b.stt(xc, cur_v(0), col(3), xc, ALU.mult, ALU.add)
        b.mm(PS0, wa, xc)
        b.mm(PS1, wx, xc)
        ra = t[1]
        b.act(ra, PS0, AF.Sigmoid, bias=col(5))
        av_ = t[2]
        b.act(av_, ra, AF.Exp, scale=col(CCH))
        a2_ = t[3]
        b.act(a2_, ra, AF.Exp, scale=col(TWOC))
        b.act(a2_, a2_, AF.Sqrt, scale=-1.0, bias=1.0)
        ix = t[1]
        b.act(ix, PS1, AF.Sigmoid, bias=col(6))
        b.tt(a2_, a2_, ix, ALU.mult)
        b.tt(a2_, a2_, xc, ALU.mult)
        hcur = hh[cur]
        b.scan(hcur, av_, a2_, 0.0 if s == 0 else hh[nxt][:, SEG - 1:SEG])
        gq = t[0]
        b.act(gq, cur_v(1), AF.Square)
        b.ts(gq, gq, 0.044715, 1.0, op0=ALU.mult, op1=ALU.add)
        b.tt(gq, gq, cur_v(1), ALU.mult)
        b.act(gq, gq, AF.Sigmoid, scale=1.5957691216057308)
        b.tt(gq, gq, cur_v(1), ALU.mult)
        ylru = t[1]
        if fused is None:
            b.tt(ylru, gq, hcur, ALU.mult)
            b.store(yT[0:128, s * SEG:(s + 1) * SEG], ylru)
        else:
            b.tt((ybf[:, 0, :], ("ybf", 0)), gq, hcur, ALU.mult)
        if phase < 3:
            continue
        shf = []
        for i, c in enumerate((2, 3, 4, 5, 6)):
            d = t[4 + i]
            b.tt(d, sh_v(c, 1), cur_v(c), ALU.subtract)
            b.stt(d, d, col(8 + i), cur_v(c), ALU.mult, ALU.add)
            shf.append(d)
        rs, ks, vs, xwa, xgs = shf
        tw = t[9]
        b.act(tw[0:64, :], xwa[0:64, :], AF.Tanh)
        b.mm(PS0, w2a2[0:64, :], tw[0:64, :])
        b.mm(PS1, w2a2[64:128, :], xwa[64:128, :])
        sgz = t[9]
        b.act(sgz, PS0, AF.Sigmoid, bias=col(13))
        avv = t[10]
        b.act(avv, PS1, AF.Sigmoid, bias=col(14))
        sg = t[11]
        b.act(sg, xgs, AF.Sigmoid)
        cs = t[12]
        b.scan(cs, rmask, sgz, 0.0)
        csm1 = t[13]
        b.tt(csm1, cs, sgz, ALU.subtract)
        Wt = t[14]; iW = t[15]; Wm1 = t[13]
        b.act(Wt, cs, AF.Exp, scale=-C0)
        b.act(iW, cs, AF.Exp, scale=C0)
        b.act(Wm1, csm1, AF.Exp, scale=-C0)
        b.mm(PS0, g2, sg)
        gv = t[11]
        b.copy(gv, PS0, eng="act")
        kq = t[9]
        b.ts(kq, ks, col(15), None, op0=ALU.mult)
        kq2 = t[12]
        b.act(kq2, kq, AF.Square)
        b.mm(PS1, bones, kq2)
        rn = t[12]
        b.act(rn, PS1, AF.Sqrt)
        b.ts(rn, rn, 1e-12, None, op0=ALU.max)
        b.recip(rn, rn)
        kkn = t[9]
        b.tt(kkn, kq, rn, ALU.mult)
        kmod = t[12]
        b.ts(kmod, avv, col(16), col(OMKA), op0=ALU.mult, op1=ALU.add)
        b.tt(kmod, kmod, ks, ALU.mult)
        bb = t[10]
        b.tt(bb, kkn, avv, ALU.mult)
        AFm = t[16]; RF = t[17]; BT = t[18]; KT = t[19]; Bh = t[20]; Kh = t[21]
        b.stt(AFm, kkn, -1.0, Wm1, ALU.mult, ALU.mult)
        b.tt(RF, rs, Wt, ALU.mult)
        b.tt(BT, bb, iW, ALU.mult)
        b.tt(KT, kmod, iW, ALU.mult)
        v3 = lambda tl: tl[:, :].rearrange("p (c s) -> p c s", s=CH)
        wcb = (v3(Wt)[:, :, CH - 1:CH].broadcast_to([128, SEG // CH, CH]), Wt.tensor.name)
        b.tt((v3(Bh), Bh.tensor.name), (v3(BT), BT.tensor.name), wcb, ALU.mult)
        b.tt((v3(Kh), Kh.tensor.name), (v3(KT), KT.tensor.name), wcb, ALU.mult)
        rk_ = t[9]
        b.tt(rk_, rs, kmod, ALU.mult)
        b.mm(PS1, rkbd, rk_)
        bonus = t[22]
        b.tt(bonus, PS1, vs, ALU.mult)
        if phase < 4:
            continue
        for h in range(2):
            b.mm((PS1[0:64, :], KEYMAP["PS1"]), ident[:, 64 * h:64 * h + 64], RF)
            b.copy((RFh[:, h, :], "RFh"), (PS1[0:64, :], KEYMAP["PS1"]), eng="act")
            b.mm((PS0[0:64, h * 8:h * 8 + 8], PS0a_), ident[:, 64 * h:64 * h + 64], (v3(Wt)[:, :, CH - 1], Wt.tensor.name))
        b.copy((WCh[:, :, :].rearrange("p h c -> p (h c)"), "WCh"), (PS0[0:64, 0:16], PS0a_), eng="act")
        for tl, tx in ((AFm, AFx), (RF, RFx), (BT, BTx)):
            b.copy((tx[0:64, :, 0, :], tx.tensor.name), (v3(tl)[0:64], tl.tensor.name), eng="pool")
            b.copy((tx[64:128, :, 1, :], tx.tensor.name), (v3(tl)[64:128], tl.tensor.name), eng="pool")
        PS0a, PS0b, PS1a, PS1b = ("PS0", 0), ("PS0", 1), ("PS1", 0), ("PS1", 1)
        sel = [ident[:, 0:64], ident[:, 64:128]]
        for cp in range(SEG // 128):
            tok = slice(cp * 128, (cp + 1) * 128)
            for q in range(2):
                c_ = cp * 2 + q
                ck = slice(c_ * CH, (c_ + 1) * CH)
                for qi, src in enumerate((AFm, Bh, Kh, vs)):
                    b.tr((PX[0:64, qi * 128:(qi + 1) * 128], "PX"), src[:, ck], ident)
                b.copy((TM[:, q, :, :].rearrange("p a b -> p (a b)"), ("TM", q)), (PX[0:64, :], "PX"), eng="act")
                for j, (l_, rx) in enumerate(((BT, AFx), (BT, RFx), (KT, AFx), (KT, RFx))):
                    b.mm((PA[0:64, j * 128:(j + 1) * 128], "PA"), l_[:, ck], rx[:, c_, :, :].rearrange("p h s -> p (h s)"))
                b.mm((PS1[0:64, q * 128:(q + 1) * 128], PS1a), AFm[:, ck], BTx[:, c_, :, :].rearrange("p h s -> p (h s)"))
                b.tt((SA[:, q, :, :, :].rearrange("p j h s -> p (j h s)"), ("SA", q)), (PA[0:64, :], "PA"), maska[0:64, :], ALU.mult)
            b.tt(SBm, (PS1[0:64, 0:256], PS1a), maskb[0:64, :], ALU.mult)
            TMv = lambda qi, q, h: (TM[:, q, qi, 64 * h:64 * h + 64], ("TM", q))
            SAv = lambda j, q, h: (SA[:, q, j, h, :], ("SA", q))
            SAK = KL([("SA", 0), ("SA", 1)])
            QH = [(q, h) for q in range(2) for h in range(2)]
            for q, h in QH:
                b.mm((PS1[0:64, 256 + (q * 2 + h) * 64:256 + (q * 2 + h) * 64 + 64], PS1b), SAv(2, q, h), TMv(3, q, h))
            b.copy((ZS[:, :, :, :].rearrange("p q h s -> p (q h s)"), "ZS"), (PS1[0:64, 256:512], PS1b), eng="act")
            if phase < 5:
                continue
            b.copy((PQ[0][:, 0, :, :, :], "PQ0"), (SA[:, :, 0, :, :], SAK), eng="pool")
            b.copy((PQ[0][:, 1, :, :, :].rearrange("p q h s -> p (q h s)"), "PQ0"), SBm, eng="pool")
            idb = (ident[0:64, 0:64].rearrange("p (a c s) -> p a c s", a=1, c=1).broadcast_to([64, 2, 2, 64]), ident.tensor.name)
            b.tt((Tb[0][:, :, :, :], "Tb0"), (SA[:, :, 0, :, :], SAK), idb, ALU.add)
            PC5 = PC[0:64, :].rearrange("p (j q h s) -> p j q h s", j=2, q=2, h=2)
            POt = PO[0:64, 256:512].rearrange("p (q h s) -> p q h s", q=2, h=2)
            for k in range(0, 6):
                i = k % 2
                pqk = "PQ%d" % i
                if k >= 1:
                    for q, h in QH:
                        b.mm((POt[:, q, h, :], ("PO", 2)), (PQ[i][:, 1, q, h, :], pqk), (Tb[1 - i][:, q, h, :], "Tb%d" % (1 - i)))
                if k <= 3:
                    for q, h in QH:
                        b.mm((PC5[:, 0, q, h, :], ("PC", 0)), (PQ[i][:, 1, q, h, :], pqk), (PQ[i][:, 0, q, h, :], pqk))
                if k <= 4:
                    for q, h in QH:
                        b.mm((PC5[:, 1, q, h, :], ("PC", 1)), (PQ[i][:, 0, q, h, :], pqk), (PQ[i][:, 1, q, h, :], pqk))
                if k <= 3:
                    b.copy((PQ[1 - i][:, :, :, :, :].rearrange("p j q h s -> p (j q h s)"), "PQ%d" % (1 - i)),
                           (PC[0:64, :], KL([("PC", 0), ("PC", 1)])), eng="act")
                elif k == 4:
                    b.copy((PQ[1 - i][:, 1, :, :, :].rearrange("p q h s -> p (q h s)"), "PQ%d" % (1 - i)),
                           (PC[0:64, 256:512], ("PC", 1)), eng="act")
                if k >= 1:
                    b.tt((Tb[i][:, :, :, :].rearrange("p q h s -> p (q h s)"), "Tb%d" % i),
                         (Tb[1 - i][:, :, :, :].rearrange("p q h s -> p (q h s)"), "Tb%d" % (1 - i)),
                         (PO[0:64, 256:512], ("PO", 2)), ALU.add)
            if phase < 5.2:
                continue
            for q, h in QH:
                o0 = ((q * 2 + h) * 2) * 64
                b.mm((PX[0:64, o0:o0 + 64], "PX"), (Tb[1][:, q, h, :], "Tb1"), TMv(0, q, h))
                b.mm((PX[0:64, o0 + 64:o0 + 128], "PX"), (Tb[1][:, q, h, :], "Tb1"), (ZS[:, q, h, :], "ZS"))
            b.copy((MS[:, :, :, :, :].rearrange("p q h m s -> p (q h m s)"), "MS"), (PX[0:64, :], "PX"), eng="act")
            M1T = lambda q, h: (MS[:, q, h, 0, :], "MS")
            M2T = lambda q, h: (MS[:, q, h, 1, :], "MS")
            if phase < 5.4:
                continue
            for q, h in QH:
                o0 = (q * 2 + h) * 64
                b.mm((PS0[0:64, o0:o0 + 64], PS0a), M1T(q, h), TMv(1, q, h))
                b.mm((PS0[0:64, 256 + o0:256 + o0 + 64], PS0b), M1T(q, h), SAv(1, q, h))
            for q, h in QH:
                c_ = cp * 2 + q
                o0 = (q * 2 + h) * 64
                b.stt((GS[:, q, h, :], "GS"), ident[0:64, 0:64], (WCh[:, h, c_:c_ + 1], "WCh"), (PS0[0:64, o0:o0 + 64], PS0a), ALU.mult, ALU.add)
            b.tt((PSm[:, :, :, :], "PSm"), (PS0[0:64, 256:512].rearrange("p (q h s) -> p q h s", q=2, h=2), PS0b),
                 (RFh[:, :, tok].rearrange("p h (q s) -> p q h s", q=2), "RFh"), ALU.add)
            if phase < 5.6:
                continue
            for q in range(2):
                ocol = slice(q * 64, q * 64 + 64)
                stn = "STT%d" % st_i
                for h in range(2):
                    if 5.8 <= phase < 5.9:
                        continue
                    pr = slice(64 * h, 64 * h + 64)
                    ob = (PO[pr, ocol], ("PO", 0)) if h == 0 else (PX[pr, ocol], "PX")
                    b.mm(ob, M2T(q, h), SAv(1, q, h), start=True, stop=False)
                    b.mm(ob, TMv(3, q, h), SAv(3, q, h), start=False, stop=False)
                    b.mm(ob, (STT[st_i][:, h, :], stn), (PSm[:, q, h, :], "PSm"), start=False, stop=True)
                for h in range(2):
                    if phase == 5.7:
                        continue
                    so = slice(h * 64, h * 64 + 64)
                    b.mm((PS1[0:64, so], PS1a), TMv(1, q, h), M2T(q, h), start=True, stop=False)
                    b.mm((PS1[0:64, so], PS1a), TMv(2, q, h), TMv(3, q, h), start=False, stop=False)
                    b.mm((PS1[0:64, so], PS1a), (GS[:, q, h, :], "GS"), (STT[st_i][:, h, :], stn), start=False, stop=True)
                b.copy((STT[1 - st_i][:, :, :].rearrange("p h s -> p (h s)"), "STT%d" % (1 - st_i)), (PS1[0:64, 0:128], PS1a), eng="dve")
                st_i = 1 - st_i
            b.copy((OS[0:64, tok], ("OS", cp)), (PO[0:64, 0:128], ("PO", 0)), eng="dve")
            b.copy((OS[64:128, tok], ("OS", cp)), (PX[64:128, 0:128], "PX"), eng="dve")
        if phase < 6:
            continue
        OSK = KL([("OS", i) for i in range(4)])
        b.mm(PS0, bones, (OS[:, :], OSK))
        cen = t[23]
        b.stt(cen, PS0, -1.0 / 64, (OS[:, :], OSK), ALU.mult, ALU.add)
        sq = t[24]
        b.act(sq, cen, AF.Square)
        b.mm(PS1, bones, sq)
        b.act(sq, PS1, AF.Sqrt, scale=1.0 / 64, bias=64e-5)
        b.recip(sq, sq)
        b.tt(cen, cen, sq, ALU.mult)
        b.ts(cen, cen, col(18), col(19), op0=ALU.mult, op1=ALU.add)
        b.tt(cen, cen, bonus, ALU.add)
        if fused is None:
            yrw = t[25 + (s % 2)]
            b.tt(yrw, cen, gv, ALU.mult)
            b.store(yT[128:256, s * SEG:(s + 1) * SEG], yrw)
        else:
            b.tt((ybf[:, 1, :], ("ybf", 1)), cen, gv, ALU.mult)
            for j in range(4):
                pt_ = p1t[j % 2]
                for n in range(2):
                    for kc in range(2):
                        b.mm(PP, (ybf[:, kc, j * 128:(j + 1) * 128], ("ybf", kc)), (wop[:, kc, n * 512:(n + 1) * 512], ("wop", kc)),
                             start=(kc == 0), stop=(kc == 1))
                    b.copy((pt_[:, n * 512:(n + 1) * 512], (pt_.tensor.name, n)), PP, eng=("act" if n == 0 else "dve"))
                b.store(fused["rs_in"][s * SEG + j * 128:s * SEG + (j + 1) * 128, :], (pt_, KL([(pt_.tensor.name, 0), (pt_.tensor.name, 1)])))
    b.finish(sem_stack=(fused or {}).get("sem_stack"))
    return nc


def _consts_l1():
    p = np.arange(128)
    ident = np.eye(128, dtype=np.float32)
    bones = (p[:, None] // 64 == p[None, :] // 64).astype(np.float32)
    s_ = (p % 64)[:, None]
    t_ = np.arange(64)[None, :]
    lt = (s_ < t_).astype(np.float32)
    le = (s_ <= t_).astype(np.float32)
    gt = (s_ > t_).astype(np.float32)
    eq = (s_ == t_).astype(np.float32)
    maska = np.concatenate([lt, lt, le, le, lt, lt, le, le], axis=1)
    maskb = np.concatenate([gt, gt, gt, gt], axis=1)
    id2 = np.concatenate([eq, eq], axis=1)
    rmask = np.ones((128, 512), np.float32)
    rmask[:, ::64] = 0.0
    return dict(ident=ident, bones=bones, maska=np.ascontiguousarray(maska), maskb=np.ascontiguousarray(maskb),
                id2=np.ascontiguousarray(id2), rmask=rmask)


def _blockdiag2(w2):
    o = np.zeros((128, 128), np.float32)
    o[0:64, 0:64] = w2[0]
    o[64:128, 64:128] = w2[1]
    return o


def l1_inputs(inp, bi, g):
    f = lambda k: np.asarray(inp[k][0], np.float32)
    ls = slice(g * 128, (g + 1) * 128)
    W = f("e_w_in")
    rw0 = 1024
    cols = np.concatenate([np.arange(512)[ls], 512 + np.arange(512)[ls], rw0 + np.arange(512)[ls], rw0 + 512 + np.arange(512)[ls],
                           rw0 + 1024 + np.arange(512)[ls], rw0 + 1536 + np.arange(128), rw0 + 1664 + np.arange(128)])
    mu = f("e_shift_mu")
    cvec = np.zeros((128, NV1), np.float32)
    cvec[:, 0:4] = f("e_conv_w")[:, ls].T
    cvec[:, 4] = f("e_conv_b")[ls]
    cvec[:, 5] = f("e_gate_a_b")[ls]
    cvec[:, 6] = f("e_gate_x_b")[ls]
    cvec[:, 7] = f("e_lru_lambda")[ls]
    cvec[:, 8] = mu[0:512][ls]
    cvec[:, 9] = mu[512:1024][ls]
    cvec[:, 10] = mu[1024:1536][ls]
    cvec[:, 11] = mu[1536:1664]
    cvec[:, 12] = mu[1664:1792]
    cvec[:, 13] = f("e_w0")[ls]
    cvec[:, 14] = f("e_a0")[ls]
    cvec[:, 15] = f("e_k_k")[ls]
    cvec[:, 16] = f("e_k_a")[ls]
    cvec[:, 17] = f("e_r_k").reshape(-1)[ls]
    cvec[:, 18] = f("e_gn_w")[ls]
    cvec[:, 19] = f("e_gn_b")[ls]
    d = dict(
        x=np.ascontiguousarray(inp["x"][bi]),
        w_in=np.ascontiguousarray(W[:, cols]),
        gmix=np.ascontiguousarray(f("e_ln_mix").reshape(8, 128).T),
        cvec=cvec,
        wa=_blockdiag2(f("e_gate_a_w")[2 * g:2 * g + 2]),
        wx=_blockdiag2(f("e_gate_x_w")[2 * g:2 * g + 2]),
        w2a2=np.ascontiguousarray(np.concatenate([f("e_w2")[:, ls], f("e_a2")[:, ls]], axis=0)),
        g2=np.ascontiguousarray(f("e_g2")[:, ls]),
    )
    d.update(_consts_l1())
    return d


def build_ffn(NT=2048, F=2816, E=1, G=512, moe=False, ngroups=None, phase=99, nc=None, pre="", fused=None):
    if nc is None:
        nc = bass.Bass("TRN2", target_bir_lowering=False)
    dr = lambda n, s, dt=F32, kind="ExternalInput": nc.dram_tensor(pre + n, s, dt, kind=kind).ap()
    nF = F // 128
    TG = G // 128
    if fused is None or fused.get("xres") is None:
        xres = dr("xres", [NT, 1024])
    else:
        xres = fused["xres"]
    if fused is None:
        aT_d = dr("aT", [1024, NT])
        wproj = dr("wproj", [1024, 1024])
    gain_d = dr("gain", [1, 1024])
    wg_d = dr("wg", [E, 1024, F])
    wu_d = dr("wu", [E, 1024, F])
    wd_d = dr("wd", [E, F, 1024])
    ident_d = dr("ident", [128, 128])
    if moe:
        router_d = dr("router", [1024, 8])
    if fused is None or fused.get("out") is None:
        out = dr("out", [NT, 1024], kind="ExternalOutput")
    else:
        out = fused["out"]

    b = B(nc, pre)
    sb, ps = b.sb, b.ps
    if fused is None:
        wo = sb("wo", [128, 8, 1024], BF16)
    gbc = sb("gbc", [128, 1024])
    ident = sb("ident_s", [128, 128])
    identb = sb("identb", [128, 128], BF16)
    b.load(gbc, gain_d.partition_broadcast(128), dkey="const", grp=True)
    b.load(ident, ident_d, dkey="const", grp=True)
    b.load(identb, ident_d, q="pool", dkey="constp", grp=True)
    if fused is None:
        wpv = wproj.rearrange("(kc p) n -> p kc n", p=128)
        for kc in range(8):
            b.load((wo[:, kc, :], ("wo", kc)), wpv[:, kc, :], q="pool")
    WOK = lambda kc: ("wo", kc)
    if moe:
        rt = sb("rt", [128, 8, 8])
        b.load(rt, router_d.rearrange("(kc p) e -> p kc e", p=128), dkey="const", grp=True)
        hT32 = sb("hT32", [128, 8, 128])
        lg = sb("lg", [128, 8]); lg2 = sb("lg2", [128, 8]); mk1 = sb("mk1", [128, 8]); mk2 = sb("mk2", [128, 8])
        sm = sb("sm", [128, 8])
        comb = sb("comb", [128, TG, 8])
        xs32 = sb("xs32", [128, 1024])
        PTa = ps("PTa", [128, 4, 128]); PTb = ps("PTb", [128, 4, 128])
    else:
        xs = sb("xs", [128, 1024], BF16)
        PT = ps("PT", [128, 8, 128], BF16)
    NXB = 2 if G <= 512 else 1
    xt = [sb("xt%d" % i, [128, 1024]) for i in range(NXB)]
    if fused is None:
        at = [sb("at%d" % i, [128, 8, 128], BF16) for i in range(NXB)]
    else:
        at = [sb("at%d" % i, [128, 1024]) for i in range(NXB)]
    acc = [sb("acc%d" % i, [128, 1024]) for i in range(TG)]
    junk = sb("junk", [128, 1024], BF16)
    ss = sb("ss", [128, 1]); rstd = sb("rstd", [128, 1])
    hT = sb("hT", [128, 8, G], BF16)
    hid = sb("hid", [128, nF, G], BF16)
    NWB = 3
    wgb = [sb("wgb%d" % i, [128, 8, 128], BF16) for i in range(NWB)]
    wub = [sb("wub%d" % i, [128, 8, 128], BF16) for i in range(NWB)]
    NS = 2 if G <= 512 else 4
    WN = 1024 // NS
    NH = G // 512
    wdh = [sb("wdh%d" % i, [128, nF, WN], BF16) for i in range(2)]
    sg = [sb("sg%d" % i, [128, 512]) for i in range(2)]
    PP = [ps("PP%d" % i, [128, 512]) for i in range(2)]
    PG = [ps("PG%d" % i, [128, 512]) for i in range(2)]
    PU = [ps("PU%d" % i, [128, 512]) for i in range(2)]
    if fused is None:
        aTv = aT_d.rearrange("(kc p) t -> p kc t", p=128)
    wi = 0
    di = 0
    for g in range(ngroups if ngroups is not None else NT // G):
        for j in range(TG):
            tok0 = g * G + j * 128
            x_ = xt[j % NXB]
            a_ = at[j % NXB]
            b.load(x_, xres(tok0) if callable(xres) else xres[tok0:tok0 + 128, :])
            if fused is None:
                b.load(a_, aTv[:, :, tok0:tok0 + 128], q="pool")
                for n in range(2):
                    for kc in range(8):
                        b.mm(PP[n], a_[:, kc, :], (wo[:, kc, n * 512:(n + 1) * 512], WOK(kc)), start=(kc == 0), stop=(kc == 7))
                    b.tt((acc[j][:, n * 512:(n + 1) * 512], ("acc", j, n)), PP[n], x_[:, n * 512:(n + 1) * 512], ALU.add)
            else:
                b.load(a_, fused["add"][tok0:tok0 + 128, :])
                for n in range(2):
                    b.tt((acc[j][:, n * 512:(n + 1) * 512], ("acc", j, n)), a_[:, n * 512:(n + 1) * 512], x_[:, n * 512:(n + 1) * 512], ALU.add)
            ACCK = KL([("acc", j, 0), ("acc", j, 1)])
            b.act(junk, (acc[j], ACCK), AF.Square, accum=ss)
            b.act(rstd, ss, AF.Sqrt, scale=1.0 / 1024, bias=1e-6)
            b.recip(rstd, rstd)
            if not moe:
                b.stt(xs, (acc[j], ACCK), rstd, gbc, ALU.mult, ALU.mult)
                for kc in range(8):
                    b.tr((PT[:, kc, :], ("PT", kc)), xs[:, kc * 128:(kc + 1) * 128], identb)
                b.copy((hT[:, :, j * 128:(j + 1) * 128], ("hT", j)), (PT, KL([("PT", kc) for kc in range(8)])), eng="act")
            else:
                b.stt(xs32, (acc[j], ACCK), rstd, gbc, ALU.mult, ALU.mult)
                for kc in range(8):
                    pt_ = PTa if kc < 4 else PTb
                    b.tr((pt_[:, kc % 4, :], (pt_.tensor.name, kc % 4)), xs32[:, kc * 128:(kc + 1) * 128], ident)
                for kc in range(8):
                    pt_ = PTa if kc < 4 else PTb
                    pass
                b.copy((hT32[:, 0:4, :], ("hT32", 0)), (PTa, KL([(PTa.tensor.name, i) for i in range(4)])), eng="act")
                b.copy((hT32[:, 4:8, :], ("hT32", 1)), (PTb, KL([(PTb.tensor.name, i) for i in range(4)])), eng="dve")
                H32 = KL([("hT32", 0), ("hT32", 1)])
                b.copy((hT[:, :, j * 128:(j + 1) * 128], ("hT", j)), (hT32, H32), eng="act")
                for kc in range(8):
                    b.mm(PP[0][:, 0:8], (hT32[:, kc, :], ("hT32", kc // 4)), rt[:, kc, :], start=(kc == 0), stop=(kc == 7))
                b.copy(lg, PP[0][:, 0:8])
                b.P.add("dve", (lambda o, i: (lambda e: e.reduce_max(out=o, in_=i, axis=AX.X)))(sm[:, 0:1], lg), reads=[lg.tensor.name], writes=[sm.tensor.name])
                b.ts(mk1, lg, sm[:, 0:1], None, op0=ALU.is_equal)
                b.stt(lg2, mk1, -1e30, lg, ALU.mult, ALU.add)
                b.P.add("dve", (lambda o, i: (lambda e: e.reduce_max(out=o, in_=i, axis=AX.X)))(sm[:, 1:2], lg2), reads=[lg2.tensor.name], writes=[sm.tensor.name])
                b.ts(mk2, lg2, sm[:, 1:2], None, op0=ALU.is_equal)
                b.ts(sm[:, 2:3], sm[:, 0:1], -1.0, None, op0=ALU.mult)
                b.act(sm[:, 3:4], sm[:, 1:2], AF.Exp, bias=sm[:, 2:3])
                b.ts(sm[:, 4:5], sm[:, 3:4], 1.0, None, op0=ALU.add)
                b.recip(sm[:, 4:5], sm[:, 4:5])
                b.tt(sm[:, 5:6], sm[:, 3:4], sm[:, 4:5], ALU.mult)
                b.ts(mk1, mk1, sm[:, 4:5], None, op0=ALU.mult)
                b.stt((comb[:, j, :], ("comb", j)), mk2, sm[:, 5:6], mk1, ALU.mult, ALU.add)
        HT = KL([("hT", j) for j in range(TG)])
        for e in range(E if phase >= 2 else 0):
            wgv = wg_d[e].rearrange("(kc p) f -> p kc f", p=128)
            wuv = wu_d[e].rearrange("(kc p) f -> p kc f", p=128)
            wdv = wd_d[e].rearrange("(fc p) n -> p fc n", p=128)
            for fc in range(nF):
                w1, w2 = wgb[wi % NWB], wub[wi % NWB]
                b.load(w1, wgv[:, :, fc * 128:(fc + 1) * 128], q="pool")
                b.load(w2, wuv[:, :, fc * 128:(fc + 1) * 128], q="pool")
                for hh in range(NH):
                    bi_ = (wi * NH + hh) % 2
                    pg, pu, sg_ = PG[bi_], PU[bi_], sg[bi_]
                    hs = slice(hh * 512, (hh + 1) * 512)
                    for kc in range(8):
                        b.mm(pg, w1[:, kc, :], (hT[:, kc, hs], HT), start=(kc == 0), stop=(kc == 7))
                    for kc in range(8):
                        b.mm(pu, w2[:, kc, :], (hT[:, kc, hs], HT), start=(kc == 0), stop=(kc == 7))
                    b.act(sg_, pg, AF.Silu)
                    b.tt((hid[:, fc, hs], ("hid", fc)), sg_, pu, ALU.mult)
                wi += 1
            HID = KL([("hid", fc) for fc in range(nF)])
            for n in range(NS if phase >= 3 else 0):
                wd_ = wdh[di % 2]
                di += 1
                for q4 in range(4):
                    f0, f1 = (nF * q4) // 4, (nF * (q4 + 1)) // 4
                    b.load((wd_[:, f0:f1, :], (wd_.tensor.name, q4)), wdv[:, f0:f1, n * WN:(n + 1) * WN], q="pool", dkey=wd_.tensor.name)
                WDK = KL([(wd_.tensor.name, q4) for q4 in range(4)])
                for j in range(TG):
                    pp = PP[j % 2]
                    for fc in range(nF):
                        b.mm(pp[:, 0:WN], (hid[:, fc, j * 128:(j + 1) * 128], HID), (wd_[:, fc, :], WDK), start=(fc == 0), stop=(fc == nF - 1))
                    av = (acc[j][:, n * WN:(n + 1) * WN], ("acc", j, (n * WN) // 512))
                    if moe:
                        b.stt(av, pp[:, 0:WN], (comb[:, j, e:e + 1], ("comb", j)), av, ALU.mult, ALU.add)
                    else:
                        b.tt(av, pp[:, 0:WN], av, ALU.add)
        for j in range(TG):
            tok0 = g * G + j * 128
            b.store(out(tok0) if callable(out) else out[tok0:tok0 + 128, :], (acc[j], KL([("acc", j, 0), ("acc", j, 1)])))
    b.finish(sem_stack=(fused or {}).get("sem_stack"))
    return nc


LAMBDA_INIT1 = 0.8 - 0.6 * float(np.exp(-0.3 * 1))
TWO_PI = 6.283185307179586
CW1 = 6.28125
CW2 = TWO_PI - 6.28125
MAGIC = 12582912.0


def build_l3(nblk=T_SEQ // 512, phase=99, nc=None, pre="", fused=None):
    if nc is None:
        nc = bass.Bass("TRN2", target_bir_lowering=False)
    dr = lambda n, s, dt=F32, kind="ExternalInput": nc.dram_tensor(pre + n, s, dt, kind=kind).ap()
    if fused is None:
        x = dr("x", [T_SEQ, 1024])
    else:
        x = fused["x"]
        wop_d = dr("wo_part", [256, 1024])
    pos_d = dr("pos", [1, T_SEQ], I32)
    w_d = dr("w", [1024, 768])
    gmix = dr("gmix", [128, 8])
    cvec = dr("cvec", [128, 4])
    lams = dr("lams", [1, 256])
    ident_d = dr("ident", [128, 128])
    bones_d = dr("bones", [128, 128])
    rot_d = dr("rot", [128, 128])
    cmask_d = dr("cmask", [128, 4 * 512])
    if fused is None:
        oT = dr("oT", [256, T_SEQ], kind="ExternalOutput")

    b = B(nc, pre)
    sb, ps = b.sb, b.ps
    wb = sb("wb", [128, 8, 768], BF16)
    if fused is not None:
        wop = sb("wop", [128, 2, 1024], BF16)
        onb = [sb("onb%d" % i, [128, 512], BF16) for i in range(2)]
        p1t = [sb("p1t%d" % i, [128, 1024]) for i in range(2)]
        for kc in range(2):
            b.load((wop[:, kc, :], ("wop", kc)), wop_d[kc * 128:(kc + 1) * 128, :], q="pool", dkey="constp", grp=True)
    wst = [sb("wst%d" % i, [128, 768]) for i in range(2)]
    gm = sb("gm", [128, 8]); cv = sb("cv", [128, 8])
    ident = sb("ident_s", [128, 128]); identb = sb("identb", [128, 128], BF16)
    bones = sb("bones_s", [128, 128]); rot = sb("rot_s", [128, 128])
    ones_f = sb("ones_f", [128, 128]); ones_b = sb("ones_b", [128, 128], BF16)
    cmask = sb("cmask_s", [128, 4, 512], BF16)
    lm = sb("lm", [128, 256]); lmp = sb("lmp", [128, 128]); lsc = sb("lsc", [128, 8])
    for t_, d_ in ((gm, gmix), (cv[:, 0:4], cvec), (ident, ident_d), (bones, bones_d), (rot, rot_d), (lm, lams.partition_broadcast(128))):
        b.load(t_, d_, dkey="const", grp=True)
    b.load(identb, ident_d, q="pool", dkey="constp", grp=True)
    b.load((cmask[:, :, :].rearrange("p a b -> p (a b)"), "cmask_s"), cmask_d, q="pool", dkey="constp", grp=True)
    b.memset(ones_f, 1.0)
    b.memset(ones_b, 1.0)
    for kc in range(8):
        b.load(wst[kc % 2], w_d[kc * 128:(kc + 1) * 128, :])
        b.ts((wb[:, kc, :], ("wb", kc)), wst[kc % 2], gm[:, kc:kc + 1], None, op0=ALU.mult)
    WBK = lambda kc: ("wb", kc)
    col = lambda i: cv[:, i:i + 1]
    b.tt((lmp[:, 0:64], "lmp"), lm[:, 0:64], lm[:, 64:128], ALU.mult)
    b.tt((lmp[:, 64:128], "lmp"), lm[:, 128:192], lm[:, 192:256], ALU.mult)
    b.P.add("dve", lambda e: e.reduce_sum(out=lsc[:, 0:1], in_=lmp[:, 0:64], axis=AX.X), reads=["lmp"], writes=[lsc.tensor.name])
    b.P.add("dve", lambda e: e.reduce_sum(out=lsc[:, 1:2], in_=lmp[:, 64:128], axis=AX.X), reads=["lmp"], writes=[lsc.tensor.name])
    b.act(lsc[:, 0:2], lsc[:, 0:2], AF.Exp)
    b.tt(lsc[:, 2:3], lsc[:, 0:1], lsc[:, 1:2], ALU.subtract)
    b.ts(lsc[:, 3:4], lsc[:, 2:3], LAMBDA_INIT1, -1.0, op0=ALU.add, op1=ALU.mult)
    b.ts(col(4), col(2), 1.0 - LAMBDA_INIT1, None, op0=ALU.mult)
    NEGLAM = lsc[:, 3:4]

    TA = max(nblk, 1) * 512
    QT = [sb("QT%d" % h, [128, TA], BF16) for h in range(2)]
    KT = [sb("KT%d" % h, [128, TA], BF16) for h in range(2)]
    VS = [sb("VS%d" % h, [128, TA // 128, 128], BF16) for h in range(2)]
    xt = [sb("xt%d" % i, [128, 1024]) for i in range(2)]
    xs = [sb("xs%d" % i, [128, 1024], BF16) for i in range(2)]
    junk = sb("junk", [128, 1024], BF16)
    ss = sb("ss", [128, 1]); rstd = sb("rstd", [128, 1])
    xnT = sb("xnT", [128, 8, 512], BF16)
    posi = sb("posi", [128, 512], I32)
    tmp = [sb("t%d" % i, [128, 512]) for i in range(8)]
    Eb = [sb("Eb%d" % i, [128, 512], BF16) for i in range(4)]
    KX = [sb("KX%d" % i, [128, 128], BF16) for i in range(6)]
    PSB = [ps("PSB%d" % i, [128, 512]) for i in range(4)]
    PVA = [ps("PVA%d" % i, [128, 512]) for i in range(2)]
    PSM = [ps("PSM%d" % i, [128, 512]) for i in range(2)]
    PSX = PSM[0]
    PTB = PSB[3].bitcast(BF16).rearrange("p (a b) -> p a b", a=8)
    for s in range(nblk):
        for j in range(4):
            tix = (s * 4 + j) % 2
            t0_ = s * 512 + j * 128
            b.load(xt[tix], x(t0_) if callable(x) else x[t0_:t0_ + 128, :])
            rms_tile_T(b, xt[tix], xs[tix], ss, rstd, PTB, (xnT[:, :, j * 128:(j + 1) * 128], ("xnT", j)), identb, junk)
        XN = KL([("xnT", j) for j in range(4)])
        if phase < 0.4:
            continue
        b.load(posi, pos_d[:, s * 512:(s + 1) * 512].partition_broadcast(128))
        ang = tmp[2]
        b.copy(ang, posi)
        b.ts(ang, ang, col(3), None, op0=ALU.mult)
        for which, shift in ((0, 0.0), (1, 1.5707963267948966)):
            a_ = tmp[3]
            kf = tmp[4]
            if shift:
                b.ts(a_, ang, shift, None, op0=ALU.add)
            else:
                a_ = ang
            b.ts(kf, a_, 1.0 / TWO_PI, MAGIC, op0=ALU.mult, op1=ALU.add)
            b.ts(kf, kf, -MAGIC, None, op0=ALU.add)
            r_ = tmp[5]
            b.stt(r_, kf, -CW1, a_, ALU.mult, ALU.add)
            b.stt(r_, kf, -CW2, r_, ALU.mult, ALU.add)
            b.ts(r_, r_, 3.1415925, -3.1415925, op0=ALU.min, op1=ALU.max)
            b.act(tmp[which], r_, AF.Sin)
        SIN, COS = tmp[0], tmp[1]
        if phase < 0.6:
            continue
        for cc in range(4):
            pp = PSB[cc % 2]
            for kc in range(8):
                b.mm(pp, (wb[:, kc, cc * 128:(cc + 1) * 128], WBK(kc)), (xnT[:, kc, :], XN), start=(kc == 0), stop=(kc == 7))
            sq = tmp[2]
            b.act(sq, pp, AF.Square)
            b.mm(PSX, bones, sq)
            b.act(sq, PSX, AF.Sqrt, scale=1.0 / 64, bias=1e-6)
            b.recip(sq, sq)
            qn = tmp[3]
            b.stt(qn, pp, col(0 if cc < 2 else 1), sq, ALU.mult, ALU.mult)
            if phase < 0.7:
                continue
            b.mm(PVA[0], rot, qn)
            t1 = tmp[4]
            b.tt(t1, qn, COS, ALU.mult)
            t2 = tmp[5]
            b.tt(t2, PVA[0], SIN, ALU.mult)
            if phase < 0.75:
                continue
            dst = (QT if cc < 2 else KT)[cc % 2]
            b.stt((dst[:, s * 512:(s + 1) * 512], (dst.tensor.name, s)), t1, 1.0, t2, ALU.mult, ALU.add)
        if phase < 0.8:
            continue
        for j in range(4):
            pp = PSB[j % 2]
            for kc in range(8):
                b.mm(pp[:, 0:256], (xnT[:, kc, j * 128:(j + 1) * 128], XN), (wb[:, kc, 512:768], WBK(kc)), start=(kc == 0), stop=(kc == 7))
            for h in range(2):
                b.copy((VS[h][:, s * 4 + j, :], (VS[h].tensor.name, s)), pp[:, h * 128:(h + 1) * 128], eng="act")
    if phase < 2:
        nb2 = 0
    else:
        nb2 = nblk
    scale = 64 ** -0.5
    for i in range(nb2):
        for h in range(2):
            qs = slice(i * 512, (i + 1) * 512)
            QK = (QT[h].tensor.name, i)
            nkb = 4 * i + 4
            steps = [(kb, c) for kb in range(nkb) for c in range(2)]
            nst = len(steps)

            def emit_s(t):
                kb, c = steps[t]
                kx = KX[t % 6]
                b.ts(kx, (KT[h][:, kb * 128:(kb + 1) * 128], (KT[h].tensor.name, kb // 4)), bones[:, 64 * c:64 * c + 1], None, op0=ALU.mult)
                b.mm(PSB[t % 4], kx, (QT[h][:, qs], QK))

            def emit_e(t):
                kb, c = steps[t]
                b.act(Eb[t % 4], PSB[t % 4], AF.Exp, scale=scale)
                if kb >= 4 * i:
                    b.tt(Eb[t % 4], Eb[t % 4], (cmask[:, kb - 4 * i, :], "cmask_s"), ALU.mult)

            def emit_pv(t):
                kb, c = steps[t]
                b.mm(PVA[c], (VS[h][:, kb, :], (VS[h].tensor.name, kb // 4)), Eb[t % 4], start=(kb == 0), stop=(kb == nkb - 1))
                b.mm(PSM[c], ones_b, Eb[t % 4], start=(kb == 0), stop=(kb == nkb - 1))
            LA = 3
            for t in range(min(LA, nst)):
                emit_s(t)
            for t in range(nst):
                emit_e(t)
                if t + LA < nst:
                    emit_s(t + LA)
                emit_pv(t)
            r0, r1, o_ = tmp[0], tmp[1], tmp[2]
            b.recip(r0, PSM[0])
            b.tt(r0, PVA[0], r0, ALU.mult)
            b.recip(r1, PSM[1])
            b.tt(r1, PVA[1], r1, ALU.mult)
            b.stt(o_, r1, NEGLAM, r0, ALU.mult, ALU.add)
            sq = tmp[3]
            b.act(sq, o_, AF.Square)
            b.mm(PVA[0], ones_f, sq)
            b.act(sq, PVA[0], AF.Sqrt, scale=1.0 / 128, bias=1e-6)
            b.recip(sq, sq)
            if fused is None:
                on = tmp[4 + ((i * 2 + h) % 2)]
                b.stt(on, o_, col(4), sq, ALU.mult, ALU.mult)
                b.store(oT[h * 128:(h + 1) * 128, qs], on)
            else:
                b.stt(onb[h], o_, col(4), sq, ALU.mult, ALU.mult)
        if fused is not None:
            for j in range(4):
                pt_ = p1t[j % 2]
                for n in range(2):
                    for h in range(2):
                        b.mm(PVA[1], onb[h][:, j * 128:(j + 1) * 128], (wop[:, h, n * 512:(n + 1) * 512], ("wop", h)), start=(h == 0), stop=(h == 1))
                    b.copy((pt_[:, n * 512:(n + 1) * 512], (pt_.tensor.name, n)), PVA[1], eng=("act" if n == 0 else "dve"))
                b.store(fused["rs_in"][i * 512 + j * 128:i * 512 + (j + 1) * 128, :], (pt_, KL([(pt_.tensor.name, 0), (pt_.tensor.name, 1)])))
    b.finish(sem_stack=(fused or {}).get("sem_stack"))
    return nc


def _consts_l3():
    p = np.arange(128)
    bones = (p[:, None] // 64 == p[None, :] // 64).astype(np.float32)
    rot = np.zeros((128, 128), np.float32)
    for d in range(128):
        dm = d % 64
        if dm < 8:
            rot[d + 8, d] = -1.0
        elif dm < 16:
            rot[d - 8, d] = 1.0
    invf = np.zeros((128,), np.float32)
    for q in range(128):
        if q % 64 < 16:
            invf[q] = np.float32(500000.0) ** np.float32(-(2 * (q % 8)) / 16.0)
    kp = np.arange(128)[:, None]
    qc = np.arange(512)[None, :]
    cm = np.concatenate([((128 * j + kp) <= qc).astype(np.float32) for j in range(4)], axis=1)
    return dict(ident=np.eye(128, dtype=np.float32), bones=bones, rot=rot, cmask=np.ascontiguousarray(cm)), invf


def l3_inputs(inp, x1, bi, g):
    f = lambda k: np.asarray(inp[k][0], np.float32)
    W = f("o_w_qkv")
    hs = [2 * g, 2 * g + 1]
    cols = np.concatenate([np.arange(h * 128, (h + 1) * 128) for h in hs] + [1024 + np.arange(h * 128, (h + 1) * 128) for h in hs]
                          + [2048 + np.arange(h * 128, (h + 1) * 128) for h in hs])
    consts, invf = _consts_l3()
    cvec = np.zeros((128, 4), np.float32)
    cvec[:, 0] = np.tile(f("o_q_norm"), 2)
    cvec[:, 1] = np.tile(f("o_k_norm"), 2)
    cvec[:, 2] = f("o_subln")
    cvec[:, 3] = invf
    d = dict(x=(None if x1 is None else np.ascontiguousarray(x1[bi])), pos=np.ascontiguousarray(inp["positions"][bi].reshape(1, -1).astype(np.int32)),
             w=np.ascontiguousarray(W[:, cols]), gmix=np.ascontiguousarray(f("o_ln_mix").reshape(8, 128).T), cvec=cvec,
             lams=np.concatenate([f("o_lambda_q1"), f("o_lambda_k1"), f("o_lambda_q2"), f("o_lambda_k2")]).reshape(1, 256))
    d.update(consts)
    return d


N_CORES = 8
CC_GROUPS = [[0, 1, 2, 3], [4, 5, 6, 7]]


_CC_STATE = {}


def _cc_block(nc, kind, src, dst, name, sem_stack):
    st = _CC_STATE.setdefault(id(nc), {})
    if "sem" not in st:
        st["sem"] = sem_stack.enter_context(nc.semaphore("cc_sem"))
        st["n"] = 0
    sem = st["sem"]
    st["n"] += 1
    cnt = st["n"]
    with nc.Block() as block:
        @block.gpsimd
        def _(g):
            g.collective_compute(kind, ALU.bypass if kind == "AllGather" else ALU.add, replica_groups=CC_GROUPS,
                                 ins=[src.ap().opt()], outs=[dst.ap().opt()]).then_inc(sem)
            g.wait_ge(sem, cnt)


AG_CH = 256


def build_fused():
    nc = bass.Bass("TRN2", target_bir_lowering=False)
    NQ = T_SEQ // 4
    nch = NQ // AG_CH
    rs1_in = nc.dram_tensor("rs1_in", [T_SEQ, 1024], F32)
    rs1_out = nc.dram_tensor("rs1_out", [NQ, 1024], F32)
    ag_in = [nc.dram_tensor("ag_in%d" % k, [AG_CH, 1024], F32) for k in range(nch)]
    ag_out = [nc.dram_tensor("ag_out%d" % k, [4 * AG_CH, 1024], F32) for k in range(nch)]
    rs2_in = nc.dram_tensor("rs2_in", [T_SEQ, 1024], F32)
    rs2_out = nc.dram_tensor("rs2_out", [NQ, 1024], F32)

    def q_tile(tok0):
        return ag_in[tok0 // AG_CH].ap()[tok0 % AG_CH:tok0 % AG_CH + 128, :]

    def full_tile(t0):
        r, w = t0 // NQ, t0 % NQ
        k, i = w // AG_CH, w % AG_CH
        return ag_out[k].ap()[r * AG_CH + i:r * AG_CH + i + 128, :]

    with contextlib.ExitStack() as ss:
        build_l1(nc=nc, pre="a__", fused=dict(rs_in=rs1_in.ap(), sem_stack=ss))
        _cc_block(nc, "ReduceScatter", rs1_in, rs1_out, "cc1", ss)
        build_ffn(NT=2048, F=2816, E=1, G=512, moe=False, nc=nc, pre="b__", fused=dict(add=rs1_out.ap(), xres=None, out=q_tile, sem_stack=ss))
        for k in range(nch):
            _cc_block(nc, "AllGather", ag_in[k], ag_out[k], "cc2_%d" % k, ss)
        build_l3(nc=nc, pre="c__", fused=dict(x=full_tile, rs_in=rs2_in.ap(), sem_stack=ss))
        _cc_block(nc, "ReduceScatter", rs2_in, rs2_out, "cc3", ss)
        build_ffn(NT=2048, F=3584, E=8, G=1024, moe=True, nc=nc, pre="d__", fused=dict(add=rs2_out.ap(), xres=q_tile, out=None, sem_stack=ss))
    return nc


def fused_inputs(inp, c):
    bi, g = c // 4, c % 4
    f = lambda k: np.asarray(inp[k][0], np.float32)
    d = {}
    l1 = l1_inputs(inp, bi, g)
    wout = f("e_w_out")
    l1["wo_part"] = np.ascontiguousarray(np.concatenate([wout[g * 128:(g + 1) * 128], wout[512 + g * 128:512 + (g + 1) * 128]], axis=0))
    for k, v in l1.items():
        d["a__" + k] = v
    ident = np.eye(128, dtype=np.float32)
    d["b__xres"] = np.ascontiguousarray(np.asarray(inp["x"], np.float32)[bi, g * 2048:(g + 1) * 2048])
    d["b__gain"] = np.ascontiguousarray(f("e_ln_ffn").reshape(1, 1024))
    d["b__wg"] = np.asarray(inp["e_ffn_gate"], np.float32)
    d["b__wu"] = np.asarray(inp["e_ffn_up"], np.float32)
    d["b__wd"] = np.asarray(inp["e_ffn_down"], np.float32)
    d["b__ident"] = ident
    l3 = l3_inputs(inp, None, bi, g)
    del l3["x"]
    l3["wo_part"] = np.ascontiguousarray(f("o_w_o")[g * 256:(g + 1) * 256])
    for k, v in l3.items():
        d["c__" + k] = v
    d["d__gain"] = np.ascontiguousarray(f("o_ln_ffn").reshape(1, 1024))
    d["d__wg"] = np.asarray(inp["o_moe_gate"][0], np.float32)
    d["d__wu"] = np.asarray(inp["o_moe_up"][0], np.float32)
    d["d__wd"] = np.asarray(inp["o_moe_down"][0], np.float32)
    d["d__router"] = f("o_router")
    d["d__ident"] = ident
    return d


def kernel(**inp):
    inp = {k: np.asarray(v) for k, v in inp.items()}
    Bn, T, D = inp["x"].shape
    nc = build_fused()
    res = run_bass_kernel_spmd(nc, [fused_inputs(inp, c) for c in range(N_CORES)], core_ids=list(range(N_CORES)))
    out = np.zeros((Bn, T, D), np.float32)
    for c in range(N_CORES):
        bi, tq = c // 4, c % 4
        out[bi, tq * 2048:(tq + 1) * 2048] = res.results[c]["d__out"]
    return out
```

```python
import contextlib
import numpy as np
import ml_dtypes
import concourse.bass as bass
import concourse.mybir as mybir
from concourse.alu_op_type import AluOpType as ALU
from concourse.bass_utils import run_bass_kernel_spmd

AF = mybir.ActivationFunctionType
F32 = mybir.dt.float32
BF16 = mybir.dt.bfloat16
I32 = mybir.dt.int32
AX = mybir.AxisListType

COMPUTE = ("pe", "act", "dve", "pool")


class _Op:
    __slots__ = ("eng", "fn", "reads", "writes", "kind", "waits", "signal", "val", "dkey", "seq")

    def __init__(self, eng, fn, reads, writes, kind):
        self.eng = eng
        self.fn = fn
        self.reads = tuple(reads)
        self.writes = tuple(writes)
        self.kind = kind
        self.waits = {}
        self.signal = False
        self.val = None
        self.dkey = None


class Prog:
    def __init__(self, nc):
        self.nc = nc
        self.ops = []
        self.last_w = {}
        self.readers = {}
        self.deps = []
        self.group_keys = set()

    def _add(self, op):
        deps = set()
        for k in op.reads:
            w = self.last_w.get(k)
            if w is not None:
                deps.add((w, "raw"))
        for k in op.writes:
            w = self.last_w.get(k)
            if w is not None:
                deps.add((w, "waw"))
            for r in self.readers.get(k, ()):
                if r is not op:
                    deps.add((r, "war"))
        for k in op.reads:
            self.readers.setdefault(k, []).append(op)
        for k in op.writes:
            self.last_w[k] = op
            self.readers[k] = []
        op.seq = len(self.ops)
        self.ops.append(op)
        self.deps.append(deps)
        return op

    def add(self, eng, fn, reads=(), writes=()):
        return self._add(_Op(eng, fn, reads, writes, "c"))

    def dma(self, q, fn, reads=(), writes=(), key=None):
        op = _Op(q, fn, reads, writes, "d")
        op.dkey = key
        return self._add(op)

    def emit(self, final_keys=(), sem_stack=None):
        nc = self.nc
        ops = self.ops
        for op, deps in zip(ops, self.deps):
            for d, kind in deps:
                if d.kind == "c":
                    if d.eng == op.eng and op.kind == "c":
                        if op.eng == "pe" or kind == "war":
                            continue
                    d.signal = True
        finals = [self.last_w[k] for k in final_keys if k in self.last_w]
        for d in finals:
            if d.kind == "c":
                d.signal = True
        cnt = {e: 0 for e in COMPUTE}
        dcnt = {}
        dkeys = []
        for op in ops:
            if op.kind == "c":
                if op.signal:
                    cnt[op.eng] += 1
                    op.val = cnt[op.eng]
            else:
                if op.dkey not in dcnt:
                    dcnt[op.dkey] = 0
                    dkeys.append(op.dkey)
                dcnt[op.dkey] += 16
                op.val = dcnt[op.dkey]
        for op in ops:
            if op.kind == "d" and op.dkey in self.group_keys:
                op.val = dcnt[op.dkey]
        with contextlib.ExitStack() as st_local:
            st = sem_stack if sem_stack is not None else st_local
            tag = "_%d" % len(getattr(st, "_exit_callbacks", ())) if sem_stack is not None else ""
            csem = {e: st.enter_context(nc.semaphore("cs_" + e + tag)) for e in COMPUTE}
            dsem = {k: st.enter_context(nc.semaphore("ds%d%s" % (i, tag))) for i, k in enumerate(dkeys)}

            def semof(d):
                return csem[d.eng] if d.kind == "c" else dsem[d.dkey]

            streams = {}
            waited = {}
            for op, deps in zip(ops, self.deps):
                need = {}
                for d, kind in deps:
                    if d.kind == "c" and d.eng == op.eng and op.kind == "c":
                        if op.eng == "pe" or kind == "war":
                            continue
                    s = semof(d)
                    sid = id(s)
                    if need.get(sid, (None, 0))[1] < d.val:
                        need[sid] = (s, d.val)
                w = waited.setdefault(op.eng, {})
                op.waits = []
                for sid, (s, v) in need.items():
                    if w.get(sid, 0) < v:
                        w[sid] = v
                        op.waits.append((s, v))
                streams.setdefault(op.eng, []).append(op)
            fin_waits = []
            for d in finals:
                fin_waits.append((semof(d), d.val))

            def run_stream(name, engine, extra=None):
                for op in streams.get(name, []):
                    for s, v in op.waits:
                        engine.wait_ge(s, v)
                    ins = op.fn(engine)
                    if op.kind == "c":
                        if op.signal:
                            ins.then_inc(csem[op.eng], 1)
                    else:
                        ins.then_inc(dsem[op.dkey], 16)
                if extra:
                    for s, v in extra:
                        engine.wait_ge(s, v)

            with nc.Block() as block:
                @block.tensor
                def _(e):
                    run_stream("pe", e)

                @block.scalar
                def _(e):
                    run_stream("act", e)

                @block.vector
                def _(e):
                    run_stream("dve", e)

                @block.gpsimd
                def _(e):
                    run_stream("pool", e)

                @block.sync
                def _(e):
                    run_stream("sp", e, extra=fin_waits)


class KL(list):
    pass


KEYMAP = {"PS0": KL([("PS0", 0), ("PS0", 1)]), "PS1": KL([("PS1", 0), ("PS1", 1)])}


def _ak(x):
    if isinstance(x, tuple):
        ap, k = x
        return (ap, k if isinstance(k, KL) else KL([k]))
    n = x.tensor.name
    return (x, KEYMAP.get(n.split("__")[-1]) or KL([n]))


class B:
    def __init__(self, nc, pre=""):
        self.nc = nc
        self.pre = pre
        self.P = Prog(nc)
        self.st = contextlib.ExitStack()
        self.nout = 0

    def sb(self, name, shape, dt=F32):
        return self.st.enter_context(self.nc.sbuf_tensor(self.pre + name, shape, dt))[:]

    def ps(self, name, shape, dt=F32):
        return self.st.enter_context(self.nc.psum_tensor(self.pre + name, shape, dt))[:]

    def mm(self, out, lhsT, rhs, start=True, stop=True):
        (o, ok), (l, lk), (r, rk) = _ak(out), _ak(lhsT), _ak(rhs)
        self.P.add("pe", lambda e: e.matmul(o, l, r, start=start, stop=stop), reads=lk + rk, writes=ok)

    def tr(self, out, in_, ident):
        (o, ok), (i, ik), (d, dk) = _ak(out), _ak(in_), _ak(ident)
        self.P.add("pe", lambda e: e.transpose(o, i, d), reads=ik + dk, writes=ok)

    def act(self, out, in_, func, scale=None, bias=None, accum=None, eng="act"):
        (o, ok), (i, ik) = _ak(out), _ak(in_)
        reads = list(ik)
        writes = list(ok)
        kw = {}
        if scale is not None:
            if isinstance(scale, (int, float)):
                kw["scale"] = float(scale)
            else:
                s, sk = _ak(scale)
                kw["scale"] = s
                reads.extend(sk)
        if bias is not None:
            if isinstance(bias, (int, float)):
                kw["bias"] = float(bias)
            else:
                b_, bk = _ak(bias)
                kw["bias"] = b_
                reads.extend(bk)
        if accum is not None:
            a_, ak_ = _ak(accum)
            kw["accum_out"] = a_
            writes.extend(ak_)
        self.P.add("act", lambda e: e.activation(out=o, in_=i, func=func, **kw), reads=reads, writes=writes)

    def tt(self, out, in0, in1, op, eng="dve"):
        (o, ok), (a, ak_), (b_, bk) = _ak(out), _ak(in0), _ak(in1)
        self.P.add(eng, lambda e: e.tensor_tensor(out=o, in0=a, in1=b_, op=op), reads=ak_ + bk, writes=ok)

    def ts(self, out, in0, s1, s2=None, op0=ALU.mult, op1=None, eng="dve", accum=None):
        (o, ok), (a, ak_) = _ak(out), _ak(in0)
        reads = list(ak_)
        writes = list(ok)

        def sc(s):
            if s is None or isinstance(s, (int, float)):
                return s
            ap, k = _ak(s)
            reads.extend(k)
            return ap
        v1, v2 = sc(s1), sc(s2)
        kw = {}
        if op1 is not None:
            kw["op1"] = op1
        if accum is not None:
            a2, a2k = _ak(accum)
            kw["accum_out"] = a2
            writes.extend(a2k)
        self.P.add(eng, lambda e: e.tensor_scalar(out=o, in0=a, scalar1=v1, scalar2=v2, op0=op0, **kw), reads=reads, writes=writes)

    def stt(self, out, in0, scalar, in1, op0, op1):
        (o, ok), (a, ak_), (b_, bk) = _ak(out), _ak(in0), _ak(in1)
        reads = list(ak_ + bk)
        if isinstance(scalar, (int, float)):
            s = float(scalar)
        else:
            s, sk = _ak(scalar)
            reads.extend(sk)
        self.P.add("dve", lambda e: e.scalar_tensor_tensor(out=o, in0=a, scalar=s, in1=b_, op0=op0, op1=op1), reads=reads, writes=ok)

    def copy(self, out, in_, eng="dve"):
        (o, ok), (i, ik) = _ak(out), _ak(in_)
        if eng == "act":
            self.P.add("act", lambda e: e.activation(out=o, in_=i, func=AF.Copy), reads=ik, writes=ok)
        else:
            self.P.add(eng, lambda e: e.tensor_copy(out=o, in_=i), reads=ik, writes=ok)

    def scan(self, out, d0, d1, init):
        (o, ok), (a, ak_), (b_, bk) = _ak(out), _ak(d0), _ak(d1)
        reads = list(ak_ + bk)
        if isinstance(init, (int, float)):
            iv = float(init)
        else:
            iv, ik = _ak(init)
            reads.extend(ik)
        self.P.add("dve", lambda e: e.tensor_tensor_scan(out=o, data0=a, data1=b_, initial=iv, op0=ALU.mult, op1=ALU.add), reads=reads, writes=ok)

    def recip(self, out, in_):
        (o, ok), (i, ik) = _ak(out), _ak(in_)
        self.P.add("dve", lambda e: e.reciprocal(out=o, in_=i), reads=ik, writes=ok)

    def memset(self, out, val, eng="dve"):
        (o, ok) = _ak(out)
        self.P.add(eng, lambda e: e.memset(o, val), writes=ok)

    def load(self, out, in_, q="sp", dkey=None, grp=False):
        (o, ok) = _ak(out)
        if grp:
            self.P.group_keys.add(dkey)
        self.P.dma(q, lambda e: e.dma_start(out=o, in_=in_), writes=ok, key=(dkey if dkey is not None else ok[0]))

    def store(self, out_dram, in_, q="sp"):
        (i, ik) = _ak(in_)
        self.nout += 1
        k = ("__out", self.nout)
        self.P.dma(q, lambda e: e.dma_start(out=out_dram, in_=i), reads=ik, writes=[k], key=ik[0])

    def finish(self, sem_stack=None):
        self.P.emit(final_keys=[("__out", i + 1) for i in range(self.nout)], sem_stack=sem_stack)
        self.st.close()


def rms_tile_T(b, xt, xs, ss, rstd, PT, xnT_dst, identb, junk, eps=1e-6, D=1024):
    b.act(junk, xt, AF.Square, accum=ss)
    b.act(rstd, ss, AF.Sqrt, scale=1.0 / D, bias=eps)
    b.recip(rstd, rstd)
    b.ts(xs, xt, rstd, None, op0=ALU.mult)
    n = D // 128
    for kc in range(n):
        b.tr((PT[:, kc, :], (PT.tensor.name, kc)), xs[:, kc * 128:(kc + 1) * 128], identb)
    b.copy(xnT_dst, (PT[:, :, :], KL([(PT.tensor.name, kc) for kc in range(n)])), eng="act")


T_SEQ = 8192
SEG = 512
CH = 64
C0 = 0.6065306597126334
NV1 = 20


def build_l1(nseg=T_SEQ // SEG, phase=99, nc=None, pre="", fused=None):
    if nc is None:
        nc = bass.Bass("TRN2", target_bir_lowering=False)
    dr = lambda n, s, dt=F32, kind="ExternalInput": nc.dram_tensor(pre + n, s, dt, kind=kind).ap()
    x = dr("x", [T_SEQ, 1024])
    w_in = dr("w_in", [1024, 896])
    gmix = dr("gmix", [128, 8])
    cvec = dr("cvec", [128, NV1])
    wa_d = dr("wa", [128, 128])
    wx_d = dr("wx", [128, 128])
    w2a2_d = dr("w2a2", [128, 128])
    g2_d = dr("g2", [128, 128])
    ident_d = dr("ident", [128, 128])
    bones_d = dr("bones", [128, 128])
    maska_d = dr("maska", [128, 512])
    maskb_d = dr("maskb", [128, 256])
    rmask_d = dr("rmask", [128, 512])
    id2_d = dr("id2", [128, 128])
    if fused is None:
        yT = dr("yT", [256, T_SEQ], kind="ExternalOutput")
    else:
        wop_d = dr("wo_part", [256, 1024])

    b = B(nc, pre)
    sb, ps = b.sb, b.ps
    wb = sb("wb", [128, 8, 896], BF16)
    if fused is not None:
        wop = sb("wop", [128, 2, 1024], BF16)
        ybf = sb("ybf", [128, 2, SEG], BF16)
        p1t = [sb("p1t%d" % i, [128, 1024]) for i in range(2)]
        for kc in range(2):
            b.load((wop[:, kc, :], ("wop", kc)), wop_d[kc * 128:(kc + 1) * 128, :], q="pool", dkey="constp", grp=True)
    wst = [sb("wst%d" % i, [128, 896]) for i in range(2)]
    gm = sb("gm", [128, 8])
    cv = sb("cv", [128, NV1 + 4])
    wa = sb("wa_s", [128, 128]); wx = sb("wx_s", [128, 128])
    w2a2 = sb("w2a2_s", [128, 128]); g2 = sb("g2_s", [128, 128])
    ident = sb("ident_s", [128, 128]); identb = sb("identb", [128, 128], BF16)
    bones = sb("bones_s", [128, 128]); rkbd = sb("rkbd", [128, 128])
    id2 = sb("id2_s", [128, 2, 64])
    maska = sb("maska_s", [128, 512]); maskb = sb("maskb_s", [128, 256]); rmask = sb("rmask_s", [128, 512])
    for t_, d_ in ((gm, gmix), (cv[:, 0:NV1], cvec), (wa, wa_d), (wx, wx_d), (w2a2, w2a2_d), (g2, g2_d), (ident, ident_d),
                   (bones, bones_d), ((id2[:, :, :].rearrange("p h s -> p (h s)"), "id2_s"), id2_d), (maska, maska_d), (maskb, maskb_d), (rmask, rmask_d)):
        b.load(t_, d_, dkey="const", grp=True)
    b.load(identb, ident_d, q="pool", dkey="constp", grp=True)
    for kc in range(8):
        b.load(wst[kc % 2], w_in[kc * 128:(kc + 1) * 128, :])
        b.ts((wb[:, kc, :], ("wb", kc)), wst[kc % 2], gm[:, kc:kc + 1], None, op0=ALU.mult)
    WBK = lambda kc: ("wb", kc)
    col = lambda i: cv[:, i:i + 1]
    CCH, OMKA, TWOC = NV1, NV1 + 1, NV1 + 2
    b.act(col(CCH), col(7), AF.Exp, scale=-1.0)
    b.act(col(CCH), col(CCH), AF.Ln, bias=1.0)
    b.ts(col(TWOC), col(CCH), -16.0, None, op0=ALU.mult)
    b.ts(col(CCH), col(CCH), -8.0, None, op0=ALU.mult)
    b.ts(col(OMKA), col(16), -1.0, 1.0, op0=ALU.mult, op1=ALU.add)
    b.ts(rkbd, bones, col(17), None, op0=ALU.mult)

    xt = [sb("xt%d" % i, [128, 1024]) for i in range(2)]
    xs = [sb("xs%d" % i, [128, 1024], BF16) for i in range(2)]
    junk = sb("junk", [128, 1024], BF16)
    ss = sb("ss", [128, 1]); rstd = sb("rstd", [128, 1])
    xnT = sb("xnT", [128, 8, SEG], BF16)
    pj = [[sb("pj%d_%d" % (i, c), [128, 4 + SEG]) for c in range(7)] for i in range(2)]
    NT = 27
    tmp = [sb("t%d" % i, [128, SEG]) for i in range(NT)]
    hh = [sb("hh%d" % i, [128, SEG]) for i in range(2)]
    TM = sb("TM", [64, 2, 4, 128])
    SA = sb("SA", [64, 2, 4, 2, 64]); SBm = sb("SBm", [64, 256])
    PQ = [sb("PQ%d" % i, [64, 2, 2, 2, 64]) for i in range(2)]
    Tb = [sb("Tb%d" % i, [64, 2, 2, 64]) for i in range(2)]
    ZS = sb("ZS", [64, 2, 2, 64]); MS = sb("MS", [64, 2, 2, 2, 64])
    GP = sb("GP", [64, 2, 2, 2, 64]); GS = GP[:, 0, :, :, :]; PSm = GP[:, 1, :, :, :]
    STT = [sb("STT%d" % i, [64, 2, 64]) for i in range(2)]
    AFx = sb("AFx", [128, SEG // CH, 2, CH]); RFx = sb("RFx", [128, SEG // CH, 2, CH]); BTx = sb("BTx", [128, SEG // CH, 2, CH])
    RFh = sb("RFh", [64, 2, SEG]); WCh = sb("WCh", [64, 2, SEG // CH])
    OS = sb("OS", [128, SEG])
    PP = ps("PP", [128, 512]); PT = ps("PT", [128, 8, 128], BF16)
    PS0 = ps("PS0", [128, 512]); PS1 = ps("PS1", [128, 512])
    PA = ps("PA", [128, 512]); PC = ps("PC", [128, 512]); PX = ps("PX", [128, 512]); PO = ps("PO", [128, 512])

    for i in range(2):
        for c in range(7):
            b.memset((pj[i][c][:, 0:4], ("pjh", i, c)), 0.0)
    b.memset((STT[0][:, :, :], "STT0"), 0.0)
    for tx in (AFx, RFx, BTx):
        b.memset((tx[:, :, :, :], tx.tensor.name), 0.0, eng="pool")

    st_i = 0
    PS0a_ = ("PS0", 0)
    XN = KL([("xnT", j) for j in range(4)])
    PIPE = (phase >= 99)

    def emit_load(s_, j):
        tix = (s_ * 4 + j) % 2
        b.load(xt[tix], x[s_ * SEG + j * 128: s_ * SEG + (j + 1) * 128, :])

    def emit_norm(s_, j):
        tix = (s_ * 4 + j) % 2
        b.act(junk, xt[tix], AF.Square, accum=ss)
        b.act(rstd, ss, AF.Sqrt, scale=1.0 / 1024, bias=1e-6)
        b.recip(rstd, rstd)
        b.ts(xs[tix], xt[tix], rstd, None, op0=ALU.mult)

    def emit_tr(s_, j):
        tix = (s_ * 4 + j) % 2
        for kc in range(8):
            b.tr((PT[:, kc, :], (PT.tensor.name, kc)), xs[tix][:, kc * 128:(kc + 1) * 128], identb)
        b.copy((xnT[:, :, j * 128:(j + 1) * 128], ("xnT", j)), (PT[:, :, :], KL([(PT.tensor.name, kc) for kc in range(8)])), eng="act")

    def emit_B(s_):
        c_, n_ = s_ % 2, 1 - (s_ % 2)
        for cc in range(7):
            for kc in range(8):
                b.mm(PP, (wb[:, kc, cc * 128:(cc + 1) * 128], WBK(kc)), (xnT[:, kc, :], XN), start=(kc == 0), stop=(kc == 7))
            b.copy((pj[c_][cc][:, 4:4 + SEG], ("pj", c_, cc)), PP, eng=("act" if cc % 2 == 0 else "dve"))
            b.copy((pj[n_][cc][:, 0:4], ("pjh", n_, cc)), (pj[c_][cc][:, SEG:SEG + 4], ("pj", c_, cc)), eng="pool")

    def emit_AB(s_):
        for j in range(4):
            emit_load(s_, j)
            emit_norm(s_, j)
            emit_tr(s_, j)
        emit_B(s_)

    if PIPE:
        emit_AB(0)
    for s in range(nseg):
        cur = s % 2
        nxt = 1 - cur
        pjc = pj[cur]
        PJ = lambda c: ("pj", cur, c)
        PJH = lambda c: ("pjh", cur, c)
        if not PIPE:
            emit_AB(s)
        pipe_next = PIPE and (s + 1 < nseg)
        if phase < 2:
            continue
        cur_v = lambda c: (pjc[c][:, 4:4 + SEG], PJ(c))
        sh_v = lambda c, k: (pjc[c][:, 4 - k:4 - k + SEG], KL([PJ(c), PJH(c)]))
        t = tmp
        xc = t[0]
        b.ts(xc, sh_v(0, 3), col(0), col(4), op0=ALU.mult, op1=ALU.add)
        b.stt(xc, sh_v(0, 2), col(1), xc, ALU.mult, ALU.add)
        b.stt(xc, sh_v(0, 1), col(2), xc, ALU.mult, ALU.add)
        b.stt(xc, cur_v(0), col(3), xc, ALU.mult, ALU.add)
        b.mm(PS0, wa, xc)
        b.mm(PS1, wx, xc)
        ra = t[1]
        b.act(ra, PS0, AF.Sigmoid, bias=col(5))
        av_ = t[2]
        b.act(av_, ra, AF.Exp, scale=col(CCH))
        a2_ = t[3]
        b.act(a2_, ra, AF.Exp, scale=col(TWOC))
        b.act(a2_, a2_, AF.Sqrt, scale=-1.0, bias=1.0)
        ix = t[1]
        b.act(ix, PS1, AF.Sigmoid, bias=col(6))
        b.tt(a2_, a2_, ix, ALU.mult)
        b.tt(a2_, a2_, xc, ALU.mult)
        hcur = hh[cur]
        b.scan(hcur, av_, a2_, 0.0 if s == 0 else hh[nxt][:, SEG - 1:SEG])
        gq = t[0]
        b.act(gq, cur_v(1), AF.Square)
        b.ts(gq, gq, 0.044715, 1.0, op0=ALU.mult, op1=ALU.add)
        b.tt(gq, gq, cur_v(1), ALU.mult)
        b.act(gq, gq, AF.Sigmoid, scale=1.5957691216057308)
        b.tt(gq, gq, cur_v(1), ALU.mult)
        ylru = t[1]
        if fused is None:
            b.tt(ylru, gq, hcur, ALU.mult)
            b.store(yT[0:128, s * SEG:(s + 1) * SEG], ylru)
        else:
            b.tt((ybf[:, 0, :], ("ybf", 0)), gq, hcur, ALU.mult)
        if phase < 3:
            continue
        shf = []
        for i, c in enumerate((2, 3, 4, 5, 6)):
            d = t[4 + i]
            b.tt(d, sh_v(c, 1), cur_v(c), ALU.subtract)
            b.stt(d, d, col(8 + i), cur_v(c), ALU.mult, ALU.add)
            shf.append(d)
        rs, ks, vs, xwa, xgs = shf
        tw = t[9]
        b.act(tw[0:64, :], xwa[0:64, :], AF.Tanh)
        b.mm(PS0, w2a2[0:64, :], tw[0:64, :])
        b.mm(PS1, w2a2[64:128, :], xwa[64:128, :])
        sgz = t[9]
        b.act(sgz, PS0, AF.Sigmoid, bias=col(13))
        avv = t[10]
        b.act(avv, PS1, AF.Sigmoid, bias=col(14))
        sg = t[11]
        b.act(sg, xgs, AF.Sigmoid)
        cs = t[12]
        b.scan(cs, rmask, sgz, 0.0)
        csm1 = t[13]
        b.tt(csm1, cs, sgz, ALU.subtract)
        Wt = t[14]; iW = t[15]; Wm1 = t[13]
        b.act(Wt, cs, AF.Exp, scale=-C0)
        b.act(iW, cs, AF.Exp, scale=C0)
        b.act(Wm1, csm1, AF.Exp, scale=-C0)
        b.mm(PS0, g2, sg)
        gv = t[11]
        b.copy(gv, PS0, eng="act")
        kq = t[9]
        b.ts(kq, ks, col(15), None, op0=ALU.mult)
        kq2 = t[12]
        b.act(kq2, kq, AF.Square)
        b.mm(PS1, bones, kq2)
        rn = t[12]
        b.act(rn, PS1, AF.Sqrt)
        b.ts(rn, rn, 1e-12, None, op0=ALU.max)
        b.recip(rn, rn)
        kkn = t[9]
        b.tt(kkn, kq, rn, ALU.mult)
        kmod = t[12]
        b.ts(kmod, avv, col(16), col(OMKA), op0=ALU.mult, op1=ALU.add)
        b.tt(kmod, kmod, ks, ALU.mult)
        bb = t[10]
        b.tt(bb, kkn, avv, ALU.mult)
        AFm = t[16]; RF = t[17]; BT = t[18]; KT = t[19]; Bh = t[20]; Kh = t[21]
        b.stt(AFm, kkn, -1.0, Wm1, ALU.mult, ALU.mult)
        b.tt(RF, rs, Wt, ALU.mult)
        b.tt(BT, bb, iW, ALU.mult)
        b.tt(KT, kmod, iW, ALU.mult)
        v3 = lambda tl: tl[:, :].rearrange("p (c s) -> p c s", s=CH)
        wcb = (v3(Wt)[:, :, CH - 1:CH].broadcast_to([128, SEG // CH, CH]), Wt.tensor.name)
        b.tt((v3(Bh), Bh.tensor.name), (v3(BT), BT.tensor.name), wcb, ALU.mult)
        b.tt((v3(Kh), Kh.tensor.name), (v3(KT), KT.tensor.name), wcb, ALU.mult)
        rk_ = t[9]
        b.tt(rk_, rs, kmod, ALU.mult)
        b.mm(PS1, rkbd, rk_)
        bonus = t[22]
        b.tt(bonus, PS1, vs, ALU.mult)
        if phase < 4:
            continue
        for h in range(2):
            b.mm((PS1[0:64, :], KEYMAP["PS1"]), ident[:, 64 * h:64 * h + 64], RF)
            b.copy((RFh[:, h, :], "RFh"), (PS1[0:64, :], KEYMAP["PS1"]), eng="act")
            b.mm((PS0[0:64, h * 8:h * 8 + 8], PS0a_), ident[:, 64 * h:64 * h + 64], (v3(Wt)[:, :, CH - 1], Wt.tensor.name))
        b.copy((WCh[:, :, :].rearrange("p h c -> p (h c)"), "WCh"), (PS0[0:64, 0:16], PS0a_), eng="act")
        for tl, tx in ((AFm, AFx), (RF, RFx), (BT, BTx)):
            b.copy((tx[0:64, :, 0, :], tx.tensor.name), (v3(tl)[0:64], tl.tensor.name), eng="pool")
            b.copy((tx[64:128, :, 1, :], tx.tensor.name), (v3(tl)[64:128], tl.tensor.name), eng="pool")
        PS0a, PS0b, PS1a, PS1b = ("PS0", 0), ("PS0", 1), ("PS1", 0), ("PS1", 1)
        sel = [ident[:, 0:64], ident[:, 64:128]]
        if pipe_next:
            emit_load(s + 1, 0)
        for cp in range(SEG // 128):
            tok = slice(cp * 128, (cp + 1) * 128)
            if pipe_next:
                if cp < 3:
                    emit_load(s + 1, cp + 1)
                emit_norm(s + 1, cp)
            for q in range(2):
                c_ = cp * 2 + q
                ck = slice(c_ * CH, (c_ + 1) * CH)
                for qi, src in enumerate((AFm, Bh, Kh, vs)):
                    b.tr((PX[0:64, qi * 128:(qi + 1) * 128], "PX"), src[:, ck], ident)
                b.copy((TM[:, q, :, :].rearrange("p a b -> p (a b)"), ("TM", q)), (PX[0:64, :], "PX"), eng="act")
                for j, (l_, rx) in enumerate(((BT, AFx), (BT, RFx), (KT, AFx), (KT, RFx))):
                    b.mm((PA[0:64, j * 128:(j + 1) * 128], "PA"), l_[:, ck], rx[:, c_, :, :].rearrange("p h s -> p (h s)"))
                b.mm((PS1[0:64, q * 128:(q + 1) * 128], PS1a), AFm[:, ck], BTx[:, c_, :, :].rearrange("p h s -> p (h s)"))
                b.tt((SA[:, q, :, :, :].rearrange("p j h s -> p (j h s)"), ("SA", q)), (PA[0:64, :], "PA"), maska[0:64, :], ALU.mult)
            b.tt(SBm, (PS1[0:64, 0:256], PS1a), maskb[0:64, :], ALU.mult)
            TMv = lambda qi, q, h: (TM[:, q, qi, 64 * h:64 * h + 64], ("TM", q))
            SAv = lambda j, q, h: (SA[:, q, j, h, :], ("SA", q))
            SAK = KL([("SA", 0), ("SA", 1)])
            QH = [(q, h) for q in range(2) for h in range(2)]
            for q, h in QH:
                b.mm((PS1[0:64, 256 + (q * 2 + h) * 64:256 + (q * 2 + h) * 64 + 64], PS1b), SAv(2, q, h), TMv(3, q, h))
            b.copy((ZS[:, :, :, :].rearrange("p q h s -> p (q h s)"), "ZS"), (PS1[0:64, 256:512], PS1b), eng="act")
            if phase < 5:
                continue
            b.copy((PQ[0][:, 0, :, :, :], "PQ0"), (SA[:, :, 0, :, :], SAK), eng="pool")
            b.copy((PQ[0][:, 1, :, :, :].rearrange("p q h s -> p (q h s)"), "PQ0"), SBm, eng="pool")
            idb = (ident[0:64, 0:64].rearrange("p (a c s) -> p a c s", a=1, c=1).broadcast_to([64, 2, 2, 64]), ident.tensor.name)
            b.tt((Tb[0][:, :, :, :], "Tb0"), (SA[:, :, 0, :, :], SAK), idb, ALU.add)
            PC5 = PC[0:64, :].rearrange("p (j q h s) -> p j q h s", j=2, q=2, h=2)
            POt = PO[0:64, 256:512].rearrange("p (q h s) -> p q h s", q=2, h=2)
            for k in range(0, 6):
                i = k % 2
                pqk = "PQ%d" % i
                if k >= 1:
                    for q, h in QH:
                        b.mm((POt[:, q, h, :], ("PO", 2)), (PQ[i][:, 1, q, h, :], pqk), (Tb[1 - i][:, q, h, :], "Tb%d" % (1 - i)))
                if k <= 3:
                    for q, h in QH:
                        b.mm((PC5[:, 0, q, h, :], ("PC", 0)), (PQ[i][:, 1, q, h, :], pqk), (PQ[i][:, 0, q, h, :], pqk))
                if k <= 4:
                    for q, h in QH:
                        b.mm((PC5[:, 1, q, h, :], ("PC", 1)), (PQ[i][:, 0, q, h, :], pqk), (PQ[i][:, 1, q, h, :], pqk))
                if k <= 3:
                    b.copy((PQ[1 - i][:, :, :, :, :].rearrange("p j q h s -> p (j q h s)"), "PQ%d" % (1 - i)),
                           (PC[0:64, :], KL([("PC", 0), ("PC", 1)])), eng="act")
                elif k == 4:
                    b.copy((PQ[1 - i][:, 1, :, :, :].rearrange("p q h s -> p (q h s)"), "PQ%d" % (1 - i)),
                           (PC[0:64, 256:512], ("PC", 1)), eng="act")
                if k >= 1:
                    b.tt((Tb[i][:, :, :, :].rearrange("p q h s -> p (q h s)"), "Tb%d" % i),
                         (Tb[1 - i][:, :, :, :].rearrange("p q h s -> p (q h s)"), "Tb%d" % (1 - i)),
                         (PO[0:64, 256:512], ("PO", 2)), ALU.add)
            if phase < 5.2:
                continue
            for q, h in QH:
                o0 = ((q * 2 + h) * 2) * 64
                b.mm((PX[0:64, o0:o0 + 64], "PX"), (Tb[1][:, q, h, :], "Tb1"), TMv(0, q, h))
                b.mm((PX[0:64, o0 + 64:o0 + 128], "PX"), (Tb[1][:, q, h, :], "Tb1"), (ZS[:, q, h, :], "ZS"))
            b.copy((MS[:, :, :, :, :].rearrange("p q h m s -> p (q h m s)"), "MS"), (PX[0:64, :], "PX"), eng="act")
            M1T = lambda q, h: (MS[:, q, h, 0, :], "MS")
            M2T = lambda q, h: (MS[:, q, h, 1, :], "MS")
            if phase < 5.4:
                continue
            for q, h in QH:
                o0 = (q * 2 + h) * 64
                b.mm((PS0[0:64, o0:o0 + 64], PS0a), M1T(q, h), TMv(1, q, h))
                b.mm((PS0[0:64, 256 + o0:256 + o0 + 64], PS0b), M1T(q, h), SAv(1, q, h))
            for q, h in QH:
                c_ = cp * 2 + q
                o0 = (q * 2 + h) * 64
                b.stt((GS[:, q, h, :], "GS"), ident[0:64, 0:64], (WCh[:, h, c_:c_ + 1], "WCh"), (PS0[0:64, o0:o0 + 64], PS0a), ALU.mult, ALU.add)
            b.tt((PSm[:, :, :, :], "PSm"), (PS0[0:64, 256:512].rearrange("p (q h s) -> p q h s", q=2, h=2), PS0b),
                 (RFh[:, :, tok].rearrange("p h (q s) -> p q h s", q=2), "RFh"), ALU.add)
            if phase < 5.6:
                continue
            for q in range(2):
                ocol = slice(q * 64, q * 64 + 64)
                stn = "STT%d" % st_i
                for h in range(2):
                    if 5.8 <= phase < 5.9:
                        continue
                    pr = slice(64 * h, 64 * h + 64)
                    ob = (PO[pr, ocol], ("PO", 0)) if h == 0 else (PX[pr, ocol], "PX")
                    b.mm(ob, M2T(q, h), SAv(1, q, h), start=True, stop=False)
                    b.mm(ob, TMv(3, q, h), SAv(3, q, h), start=False, stop=False)
                    b.mm(ob, (STT[st_i][:, h, :], stn), (PSm[:, q, h, :], "PSm"), start=False, stop=True)
                for h in range(2):
                    if phase == 5.7:
                        continue
                    so = slice(h * 64, h * 64 + 64)
                    b.mm((PS1[0:64, so], PS1a), TMv(1, q, h), M2T(q, h), start=True, stop=False)
                    b.mm((PS1[0:64, so], PS1a), TMv(2, q, h), TMv(3, q, h), start=False, stop=False)
                    b.mm((PS1[0:64, so], PS1a), (GS[:, q, h, :], "GS"), (STT[st_i][:, h, :], stn), start=False, stop=True)
                b.copy((STT[1 - st_i][:, :, :].rearrange("p h s -> p (h s)"), "STT%d" % (1 - st_i)), (PS1[0:64, 0:128], PS1a), eng="dve")
                st_i = 1 - st_i
            b.copy((OS[0:64, tok], ("OS", cp)), (PO[0:64, 0:128], ("PO", 0)), eng="dve")
            b.copy((OS[64:128, tok], ("OS", cp)), (PX[64:128, 0:128], "PX"), eng="dve")
            if pipe_next:
                emit_tr(s + 1, cp)
                if cp == 3:
                    emit_B(s + 1)
        if phase < 6:
            continue
        OSK = KL([("OS", i) for i in range(4)])
        b.mm(PS0, bones, (OS[:, :], OSK))
        cen = t[23]
        b.stt(cen, PS0, -1.0 / 64, (OS[:, :], OSK), ALU.mult, ALU.add)
        sq = t[24]
        b.act(sq, cen, AF.Square)
        b.mm(PS1, bones, sq)
        b.act(sq, PS1, AF.Sqrt, scale=1.0 / 64, bias=64e-5)
        b.recip(sq, sq)
        b.tt(cen, cen, sq, ALU.mult)
        b.ts(cen, cen, col(18), col(19), op0=ALU.mult, op1=ALU.add)
        b.tt(cen, cen, bonus, ALU.add)
        if fused is None:
            yrw = t[25 + (s % 2)]
            b.tt(yrw, cen, gv, ALU.mult)
            b.store(yT[128:256, s * SEG:(s + 1) * SEG], yrw)
        else:
            b.tt((ybf[:, 1, :], ("ybf", 1)), cen, gv, ALU.mult)
            for j in range(4):
                pt_ = p1t[j % 2]
                for n in range(2):
                    for kc in range(2):
                        b.mm(PP, (ybf[:, kc, j * 128:(j + 1) * 128], ("ybf", kc)), (wop[:, kc, n * 512:(n + 1) * 512], ("wop", kc)),
                             start=(kc == 0), stop=(kc == 1))
                    b.copy((pt_[:, n * 512:(n + 1) * 512], (pt_.tensor.name, n)), PP, eng=("act" if n == 0 else "dve"))
                b.store(fused["rs_in"][s * SEG + j * 128:s * SEG + (j + 1) * 128, :], (pt_, KL([(pt_.tensor.name, 0), (pt_.tensor.name, 1)])))
    b.finish(sem_stack=(fused or {}).get("sem_stack"))
    return nc


def _consts_l1():
    p = np.arange(128)
    ident = np.eye(128, dtype=np.float32)
    bones = (p[:, None] // 64 == p[None, :] // 64).astype(np.float32)
    s_ = (p % 64)[:, None]
    t_ = np.arange(64)[None, :]
    lt = (s_ < t_).astype(np.float32)
    le = (s_ <= t_).astype(np.float32)
    gt = (s_ > t_).astype(np.float32)
    eq = (s_ == t_).astype(np.float32)
    maska = np.concatenate([lt, lt, le, le, lt, lt, le, le], axis=1)
    maskb = np.concatenate([gt, gt, gt, gt], axis=1)
    id2 = np.concatenate([eq, eq], axis=1)
    rmask = np.ones((128, 512), np.float32)
    rmask[:, ::64] = 0.0
    return dict(ident=ident, bones=bones, maska=np.ascontiguousarray(maska), maskb=np.ascontiguousarray(maskb),
                id2=np.ascontiguousarray(id2), rmask=rmask)


def _blockdiag2(w2):
    o = np.zeros((128, 128), np.float32)
    o[0:64, 0:64] = w2[0]
    o[64:128, 64:128] = w2[1]
    return o


def l1_inputs(inp, bi, g):
    f = lambda k: np.asarray(inp[k][0], np.float32)
    ls = slice(g * 128, (g + 1) * 128)
    W = f("e_w_in")
    rw0 = 1024
    cols = np.concatenate([np.arange(512)[ls], 512 + np.arange(512)[ls], rw0 + np.arange(512)[ls], rw0 + 512 + np.arange(512)[ls],
                           rw0 + 1024 + np.arange(512)[ls], rw0 + 1536 + np.arange(128), rw0 + 1664 + np.arange(128)])
    mu = f("e_shift_mu")
    cvec = np.zeros((128, NV1), np.float32)
    cvec[:, 0:4] = f("e_conv_w")[:, ls].T
    cvec[:, 4] = f("e_conv_b")[ls]
    cvec[:, 5] = f("e_gate_a_b")[ls]
    cvec[:, 6] = f("e_gate_x_b")[ls]
    cvec[:, 7] = f("e_lru_lambda")[ls]
    cvec[:, 8] = mu[0:512][ls]
    cvec[:, 9] = mu[512:1024][ls]
    cvec[:, 10] = mu[1024:1536][ls]
    cvec[:, 11] = mu[1536:1664]
    cvec[:, 12] = mu[1664:1792]
    cvec[:, 13] = f("e_w0")[ls]
    cvec[:, 14] = f("e_a0")[ls]
    cvec[:, 15] = f("e_k_k")[ls]
    cvec[:, 16] = f("e_k_a")[ls]
    cvec[:, 17] = f("e_r_k").reshape(-1)[ls]
    cvec[:, 18] = f("e_gn_w")[ls]
    cvec[:, 19] = f("e_gn_b")[ls]
    d = dict(
        x=np.ascontiguousarray(inp["x"][bi]),
        w_in=np.ascontiguousarray(W[:, cols]),
        gmix=np.ascontiguousarray(f("e_ln_mix").reshape(8, 128).T),
        cvec=cvec,
        wa=_blockdiag2(f("e_gate_a_w")[2 * g:2 * g + 2]),
        wx=_blockdiag2(f("e_gate_x_w")[2 * g:2 * g + 2]),
        w2a2=np.ascontiguousarray(np.concatenate([f("e_w2")[:, ls], f("e_a2")[:, ls]], axis=0)),
        g2=np.ascontiguousarray(f("e_g2")[:, ls]),
    )
    d.update(_consts_l1())
    return d


def build_ffn(NT=2048, F=2816, E=1, G=512, moe=False, ngroups=None, phase=99, nc=None, pre="", fused=None):
    if nc is None:
        nc = bass.Bass("TRN2", target_bir_lowering=False)
    dr = lambda n, s, dt=F32, kind="ExternalInput": nc.dram_tensor(pre + n, s, dt, kind=kind).ap()
    nF = F // 128
    TG = G // 128
    if fused is None or fused.get("xres") is None:
        xres = dr("xres", [NT, 1024])
    else:
        xres = fused["xres"]
    if fused is None:
        aT_d = dr("aT", [1024, NT])
        wproj = dr("wproj", [1024, 1024])
    gain_d = dr("gain", [1, 1024])
    wg_d = dr("wg", [E, 1024, F])
    wu_d = dr("wu", [E, 1024, F])
    wd_d = dr("wd", [E, F, 1024])
    ident_d = dr("ident", [128, 128])
    if moe:
        router_d = dr("router", [1024, 8])
    if fused is None or fused.get("out") is None:
        out = dr("out", [NT, 1024], kind="ExternalOutput")
    else:
        out = fused["out"]

    b = B(nc, pre)
    sb, ps = b.sb, b.ps
    if fused is None:
        wo = sb("wo", [128, 8, 1024], BF16)
    gbc = sb("gbc", [128, 1024])
    ident = sb("ident_s", [128, 128])
    identb = sb("identb", [128, 128], BF16)
    b.load(gbc, gain_d.partition_broadcast(128), dkey="const", grp=True)
    b.load(ident, ident_d, dkey="const", grp=True)
    b.load(identb, ident_d, q="pool", dkey="constp", grp=True)
    if fused is None:
        wpv = wproj.rearrange("(kc p) n -> p kc n", p=128)
        for kc in range(8):
            b.load((wo[:, kc, :], ("wo", kc)), wpv[:, kc, :], q="pool")
    WOK = lambda kc: ("wo", kc)
    if moe:
        rt = sb("rt", [128, 8, 8])
        b.load(rt, router_d.rearrange("(kc p) e -> p kc e", p=128), dkey="const", grp=True)
        hT32 = sb("hT32", [128, 8, 128])
        lg = sb("lg", [128, 8]); lg2 = sb("lg2", [128, 8]); mk1 = sb("mk1", [128, 8]); mk2 = sb("mk2", [128, 8])
        sm = sb("sm", [128, 8])
        comb = sb("comb", [128, TG, 8])
        xs32 = sb("xs32", [128, 1024])
        PTa = ps("PTa", [128, 4, 128]); PTb = ps("PTb", [128, 4, 128])
    else:
        xs = sb("xs", [128, 1024], BF16)
        PT = ps("PT", [128, 8, 128], BF16)
    NXB = 2 if G <= 512 else 1
    xt = [sb("xt%d" % i, [128, 1024]) for i in range(NXB)]
    if fused is None:
        at = [sb("at%d" % i, [128, 8, 128], BF16) for i in range(NXB)]
    else:
        at = [sb("at%d" % i, [128, 1024]) for i in range(NXB)]
    acc = [sb("acc%d" % i, [128, 1024]) for i in range(TG)]
    junk = sb("junk", [128, 1024], BF16)
    ss = sb("ss", [128, 1]); rstd = sb("rstd", [128, 1])
    hT = sb("hT", [128, 8, G], BF16)
    hid = sb("hid", [128, nF, G], BF16)
    NWB = 3
    wgb = [sb("wgb%d" % i, [128, 8, 128], BF16) for i in range(NWB)]
    wub = [sb("wub%d" % i, [128, 8, 128], BF16) for i in range(NWB)]
    NS = 2 if G <= 512 else 4
    WN = 1024 // NS
    NH = G // 512
    wdh = [sb("wdh%d" % i, [128, nF, WN], BF16) for i in range(2)]
    sg = [sb("sg%d" % i, [128, 512]) for i in range(2)]
    PP = [ps("PP%d" % i, [128, 512]) for i in range(2)]
    PG = [ps("PG%d" % i, [128, 512]) for i in range(2)]
    PU = [ps("PU%d" % i, [128, 512]) for i in range(2)]
    if fused is None:
        aTv = aT_d.rearrange("(kc p) t -> p kc t", p=128)
    wi = 0
    di = 0
    for g in range(ngroups if ngroups is not None else NT // G):
        for j in range(TG):
            tok0 = g * G + j * 128
            x_ = xt[j % NXB]
            a_ = at[j % NXB]
            b.load(x_, xres(tok0) if callable(xres) else xres[tok0:tok0 + 128, :])
            if fused is None:
                b.load(a_, aTv[:, :, tok0:tok0 + 128], q="pool")
                for n in range(2):
                    for kc in range(8):
                        b.mm(PP[n], a_[:, kc, :], (wo[:, kc, n * 512:(n + 1) * 512], WOK(kc)), start=(kc == 0), stop=(kc == 7))
                    b.tt((acc[j][:, n * 512:(n + 1) * 512], ("acc", j, n)), PP[n], x_[:, n * 512:(n + 1) * 512], ALU.add)
            else:
                b.load(a_, fused["add"][tok0:tok0 + 128, :])
                for n in range(2):
                    b.tt((acc[j][:, n * 512:(n + 1) * 512], ("acc", j, n)), a_[:, n * 512:(n + 1) * 512], x_[:, n * 512:(n + 1) * 512], ALU.add)
            ACCK = KL([("acc", j, 0), ("acc", j, 1)])
            b.act(junk, (acc[j], ACCK), AF.Square, accum=ss)
            b.act(rstd, ss, AF.Sqrt, scale=1.0 / 1024, bias=1e-6)
            b.recip(rstd, rstd)
            if not moe:
                b.stt(xs, (acc[j], ACCK), rstd, gbc, ALU.mult, ALU.mult)
                for kc in range(8):
                    b.tr((PT[:, kc, :], ("PT", kc)), xs[:, kc * 128:(kc + 1) * 128], identb)
                b.copy((hT[:, :, j * 128:(j + 1) * 128], ("hT", j)), (PT, KL([("PT", kc) for kc in range(8)])), eng="act")
            else:
                b.stt(xs32, (acc[j], ACCK), rstd, gbc, ALU.mult, ALU.mult)
                for kc in range(8):
                    pt_ = PTa if kc < 4 else PTb
                    b.tr((pt_[:, kc % 4, :], (pt_.tensor.name, kc % 4)), xs32[:, kc * 128:(kc + 1) * 128], ident)
                for kc in range(8):
                    pt_ = PTa if kc < 4 else PTb
                    pass
                b.copy((hT32[:, 0:4, :], ("hT32", 0)), (PTa, KL([(PTa.tensor.name, i) for i in range(4)])), eng="act")
                b.copy((hT32[:, 4:8, :], ("hT32", 1)), (PTb, KL([(PTb.tensor.name, i) for i in range(4)])), eng="dve")
                H32 = KL([("hT32", 0), ("hT32", 1)])
                b.copy((hT[:, :, j * 128:(j + 1) * 128], ("hT", j)), (hT32, H32), eng="act")
                for kc in range(8):
                    b.mm(PP[0][:, 0:8], (hT32[:, kc, :], ("hT32", kc // 4)), rt[:, kc, :], start=(kc == 0), stop=(kc == 7))
                b.copy(lg, PP[0][:, 0:8])
                b.P.add("dve", (lambda o, i: (lambda e: e.reduce_max(out=o, in_=i, axis=AX.X)))(sm[:, 0:1], lg), reads=[lg.tensor.name], writes=[sm.tensor.name])
                b.ts(mk1, lg, sm[:, 0:1], None, op0=ALU.is_equal)
                b.stt(lg2, mk1, -1e30, lg, ALU.mult, ALU.add)
                b.P.add("dve", (lambda o, i: (lambda e: e.reduce_max(out=o, in_=i, axis=AX.X)))(sm[:, 1:2], lg2), reads=[lg2.tensor.name], writes=[sm.tensor.name])
                b.ts(mk2, lg2, sm[:, 1:2], None, op0=ALU.is_equal)
                b.ts(sm[:, 2:3], sm[:, 0:1], -1.0, None, op0=ALU.mult)
                b.act(sm[:, 3:4], sm[:, 1:2], AF.Exp, bias=sm[:, 2:3])
                b.ts(sm[:, 4:5], sm[:, 3:4], 1.0, None, op0=ALU.add)
                b.recip(sm[:, 4:5], sm[:, 4:5])
                b.tt(sm[:, 5:6], sm[:, 3:4], sm[:, 4:5], ALU.mult)
                b.ts(mk1, mk1, sm[:, 4:5], None, op0=ALU.mult)
                b.stt((comb[:, j, :], ("comb", j)), mk2, sm[:, 5:6], mk1, ALU.mult, ALU.add)
        HT = KL([("hT", j) for j in range(TG)])
        for e in range(E if phase >= 2 else 0):
            wgv = wg_d[e].rearrange("(kc p) f -> p kc f", p=128)
            wuv = wu_d[e].rearrange("(kc p) f -> p kc f", p=128)
            wdv = wd_d[e].rearrange("(fc p) n -> p fc n", p=128)
            for fc in range(nF):
                w1, w2 = wgb[wi % NWB], wub[wi % NWB]
                b.load(w1, wgv[:, :, fc * 128:(fc + 1) * 128], q="pool")
                b.load(w2, wuv[:, :, fc * 128:(fc + 1) * 128], q="pool")
                for hh in range(NH):
                    bi_ = (wi * NH + hh) % 2
                    pg, pu, sg_ = PG[bi_], PU[bi_], sg[bi_]
                    hs = slice(hh * 512, (hh + 1) * 512)
                    for kc in range(8):
                        b.mm(pg, w1[:, kc, :], (hT[:, kc, hs], HT), start=(kc == 0), stop=(kc == 7))
                    for kc in range(8):
                        b.mm(pu, w2[:, kc, :], (hT[:, kc, hs], HT), start=(kc == 0), stop=(kc == 7))
                    b.act(sg_, pg, AF.Silu)
                    b.tt((hid[:, fc, hs], ("hid", fc)), sg_, pu, ALU.mult)
                wi += 1
            HID = KL([("hid", fc) for fc in range(nF)])
            for n in range(NS if phase >= 3 else 0):
                wd_ = wdh[di % 2]
                di += 1
                for q4 in range(4):
                    f0, f1 = (nF * q4) // 4, (nF * (q4 + 1)) // 4
                    b.load((wd_[:, f0:f1, :], (wd_.tensor.name, q4)), wdv[:, f0:f1, n * WN:(n + 1) * WN], q="pool", dkey=wd_.tensor.name)
                WDK = KL([(wd_.tensor.name, q4) for q4 in range(4)])
                for j in range(TG):
                    pp = PP[j % 2]
                    for fc in range(nF):
                        b.mm(pp[:, 0:WN], (hid[:, fc, j * 128:(j + 1) * 128], HID), (wd_[:, fc, :], WDK), start=(fc == 0), stop=(fc == nF - 1))
                    av = (acc[j][:, n * WN:(n + 1) * WN], ("acc", j, (n * WN) // 512))
                    if moe:
                        b.stt(av, pp[:, 0:WN], (comb[:, j, e:e + 1], ("comb", j)), av, ALU.mult, ALU.add)
                    else:
                        b.tt(av, pp[:, 0:WN], av, ALU.add)
        for j in range(TG):
            tok0 = g * G + j * 128
            b.store(out(tok0) if callable(out) else out[tok0:tok0 + 128, :], (acc[j], KL([("acc", j, 0), ("acc", j, 1)])))
    b.finish(sem_stack=(fused or {}).get("sem_stack"))
    return nc


LAMBDA_INIT1 = 0.8 - 0.6 * float(np.exp(-0.3 * 1))
TWO_PI = 6.283185307179586
CW1 = 6.28125
CW2 = TWO_PI - 6.28125
MAGIC = 12582912.0


def build_l3(nblk=T_SEQ // 512, phase=99, nc=None, pre="", fused=None):
    if nc is None:
        nc = bass.Bass("TRN2", target_bir_lowering=False)
    dr = lambda n, s, dt=F32, kind="ExternalInput": nc.dram_tensor(pre + n, s, dt, kind=kind).ap()
    if fused is None:
        x = dr("x", [T_SEQ, 1024])
    else:
        x = fused["x"]
        wop_d = dr("wo_part", [256, 1024])
    pos_d = dr("pos", [1, T_SEQ], I32)
    w_d = dr("w", [1024, 768])
    gmix = dr("gmix", [128, 8])
    cvec = dr("cvec", [128, 4])
    lams = dr("lams", [1, 256])
    ident_d = dr("ident", [128, 128])
    bones_d = dr("bones", [128, 128])
    rot_d = dr("rot", [128, 128])
    cmask_d = dr("cmask", [128, 4 * 512])
    if fused is None:
        oT = dr("oT", [256, T_SEQ], kind="ExternalOutput")

    b = B(nc, pre)
    sb, ps = b.sb, b.ps
    wb = sb("wb", [128, 8, 768], BF16)
    if fused is not None:
        wop = sb("wop", [128, 2, 1024], BF16)
        onb = [sb("onb%d" % i, [128, 512], BF16) for i in range(2)]
        p1t = [sb("p1t%d" % i, [128, 1024]) for i in range(2)]
        for kc in range(2):
            b.load((wop[:, kc, :], ("wop", kc)), wop_d[kc * 128:(kc + 1) * 128, :], q="pool", dkey="constp", grp=True)
    wst = [sb("wst%d" % i, [128, 768]) for i in range(2)]
    gm = sb("gm", [128, 8]); cv = sb("cv", [128, 8])
    ident = sb("ident_s", [128, 128]); identb = sb("identb", [128, 128], BF16)
    bones = sb("bones_s", [128, 128]); rot = sb("rot_s", [128, 128])
    ones_f = sb("ones_f", [128, 128]); ones_b = sb("ones_b", [128, 128], BF16)
    cmask = sb("cmask_s", [128, 4, 512], BF16)
    lm = sb("lm", [128, 256]); lmp = sb("lmp", [128, 128]); lsc = sb("lsc", [128, 8])
    for t_, d_ in ((gm, gmix), (cv[:, 0:4], cvec), (ident, ident_d), (bones, bones_d), (rot, rot_d), (lm, lams.partition_broadcast(128))):
        b.load(t_, d_, dkey="const", grp=True)
    b.load(identb, ident_d, q="pool", dkey="constp", grp=True)
    b.load((cmask[:, :, :].rearrange("p a b -> p (a b)"), "cmask_s"), cmask_d, q="pool", dkey="constp", grp=True)
    b.memset(ones_f, 1.0)
    b.memset(ones_b, 1.0)
    for kc in range(8):
        b.load(wst[kc % 2], w_d[kc * 128:(kc + 1) * 128, :])
        b.ts((wb[:, kc, :], ("wb", kc)), wst[kc % 2], gm[:, kc:kc + 1], None, op0=ALU.mult)
    WBK = lambda kc: ("wb", kc)
    col = lambda i: cv[:, i:i + 1]
    b.tt((lmp[:, 0:64], "lmp"), lm[:, 0:64], lm[:, 64:128], ALU.mult)
    b.tt((lmp[:, 64:128], "lmp"), lm[:, 128:192], lm[:, 192:256], ALU.mult)
    b.P.add("dve", lambda e: e.reduce_sum(out=lsc[:, 0:1], in_=lmp[:, 0:64], axis=AX.X), reads=["lmp"], writes=[lsc.tensor.name])
    b.P.add("dve", lambda e: e.reduce_sum(out=lsc[:, 1:2], in_=lmp[:, 64:128], axis=AX.X), reads=["lmp"], writes=[lsc.tensor.name])
    b.act(lsc[:, 0:2], lsc[:, 0:2], AF.Exp)
    b.tt(lsc[:, 2:3], lsc[:, 0:1], lsc[:, 1:2], ALU.subtract)
    b.ts(lsc[:, 3:4], lsc[:, 2:3], LAMBDA_INIT1, -1.0, op0=ALU.add, op1=ALU.mult)
    b.ts(col(4), col(2), 1.0 - LAMBDA_INIT1, None, op0=ALU.mult)
    NEGLAM = lsc[:, 3:4]

    TA = max(nblk, 1) * 512
    QT = [sb("QT%d" % h, [128, TA], BF16) for h in range(2)]
    KT = [sb("KT%d" % h, [128, TA], BF16) for h in range(2)]
    VS = [sb("VS%d" % h, [128, TA // 128, 128], BF16) for h in range(2)]
    xt = [sb("xt%d" % i, [128, 1024]) for i in range(2)]
    xs = [sb("xs%d" % i, [128, 1024], BF16) for i in range(2)]
    junk = sb("junk", [128, 1024], BF16)
    ss = sb("ss", [128, 1]); rstd = sb("rstd", [128, 1])
    xnT = sb("xnT", [128, 8, 512], BF16)
    posi = sb("posi", [128, 512], I32)
    tmp = [sb("t%d" % i, [128, 512]) for i in range(8)]
    Eb = [sb("Eb%d" % i, [128, 512], BF16) for i in range(4)]
    KX = [sb("KX%d" % i, [128, 128], BF16) for i in range(6)]
    PSB = [ps("PSB%d" % i, [128, 512]) for i in range(4)]
    PVA = [ps("PVA%d" % i, [128, 512]) for i in range(2)]
    PSM = [ps("PSM%d" % i, [128, 512]) for i in range(2)]
    PSX = PSM[0]
    PTB = PSB[3].bitcast(BF16).rearrange("p (a b) -> p a b", a=8)
    for s in range(nblk):
        for j in range(4):
            tix = (s * 4 + j) % 2
            t0_ = s * 512 + j * 128
            b.load(xt[tix], x(t0_) if callable(x) else x[t0_:t0_ + 128, :])
            rms_tile_T(b, xt[tix], xs[tix], ss, rstd, PTB, (xnT[:, :, j * 128:(j + 1) * 128], ("xnT", j)), identb, junk)
        XN = KL([("xnT", j) for j in range(4)])
        if phase < 0.4:
            continue
        b.load(posi, pos_d[:, s * 512:(s + 1) * 512].partition_broadcast(128))
        ang = tmp[2]
        b.copy(ang, posi)
        b.ts(ang, ang, col(3), None, op0=ALU.mult)
        for which, shift in ((0, 0.0), (1, 1.5707963267948966)):
            a_ = tmp[3]
            kf = tmp[4]
            if shift:
                b.ts(a_, ang, shift, None, op0=ALU.add)
            else:
                a_ = ang
            b.ts(kf, a_, 1.0 / TWO_PI, MAGIC, op0=ALU.mult, op1=ALU.add)
            b.ts(kf, kf, -MAGIC, None, op0=ALU.add)
            r_ = tmp[5]
            b.stt(r_, kf, -CW1, a_, ALU.mult, ALU.add)
            b.stt(r_, kf, -CW2, r_, ALU.mult, ALU.add)
            b.ts(r_, r_, 3.1415925, -3.1415925, op0=ALU.min, op1=ALU.max)
            b.act(tmp[which], r_, AF.Sin)
        SIN, COS = tmp[0], tmp[1]
        if phase < 0.6:
            continue
        for cc in range(4):
            pp = PSB[cc % 2]
            for kc in range(8):
                b.mm(pp, (wb[:, kc, cc * 128:(cc + 1) * 128], WBK(kc)), (xnT[:, kc, :], XN), start=(kc == 0), stop=(kc == 7))
            sq = tmp[2]
            b.act(sq, pp, AF.Square)
            b.mm(PSX, bones, sq)
            b.act(sq, PSX, AF.Sqrt, scale=1.0 / 64, bias=1e-6)
            b.recip(sq, sq)
            qn = tmp[3]
            b.stt(qn, pp, col(0 if cc < 2 else 1), sq, ALU.mult, ALU.mult)
            if phase < 0.7:
                continue
            b.mm(PVA[0], rot, qn)
            t1 = tmp[4]
            b.tt(t1, qn, COS, ALU.mult)
            t2 = tmp[5]
            b.tt(t2, PVA[0], SIN, ALU.mult)
            if phase < 0.75:
                continue
            dst = (QT if cc < 2 else KT)[cc % 2]
            b.stt((dst[:, s * 512:(s + 1) * 512], (dst.tensor.name, s)), t1, 1.0, t2, ALU.mult, ALU.add)
        if phase < 0.8:
            continue
        for j in range(4):
            pp = PSB[j % 2]
            for kc in range(8):
                b.mm(pp[:, 0:256], (xnT[:, kc, j * 128:(j + 1) * 128], XN), (wb[:, kc, 512:768], WBK(kc)), start=(kc == 0), stop=(kc == 7))
            for h in range(2):
                b.copy((VS[h][:, s * 4 + j, :], (VS[h].tensor.name, s)), pp[:, h * 128:(h + 1) * 128], eng="act")
    if phase < 2:
        nb2 = 0
    else:
        nb2 = nblk
    scale = 64 ** -0.5
    for i in range(nb2):
        for h in range(2):
            qs = slice(i * 512, (i + 1) * 512)
            QK = (QT[h].tensor.name, i)
            nkb = 4 * i + 4
            steps = [(kb, c) for kb in range(nkb) for c in range(2)]
            nst = len(steps)

            def emit_s(t):
                kb, c = steps[t]
                kx = KX[t % 6]
                b.ts(kx, (KT[h][:, kb * 128:(kb + 1) * 128], (KT[h].tensor.name, kb // 4)), bones[:, 64 * c:64 * c + 1], None, op0=ALU.mult)
                b.mm(PSB[t % 4], kx, (QT[h][:, qs], QK))

            def emit_e(t):
                kb, c = steps[t]
                b.act(Eb[t % 4], PSB[t % 4], AF.Exp, scale=scale)
                if kb >= 4 * i:
                    b.tt(Eb[t % 4], Eb[t % 4], (cmask[:, kb - 4 * i, :], "cmask_s"), ALU.mult)

            def emit_pv(t):
                kb, c = steps[t]
                b.mm(PVA[c], (VS[h][:, kb, :], (VS[h].tensor.name, kb // 4)), Eb[t % 4], start=(kb == 0), stop=(kb == nkb - 1))
                b.mm(PSM[c], ones_b, Eb[t % 4], start=(kb == 0), stop=(kb == nkb - 1))
            LA = 3
            for t in range(min(LA, nst)):
                emit_s(t)
            for t in range(nst):
                emit_e(t)
                if t + LA < nst:
                    emit_s(t + LA)
                emit_pv(t)
            r0, r1, o_ = tmp[0], tmp[1], tmp[2]
            b.recip(r0, PSM[0])
            b.tt(r0, PVA[0], r0, ALU.mult)
            b.recip(r1, PSM[1])
            b.tt(r1, PVA[1], r1, ALU.mult)
            b.stt(o_, r1, NEGLAM, r0, ALU.mult, ALU.add)
            sq = tmp[3]
            b.act(sq, o_, AF.Square)
            b.mm(PVA[0], ones_f, sq)
            b.act(sq, PVA[0], AF.Sqrt, scale=1.0 / 128, bias=1e-6)
            b.recip(sq, sq)
            if fused is None:
                on = tmp[4 + ((i * 2 + h) % 2)]
                b.stt(on, o_, col(4), sq, ALU.mult, ALU.mult)
                b.store(oT[h * 128:(h + 1) * 128, qs], on)
            else:
                b.stt(onb[h], o_, col(4), sq, ALU.mult, ALU.mult)
        if fused is not None:
            for j in range(4):
                pt_ = p1t[j % 2]
                for n in range(2):
                    for h in range(2):
                        b.mm(PVA[1], onb[h][:, j * 128:(j + 1) * 128], (wop[:, h, n * 512:(n + 1) * 512], ("wop", h)), start=(h == 0), stop=(h == 1))
                    b.copy((pt_[:, n * 512:(n + 1) * 512], (pt_.tensor.name, n)), PVA[1], eng=("act" if n == 0 else "dve"))
                b.store(fused["rs_in"][i * 512 + j * 128:i * 512 + (j + 1) * 128, :], (pt_, KL([(pt_.tensor.name, 0), (pt_.tensor.name, 1)])))
    b.finish(sem_stack=(fused or {}).get("sem_stack"))
    return nc


def _consts_l3():
    p = np.arange(128)
    bones = (p[:, None] // 64 == p[None, :] // 64).astype(np.float32)
    rot = np.zeros((128, 128), np.float32)
    for d in range(128):
        dm = d % 64
        if dm < 8:
            rot[d + 8, d] = -1.0
        elif dm < 16:
            rot[d - 8, d] = 1.0
    invf = np.zeros((128,), np.float32)
    for q in range(128):
        if q % 64 < 16:
            invf[q] = np.float32(500000.0) ** np.float32(-(2 * (q % 8)) / 16.0)
    kp = np.arange(128)[:, None]
    qc = np.arange(512)[None, :]
    cm = np.concatenate([((128 * j + kp) <= qc).astype(np.float32) for j in range(4)], axis=1)
    return dict(ident=np.eye(128, dtype=np.float32), bones=bones, rot=rot, cmask=np.ascontiguousarray(cm)), invf


def l3_inputs(inp, x1, bi, g):
    f = lambda k: np.asarray(inp[k][0], np.float32)
    W = f("o_w_qkv")
    hs = [2 * g, 2 * g + 1]
    cols = np.concatenate([np.arange(h * 128, (h + 1) * 128) for h in hs] + [1024 + np.arange(h * 128, (h + 1) * 128) for h in hs]
                          + [2048 + np.arange(h * 128, (h + 1) * 128) for h in hs])
    consts, invf = _consts_l3()
    cvec = np.zeros((128, 4), np.float32)
    cvec[:, 0] = np.tile(f("o_q_norm"), 2)
    cvec[:, 1] = np.tile(f("o_k_norm"), 2)
    cvec[:, 2] = f("o_subln")
    cvec[:, 3] = invf
    d = dict(x=(None if x1 is None else np.ascontiguousarray(x1[bi])), pos=np.ascontiguousarray(inp["positions"][bi].reshape(1, -1).astype(np.int32)),
             w=np.ascontiguousarray(W[:, cols]), gmix=np.ascontiguousarray(f("o_ln_mix").reshape(8, 128).T), cvec=cvec,
             lams=np.concatenate([f("o_lambda_q1"), f("o_lambda_k1"), f("o_lambda_q2"), f("o_lambda_k2")]).reshape(1, 256))
    d.update(consts)
    return d


N_CORES = 8
CC_GROUPS = [[0, 1, 2, 3], [4, 5, 6, 7]]


_CC_STATE = {}


def _cc_block(nc, kind, src, dst, name, sem_stack):
    st = _CC_STATE.setdefault(id(nc), {})
    if "sem" not in st:
        st["sem"] = sem_stack.enter_context(nc.semaphore("cc_sem"))
        st["n"] = 0
    sem = st["sem"]
    st["n"] += 1
    cnt = st["n"]
    with nc.Block() as block:
        @block.gpsimd
        def _(g):
            g.collective_compute(kind, ALU.bypass if kind == "AllGather" else ALU.add, replica_groups=CC_GROUPS,
                                 ins=[src.ap().opt()], outs=[dst.ap().opt()]).then_inc(sem)
            g.wait_ge(sem, cnt)


N_EXPERTS_ = 8
AG_CH = 256


def _cc_multi(nc, kind, pairs, sem_stack):
    st = _CC_STATE.setdefault(id(nc), {})
    if "sem" not in st:
        st["sem"] = sem_stack.enter_context(nc.semaphore("cc_sem"))
        st["n"] = 0
    sem = st["sem"]
    with nc.Block() as block:
        @block.gpsimd
        def _(g):
            for src, dst in pairs:
                st["n"] += 1
                g.collective_compute(kind, ALU.bypass if kind == "AllGather" else ALU.add, replica_groups=CC_GROUPS,
                                     ins=[src.ap().opt()], outs=[dst.ap().opt()]).then_inc(sem)
            g.wait_ge(sem, st["n"])


def build_fused():
    nc = bass.Bass("TRN2", target_bir_lowering=False)
    NQ = T_SEQ // 4
    nch = NQ // AG_CH
    rs1_in = nc.dram_tensor("rs1_in", [T_SEQ, 1024], F32)
    rs1_out = nc.dram_tensor("rs1_out", [NQ, 1024], F32)
    ag_in = [nc.dram_tensor("ag_in%d" % k, [AG_CH, 1024], F32) for k in range(nch)]
    ag_out = [nc.dram_tensor("ag_out%d" % k, [4 * AG_CH, 1024], F32) for k in range(nch)]
    rs2_in = nc.dram_tensor("rs2_in", [T_SEQ, 1024], F32)
    rs2_out = nc.dram_tensor("rs2_out", [NQ, 1024], F32)

    def q_tile(tok0):
        return ag_in[tok0 // AG_CH].ap()[tok0 % AG_CH:tok0 % AG_CH + 128, :]

    def full_tile(t0):
        r, w = t0 // NQ, t0 % NQ
        k, i = w // AG_CH, w % AG_CH
        return ag_out[k].ap()[r * AG_CH + i:r * AG_CH + i + 128, :]

    with contextlib.ExitStack() as ss:
        build_l1(nc=nc, pre="a__", fused=dict(rs_in=rs1_in.ap(), sem_stack=ss))
        _cc_block(nc, "ReduceScatter", rs1_in, rs1_out, "cc1", ss)
        build_ffn(NT=2048, F=2816, E=1, G=512, moe=False, nc=nc, pre="b__", fused=dict(add=rs1_out.ap(), xres=None, out=q_tile, sem_stack=ss))
        _cc_multi(nc, "AllGather", [(ag_in[k], ag_out[k]) for k in range(nch)], ss)
        build_l3(nc=nc, pre="c__", fused=dict(x=full_tile, rs_in=rs2_in.ap(), sem_stack=ss))
        _cc_block(nc, "ReduceScatter", rs2_in, rs2_out, "cc3", ss)
        build_ffn(NT=2048, F=3584, E=8, G=1024, moe=True, nc=nc, pre="d__", fused=dict(add=rs2_out.ap(), xres=q_tile, out=None, sem_stack=ss))
    return nc


def fused_inputs(inp, c):
    bi, g = c // 4, c % 4
    f = lambda k: np.asarray(inp[k][0], np.float32)
    d = {}
    l1 = l1_inputs(inp, bi, g)
    wout = f("e_w_out")
    l1["wo_part"] = np.ascontiguousarray(np.concatenate([wout[g * 128:(g + 1) * 128], wout[512 + g * 128:512 + (g + 1) * 128]], axis=0))
    for k, v in l1.items():
        d["a__" + k] = v
    ident = np.eye(128, dtype=np.float32)
    d["b__xres"] = np.ascontiguousarray(np.asarray(inp["x"], np.float32)[bi, g * 2048:(g + 1) * 2048])
    d["b__gain"] = np.ascontiguousarray(f("e_ln_ffn").reshape(1, 1024))
    d["b__wg"] = np.asarray(inp["e_ffn_gate"], np.float32)
    d["b__wu"] = np.asarray(inp["e_ffn_up"], np.float32)
    d["b__wd"] = np.asarray(inp["e_ffn_down"], np.float32)
    d["b__ident"] = ident
    l3 = l3_inputs(inp, None, bi, g)
    del l3["x"]
    l3["wo_part"] = np.ascontiguousarray(f("o_w_o")[g * 256:(g + 1) * 256])
    for k, v in l3.items():
        d["c__" + k] = v
    d["d__gain"] = np.ascontiguousarray(f("o_ln_ffn").reshape(1, 1024))
    sh = c % N_EXPERTS_
    d["d__wg"] = np.roll(np.asarray(inp["o_moe_gate"][0], np.float32), -sh, axis=0)
    d["d__wu"] = np.roll(np.asarray(inp["o_moe_up"][0], np.float32), -sh, axis=0)
    d["d__wd"] = np.roll(np.asarray(inp["o_moe_down"][0], np.float32), -sh, axis=0)
    d["d__router"] = np.ascontiguousarray(np.roll(f("o_router"), -sh, axis=1))
    d["d__ident"] = ident
    return d


def kernel(**inp):
    inp = {k: np.asarray(v) for k, v in inp.items()}
    Bn, T, D = inp["x"].shape
    nc = build_fused()
    res = run_bass_kernel_spmd(nc, [fused_inputs(inp, c) for c in range(N_CORES)], core_ids=list(range(N_CORES)))
    out = np.zeros((Bn, T, D), np.float32)
    for c in range(N_CORES):
        bi, tq = c // 4, c % 4
        out[bi, tq * 2048:(tq + 1) * 2048] = res.results[c]["d__out"]
    return out
```

```python
import contextlib
import numpy as np
import ml_dtypes
import concourse.bass as bass
import concourse.mybir as mybir
from concourse.alu_op_type import AluOpType as ALU
from concourse.bass_utils import run_bass_kernel_spmd

AF = mybir.ActivationFunctionType
F32 = mybir.dt.float32
BF16 = mybir.dt.bfloat16
I32 = mybir.dt.int32
AX = mybir.AxisListType

COMPUTE = ("pe", "act", "dve", "pool")


class _Op:
    __slots__ = ("eng", "fn", "reads", "writes", "kind", "waits", "signal", "val", "dkey", "seq")

    def __init__(self, eng, fn, reads, writes, kind):
        self.eng = eng
        self.fn = fn
        self.reads = tuple(reads)
        self.writes = tuple(writes)
        self.kind = kind
        self.waits = {}
        self.signal = False
        self.val = None
        self.dkey = None


class Prog:
    def __init__(self, nc):
        self.nc = nc
        self.ops = []
        self.last_w = {}
        self.readers = {}
        self.deps = []
        self.group_keys = set()

    def _add(self, op):
        deps = set()
        for k in op.reads:
            w = self.last_w.get(k)
            if w is not None:
                deps.add((w, "raw"))
        for k in op.writes:
            w = self.last_w.get(k)
            if w is not None:
                deps.add((w, "waw"))
            for r in self.readers.get(k, ()):
                if r is not op:
                    deps.add((r, "war"))
        for k in op.reads:
            self.readers.setdefault(k, []).append(op)
        for k in op.writes:
            self.last_w[k] = op
            self.readers[k] = []
        op.seq = len(self.ops)
        self.ops.append(op)
        self.deps.append(deps)
        return op

    def add(self, eng, fn, reads=(), writes=()):
        return self._add(_Op(eng, fn, reads, writes, "c"))

    def dma(self, q, fn, reads=(), writes=(), key=None):
        op = _Op(q, fn, reads, writes, "d")
        op.dkey = key
        return self._add(op)

    def emit(self, final_keys=(), sem_stack=None):
        nc = self.nc
        ops = self.ops
        for op, deps in zip(ops, self.deps):
            for d, kind in deps:
                if d.kind == "c":
                    if d.eng == op.eng and op.kind == "c":
                        if op.eng == "pe" or kind == "war":
                            continue
                    d.signal = True
        finals = [self.last_w[k] for k in final_keys if k in self.last_w]
        for d in finals:
            if d.kind == "c":
                d.signal = True
        cnt = {e: 0 for e in COMPUTE}
        dcnt = {}
        dkeys = []
        for op in ops:
            if op.kind == "c":
                if op.signal:
                    cnt[op.eng] += 1
                    op.val = cnt[op.eng]
            else:
                if op.dkey not in dcnt:
                    dcnt[op.dkey] = 0
                    dkeys.append(op.dkey)
                dcnt[op.dkey] += 16
                op.val = dcnt[op.dkey]
        for op in ops:
            if op.kind == "d" and op.dkey in self.group_keys:
                op.val = dcnt[op.dkey]
        with contextlib.ExitStack() as st_local:
            st = sem_stack if sem_stack is not None else st_local
            tag = "_%d" % len(getattr(st, "_exit_callbacks", ())) if sem_stack is not None else ""
            csem = {e: st.enter_context(nc.semaphore("cs_" + e + tag)) for e in COMPUTE}
            dsem = {k: st.enter_context(nc.semaphore("ds%d%s" % (i, tag))) for i, k in enumerate(dkeys)}

            def semof(d):
                return csem[d.eng] if d.kind == "c" else dsem[d.dkey]

            streams = {}
            waited = {}
            for op, deps in zip(ops, self.deps):
                need = {}
                for d, kind in deps:
                    if d.kind == "c" and d.eng == op.eng and op.kind == "c":
                        if op.eng == "pe" or kind == "war":
                            continue
                    s = semof(d)
                    sid = id(s)
                    if need.get(sid, (None, 0))[1] < d.val:
                        need[sid] = (s, d.val)
                w = waited.setdefault(op.eng, {})
                op.waits = []
                for sid, (s, v) in need.items():
                    if w.get(sid, 0) < v:
                        w[sid] = v
                        op.waits.append((s, v))
                streams.setdefault(op.eng, []).append(op)
            fin_waits = []
            for d in finals:
                fin_waits.append((semof(d), d.val))

            def run_stream(name, engine, extra=None):
                for op in streams.get(name, []):
                    for s, v in op.waits:
                        engine.wait_ge(s, v)
                    ins = op.fn(engine)
                    if op.kind == "c":
                        if op.signal:
                            ins.then_inc(csem[op.eng], 1)
                    else:
                        ins.then_inc(dsem[op.dkey], 16)
                if extra:
                    for s, v in extra:
                        engine.wait_ge(s, v)

            with nc.Block() as block:
                @block.tensor
                def _(e):
                    run_stream("pe", e)

                @block.scalar
                def _(e):
                    run_stream("act", e)

                @block.vector
                def _(e):
                    run_stream("dve", e)

                @block.gpsimd
                def _(e):
                    run_stream("pool", e)

                @block.sync
                def _(e):
                    run_stream("sp", e, extra=fin_waits)


class KL(list):
    pass


KEYMAP = {"PS0": KL([("PS0", 0), ("PS0", 1)]), "PS1": KL([("PS1", 0), ("PS1", 1)])}


def _ak(x):
    if isinstance(x, tuple):
        ap, k = x
        return (ap, k if isinstance(k, KL) else KL([k]))
    n = x.tensor.name
    return (x, KEYMAP.get(n.split("__")[-1]) or KL([n]))


class B:
    def __init__(self, nc, pre=""):
        self.nc = nc
        self.pre = pre
        self.P = Prog(nc)
        self.st = contextlib.ExitStack()
        self.nout = 0

    def sb(self, name, shape, dt=F32):
        return self.st.enter_context(self.nc.sbuf_tensor(self.pre + name, shape, dt))[:]

    def ps(self, name, shape, dt=F32):
        return self.st.enter_context(self.nc.psum_tensor(self.pre + name, shape, dt))[:]

    def mm(self, out, lhsT, rhs, start=True, stop=True):
        (o, ok), (l, lk), (r, rk) = _ak(out), _ak(lhsT), _ak(rhs)
        self.P.add("pe", lambda e: e.matmul(o, l, r, start=start, stop=stop), reads=lk + rk, writes=ok)

    def tr(self, out, in_, ident):
        (o, ok), (i, ik), (d, dk) = _ak(out), _ak(in_), _ak(ident)
        self.P.add("pe", lambda e: e.transpose(o, i, d), reads=ik + dk, writes=ok)

    def act(self, out, in_, func, scale=None, bias=None, accum=None, eng="act"):
        (o, ok), (i, ik) = _ak(out), _ak(in_)
        reads = list(ik)
        writes = list(ok)
        kw = {}
        if scale is not None:
            if isinstance(scale, (int, float)):
                kw["scale"] = float(scale)
            else:
                s, sk = _ak(scale)
                kw["scale"] = s
                reads.extend(sk)
        if bias is not None:
            if isinstance(bias, (int, float)):
                kw["bias"] = float(bias)
            else:
                b_, bk = _ak(bias)
                kw["bias"] = b_
                reads.extend(bk)
        if accum is not None:
            a_, ak_ = _ak(accum)
            kw["accum_out"] = a_
            writes.extend(ak_)
        self.P.add("act", lambda e: e.activation(out=o, in_=i, func=func, **kw), reads=reads, writes=writes)

    def tt(self, out, in0, in1, op, eng="dve"):
        (o, ok), (a, ak_), (b_, bk) = _ak(out), _ak(in0), _ak(in1)
        self.P.add(eng, lambda e: e.tensor_tensor(out=o, in0=a, in1=b_, op=op), reads=ak_ + bk, writes=ok)

    def ts(self, out, in0, s1, s2=None, op0=ALU.mult, op1=None, eng="dve", accum=None):
        (o, ok), (a, ak_) = _ak(out), _ak(in0)
        reads = list(ak_)
        writes = list(ok)

        def sc(s):
            if s is None or isinstance(s, (int, float)):
                return s
            ap, k = _ak(s)
            reads.extend(k)
            return ap
        v1, v2 = sc(s1), sc(s2)
        kw = {}
        if op1 is not None:
            kw["op1"] = op1
        if accum is not None:
            a2, a2k = _ak(accum)
            kw["accum_out"] = a2
            writes.extend(a2k)
        self.P.add(eng, lambda e: e.tensor_scalar(out=o, in0=a, scalar1=v1, scalar2=v2, op0=op0, **kw), reads=reads, writes=writes)

    def stt(self, out, in0, scalar, in1, op0, op1):
        (o, ok), (a, ak_), (b_, bk) = _ak(out), _ak(in0), _ak(in1)
        reads = list(ak_ + bk)
        if isinstance(scalar, (int, float)):
            s = float(scalar)
        else:
            s, sk = _ak(scalar)
            reads.extend(sk)
        self.P.add("dve", lambda e: e.scalar_tensor_tensor(out=o, in0=a, scalar=s, in1=b_, op0=op0, op1=op1), reads=reads, writes=ok)

    def copy(self, out, in_, eng="dve"):
        (o, ok), (i, ik) = _ak(out), _ak(in_)
        if eng == "act":
            self.P.add("act", lambda e: e.activation(out=o, in_=i, func=AF.Copy), reads=ik, writes=ok)
        else:
            self.P.add(eng, lambda e: e.tensor_copy(out=o, in_=i), reads=ik, writes=ok)

    def scan(self, out, d0, d1, init):
        (o, ok), (a, ak_), (b_, bk) = _ak(out), _ak(d0), _ak(d1)
        reads = list(ak_ + bk)
        if isinstance(init, (int, float)):
            iv = float(init)
        else:
            iv, ik = _ak(init)
            reads.extend(ik)
        self.P.add("dve", lambda e: e.tensor_tensor_scan(out=o, data0=a, data1=b_, initial=iv, op0=ALU.mult, op1=ALU.add), reads=reads, writes=ok)

    def recip(self, out, in_):
        (o, ok), (i, ik) = _ak(out), _ak(in_)
        self.P.add("dve", lambda e: e.reciprocal(out=o, in_=i), reads=ik, writes=ok)

    def memset(self, out, val, eng="dve"):
        (o, ok) = _ak(out)
        self.P.add(eng, lambda e: e.memset(o, val), writes=ok)

    def load(self, out, in_, q="sp", dkey=None, grp=False):
        (o, ok) = _ak(out)
        if grp:
            self.P.group_keys.add(dkey)
        self.P.dma(q, lambda e: e.dma_start(out=o, in_=in_), writes=ok, key=(dkey if dkey is not None else ok[0]))

    def store(self, out_dram, in_, q="sp"):
        (i, ik) = _ak(in_)
        self.nout += 1
        k = ("__out", self.nout)
        self.P.dma(q, lambda e: e.dma_start(out=out_dram, in_=i), reads=ik, writes=[k], key=ik[0])

    def finish(self, sem_stack=None):
        self.P.emit(final_keys=[("__out", i + 1) for i in range(self.nout)], sem_stack=sem_stack)
        self.st.close()


def rms_tile_T(b, xt, xs, ss, rstd, PT, xnT_dst, identb, junk, eps=1e-6, D=1024):
    b.act(junk, xt, AF.Square, accum=ss)
    b.act(rstd, ss, AF.Sqrt, scale=1.0 / D, bias=eps)
    b.recip(rstd, rstd)
    b.ts(xs, xt, rstd, None, op0=ALU.mult)
    n = D // 128
    for kc in range(n):
        b.tr((PT[:, kc, :], (PT.tensor.name, kc)), xs[:, kc * 128:(kc + 1) * 128], identb)
    b.copy(xnT_dst, (PT[:, :, :], KL([(PT.tensor.name, kc) for kc in range(n)])), eng="act")


T_SEQ = 8192
SEG = 512
CH = 64
C0 = 0.6065306597126334
NV1 = 20


def build_l1(nseg=T_SEQ // SEG, phase=99, nc=None, pre="", fused=None):
    if nc is None:
        nc = bass.Bass("TRN2", target_bir_lowering=False)
    dr = lambda n, s, dt=F32, kind="ExternalInput": nc.dram_tensor(pre + n, s, dt, kind=kind).ap()
    x = dr("x", [T_SEQ, 1024])
    w_in = dr("w_in", [1024, 896])
    gmix = dr("gmix", [128, 8])
    cvec = dr("cvec", [128, NV1])
    wa_d = dr("wa", [128, 128])
    wx_d = dr("wx", [128, 128])
    w2a2_d = dr("w2a2", [128, 128])
    g2_d = dr("g2", [128, 128])
    ident_d = dr("ident", [128, 128])
    bones_d = dr("bones", [128, 128])
    maska_d = dr("maska", [128, 512])
    maskb_d = dr("maskb", [128, 256])
    rmask_d = dr("rmask", [128, 512])
    id2_d = dr("id2", [128, 128])
    if fused is None:
        yT = dr("yT", [256, T_SEQ], kind="ExternalOutput")
    else:
        wop_d = dr("wo_part", [256, 1024])

    b = B(nc, pre)
    sb, ps = b.sb, b.ps
    wb = sb("wb", [128, 8, 896], BF16)
    if fused is not None:
        wop = sb("wop", [128, 2, 1024], BF16)
        ybf = sb("ybf", [128, 2, SEG], BF16)
        p1t = [sb("p1t%d" % i, [128, 1024]) for i in range(2)]
        for kc in range(2):
            b.load((wop[:, kc, :], ("wop", kc)), wop_d[kc * 128:(kc + 1) * 128, :], q="pool", dkey="constp", grp=True)
    wst = [sb("wst%d" % i, [128, 896]) for i in range(2)]
    gm = sb("gm", [128, 8])
    cv = sb("cv", [128, NV1 + 4])
    wa = sb("wa_s", [128, 128]); wx = sb("wx_s", [128, 128])
    w2a2 = sb("w2a2_s", [128, 128]); g2 = sb("g2_s", [128, 128])
    ident = sb("ident_s", [128, 128]); identb = sb("identb", [128, 128], BF16)
    bones = sb("bones_s", [128, 128]); rkbd = sb("rkbd", [128, 128])
    id2 = sb("id2_s", [128, 2, 64])
    maska = sb("maska_s", [128, 512]); maskb = sb("maskb_s", [128, 256]); rmask = sb("rmask_s", [128, 512])
    for t_, d_ in ((gm, gmix), (cv[:, 0:NV1], cvec), (wa, wa_d), (wx, wx_d), (w2a2, w2a2_d), (g2, g2_d), (ident, ident_d),
                   (bones, bones_d), ((id2[:, :, :].rearrange("p h s -> p (h s)"), "id2_s"), id2_d), (maska, maska_d), (maskb, maskb_d), (rmask, rmask_d)):
        b.load(t_, d_, dkey="const", grp=True)
    b.load(identb, ident_d, q="pool", dkey="constp", grp=True)
    for kc in range(8):
        b.load(wst[kc % 2], w_in[kc * 128:(kc + 1) * 128, :])
        b.ts((wb[:, kc, :], ("wb", kc)), wst[kc % 2], gm[:, kc:kc + 1], None, op0=ALU.mult)
    WBK = lambda kc: ("wb", kc)
    col = lambda i: cv[:, i:i + 1]
    CCH, OMKA, TWOC = NV1, NV1 + 1, NV1 + 2
    b.act(col(CCH), col(7), AF.Exp, scale=-1.0)
    b.act(col(CCH), col(CCH), AF.Ln, bias=1.0)
    b.ts(col(TWOC), col(CCH), -16.0, None, op0=ALU.mult)
    b.ts(col(CCH), col(CCH), -8.0, None, op0=ALU.mult)
    b.ts(col(OMKA), col(16), -1.0, 1.0, op0=ALU.mult, op1=ALU.add)
    b.ts(rkbd, bones, col(17), None, op0=ALU.mult)

    xt = [sb("xt%d" % i, [128, 1024]) for i in range(2)]
    xs = [sb("xs%d" % i, [128, 1024], BF16) for i in range(2)]
    junk = sb("junk", [128, 1024], BF16)
    ss = sb("ss", [128, 1]); rstd = sb("rstd", [128, 1])
    xnT = sb("xnT", [128, 8, SEG], BF16)
    pj = [[sb("pj%d_%d" % (i, c), [128, 4 + SEG]) for c in range(7)] for i in range(2)]
    NT = 27
    tmp = [sb("t%d" % i, [128, SEG]) for i in range(NT)]
    hh = [sb("hh%d" % i, [128, SEG]) for i in range(2)]
    TM = sb("TM", [64, 2, 4, 128])
    SA = sb("SA", [64, 2, 4, 2, 64]); SBm = sb("SBm", [64, 256])
    PQ = [sb("PQ%d" % i, [64, 2, 2, 2, 64]) for i in range(2)]
    Tb = [sb("Tb%d" % i, [64, 2, 2, 64]) for i in range(2)]
    ZS = sb("ZS", [64, 2, 2, 64]); MS = sb("MS", [64, 2, 2, 2, 64])
    GP = sb("GP", [64, 2, 2, 2, 64]); GS = GP[:, 0, :, :, :]; PSm = GP[:, 1, :, :, :]
    STT = [sb("STT%d" % i, [64, 2, 64]) for i in range(2)]
    AFx = sb("AFx", [128, SEG // CH, 2, CH]); RFx = sb("RFx", [128, SEG // CH, 2, CH]); BTx = sb("BTx", [128, SEG // CH, 2, CH])
    RFh = sb("RFh", [64, 2, SEG]); WCh = sb("WCh", [64, 2, SEG // CH])
    OS = sb("OS", [128, SEG])
    PP = ps("PP", [128, 512]); PT = ps("PT", [128, 8, 128], BF16)
    PS0 = ps("PS0", [128, 512]); PS1 = ps("PS1", [128, 512])
    PA = ps("PA", [128, 512]); PC = ps("PC", [128, 512]); PX = ps("PX", [128, 512]); PO = ps("PO", [128, 512])

    for i in range(2):
        for c in range(7):
            b.memset((pj[i][c][:, 0:4], ("pjh", i, c)), 0.0)
    b.memset((STT[0][:, :, :], "STT0"), 0.0)
    for tx in (AFx, RFx, BTx):
        b.memset((tx[:, :, :, :], tx.tensor.name), 0.0, eng="pool")

    st_i = 0
    PS0a_ = ("PS0", 0)
    XN = KL([("xnT", j) for j in range(4)])
    PIPE = (phase >= 99)

    def emit_load(s_, j):
        tix = (s_ * 4 + j) % 2
        b.load(xt[tix], x[s_ * SEG + j * 128: s_ * SEG + (j + 1) * 128, :])

    def emit_norm(s_, j):
        tix = (s_ * 4 + j) % 2
        b.act(junk, xt[tix], AF.Square, accum=ss)
        b.act(rstd, ss, AF.Sqrt, scale=1.0 / 1024, bias=1e-6)
        b.recip(rstd, rstd)
        b.ts(xs[tix], xt[tix], rstd, None, op0=ALU.mult)

    def emit_tr(s_, j):
        tix = (s_ * 4 + j) % 2
        for kc in range(8):
            b.tr((PT[:, kc, :], (PT.tensor.name, kc)), xs[tix][:, kc * 128:(kc + 1) * 128], identb)
        b.copy((xnT[:, :, j * 128:(j + 1) * 128], ("xnT", j)), (PT[:, :, :], KL([(PT.tensor.name, kc) for kc in range(8)])), eng="act")

    def emit_B(s_):
        c_, n_ = s_ % 2, 1 - (s_ % 2)
        for cc in range(7):
            for kc in range(8):
                b.mm(PP, (wb[:, kc, cc * 128:(cc + 1) * 128], WBK(kc)), (xnT[:, kc, :], XN), start=(kc == 0), stop=(kc == 7))
            b.copy((pj[c_][cc][:, 4:4 + SEG], ("pj", c_, cc)), PP, eng=("act" if cc % 2 == 0 else "dve"))
            b.copy((pj[n_][cc][:, 0:4], ("pjh", n_, cc)), (pj[c_][cc][:, SEG:SEG + 4], ("pj", c_, cc)), eng="pool")

    def emit_AB(s_):
        for j in range(4):
            emit_load(s_, j)
            emit_norm(s_, j)
            emit_tr(s_, j)
        emit_B(s_)

    if PIPE:
        emit_AB(0)
    for s in range(nseg):
        cur = s % 2
        nxt = 1 - cur
        pjc = pj[cur]
        PJ = lambda c: ("pj", cur, c)
        PJH = lambda c: ("pjh", cur, c)
        if not PIPE:
            emit_AB(s)
        pipe_next = PIPE and (s + 1 < nseg)
        if phase < 2:
            continue
        cur_v = lambda c: (pjc[c][:, 4:4 + SEG], PJ(c))
        sh_v = lambda c, k: (pjc[c][:, 4 - k:4 - k + SEG], KL([PJ(c), PJH(c)]))
        t = tmp
        xc = t[0]
        b.ts(xc, sh_v(0, 3), col(0), col(4), op0=ALU.mult, op1=ALU.add)
        b.stt(xc, sh_v(0, 2), col(1), xc, ALU.mult, ALU.add)
        b.stt(xc, sh_v(0, 1), col(2), xc, ALU.mult, ALU.add)
        b.stt(xc, cur_v(0), col(3), xc, ALU.mult, ALU.add)
        b.mm(PS0, wa, xc)
        b.mm(PS1, wx, xc)
        ra = t[1]
        b.act(ra, PS0, AF.Sigmoid, bias=col(5))
        av_ = t[2]
        b.act(av_, ra, AF.Exp, scale=col(CCH))
        a2_ = t[3]
        b.act(a2_, ra, AF.Exp, scale=col(TWOC))
        b.act(a2_, a2_, AF.Sqrt, scale=-1.0, bias=1.0)
        ix = t[1]
        b.act(ix, PS1, AF.Sigmoid, bias=col(6))
        b.tt(a2_, a2_, ix, ALU.mult)
        b.tt(a2_, a2_, xc, ALU.mult)
        hcur = hh[cur]
        b.scan(hcur, av_, a2_, 0.0 if s == 0 else hh[nxt][:, SEG - 1:SEG])
        gq = t[0]
        b.act(gq, cur_v(1), AF.Square)
        b.ts(gq, gq, 0.044715, 1.0, op0=ALU.mult, op1=ALU.add)
        b.tt(gq, gq, cur_v(1), ALU.mult)
        b.act(gq, gq, AF.Sigmoid, scale=1.5957691216057308)
        b.tt(gq, gq, cur_v(1), ALU.mult)
        ylru = t[1]
        if fused is None:
            b.tt(ylru, gq, hcur, ALU.mult)
            b.store(yT[0:128, s * SEG:(s + 1) * SEG], ylru)
        else:
            b.tt((ybf[:, 0, :], ("ybf", 0)), gq, hcur, ALU.mult)
        if phase < 3:
            continue
        shf = []
        for i, c in enumerate((2, 3, 4, 5, 6)):
            d = t[4 + i]
            b.tt(d, sh_v(c, 1), cur_v(c), ALU.subtract)
            b.stt(d, d, col(8 + i), cur_v(c), ALU.mult, ALU.add)
            shf.append(d)
        rs, ks, vs, xwa, xgs = shf
        tw = t[9]
        b.act(tw[0:64, :], xwa[0:64, :], AF.Tanh)
        b.mm(PS0, w2a2[0:64, :], tw[0:64, :])
        b.mm(PS1, w2a2[64:128, :], xwa[64:128, :])
        sgz = t[9]
        b.act(sgz, PS0, AF.Sigmoid, bias=col(13))
        avv = t[10]
        b.act(avv, PS1, AF.Sigmoid, bias=col(14))
        sg = t[11]
        b.act(sg, xgs, AF.Sigmoid)
        cs = t[12]
        b.scan(cs, rmask, sgz, 0.0)
        csm1 = t[13]
        b.tt(csm1, cs, sgz, ALU.subtract)
        Wt = t[14]; iW = t[15]; Wm1 = t[13]
        b.act(Wt, cs, AF.Exp, scale=-C0)
        b.act(iW, cs, AF.Exp, scale=C0)
        b.act(Wm1, csm1, AF.Exp, scale=-C0)
        b.mm(PS0, g2, sg)
        gv = t[11]
        b.copy(gv, PS0, eng="act")
        kq = t[9]
        b.ts(kq, ks, col(15), None, op0=ALU.mult)
        kq2 = t[12]
        b.act(kq2, kq, AF.Square)
        b.mm(PS1, bones, kq2)
        rn = t[12]
        b.act(rn, PS1, AF.Sqrt)
        b.ts(rn, rn, 1e-12, None, op0=ALU.max)
        b.recip(rn, rn)
        kkn = t[9]
        b.tt(kkn, kq, rn, ALU.mult)
        kmod = t[12]
        b.ts(kmod, avv, col(16), col(OMKA), op0=ALU.mult, op1=ALU.add)
        b.tt(kmod, kmod, ks, ALU.mult)
        bb = t[10]
        b.tt(bb, kkn, avv, ALU.mult)
        AFm = t[16]; RF = t[17]; BT = t[18]; KT = t[19]; Bh = t[20]; Kh = t[21]
        b.stt(AFm, kkn, -1.0, Wm1, ALU.mult, ALU.mult)
        b.tt(RF, rs, Wt, ALU.mult)
        b.tt(BT, bb, iW, ALU.mult)
        b.tt(KT, kmod, iW, ALU.mult)
        v3 = lambda tl: tl[:, :].rearrange("p (c s) -> p c s", s=CH)
        wcb = (v3(Wt)[:, :, CH - 1:CH].broadcast_to([128, SEG // CH, CH]), Wt.tensor.name)
        b.tt((v3(Bh), Bh.tensor.name), (v3(BT), BT.tensor.name), wcb, ALU.mult)
        b.tt((v3(Kh), Kh.tensor.name), (v3(KT), KT.tensor.name), wcb, ALU.mult)
        rk_ = t[9]
        b.tt(rk_, rs, kmod, ALU.mult)
        b.mm(PS1, rkbd, rk_)
        bonus = t[22]
        b.tt(bonus, PS1, vs, ALU.mult)
        if phase < 4:
            continue
        for h in range(2):
            b.mm((PS1[0:64, :], KEYMAP["PS1"]), ident[:, 64 * h:64 * h + 64], RF)
            b.copy((RFh[:, h, :], "RFh"), (PS1[0:64, :], KEYMAP["PS1"]), eng="act")
            b.mm((PS0[0:64, h * 8:h * 8 + 8], PS0a_), ident[:, 64 * h:64 * h + 64], (v3(Wt)[:, :, CH - 1], Wt.tensor.name))
        b.copy((WCh[:, :, :].rearrange("p h c -> p (h c)"), "WCh"), (PS0[0:64, 0:16], PS0a_), eng="act")
        for tl, tx in ((AFm, AFx), (RF, RFx), (BT, BTx)):
            b.copy((tx[0:64, :, 0, :], tx.tensor.name), (v3(tl)[0:64], tl.tensor.name), eng="pool")
            b.copy((tx[64:128, :, 1, :], tx.tensor.name), (v3(tl)[64:128], tl.tensor.name), eng="pool")
        PS0a, PS0b, PS1a, PS1b = ("PS0", 0), ("PS0", 1), ("PS1", 0), ("PS1", 1)
        sel = [ident[:, 0:64], ident[:, 64:128]]
        if pipe_next:
            emit_load(s + 1, 0)
        for cp in range(SEG // 128):
            tok = slice(cp * 128, (cp + 1) * 128)
            if pipe_next:
                if cp < 3:
                    emit_load(s + 1, cp + 1)
                emit_norm(s + 1, cp)
            for q in range(2):
                c_ = cp * 2 + q
                ck = slice(c_ * CH, (c_ + 1) * CH)
                for qi, src in enumerate((AFm, Bh, Kh, vs)):
                    b.tr((PX[0:64, qi * 128:(qi + 1) * 128], "PX"), src[:, ck], ident)
                b.copy((TM[:, q, :, :].rearrange("p a b -> p (a b)"), ("TM", q)), (PX[0:64, :], "PX"), eng="act")
                for j, (l_, rx) in enumerate(((BT, AFx), (BT, RFx), (KT, AFx), (KT, RFx))):
                    b.mm((PA[0:64, j * 128:(j + 1) * 128], "PA"), l_[:, ck], rx[:, c_, :, :].rearrange("p h s -> p (h s)"))
                b.mm((PS1[0:64, q * 128:(q + 1) * 128], PS1a), AFm[:, ck], BTx[:, c_, :, :].rearrange("p h s -> p (h s)"))
                b.tt((SA[:, q, :, :, :].rearrange("p j h s -> p (j h s)"), ("SA", q)), (PA[0:64, :], "PA"), maska[0:64, :], ALU.mult)
            b.tt(SBm, (PS1[0:64, 0:256], PS1a), maskb[0:64, :], ALU.mult)
            TMv = lambda qi, q, h: (TM[:, q, qi, 64 * h:64 * h + 64], ("TM", q))
            SAv = lambda j, q, h: (SA[:, q, j, h, :], ("SA", q))
            SAK = KL([("SA", 0), ("SA", 1)])
            QH = [(q, h) for q in range(2) for h in range(2)]
            for q, h in QH:
                b.mm((PS1[0:64, 256 + (q * 2 + h) * 64:256 + (q * 2 + h) * 64 + 64], PS1b), SAv(2, q, h), TMv(3, q, h))
            b.copy((ZS[:, :, :, :].rearrange("p q h s -> p (q h s)"), "ZS"), (PS1[0:64, 256:512], PS1b), eng="act")
            if phase < 5:
                continue
            b.copy((PQ[0][:, 0, :, :, :], "PQ0"), (SA[:, :, 0, :, :], SAK), eng="pool")
            b.copy((PQ[0][:, 1, :, :, :].rearrange("p q h s -> p (q h s)"), "PQ0"), SBm, eng="pool")
            idb = (ident[0:64, 0:64].rearrange("p (a c s) -> p a c s", a=1, c=1).broadcast_to([64, 2, 2, 64]), ident.tensor.name)
            b.tt((Tb[0][:, :, :, :], "Tb0"), (SA[:, :, 0, :, :], SAK), idb, ALU.add)
            PC5 = PC[0:64, :].rearrange("p (j q h s) -> p j q h s", j=2, q=2, h=2)
            POt = PO[0:64, 256:512].rearrange("p (q h s) -> p q h s", q=2, h=2)
            for k in range(0, 6):
                i = k % 2
                pqk = "PQ%d" % i
                if k >= 1:
                    for q, h in QH:
                        b.mm((POt[:, q, h, :], ("PO", 2)), (PQ[i][:, 1, q, h, :], pqk), (Tb[1 - i][:, q, h, :], "Tb%d" % (1 - i)))
                if k <= 3:
                    for q, h in QH:
                        b.mm((PC5[:, 0, q, h, :], ("PC", 0)), (PQ[i][:, 1, q, h, :], pqk), (PQ[i][:, 0, q, h, :], pqk))
                if k <= 4:
                    for q, h in QH:
                        b.mm((PC5[:, 1, q, h, :], ("PC", 1)), (PQ[i][:, 0, q, h, :], pqk), (PQ[i][:, 1, q, h, :], pqk))
                if k <= 3:
                    b.copy((PQ[1 - i][:, :, :, :, :].rearrange("p j q h s -> p (j q h s)"), "PQ%d" % (1 - i)),
                           (PC[0:64, :], KL([("PC", 0), ("PC", 1)])), eng="act")
                elif k == 4:
                    b.copy((PQ[1 - i][:, 1, :, :, :].rearrange("p q h s -> p (q h s)"), "PQ%d" % (1 - i)),
                           (PC[0:64, 256:512], ("PC", 1)), eng="act")
                if k >= 1:
                    b.tt((Tb[i][:, :, :, :].rearrange("p q h s -> p (q h s)"), "Tb%d" % i),
                         (Tb[1 - i][:, :, :, :].rearrange("p q h s -> p (q h s)"), "Tb%d" % (1 - i)),
                         (PO[0:64, 256:512], ("PO", 2)), ALU.add)
            if phase < 5.2:
                continue
            for q, h in QH:
                o0 = ((q * 2 + h) * 2) * 64
                b.mm((PX[0:64, o0:o0 + 64], "PX"), (Tb[1][:, q, h, :], "Tb1"), TMv(0, q, h))
                b.mm((PX[0:64, o0 + 64:o0 + 128], "PX"), (Tb[1][:, q, h, :], "Tb1"), (ZS[:, q, h, :], "ZS"))
            b.copy((MS[:, :, :, :, :].rearrange("p q h m s -> p (q h m s)"), "MS"), (PX[0:64, :], "PX"), eng="act")
            M1T = lambda q, h: (MS[:, q, h, 0, :], "MS")
            M2T = lambda q, h: (MS[:, q, h, 1, :], "MS")
            if phase < 5.4:
                continue
            for q, h in QH:
                o0 = (q * 2 + h) * 64
                b.mm((PS0[0:64, o0:o0 + 64], PS0a), M1T(q, h), TMv(1, q, h))
                b.mm((PS0[0:64, 256 + o0:256 + o0 + 64], PS0b), M1T(q, h), SAv(1, q, h))
            for q, h in QH:
                c_ = cp * 2 + q
                o0 = (q * 2 + h) * 64
                b.stt((GS[:, q, h, :], "GS"), ident[0:64, 0:64], (WCh[:, h, c_:c_ + 1], "WCh"), (PS0[0:64, o0:o0 + 64], PS0a), ALU.mult, ALU.add)
            b.tt((PSm[:, :, :, :], "PSm"), (PS0[0:64, 256:512].rearrange("p (q h s) -> p q h s", q=2, h=2), PS0b),
                 (RFh[:, :, tok].rearrange("p h (q s) -> p q h s", q=2), "RFh"), ALU.add)
            if phase < 5.6:
                continue
            for q in range(2):
                ocol = slice(q * 64, q * 64 + 64)
                stn = "STT%d" % st_i
                for h in range(2):
                    if 5.8 <= phase < 5.9:
                        continue
                    pr = slice(64 * h, 64 * h + 64)
                    ob = (PO[pr, ocol], ("PO", 0)) if h == 0 else (PX[pr, ocol], "PX")
                    b.mm(ob, M2T(q, h), SAv(1, q, h), start=True, stop=False)
                    b.mm(ob, TMv(3, q, h), SAv(3, q, h), start=False, stop=False)
                    b.mm(ob, (STT[st_i][:, h, :], stn), (PSm[:, q, h, :], "PSm"), start=False, stop=True)
                for h in range(2):
                    if phase == 5.7:
                        continue
                    so = slice(h * 64, h * 64 + 64)
                    b.mm((PS1[0:64, so], PS1a), TMv(1, q, h), M2T(q, h), start=True, stop=False)
                    b.mm((PS1[0:64, so], PS1a), TMv(2, q, h), TMv(3, q, h), start=False, stop=False)
                    b.mm((PS1[0:64, so], PS1a), (GS[:, q, h, :], "GS"), (STT[st_i][:, h, :], stn), start=False, stop=True)
                b.copy((STT[1 - st_i][:, :, :].rearrange("p h s -> p (h s)"), "STT%d" % (1 - st_i)), (PS1[0:64, 0:128], PS1a), eng="dve")
                st_i = 1 - st_i
            b.copy((OS[0:64, tok], ("OS", cp)), (PO[0:64, 0:128], ("PO", 0)), eng="dve")
            b.copy((OS[64:128, tok], ("OS", cp)), (PX[64:128, 0:128], "PX"), eng="dve")
            if pipe_next:
                emit_tr(s + 1, cp)
                if cp == 3:
                    emit_B(s + 1)
        if phase < 6:
            continue
        OSK = KL([("OS", i) for i in range(4)])
        b.mm(PS0, bones, (OS[:, :], OSK))
        cen = t[23]
        b.stt(cen, PS0, -1.0 / 64, (OS[:, :], OSK), ALU.mult, ALU.add)
        sq = t[24]
        b.act(sq, cen, AF.Square)
        b.mm(PS1, bones, sq)
        b.act(sq, PS1, AF.Sqrt, scale=1.0 / 64, bias=64e-5)
        b.recip(sq, sq)
        b.tt(cen, cen, sq, ALU.mult)
        b.ts(cen, cen, col(18), col(19), op0=ALU.mult, op1=ALU.add)
        b.tt(cen, cen, bonus, ALU.add)
        if fused is None:
            yrw = t[25 + (s % 2)]
            b.tt(yrw, cen, gv, ALU.mult)
            b.store(yT[128:256, s * SEG:(s + 1) * SEG], yrw)
        else:
            b.tt((ybf[:, 1, :], ("ybf", 1)), cen, gv, ALU.mult)
            for j in range(4):
                pt_ = p1t[j % 2]
                for n in range(2):
                    for kc in range(2):
                        b.mm(PP, (ybf[:, kc, j * 128:(j + 1) * 128], ("ybf", kc)), (wop[:, kc, n * 512:(n + 1) * 512], ("wop", kc)),
                             start=(kc == 0), stop=(kc == 1))
                    b.copy((pt_[:, n * 512:(n + 1) * 512], (pt_.tensor.name, n)), PP, eng=("act" if n == 0 else "dve"))
                b.store(fused["rs_in"][s * SEG + j * 128:s * SEG + (j + 1) * 128, :], (pt_, KL([(pt_.tensor.name, 0), (pt_.tensor.name, 1)])))
    b.finish(sem_stack=(fused or {}).get("sem_stack"))
    return nc


def _consts_l1():
    p = np.arange(128)
    ident = np.eye(128, dtype=np.float32)
    bones = (p[:, None] // 64 == p[None, :] // 64).astype(np.float32)
    s_ = (p % 64)[:, None]
    t_ = np.arange(64)[None, :]
    lt = (s_ < t_).astype(np.float32)
    le = (s_ <= t_).astype(np.float32)
    gt = (s_ > t_).astype(np.float32)
    eq = (s_ == t_).astype(np.float32)
    maska = np.concatenate([lt, lt, le, le, lt, lt, le, le], axis=1)
    maskb = np.concatenate([gt, gt, gt, gt], axis=1)
    id2 = np.concatenate([eq, eq], axis=1)
    rmask = np.ones((128, 512), np.float32)
    rmask[:, ::64] = 0.0
    return dict(ident=ident, bones=bones, maska=np.ascontiguousarray(maska), maskb=np.ascontiguousarray(maskb),
                id2=np.ascontiguousarray(id2), rmask=rmask)


def _blockdiag2(w2):
    o = np.zeros((128, 128), np.float32)
    o[0:64, 0:64] = w2[0]
    o[64:128, 64:128] = w2[1]
    return o


def l1_inputs(inp, bi, g):
    f = lambda k: np.asarray(inp[k][0], np.float32)
    ls = slice(g * 128, (g + 1) * 128)
    W = f("e_w_in")
    rw0 = 1024
    cols = np.concatenate([np.arange(512)[ls], 512 + np.arange(512)[ls], rw0 + np.arange(512)[ls], rw0 + 512 + np.arange(512)[ls],
                           rw0 + 1024 + np.arange(512)[ls], rw0 + 1536 + np.arange(128), rw0 + 1664 + np.arange(128)])
    mu = f("e_shift_mu")
    cvec = np.zeros((128, NV1), np.float32)
    cvec[:, 0:4] = f("e_conv_w")[:, ls].T
    cvec[:, 4] = f("e_conv_b")[ls]
    cvec[:, 5] = f("e_gate_a_b")[ls]
    cvec[:, 6] = f("e_gate_x_b")[ls]
    cvec[:, 7] = f("e_lru_lambda")[ls]
    cvec[:, 8] = mu[0:512][ls]
    cvec[:, 9] = mu[512:1024][ls]
    cvec[:, 10] = mu[1024:1536][ls]
    cvec[:, 11] = mu[1536:1664]
    cvec[:, 12] = mu[1664:1792]
    cvec[:, 13] = f("e_w0")[ls]
    cvec[:, 14] = f("e_a0")[ls]
    cvec[:, 15] = f("e_k_k")[ls]
    cvec[:, 16] = f("e_k_a")[ls]
    cvec[:, 17] = f("e_r_k").reshape(-1)[ls]
    cvec[:, 18] = f("e_gn_w")[ls]
    cvec[:, 19] = f("e_gn_b")[ls]
    d = dict(
        x=np.ascontiguousarray(inp["x"][bi]),
        w_in=np.ascontiguousarray(W[:, cols]),
        gmix=np.ascontiguousarray(f("e_ln_mix").reshape(8, 128).T),
        cvec=cvec,
        wa=_blockdiag2(f("e_gate_a_w")[2 * g:2 * g + 2]),
        wx=_blockdiag2(f("e_gate_x_w")[2 * g:2 * g + 2]),
        w2a2=np.ascontiguousarray(np.concatenate([f("e_w2")[:, ls], f("e_a2")[:, ls]], axis=0)),
        g2=np.ascontiguousarray(f("e_g2")[:, ls]),
    )
    d.update(_consts_l1())
    return d


def build_ffn(NT=2048, F=2816, E=1, G=512, moe=False, ngroups=None, phase=99, nc=None, pre="", fused=None):
    if nc is None:
        nc = bass.Bass("TRN2", target_bir_lowering=False)
    dr = lambda n, s, dt=F32, kind="ExternalInput": nc.dram_tensor(pre + n, s, dt, kind=kind).ap()
    nF = F // 128
    TG = G // 128
    if fused is None or fused.get("xres") is None:
        xres = dr("xres", [NT, 1024])
    else:
        xres = fused["xres"]
    if fused is None:
        aT_d = dr("aT", [1024, NT])
        wproj = dr("wproj", [1024, 1024])
    gain_d = dr("gain", [1, 1024])
    wg_d = dr("wg", [E, 1024, F])
    wu_d = dr("wu", [E, 1024, F])
    wd_d = dr("wd", [E, F, 1024])
    ident_d = dr("ident", [128, 128])
    if moe:
        router_d = dr("router", [1024, 8])
    if fused is None or fused.get("out") is None:
        out = dr("out", [NT, 1024], kind="ExternalOutput")
    else:
        out = fused["out"]

    b = B(nc, pre)
    sb, ps = b.sb, b.ps
    if fused is None:
        wo = sb("wo", [128, 8, 1024], BF16)
    gbc = sb("gbc", [128, 1024])
    ident = sb("ident_s", [128, 128])
    identb = sb("identb", [128, 128], BF16)
    b.load(gbc, gain_d.partition_broadcast(128), dkey="const", grp=True)
    b.load(ident, ident_d, dkey="const", grp=True)
    b.load(identb, ident_d, q="pool", dkey="constp", grp=True)
    if fused is None:
        wpv = wproj.rearrange("(kc p) n -> p kc n", p=128)
        for kc in range(8):
            b.load((wo[:, kc, :], ("wo", kc)), wpv[:, kc, :], q="pool")
    WOK = lambda kc: ("wo", kc)
    if moe:
        rt = sb("rt", [128, 8, 8])
        b.load(rt, router_d.rearrange("(kc p) e -> p kc e", p=128), dkey="const", grp=True)
        hT32 = sb("hT32", [128, 8, 128])
        lg = sb("lg", [128, 8]); lg2 = sb("lg2", [128, 8]); mk1 = sb("mk1", [128, 8]); mk2 = sb("mk2", [128, 8])
        sm = sb("sm", [128, 8])
        comb = sb("comb", [128, TG, 8])
        xs32 = sb("xs32", [128, 1024])
        PTa = ps("PTa", [128, 4, 128]); PTb = ps("PTb", [128, 4, 128])
    else:
        xs = sb("xs", [128, 1024], BF16)
        PT = ps("PT", [128, 8, 128], BF16)
    NXB = 2 if G <= 512 else 1
    xt = [sb("xt%d" % i, [128, 1024]) for i in range(NXB)]
    if fused is None:
        at = [sb("at%d" % i, [128, 8, 128], BF16) for i in range(NXB)]
    else:
        at = [sb("at%d" % i, [128, 1024]) for i in range(NXB)]
    acc = [sb("acc%d" % i, [128, 1024]) for i in range(TG)]
    junk = sb("junk", [128, 1024], BF16)
    ss = sb("ss", [128, 1]); rstd = sb("rstd", [128, 1])
    hT = sb("hT", [128, 8, G], BF16)
    hid = sb("hid", [128, nF, G], BF16)
    NWB = 3
    wgb = [sb("wgb%d" % i, [128, 8, 128], BF16) for i in range(NWB)]
    wub = [sb("wub%d" % i, [128, 8, 128], BF16) for i in range(NWB)]
    NS = 2 if G <= 512 else 4
    WN = 1024 // NS
    NH = G // 512
    wdh = [sb("wdh%d" % i, [128, nF, WN], BF16) for i in range(2)]
    sg = [sb("sg%d" % i, [128, 512]) for i in range(2)]
    PP = [ps("PP%d" % i, [128, 512]) for i in range(2)]
    PG = [ps("PG%d" % i, [128, 512]) for i in range(2)]
    PU = [ps("PU%d" % i, [128, 512]) for i in range(2)]
    if fused is None:
        aTv = aT_d.rearrange("(kc p) t -> p kc t", p=128)
    wi = 0
    di = 0
    for g in range(ngroups if ngroups is not None else NT // G):
        for j in range(TG):
            tok0 = g * G + j * 128
            x_ = xt[j % NXB]
            a_ = at[j % NXB]
            b.load(x_, xres(tok0) if callable(xres) else xres[tok0:tok0 + 128, :])
            if fused is None:
                b.load(a_, aTv[:, :, tok0:tok0 + 128], q="pool")
                for n in range(2):
                    for kc in range(8):
                        b.mm(PP[n], a_[:, kc, :], (wo[:, kc, n * 512:(n + 1) * 512], WOK(kc)), start=(kc == 0), stop=(kc == 7))
                    b.tt((acc[j][:, n * 512:(n + 1) * 512], ("acc", j, n)), PP[n], x_[:, n * 512:(n + 1) * 512], ALU.add)
            else:
                b.load(a_, fused["add"][tok0:tok0 + 128, :])
                for n in range(2):
                    b.tt((acc[j][:, n * 512:(n + 1) * 512], ("acc", j, n)), a_[:, n * 512:(n + 1) * 512], x_[:, n * 512:(n + 1) * 512], ALU.add)
            ACCK = KL([("acc", j, 0), ("acc", j, 1)])
            b.act(junk, (acc[j], ACCK), AF.Square, accum=ss)
            b.act(rstd, ss, AF.Sqrt, scale=1.0 / 1024, bias=1e-6)
            b.recip(rstd, rstd)
            if not moe:
                b.stt(xs, (acc[j], ACCK), rstd, gbc, ALU.mult, ALU.mult)
                for kc in range(8):
                    b.tr((PT[:, kc, :], ("PT", kc)), xs[:, kc * 128:(kc + 1) * 128], identb)
                b.copy((hT[:, :, j * 128:(j + 1) * 128], ("hT", j)), (PT, KL([("PT", kc) for kc in range(8)])), eng="act")
            else:
                b.stt(xs32, (acc[j], ACCK), rstd, gbc, ALU.mult, ALU.mult)
                for kc in range(8):
                    pt_ = PTa if kc < 4 else PTb
                    b.tr((pt_[:, kc % 4, :], (pt_.tensor.name, kc % 4)), xs32[:, kc * 128:(kc + 1) * 128], ident)
                for kc in range(8):
                    pt_ = PTa if kc < 4 else PTb
                    pass
                b.copy((hT32[:, 0:4, :], ("hT32", 0)), (PTa, KL([(PTa.tensor.name, i) for i in range(4)])), eng="act")
                b.copy((hT32[:, 4:8, :], ("hT32", 1)), (PTb, KL([(PTb.tensor.name, i) for i in range(4)])), eng="dve")
                H32 = KL([("hT32", 0), ("hT32", 1)])
                b.copy((hT[:, :, j * 128:(j + 1) * 128], ("hT", j)), (hT32, H32), eng="act")
                for kc in range(8):
                    b.mm(PP[0][:, 0:8], (hT32[:, kc, :], ("hT32", kc // 4)), rt[:, kc, :], start=(kc == 0), stop=(kc == 7))
                b.copy(lg, PP[0][:, 0:8])
                b.P.add("dve", (lambda o, i: (lambda e: e.reduce_max(out=o, in_=i, axis=AX.X)))(sm[:, 0:1], lg), reads=[lg.tensor.name], writes=[sm.tensor.name])
                b.ts(mk1, lg, sm[:, 0:1], None, op0=ALU.is_equal)
                b.stt(lg2, mk1, -1e30, lg, ALU.mult, ALU.add)
                b.P.add("dve", (lambda o, i: (lambda e: e.reduce_max(out=o, in_=i, axis=AX.X)))(sm[:, 1:2], lg2), reads=[lg2.tensor.name], writes=[sm.tensor.name])
                b.ts(mk2, lg2, sm[:, 1:2], None, op0=ALU.is_equal)
                b.ts(sm[:, 2:3], sm[:, 0:1], -1.0, None, op0=ALU.mult)
                b.act(sm[:, 3:4], sm[:, 1:2], AF.Exp, bias=sm[:, 2:3])
                b.ts(sm[:, 4:5], sm[:, 3:4], 1.0, None, op0=ALU.add)
                b.recip(sm[:, 4:5], sm[:, 4:5])
                b.tt(sm[:, 5:6], sm[:, 3:4], sm[:, 4:5], ALU.mult)
                b.ts(mk1, mk1, sm[:, 4:5], None, op0=ALU.mult)
                b.stt((comb[:, j, :], ("comb", j)), mk2, sm[:, 5:6], mk1, ALU.mult, ALU.add)
        HT = KL([("hT", j) for j in range(TG)])
        for e in range(E if phase >= 2 else 0):
            wgv = wg_d[e].rearrange("(kc p) f -> p kc f", p=128)
            wuv = wu_d[e].rearrange("(kc p) f -> p kc f", p=128)
            wdv = wd_d[e].rearrange("(fc p) n -> p fc n", p=128)
            for fc in range(nF):
                w1, w2 = wgb[wi % NWB], wub[wi % NWB]
                b.load(w1, wgv[:, :, fc * 128:(fc + 1) * 128], q="pool")
                b.load(w2, wuv[:, :, fc * 128:(fc + 1) * 128], q="pool")
                for hh in range(NH):
                    bi_ = (wi * NH + hh) % 2
                    pg, pu, sg_ = PG[bi_], PU[bi_], sg[bi_]
                    hs = slice(hh * 512, (hh + 1) * 512)
                    for kc in range(8):
                        b.mm(pg, w1[:, kc, :], (hT[:, kc, hs], HT), start=(kc == 0), stop=(kc == 7))
                    for kc in range(8):
                        b.mm(pu, w2[:, kc, :], (hT[:, kc, hs], HT), start=(kc == 0), stop=(kc == 7))
                    b.act(sg_, pg, AF.Silu)
                    b.tt((hid[:, fc, hs], ("hid", fc)), sg_, pu, ALU.mult)
                wi += 1
            HID = KL([("hid", fc) for fc in range(nF)])
            for n in range(NS if phase >= 3 else 0):
                wd_ = wdh[di % 2]
                di += 1
                for q4 in range(4):
                    f0, f1 = (nF * q4) // 4, (nF * (q4 + 1)) // 4
                    b.load((wd_[:, f0:f1, :], (wd_.tensor.name, q4)), wdv[:, f0:f1, n * WN:(n + 1) * WN], q="pool", dkey=wd_.tensor.name)
                WDK = KL([(wd_.tensor.name, q4) for q4 in range(4)])
                for j in range(TG):
                    pp = PP[j % 2]
                    for fc in range(nF):
                        b.mm(pp[:, 0:WN], (hid[:, fc, j * 128:(j + 1) * 128], HID), (wd_[:, fc, :], WDK), start=(fc == 0), stop=(fc == nF - 1))
                    av = (acc[j][:, n * WN:(n + 1) * WN], ("acc", j, (n * WN) // 512))
                    if moe:
                        b.stt(av, pp[:, 0:WN], (comb[:, j, e:e + 1], ("comb", j)), av, ALU.mult, ALU.add)
                    else:
                        b.tt(av, pp[:, 0:WN], av, ALU.add)
        for j in range(TG):
            tok0 = g * G + j * 128
            b.store(out(tok0) if callable(out) else out[tok0:tok0 + 128, :], (acc[j], KL([("acc", j, 0), ("acc", j, 1)])))
    b.finish(sem_stack=(fused or {}).get("sem_stack"))
    return nc


LAMBDA_INIT1 = 0.8 - 0.6 * float(np.exp(-0.3 * 1))
TWO_PI = 6.283185307179586
CW1 = 6.28125
CW2 = TWO_PI - 6.28125
MAGIC = 12582912.0


def build_l3(nblk=T_SEQ // 512, phase=99, nc=None, pre="", fused=None):
    if nc is None:
        nc = bass.Bass("TRN2", target_bir_lowering=False)
    dr = lambda n, s, dt=F32, kind="ExternalInput": nc.dram_tensor(pre + n, s, dt, kind=kind).ap()
    if fused is None:
        x = dr("x", [T_SEQ, 1024])
    else:
        x = fused["x"]
        wop_d = dr("wo_part", [256, 1024])
    pos_d = dr("pos", [1, T_SEQ], I32)
    w_d = dr("w", [1024, 768])
    gmix = dr("gmix", [128, 8])
    cvec = dr("cvec", [128, 4])
    lams = dr("lams", [1, 256])
    ident_d = dr("ident", [128, 128])
    bones_d = dr("bones", [128, 128])
    rot_d = dr("rot", [128, 128])
    cmask_d = dr("cmask", [128, 4 * 512])
    if fused is None:
        oT = dr("oT", [256, T_SEQ], kind="ExternalOutput")

    b = B(nc, pre)
    sb, ps = b.sb, b.ps
    wb = sb("wb", [128, 8, 768], BF16)
    if fused is not None:
        wop = sb("wop", [128, 2, 1024], BF16)
        onb = [sb("onb%d" % i, [128, 512], BF16) for i in range(2)]
        p1t = [sb("p1t%d" % i, [128, 1024]) for i in range(2)]
        for kc in range(2):
            b.load((wop[:, kc, :], ("wop", kc)), wop_d[kc * 128:(kc + 1) * 128, :], q="pool", dkey="constp", grp=True)
    wst = [sb("wst%d" % i, [128, 768]) for i in range(2)]
    gm = sb("gm", [128, 8]); cv = sb("cv", [128, 8])
    ident = sb("ident_s", [128, 128]); identb = sb("identb", [128, 128], BF16)
    bones = sb("bones_s", [128, 128]); rot = sb("rot_s", [128, 128])
    ones_f = sb("ones_f", [128, 128]); ones_b = sb("ones_b", [128, 128], BF16)
    cmask = sb("cmask_s", [128, 4, 512], BF16)
    lm = sb("lm", [128, 256]); lmp = sb("lmp", [128, 128]); lsc = sb("lsc", [128, 8])
    for t_, d_ in ((gm, gmix), (cv[:, 0:4], cvec), (ident, ident_d), (bones, bones_d), (rot, rot_d), (lm, lams.partition_broadcast(128))):
        b.load(t_, d_, dkey="const", grp=True)
    b.load(identb, ident_d, q="pool", dkey="constp", grp=True)
    b.load((cmask[:, :, :].rearrange("p a b -> p (a b)"), "cmask_s"), cmask_d, q="pool", dkey="constp", grp=True)
    b.memset(ones_f, 1.0)
    b.memset(ones_b, 1.0)
    for kc in range(8):
        b.load(wst[kc % 2], w_d[kc * 128:(kc + 1) * 128, :])
        b.ts((wb[:, kc, :], ("wb", kc)), wst[kc % 2], gm[:, kc:kc + 1], None, op0=ALU.mult)
    WBK = lambda kc: ("wb", kc)
    col = lambda i: cv[:, i:i + 1]
    b.tt((lmp[:, 0:64], "lmp"), lm[:, 0:64], lm[:, 64:128], ALU.mult)
    b.tt((lmp[:, 64:128], "lmp"), lm[:, 128:192], lm[:, 192:256], ALU.mult)
    b.P.add("dve", lambda e: e.reduce_sum(out=lsc[:, 0:1], in_=lmp[:, 0:64], axis=AX.X), reads=["lmp"], writes=[lsc.tensor.name])
    b.P.add("dve", lambda e: e.reduce_sum(out=lsc[:, 1:2], in_=lmp[:, 64:128], axis=AX.X), reads=["lmp"], writes=[lsc.tensor.name])
    b.act(lsc[:, 0:2], lsc[:, 0:2], AF.Exp)
    b.tt(lsc[:, 2:3], lsc[:, 0:1], lsc[:, 1:2], ALU.subtract)
    b.ts(lsc[:, 3:4], lsc[:, 2:3], LAMBDA_INIT1, -1.0, op0=ALU.add, op1=ALU.mult)
    b.ts(col(4), col(2), 1.0 - LAMBDA_INIT1, None, op0=ALU.mult)
    NEGLAM = lsc[:, 3:4]

    TA = max(nblk, 1) * 512
    QT = [sb("QT%d" % h, [128, TA], BF16) for h in range(2)]
    KT = [sb("KT%d" % h, [128, TA], BF16) for h in range(2)]
    VS = [sb("VS%d" % h, [128, TA // 128, 128], BF16) for h in range(2)]
    xt = [sb("xt%d" % i, [128, 1024]) for i in range(2)]
    xs = [sb("xs%d" % i, [128, 1024], BF16) for i in range(2)]
    junk = sb("junk", [128, 1024], BF16)
    ss = sb("ss", [128, 1]); rstd = sb("rstd", [128, 1])
    xnT = sb("xnT", [128, 8, 512], BF16)
    posi = sb("posi", [128, 512], I32)
    tmp = [sb("t%d" % i, [128, 512]) for i in range(10)]
    Eb = [sb("Eb%d" % i, [128, 512], BF16) for i in range(4)]
    KX = [sb("KX%d" % i, [128, 128], BF16) for i in range(6)]
    PSB = [ps("PSB%d" % i, [128, 512]) for i in range(4)]
    PVA = [ps("PVA%d" % i, [128, 512]) for i in range(2)]
    PSM = [ps("PSM%d" % i, [128, 512]) for i in range(2)]
    PSX = PSM[0]
    PTB = PSB[3].bitcast(BF16).rearrange("p (a b) -> p a b", a=8)
    for s in range(nblk):
        def ld(g_):
            s2, j2 = g_ // 4, g_ % 4
            if s2 >= nblk:
                return
            t0_ = s2 * 512 + j2 * 128
            b.load(xt[g_ % 2], x(t0_) if callable(x) else x[t0_:t0_ + 128, :])
        if s == 0:
            ld(0)
        for j in range(4):
            tix = (s * 4 + j) % 2
            ld(s * 4 + j + 1)
            rms_tile_T(b, xt[tix], xs[tix], ss, rstd, PTB, (xnT[:, :, j * 128:(j + 1) * 128], ("xnT", j)), identb, junk)
        XN = KL([("xnT", j) for j in range(4)])
        if phase < 0.4:
            continue
        b.load(posi, pos_d[:, s * 512:(s + 1) * 512].partition_broadcast(128))
        ang = tmp[2]
        b.copy(ang, posi)
        b.ts(ang, ang, col(3), None, op0=ALU.mult)
        for which, shift in ((0, 0.0), (1, 1.5707963267948966)):
            a_ = tmp[3]
            kf = tmp[4]
            if shift:
                b.ts(a_, ang, shift, None, op0=ALU.add)
            else:
                a_ = ang
            b.ts(kf, a_, 1.0 / TWO_PI, MAGIC, op0=ALU.mult, op1=ALU.add)
            b.ts(kf, kf, -MAGIC, None, op0=ALU.add)
            r_ = tmp[5]
            b.stt(r_, kf, -CW1, a_, ALU.mult, ALU.add)
            b.stt(r_, kf, -CW2, r_, ALU.mult, ALU.add)
            b.ts(r_, r_, 3.1415925, -3.1415925, op0=ALU.min, op1=ALU.max)
            b.act(tmp[which], r_, AF.Sin)
        SIN, COS = tmp[0], tmp[1]
        if phase < 0.6:
            continue
        for c2 in range(2):
            ccs = (2 * c2, 2 * c2 + 1)
            pps = (PSB[0], PSB[1])
            pbs = (PSM[0], PSM[1])
            prs = (PVA[0], PVA[1])
            sqs = (tmp[2], tmp[6]); qns = (tmp[3], tmp[7]); t1s = (tmp[4], tmp[8]); t2s = (tmp[5], tmp[9])
            for u in range(2):
                for kc in range(8):
                    b.mm(pps[u], (wb[:, kc, ccs[u] * 128:(ccs[u] + 1) * 128], WBK(kc)), (xnT[:, kc, :], XN), start=(kc == 0), stop=(kc == 7))
            for u in range(2):
                b.act(sqs[u], pps[u], AF.Square)
            for u in range(2):
                b.mm(pbs[u], bones, sqs[u])
            for u in range(2):
                b.act(sqs[u], pbs[u], AF.Sqrt, scale=1.0 / 64, bias=1e-6)
            for u in range(2):
                b.recip(sqs[u], sqs[u])
            for u in range(2):
                b.stt(qns[u], pps[u], col(0 if ccs[u] < 2 else 1), sqs[u], ALU.mult, ALU.mult)
            for u in range(2):
                b.mm(prs[u], rot, qns[u])
            for u in range(2):
                b.tt(t1s[u], qns[u], COS, ALU.mult)
            for u in range(2):
                b.tt(t2s[u], prs[u], SIN, ALU.mult)
            for u in range(2):
                dst = (QT if ccs[u] < 2 else KT)[ccs[u] % 2]
                b.stt((dst[:, s * 512:(s + 1) * 512], (dst.tensor.name, s)), t1s[u], 1.0, t2s[u], ALU.mult, ALU.add)
        if phase < 0.8:
            continue
        for j in range(4):
            pp = PSB[j % 2]
            for kc in range(8):
                b.mm(pp[:, 0:256], (xnT[:, kc, j * 128:(j + 1) * 128], XN), (wb[:, kc, 512:768], WBK(kc)), start=(kc == 0), stop=(kc == 7))
            for h in range(2):
                b.copy((VS[h][:, s * 4 + j, :], (VS[h].tensor.name, s)), pp[:, h * 128:(h + 1) * 128], eng="act")
    if phase < 2:
        nb2 = 0
    else:
        nb2 = nblk
    scale = 64 ** -0.5
    for i in range(nb2):
        for h in range(2):
            qs = slice(i * 512, (i + 1) * 512)
            QK = (QT[h].tensor.name, i)
            nkb = 4 * i + 4
            steps = [(kb, c) for kb in range(nkb) for c in range(2)]
            nst = len(steps)

            def emit_s(t):
                kb, c = steps[t]
                kx = KX[t % 6]
                b.ts(kx, (KT[h][:, kb * 128:(kb + 1) * 128], (KT[h].tensor.name, kb // 4)), bones[:, 64 * c:64 * c + 1], None, op0=ALU.mult)
                b.mm(PSB[t % 4], kx, (QT[h][:, qs], QK))

            def emit_e(t):
                kb, c = steps[t]
                b.act(Eb[t % 4], PSB[t % 4], AF.Exp, scale=scale)
                if kb >= 4 * i:
                    b.tt(Eb[t % 4], Eb[t % 4], (cmask[:, kb - 4 * i, :], "cmask_s"), ALU.mult)

            def emit_pv(t):
                kb, c = steps[t]
                b.mm(PVA[c], (VS[h][:, kb, :], (VS[h].tensor.name, kb // 4)), Eb[t % 4], start=(kb == 0), stop=(kb == nkb - 1))
                b.mm(PSM[c], ones_b, Eb[t % 4], start=(kb == 0), stop=(kb == nkb - 1))
            LA = 3
            for t in range(min(LA, nst)):
                emit_s(t)
            for t in range(nst):
                emit_e(t)
                if t + LA < nst:
                    emit_s(t + LA)
                emit_pv(t)
            r0, r1, o_ = tmp[0], tmp[1], tmp[2]
            b.recip(r0, PSM[0])
            b.tt(r0, PVA[0], r0, ALU.mult)
            b.recip(r1, PSM[1])
            b.tt(r1, PVA[1], r1, ALU.mult)
            b.stt(o_, r1, NEGLAM, r0, ALU.mult, ALU.add)
            sq = tmp[3]
            b.act(sq, o_, AF.Square)
            b.mm(PVA[0], ones_f, sq)
            b.act(sq, PVA[0], AF.Sqrt, scale=1.0 / 128, bias=1e-6)
            b.recip(sq, sq)
            if fused is None:
                on = tmp[4 + ((i * 2 + h) % 2)]
                b.stt(on, o_, col(4), sq, ALU.mult, ALU.mult)
                b.store(oT[h * 128:(h + 1) * 128, qs], on)
            else:
                b.stt(onb[h], o_, col(4), sq, ALU.mult, ALU.mult)
        if fused is not None:
            for j in range(4):
                pt_ = p1t[j % 2]
                for n in range(2):
                    for h in range(2):
                        b.mm(PVA[1], onb[h][:, j * 128:(j + 1) * 128], (wop[:, h, n * 512:(n + 1) * 512], ("wop", h)), start=(h == 0), stop=(h == 1))
                    b.copy((pt_[:, n * 512:(n + 1) * 512], (pt_.tensor.name, n)), PVA[1], eng=("act" if n == 0 else "dve"))
                b.store(fused["rs_in"][i * 512 + j * 128:i * 512 + (j + 1) * 128, :], (pt_, KL([(pt_.tensor.name, 0), (pt_.tensor.name, 1)])))
    b.finish(sem_stack=(fused or {}).get("sem_stack"))
    return nc


def _consts_l3():
    p = np.arange(128)
    bones = (p[:, None] // 64 == p[None, :] // 64).astype(np.float32)
    rot = np.zeros((128, 128), np.float32)
    for d in range(128):
        dm = d % 64
        if dm < 8:
            rot[d + 8, d] = -1.0
        elif dm < 16:
            rot[d - 8, d] = 1.0
    invf = np.zeros((128,), np.float32)
    for q in range(128):
        if q % 64 < 16:
            invf[q] = np.float32(500000.0) ** np.float32(-(2 * (q % 8)) / 16.0)
    kp = np.arange(128)[:, None]
    qc = np.arange(512)[None, :]
    cm = np.concatenate([((128 * j + kp) <= qc).astype(np.float32) for j in range(4)], axis=1)
    return dict(ident=np.eye(128, dtype=np.float32), bones=bones, rot=rot, cmask=np.ascontiguousarray(cm)), invf


def l3_inputs(inp, x1, bi, g):
    f = lambda k: np.asarray(inp[k][0], np.float32)
    W = f("o_w_qkv")
    hs = [2 * g, 2 * g + 1]
    cols = np.concatenate([np.arange(h * 128, (h + 1) * 128) for h in hs] + [1024 + np.arange(h * 128, (h + 1) * 128) for h in hs]
                          + [2048 + np.arange(h * 128, (h + 1) * 128) for h in hs])
    consts, invf = _consts_l3()
    cvec = np.zeros((128, 4), np.float32)
    cvec[:, 0] = np.tile(f("o_q_norm"), 2)
    cvec[:, 1] = np.tile(f("o_k_norm"), 2)
    cvec[:, 2] = f("o_subln")
    cvec[:, 3] = invf
    d = dict(x=(None if x1 is None else np.ascontiguousarray(x1[bi])), pos=np.ascontiguousarray(inp["positions"][bi].reshape(1, -1).astype(np.int32)),
             w=np.ascontiguousarray(W[:, cols]), gmix=np.ascontiguousarray(f("o_ln_mix").reshape(8, 128).T), cvec=cvec,
             lams=np.concatenate([f("o_lambda_q1"), f("o_lambda_k1"), f("o_lambda_q2"), f("o_lambda_k2")]).reshape(1, 256))
    d.update(consts)
    return d


N_CORES = 8
CC_GROUPS = [[0, 1, 2, 3], [4, 5, 6, 7]]


_CC_STATE = {}


def _cc_block(nc, kind, src, dst, name, sem_stack):
    st = _CC_STATE.setdefault(id(nc), {})
    if "sem" not in st:
        st["sem"] = sem_stack.enter_context(nc.semaphore("cc_sem"))
        st["n"] = 0
    sem = st["sem"]
    st["n"] += 1
    cnt = st["n"]
    with nc.Block() as block:
        @block.gpsimd
        def _(g):
            g.collective_compute(kind, ALU.bypass if kind == "AllGather" else ALU.add, replica_groups=CC_GROUPS,
                                 ins=[src.ap().opt()], outs=[dst.ap().opt()]).then_inc(sem)
            g.wait_ge(sem, cnt)


N_EXPERTS_ = 8
AG_CH = 256


def _cc_multi(nc, kind, pairs, sem_stack):
    st = _CC_STATE.setdefault(id(nc), {})
    if "sem" not in st:
        st["sem"] = sem_stack.enter_context(nc.semaphore("cc_sem"))
        st["n"] = 0
    sem = st["sem"]
    with nc.Block() as block:
        @block.gpsimd
        def _(g):
            for src, dst in pairs:
                st["n"] += 1
                g.collective_compute(kind, ALU.bypass if kind == "AllGather" else ALU.add, replica_groups=CC_GROUPS,
                                     ins=[src.ap().opt()], outs=[dst.ap().opt()]).then_inc(sem)
            g.wait_ge(sem, st["n"])


def build_fused():
    nc = bass.Bass("TRN2", target_bir_lowering=False)
    NQ = T_SEQ // 4
    nch = NQ // AG_CH
    rs1_in = nc.dram_tensor("rs1_in", [T_SEQ, 1024], F32)
    rs1_out = nc.dram_tensor("rs1_out", [NQ, 1024], F32)
    ag_in = [nc.dram_tensor("ag_in%d" % k, [AG_CH, 1024], F32) for k in range(nch)]
    ag_out = [nc.dram_tensor("ag_out%d" % k, [4 * AG_CH, 1024], F32) for k in range(nch)]
    rs2_in = nc.dram_tensor("rs2_in", [T_SEQ, 1024], F32)
    rs2_out = nc.dram_tensor("rs2_out", [NQ, 1024], F32)

    def q_tile(tok0):
        return ag_in[tok0 // AG_CH].ap()[tok0 % AG_CH:tok0 % AG_CH + 128, :]

    def full_tile(t0):
        r, w = t0 // NQ, t0 % NQ
        k, i = w // AG_CH, w % AG_CH
        return ag_out[k].ap()[r * AG_CH + i:r * AG_CH + i + 128, :]

    with contextlib.ExitStack() as ss:
        build_l1(nc=nc, pre="a__", fused=dict(rs_in=rs1_in.ap(), sem_stack=ss))
        _cc_block(nc, "ReduceScatter", rs1_in, rs1_out, "cc1", ss)
        build_ffn(NT=2048, F=2816, E=1, G=1024, moe=False, nc=nc, pre="b__", fused=dict(add=rs1_out.ap(), xres=None, out=q_tile, sem_stack=ss))
        _cc_multi(nc, "AllGather", [(ag_in[k], ag_out[k]) for k in range(nch)], ss)
        build_l3(nc=nc, pre="c__", fused=dict(x=full_tile, rs_in=rs2_in.ap(), sem_stack=ss))
        _cc_block(nc, "ReduceScatter", rs2_in, rs2_out, "cc3", ss)
        build_ffn(NT=2048, F=3584, E=8, G=1024, moe=True, nc=nc, pre="d__", fused=dict(add=rs2_out.ap(), xres=q_tile, out=None, sem_stack=ss))
    return nc


def fused_inputs(inp, c):
    bi, g = c // 4, c % 4
    f = lambda k: np.asarray(inp[k][0], np.float32)
    d = {}
    l1 = l1_inputs(inp, bi, g)
    wout = f("e_w_out")
    l1["wo_part"] = np.ascontiguousarray(np.concatenate([wout[g * 128:(g + 1) * 128], wout[512 + g * 128:512 + (g + 1) * 128]], axis=0))
    for k, v in l1.items():
        d["a__" + k] = v
    ident = np.eye(128, dtype=np.float32)
    d["b__xres"] = np.ascontiguousarray(np.asarray(inp["x"], np.float32)[bi, g * 2048:(g + 1) * 2048])
    d["b__gain"] = np.ascontiguousarray(f("e_ln_ffn").reshape(1, 1024))
    d["b__wg"] = np.asarray(inp["e_ffn_gate"], np.float32)
    d["b__wu"] = np.asarray(inp["e_ffn_up"], np.float32)
    d["b__wd"] = np.asarray(inp["e_ffn_down"], np.float32)
    d["b__ident"] = ident
    l3 = l3_inputs(inp, None, bi, g)
    del l3["x"]
    l3["wo_part"] = np.ascontiguousarray(f("o_w_o")[g * 256:(g + 1) * 256])
    for k, v in l3.items():
        d["c__" + k] = v
    d["d__gain"] = np.ascontiguousarray(f("o_ln_ffn").reshape(1, 1024))
    sh = c % N_EXPERTS_
    d["d__wg"] = np.roll(np.asarray(inp["o_moe_gate"][0], np.float32), -sh, axis=0)
    d["d__wu"] = np.roll(np.asarray(inp["o_moe_up"][0], np.float32), -sh, axis=0)
    d["d__wd"] = np.roll(np.asarray(inp["o_moe_down"][0], np.float32), -sh, axis=0)
    d["d__router"] = np.ascontiguousarray(np.roll(f("o_router"), -sh, axis=1))
    d["d__ident"] = ident
    return d


def kernel(**inp):
    inp = {k: np.asarray(v) for k, v in inp.items()}
    Bn, T, D = inp["x"].shape
    nc = build_fused()
    res = run_bass_kernel_spmd(nc, [fused_inputs(inp, c) for c in range(N_CORES)], core_ids=list(range(N_CORES)))
    out = np.zeros((Bn, T, D), np.float32)
    for c in range(N_CORES):
        bi, tq = c // 4, c % 4
        out[bi, tq * 2048:(tq + 1) * 2048] = res.results[c]["d__out"]
    return out
```
